# Optimizing a Trainium2 kernel written in Bass

```python
import jax
import jax.numpy as jnp
from jax import lax
import numpy as np

D_MODEL = 1024
BATCH = 1
SEQ = 16384
DEPTH = 4

HEAD_DIM = 64
N_RET_HEADS = D_MODEL // (2 * HEAD_DIM)
N_DIL_HEADS = D_MODEL // (2 * HEAD_DIM)
N_NA_HEADS = D_MODEL // HEAD_DIM
RET_W = N_RET_HEADS * HEAD_DIM
DIL_W = N_DIL_HEADS * HEAD_DIM
NA_W = N_NA_HEADS * HEAD_DIM
EVEN_IN = 5 * RET_W + 3 * DIL_W
ODD_IN = 3 * NA_W
RET_CHUNK = 128
RET_DECAY_EXP0 = 5.0
ROPE_THETA = 10000.0
DIL_CONFIGS = ((128, 1), (512, 4), (2048, 16))
GRID_W = 64
NA_KH_MAX = 8
NA_KW = 16
N_EXPERTS = 32
TOP_K = 4
D_FF = D_MODEL
SWIGLU_LIMIT = 7.0
SWIGLU_ALPHA = 1.702
MOE_BLOCK = 128
LN_EPS = 1e-5
GN_EPS = 1e-6
NEG_INF = -1e30
DEEPNORM_ALPHA = (2.0 * DEPTH) ** 0.25
DEEPNORM_BETA = (8.0 * DEPTH) ** -0.25
N_EVEN = (DEPTH + 1) // 2
N_ODD = DEPTH // 2

kernel_name = "hybrid_retnet_longnet_natten_moe_encoder"


def layer_norm(x, g, b):
    xf = x.astype(jnp.float32)
    mu = xf.mean(-1, keepdims=True)
    var = jnp.square(xf - mu).mean(-1, keepdims=True)
    return ((xf - mu) * lax.rsqrt(var + LN_EPS) * g + b).astype(x.dtype)


def head_norm(x):
    xf = x.astype(jnp.float32)
    mu = xf.mean(-1, keepdims=True)
    var = jnp.square(xf - mu).mean(-1, keepdims=True)
    return (xf - mu) * lax.rsqrt(var + GN_EPS)


def rope_tables(T):
    pos = jnp.arange(T, dtype=jnp.float32)
    inv = ROPE_THETA ** (-jnp.arange(0, HEAD_DIM, 2, dtype=jnp.float32) / HEAD_DIM)
    ang = pos[:, None] * inv[None, :]
    return jnp.cos(ang), jnp.sin(ang)


def apply_rope(x, cos, sin):
    x1, x2 = jnp.split(x, 2, axis=-1)
    c = cos[None, :, None, :].astype(x.dtype)
    s = sin[None, :, None, :].astype(x.dtype)
    return jnp.concatenate([x1 * c - x2 * s, x2 * c + x1 * s], axis=-1)


def retention_one_direction(q, k, v, log_gamma, strict):
    b_, h_, t_, d_ = q.shape
    C = RET_CHUNK
    nc = t_ // C
    qc = q.reshape(b_, h_, nc, C, d_)
    kc = k.reshape(b_, h_, nc, C, d_)
    vc = v.reshape(b_, h_, nc, C, d_)
    idx = jnp.arange(C, dtype=jnp.float32)
    diff = idx[:, None] - idx[None, :]
    keep = (diff > 0) if strict else (diff >= 0)
    decay = jnp.where(keep, jnp.exp(log_gamma[:, None, None] * jnp.maximum(diff, 0.0)), 0.0)
    scores = jnp.einsum('bhncd,bhnmd->bhncm', qc, kc) * decay[None, :, None]
    intra = jnp.einsum('bhncm,bhnme->bhnce', scores, vc)
    zeta = jnp.exp(log_gamma[:, None] * (C - 1.0 - idx)[None, :])
    chunk_state = jnp.einsum('bhnmd,hm,bhnme->nbhde', kc, zeta, vc)
    chunk_decay = jnp.exp(log_gamma * C)[None, :, None, None]

    def step(R, S):
        return chunk_decay * R + S, R

    _, r_prev = lax.scan(step, jnp.zeros_like(chunk_state[0]), chunk_state)
    xi = jnp.exp(log_gamma[:, None] * (idx + 1.0)[None, :])
    cross = jnp.einsum('bhncd,nbhde->bhnce', qc, r_prev) * xi[None, :, None, :, None]
    return (intra + cross).reshape(b_, h_, t_, d_)


def retention_mixer(q, k, v, g_f, g_b, decay_logits, cos, sin):
    b_, t_, h_, d_ = q.shape
    q = apply_rope(q, cos, sin)
    k = apply_rope(k, cos, sin) * (HEAD_DIM ** -0.5)
    qf, kf, vf = [a.astype(jnp.float32).transpose(0, 2, 1, 3) for a in (q, k, v)]
    log_gamma = jax.nn.log_sigmoid(decay_logits.astype(jnp.float32))
    o_f = retention_one_direction(qf, kf, vf, log_gamma[0], strict=False)
    o_b = jnp.flip(retention_one_direction(jnp.flip(qf, 2), jnp.flip(kf, 2), jnp.flip(vf, 2),
                                           log_gamma[1], strict=True), 2)
    o_f = head_norm(o_f.transpose(0, 2, 1, 3))
    o_b = head_norm(o_b.transpose(0, 2, 1, 3))
    y = jax.nn.silu(g_f.astype(jnp.float32)) * o_f + jax.nn.silu(g_b.astype(jnp.float32)) * o_b
    return y.reshape(b_, t_, h_ * d_).astype(q.dtype)


def dilated_branch(q, k, v, dil, half):
    b_, h_, t_, d_ = q.shape
    L = t_ // dil
    nb = -(-L // half)
    Lp = nb * half

    def classes(a):
        return a.reshape(b_, h_, L, dil, d_).transpose(0, 1, 3, 2, 4)

    qr, kr, vr = classes(q), classes(k), classes(v)
    qb = jnp.pad(qr, ((0, 0), (0, 0), (0, 0), (0, Lp - L), (0, 0))).reshape(b_, h_, dil, nb, half, d_)

    def windows(a):
        ap = jnp.pad(a, ((0, 0), (0, 0), (0, 0), (half, Lp - L + half), (0, 0)))
        ap = ap.reshape(b_, h_, dil, nb + 2, half, d_)
        return jnp.concatenate([ap[:, :, :, :-2], ap[:, :, :, 1:-1], ap[:, :, :, 2:]], axis=4)

    kw, vw = windows(kr), windows(vr)
    qi = jnp.arange(nb)[:, None, None] * half + jnp.arange(half)[None, :, None]
    ki = jnp.arange(nb)[:, None, None] * half - half + jnp.arange(3 * half)[None, None, :]
    valid = (jnp.abs(ki - qi) <= half) & (ki >= 0) & (ki < L)
    s = jnp.einsum('bhrnqd,bhrnkd->bhrnqk', qb, kw).astype(jnp.float32)
    s = jnp.where(valid, s, NEG_INF)
    m = s.max(-1, keepdims=True)
    p = jnp.exp(s - m)
    den = p.sum(-1, keepdims=True)
    o = jnp.einsum('bhrnqk,bhrnkd->bhrnqd', (p / den).astype(v.dtype), vw)
    lse = (m + jnp.log(den))[..., 0]
    o = o.reshape(b_, h_, dil, Lp, d_)[:, :, :, :L].transpose(0, 1, 3, 2, 4).reshape(b_, h_, t_, d_)
    lse = lse.reshape(b_, h_, dil, Lp)[..., :L].transpose(0, 1, 3, 2).reshape(b_, h_, t_)
    return o, lse


def dilated_mixer(q, k, v, cos, sin):
    b_, t_, h_, d_ = q.shape
    q = (apply_rope(q, cos, sin) * (HEAD_DIM ** -0.5)).transpose(0, 2, 1, 3)
    k = apply_rope(k, cos, sin).transpose(0, 2, 1, 3)
    v = v.transpose(0, 2, 1, 3)
    outs, lses = [], []
    for window, dil in DIL_CONFIGS:
        o, lse = dilated_branch(q, k, v, dil, window // (2 * dil))
        outs.append(o)
        lses.append(lse)
    wts = jax.nn.softmax(jnp.stack(lses, 0), axis=0).astype(v.dtype)
    o = jnp.einsum('gbht,gbhtd->bthd', wts, jnp.stack(outs, 0))
    return o.reshape(b_, t_, h_ * d_)


def neighbourhood_attention(q, k, v, rpb):
    b_, t_, h_, d_ = q.shape
    rows = t_ // GRID_W
    kh = min(NA_KH_MAX, rows)
    qg = q.reshape(b_, rows, GRID_W, h_, d_) * (HEAD_DIM ** -0.5)
    kg = k.reshape(b_, rows, GRID_W, h_, d_)
    vg = v.reshape(b_, rows, GRID_W, h_, d_)
    cols = jnp.arange(GRID_W)
    col_start = jnp.clip(cols - NA_KW // 2, 0, GRID_W - NA_KW)
    col_idx = col_start[:, None] + jnp.arange(NA_KW)[None, :]
    col_off = col_idx - cols[:, None] + (NA_KW - 1)
    rpb_cols = rpb[:, :, col_off]

    def one_row(r):
        r0 = jnp.clip(r - kh // 2, 0, rows - kh)
        kb = lax.dynamic_slice_in_dim(kg, r0, kh, axis=1)[:, :, col_idx]
        vb = lax.dynamic_slice_in_dim(vg, r0, kh, axis=1)[:, :, col_idx]
        row_off = r0 + jnp.arange(kh) - r + (NA_KH_MAX - 1)
        bias = rpb_cols[:, row_off].transpose(0, 2, 1, 3)
        q_r = lax.dynamic_index_in_dim(qg, r, axis=1, keepdims=False)
        s = jnp.einsum('bqhd,biqjhd->bhqij', q_r, kb).astype(jnp.float32) + bias[None]
        p = jax.nn.softmax(s.reshape(b_, h_, GRID_W, kh * NA_KW), axis=-1)
        p = p.reshape(s.shape).astype(v.dtype)
        return jnp.einsum('bhqij,biqjhd->bqhd', p, vb)

    out = lax.map(one_row, jnp.arange(rows))
    return out.transpose(1, 0, 2, 3, 4).reshape(b_, t_, h_ * d_)


def even_mixer(x, w_in, w_out, decay_logits, cos, sin):
    b_, t_, _ = x.shape
    h = jnp.einsum('btd,de->bte', x, w_in)
    cuts = [RET_W, 2 * RET_W, 3 * RET_W, 4 * RET_W, 5 * RET_W, 5 * RET_W + DIL_W, 5 * RET_W + 2 * DIL_W]
    rq, rk, rv, rgf, rgb, dq, dk, dv = jnp.split(h, cuts, axis=-1)
    ret_heads = lambda a: a.reshape(b_, t_, N_RET_HEADS, HEAD_DIM)
    dil_heads = lambda a: a.reshape(b_, t_, N_DIL_HEADS, HEAD_DIM)
    y_ret = retention_mixer(ret_heads(rq), ret_heads(rk), ret_heads(rv), ret_heads(rgf), ret_heads(rgb),
                            decay_logits, cos, sin)
    y_dil = dilated_mixer(dil_heads(dq), dil_heads(dk), dil_heads(dv), cos, sin)
    return jnp.einsum('bte,ed->btd', jnp.concatenate([y_ret, y_dil], axis=-1), w_out)


def odd_mixer(x, w_in, w_out, rpb):
    b_, t_, _ = x.shape
    h = jnp.einsum('btd,de->bte', x, w_in)
    q, k, v = [a.reshape(b_, t_, N_NA_HEADS, HEAD_DIM) for a in jnp.split(h, 3, axis=-1)]
    return jnp.einsum('bte,ed->btd', neighbourhood_attention(q, k, v, rpb), w_out)


def moe(x2d, router_w, router_b, w_up, b_up, w_down, b_down):
    n_tok, d_ = x2d.shape
    logits = (x2d @ router_w + router_b).astype(jnp.float32)
    top_v, top_i = lax.top_k(logits, TOP_K)
    gates = jax.nn.softmax(top_v, axis=-1).astype(x2d.dtype)
    n_slot = n_tok * TOP_K
    flat_e = top_i.reshape(-1).astype(jnp.int32)
    flat_g = gates.reshape(-1)
    flat_tok = jnp.arange(n_slot, dtype=jnp.int32) // TOP_K
    order = jnp.argsort(flat_e)
    se = flat_e[order]
    counts = jnp.bincount(flat_e, length=N_EXPERTS).astype(jnp.int32)
    start = jnp.cumsum(counts) - counts
    pcounts = (counts + MOE_BLOCK - 1) // MOE_BLOCK * MOE_BLOCK
    pend = jnp.cumsum(pcounts)
    pstart = pend - pcounts
    dest = pstart[se] + (jnp.arange(n_slot, dtype=jnp.int32) - start[se])
    n_pad = n_slot + N_EXPERTS * MOE_BLOCK
    n_blk = n_pad // MOE_BLOCK
    slot_tok = jnp.full((n_pad,), n_tok, dtype=jnp.int32).at[dest].set(flat_tok[order])
    slot_gate = jnp.zeros((n_pad,), x2d.dtype).at[dest].set(flat_g[order])
    blk_e = jnp.minimum(jnp.searchsorted(pend, jnp.arange(n_blk, dtype=jnp.int32) * MOE_BLOCK, side='right'),
                        N_EXPERTS - 1)
    x_pad = jnp.concatenate([x2d, jnp.zeros((1, d_), x2d.dtype)], axis=0)

    def block(args):
        tok, g, e = args
        h = x_pad[tok] @ w_up[e] + b_up[e]
        x_glu = jnp.minimum(h[:, 0::2], SWIGLU_LIMIT)
        x_lin = jnp.clip(h[:, 1::2], -SWIGLU_LIMIT, SWIGLU_LIMIT)
        act = x_glu * jax.nn.sigmoid(SWIGLU_ALPHA * x_glu) * (x_lin + 1.0)
        return (act @ w_down[e] + b_down[e]) * g[:, None]

    out = lax.map(block, (slot_tok.reshape(n_blk, MOE_BLOCK), slot_gate.reshape(n_blk, MOE_BLOCK), blk_e))
    y = jnp.zeros((n_tok + 1, d_), x2d.dtype).at[slot_tok].add(out.reshape(n_pad, d_))
    return y[:n_tok]


def setup_inputs(seed: int = 0) -> dict:
    key = jax.random.key(seed)
    ks = jax.random.split(key, 16)
    nrm = lambda k, shape, scale: jax.random.normal(k, shape, jnp.float32) * scale
    base_logit = jnp.log(2.0 ** (RET_DECAY_EXP0 + jnp.arange(N_RET_HEADS, dtype=jnp.float32)) - 1.0)
    return {
        'x': nrm(ks[0], (BATCH, SEQ, D_MODEL), 1.0),
        'ab_w_in': nrm(ks[1], (N_EVEN, D_MODEL, EVEN_IN), D_MODEL ** -0.5),
        'ab_w_out': nrm(ks[2], (N_EVEN, RET_W + DIL_W, D_MODEL), DEEPNORM_BETA * (RET_W + DIL_W) ** -0.5),
        'ret_decay': base_logit[None, None, :] + nrm(ks[3], (N_EVEN, 2, N_RET_HEADS), 0.1),
        'c_w_in': nrm(ks[4], (N_ODD, D_MODEL, ODD_IN), D_MODEL ** -0.5),
        'c_w_out': nrm(ks[5], (N_ODD, NA_W, D_MODEL), DEEPNORM_BETA * NA_W ** -0.5),
        'c_rpb': nrm(ks[6], (N_ODD, N_NA_HEADS, 2 * NA_KH_MAX - 1, 2 * NA_KW - 1), 0.02),
        'ln_g': 1.0 + nrm(ks[7], (DEPTH, 2, D_MODEL), 0.02),
        'ln_b': nrm(ks[8], (DEPTH, 2, D_MODEL), 0.02),
        'router_w': nrm(ks[9], (DEPTH, D_MODEL, N_EXPERTS), D_MODEL ** -0.5),
        'router_b': nrm(ks[10], (DEPTH, N_EXPERTS), 0.01),
        'exp_w_up': nrm(ks[11], (DEPTH, N_EXPERTS, D_MODEL, 2 * D_FF), D_MODEL ** -0.5),
        'exp_b_up': nrm(ks[12], (DEPTH, N_EXPERTS, 2 * D_FF), 0.01),
        'exp_w_down': nrm(ks[13], (DEPTH, N_EXPERTS, D_FF, D_MODEL), DEEPNORM_BETA * D_FF ** -0.5),
        'exp_b_down': nrm(ks[14], (DEPTH, N_EXPERTS, D_MODEL), 0.01),
    }


def reference(x, ab_w_in, ab_w_out, ret_decay, c_w_in, c_w_out, c_rpb, ln_g, ln_b,
              router_w, router_b, exp_w_up, exp_b_up, exp_w_down, exp_b_down):
    b_, t_, d_ = x.shape
    cos, sin = rope_tables(t_)
    for layer in range(DEPTH):
        j = layer // 2
        if layer % 2 == 0:
            mix = even_mixer(x, ab_w_in[j], ab_w_out[j], ret_decay[j], cos, sin)
        else:
            mix = odd_mixer(x, c_w_in[j], c_w_out[j], c_rpb[j])
        x = layer_norm(DEEPNORM_ALPHA * x + mix, ln_g[layer, 0], ln_b[layer, 0])
        ffn = moe(x.reshape(b_ * t_, d_), router_w[layer], router_b[layer], exp_w_up[layer],
                  exp_b_up[layer], exp_w_down[layer], exp_b_down[layer]).reshape(b_, t_, d_)
        x = layer_norm(DEEPNORM_ALPHA * x + ffn, ln_g[layer, 1], ln_b[layer, 1])
    return x
```

```python
import numpy as np
import concourse.bass as bass
import concourse.mybir as mybir
from concourse.bass_utils import run_bass_kernel_spmd

F32 = mybir.dt.float32
BF16 = mybir.dt.bfloat16
AF = mybir.ActivationFunctionType
ALU = mybir.AluOpType
AX = mybir.AxisListType


class Res:
    __slots__ = ("name", "w", "r", "dsem", "dcnt")

    def __init__(self, name):
        self.name = name
        self.w = None
        self.r = []
        self.dsem = None
        self.dcnt = 0


class Ctx:
    def __init__(self, nc):
        self.nc = nc
        self.eng = {"pe": nc.tensor, "act": nc.scalar, "dve": nc.vector,
                    "pool": nc.gpsimd, "sp": nc.sync}
        self.sem = {k: nc.alloc_semaphore("c_" + k) for k in ("pe", "act", "dve", "pool")}
        self.cnt = {k: 0 for k in self.sem}
        self.seen = {k: {} for k in self.eng}
        self.semobj = dict(self.sem)
        self.n_dsem = 0
        self.n_wait = 0
        self.n_ins = 0

    def sb(self, name, shape, dt):
        return self.nc.alloc_sbuf_tensor(name, list(shape), dt)

    def ps(self, name, shape, dt=F32):
        return self.nc.alloc_psum_tensor(name, list(shape), dt)

    def _dsem(self, res):
        if res.dsem is None:
            key = "d%d" % self.n_dsem
            self.n_dsem += 1
            res.dsem = key
            self.semobj[key] = self.nc.alloc_semaphore(key)
        return res.dsem

    def _wait(self, e, deps):
        seen = self.seen[e]
        eng = self.eng[e]
        best = {}
        for d in deps:
            if d is None:
                continue
            k, v = d
            if best.get(k, 0) < v:
                best[k] = v
        for k, v in best.items():
            if seen.get(k, 0) < v:
                eng.wait_ge(self.semobj[k], v)
                seen[k] = v
                self.n_wait += 1

    def op(self, e, fn, reads=(), writes=(), acc=False):
        deps = [r.w for r in reads]
        if not acc:
            for w in writes:
                deps.append(w.w)
                deps.extend(w.r)
        self._wait(e, deps)
        ins = fn()
        self.cnt[e] += 1
        ins.then_inc(self.sem[e], 1)
        ev = (e, self.cnt[e])
        self.seen[e][e] = max(self.seen[e].get(e, 0), 0)
        for r in reads:
            r.r.append(ev)
        for w in writes:
            w.w = ev
            if not acc:
                w.r = []
        self.n_ins += 1
        return ins

    def dma(self, q, out, in_, reads=(), writes=(), **kw):
        assert len(writes) == 1
        wres = writes[0]
        deps = [r.w for r in reads]
        deps.append(wres.w)
        deps.extend(wres.r)
        self._wait(q, deps)
        key = self._dsem(wres)
        ins = self.eng[q].dma_start(out=out, in_=in_, **kw)
        wres.dcnt += 16
        ins.then_inc(self.semobj[key], 16)
        ev = (key, wres.dcnt)
        for r in reads:
            r.r.append(ev)
        wres.w = ev
        wres.r = []
        self.n_ins += 1
        return ins

    def dma_more(self, q, out, in_, reads=(), writes=(), **kw):
        wres = writes[0]
        deps = [r.w for r in reads]
        self._wait(q, deps)
        key = self._dsem(wres)
        ins = self.eng[q].dma_start(out=out, in_=in_, **kw)
        wres.dcnt += 16
        ins.then_inc(self.semobj[key], 16)
        ev = (key, wres.dcnt)
        for r in reads:
            r.r.append(ev)
        wres.w = ev
        self.n_ins += 1
        return ins

    def finish(self, q, resources):
        self._wait(q, [r.w for r in resources])


ALPHA = (2.0 * 4) ** 0.25
LN_EPS = 1e-5


def build_F(n_exp=32, n_blk=2):
    nc = bass.Bass("TRN2", target_bir_lowering=False)
    c = Ctx(nc)
    TB = 1024
    NT = TB * n_blk
    D = 1024
    x = nc.dram_tensor("x", [NT, D], F32, kind="ExternalInput").ap()
    yT = nc.dram_tensor("yT", [D, NT], F32, kind="ExternalInput").ap()
    w_out = nc.dram_tensor("w_out", [D, D], F32, kind="ExternalInput").ap()
    lnp = nc.dram_tensor("lnp", [4, D], F32, kind="ExternalInput").ap()
    rw = nc.dram_tensor("rw", [D, 32], F32, kind="ExternalInput").ap()
    rb = nc.dram_tensor("rb", [1, 32], F32, kind="ExternalInput").ap()
    wup = nc.dram_tensor("wup", [n_exp, D, 2 * D], F32, kind="ExternalInput").ap()
    bup = nc.dram_tensor("bup", [256, 256], F32, kind="ExternalInput").ap()
    wdn = nc.dram_tensor("wdn", [n_exp, D, D], F32, kind="ExternalInput").ap()
    bdn = nc.dram_tensor("bdn", [32, D], F32, kind="ExternalInput").ap()
    idn = nc.dram_tensor("idn", [128, 128], F32, kind="ExternalInput").ap()
    xo = nc.dram_tensor("xo", [NT, D], F32, kind="ExternalOutput").ap()

    emit_F(nc, c, x, yT, w_out, lnp, rw, rb, wup, bup, wdn, bdn, idn, xo, n_exp=n_exp, n_blk=n_blk)
    return nc


def emit_F(nc, c, x, yT, w_out, lnp, rw, rb, wup, bup, wdn, bdn, idn, xo, n_exp=32, n_blk=2, pfx="F"):
    TB = 1024
    D = 1024
    NTL = TB // 128
    sb = lambda n, s, d: c.sb(pfx + n, s, d)
    ident = sb("ident", [128, 128], F32); r_ident = Res("ident")
    c.dma("sp", ident[:], idn, writes=[r_ident])
    rw32 = sb("rw32", [128, 8, 32], F32); r_rw = Res("rw")
    c.dma("sp", rw32[:], rw.rearrange("(c p) e -> p c e", p=128), writes=[r_rw])
    rbB = sb("rbB", [128, 32], F32); r_rb = Res("rb")
    c.dma("sp", rbB[:], rb.partition_broadcast(128), writes=[r_rb])
    bdn32 = sb("bdn32", [32, D], F32); r_bdn = Res("bdn")
    c.dma("sp", bdn32[:], bdn, writes=[r_bdn])
    lnB = [sb("lnB%d" % i, [128, D], F32) for i in range(4)]
    r_ln = [Res("ln%d" % i) for i in range(4)]
    for i in range(4):
        c.dma("sp", lnB[i][:], lnp[i:i + 1, :].partition_broadcast(128), writes=[r_ln[i]])
    B = [c.ps(pfx + "bank%d" % i, [128, 512], F32) for i in range(8)]
    rB = [Res("bank%d" % i) for i in range(8)]
    bupraw = sb("bupraw", [128, 2, 256], F32); r_bupraw = Res("bupraw")
    c.dma("sp", bupraw[:], bup.rearrange("(g p) k -> p g k", p=128), writes=[r_bupraw])
    bupT = sb("bupT", [128, 2, 256], F32); r_bupT = Res("bupT")
    for two in range(2):
        for g in range(2):
            c.op("pe", lambda: nc.tensor.transpose(B[0][:, (two * 2 + g) * 128:(two * 2 + g + 1) * 128],
                                                   bupraw[:, g, two:256:2], ident[:]),
                 reads=[r_bupraw, r_ident], writes=[rB[0]], acc=(two + g > 0))
    c.op("act", lambda: nc.scalar.copy(bupT[:].rearrange("p a b -> p (a b)"), B[0][:]), reads=[rB[0]], writes=[r_bupT])

    c.op("dve", lambda: nc.vector.tensor_scalar(bupT[:, 1, :], bupT[:, 1, :], 1.0, None, ALU.add), reads=[r_bupT], writes=[r_bupT])

    yacc = sb("yacc", [128, NTL, D], F32); r_yacc = [Res("yacc%d" % i) for i in range(NTL)]
    x1Tb = sb("x1Tb", [128, 8, TB], BF16); r_x1Tb = [Res("x1Tb%d" % i) for i in range(NTL)]
    gates = sb("gates", [128, NTL, 32], F32); r_gates = [Res("gates%d" % i) for i in range(NTL)]
    gT = sb("gT", [32, TB], F32); r_gT = [Res("gT%d" % i) for i in range(NTL)]
    xt = [sb("xt%d" % i, [128, D], F32) for i in range(2)]; r_xt = [Res("xt%d" % i) for i in range(2)]
    x1T32 = [sb("x1T32_%d" % i, [128, 8, 128], F32) for i in range(2)]; r_x1T32 = [Res("x1T32_%d" % i) for i in range(2)]
    sm = [sb("sm%d" % i, [128, 128], F32) for i in range(2)]; r_sm = [Res("sm%d" % i) for i in range(2)]
    wuc = [sb("wuc%d" % i, [128, 8, 256], BF16) for i in range(4)]; r_wuc = [Res("wuc%d" % i) for i in range(4)]
    wd = [sb("wd%d" % i, [128, 8, D], BF16) for i in range(2)]; r_wd = [Res("wd%d" % i) for i in range(2)]
    act = [sb("act%d" % i, [128, 8, TB], BF16) for i in range(2)]
    r_act = [Res("act%d" % i) for i in range(2)]
    yTb, r_yTb = act[0], r_act[0]
    woutb, r_wout = act[1], r_act[1]
    NTMP = 3
    tg = [sb("tg%d" % i, [128, 512], F32) for i in range(NTMP)]; r_tg = [Res("tg%d" % i) for i in range(NTMP)]
    tsg = [sb("tsg%d" % i, [128, 512], F32) for i in range(NTMP)]; r_tsg = [Res("tsg%d" % i) for i in range(NTMP)]
    tl = [sb("tl%d" % i, [128, 512], F32) for i in range(NTMP)]; r_tl = [Res("tl%d" % i) for i in range(NTMP)]
    ot = [sb("ot%d" % i, [128, D], F32) for i in range(2)]; r_ot = [Res("ot%d" % i) for i in range(2)]
    zt, r_zt = ot, r_ot
    r_out = [Res("xo0"), Res("xo1")]

    def layer_norm(src, r_src, dst, r_dst, s, r_s, gi, bi):
        for h in range(2):
            c.op("dve", lambda: nc.vector.bn_stats(s[:, h * 6:(h + 1) * 6], src[:, h * 512:(h + 1) * 512]),
                 reads=[r_src], writes=[r_s], acc=(h > 0))
        c.op("dve", lambda: nc.vector.bn_aggr(s[:, 12:14], s[:, 0:12]),
             reads=[r_s], writes=[r_s])
        c.op("act", lambda: nc.scalar.activation(s[:, 14:15], s[:, 13:14], AF.Sqrt, bias=epsT[:, 0:1], scale=1.0),
             reads=[r_s, r_eps], writes=[r_s])
        c.op("dve", lambda: nc.vector.reciprocal(s[:, 15:16], s[:, 14:15]), reads=[r_s], writes=[r_s])
        c.op("dve", lambda: nc.vector.tensor_scalar(dst, src, s[:, 12:13], s[:, 15:16], ALU.subtract, ALU.mult),
             reads=[r_src, r_s], writes=[r_dst])
        c.op("pool", lambda: nc.gpsimd.tensor_tensor(dst, dst, lnB[gi][:], ALU.mult), reads=[r_dst, r_ln[gi]], writes=[r_dst])
        c.op("pool", lambda: nc.gpsimd.tensor_tensor(dst, dst, lnB[bi][:], ALU.add), reads=[r_dst, r_ln[bi]], writes=[r_dst])

    epsT = sb("epsT", [128, 1], F32); r_eps = Res("eps")
    c.op("dve", lambda: nc.vector.memset(epsT[:], LN_EPS), writes=[r_eps])

    def issue_wd(q):
        e_ = q % n_exp
        c.dma("pool", wd[q % 2][:], wdn[e_].rearrange("(c p) d -> p c d", p=128), writes=[r_wd[q % 2]])

    def issue_wu(gi):
        e_ = (gi // 8) % n_exp
        fc_ = gi % 8
        c.dma("pool", wuc[gi % 4][:], wup[e_][:, fc_ * 256:(fc_ + 1) * 256].rearrange("(c p) f -> p c f", p=128), writes=[r_wuc[gi % 4]])

    issue_wd(0)
    issue_wu(0)
    issue_wu(1)
    tmp_i = 0
    pg_i = 0
    po_i = 0
    for blk in range(n_blk):
        t0 = blk * TB
        c.dma("pool", yTb[:], yT[:, t0:t0 + TB].rearrange("(c p) t -> p c t", p=128), writes=[r_yTb])
        c.dma("pool", woutb[:], w_out.rearrange("(c p) d -> p c d", p=128), writes=[r_wout])
        for i in range(NTL):
            b2 = i % 2
            c.dma("sp", xt[b2][:], x[t0 + i * 128:t0 + (i + 1) * 128, :], writes=[r_xt[b2]])
            for h in range(2):
                for k in range(8):
                    c.op("pe", lambda: nc.tensor.matmul(B[h][:], yTb[:, k, i * 128:(i + 1) * 128],
                                                        woutb[:, k, h * 512:(h + 1) * 512], start=(k == 0), stop=(k == 7)),
                         reads=[r_yTb, r_wout], writes=[rB[h]], acc=(k > 0))
                c.op("dve", lambda: nc.vector.scalar_tensor_tensor(zt[b2][:, h * 512:(h + 1) * 512], xt[b2][:, h * 512:(h + 1) * 512],
                                                                   ALPHA, B[h][:], ALU.mult, ALU.add),
                     reads=[r_xt[b2], rB[h]], writes=[r_zt[b2]], acc=(h > 0))
            layer_norm(zt[b2][:], r_zt[b2], yacc[:, i, :], r_yacc[i], sm[b2], r_sm[b2], 0, 1)
            for k in range(8):
                bk = 2 + k // 4
                c.op("pe", lambda: nc.tensor.transpose(B[bk][:, (k % 4) * 128:(k % 4 + 1) * 128],
                                                       yacc[:, i, k * 128:(k + 1) * 128], ident[:]),
                     reads=[r_yacc[i], r_ident], writes=[rB[bk]], acc=(k % 4 > 0))
            for hh in range(2):
                c.op("act", lambda: nc.scalar.copy(x1T32[b2][:, hh * 4:(hh + 1) * 4, :].rearrange("p a b -> p (a b)"), B[2 + hh][:]),
                     reads=[rB[2 + hh]], writes=[r_x1T32[b2]], acc=(hh > 0))
            c.op("pool", lambda: nc.gpsimd.tensor_copy(x1Tb[:, :, i * 128:(i + 1) * 128], x1T32[b2][:]),
                 reads=[r_x1T32[b2]], writes=[r_x1Tb[i]])
            for k in range(8):
                c.op("pe", lambda: nc.tensor.matmul(B[4][:, 0:32], x1T32[b2][:, k, :], rw32[:, k, :], start=(k == 0), stop=(k == 7)),
                     reads=[r_x1T32[b2], r_rw], writes=[rB[4]], acc=(k > 0))
            s = sm[b2]; r_s = r_sm[b2]
            c.op("dve", lambda: nc.vector.tensor_tensor(s[:, 32:64], B[4][:, 0:32], rbB[:], ALU.add), reads=[rB[4], r_rb], writes=[r_s])
            c.op("dve", lambda: nc.vector.max(s[:, 16:24], s[:, 32:64]), reads=[r_s], writes=[r_s])
            c.op("dve", lambda: nc.vector.tensor_scalar(s[:, 64:96], s[:, 32:64], s[:, 19:20], None, ALU.is_ge), reads=[r_s], writes=[r_s])
            c.op("dve", lambda: nc.vector.tensor_scalar(s[:, 24:25], s[:, 16:17], -1.0, None, ALU.mult), reads=[r_s], writes=[r_s])
            c.op("act", lambda: nc.scalar.activation(s[:, 96:128], s[:, 32:64], AF.Exp, bias=s[:, 24:25], scale=1.0), reads=[r_s], writes=[r_s])
            c.op("dve", lambda: nc.vector.tensor_tensor(s[:, 96:128], s[:, 96:128], s[:, 64:96], ALU.mult), reads=[r_s], writes=[r_s])
            c.op("dve", lambda: nc.vector.reduce_sum(s[:, 25:26], s[:, 96:128], AX.X), reads=[r_s], writes=[r_s])
            c.op("dve", lambda: nc.vector.reciprocal(s[:, 26:27], s[:, 25:26]), reads=[r_s], writes=[r_s])
            c.op("dve", lambda: nc.vector.tensor_scalar(gates[:, i, :], s[:, 96:128], s[:, 26:27], None, ALU.mult), reads=[r_s], writes=[r_gates[i]])
            c.op("pe", lambda: nc.tensor.transpose(B[4][0:32, 128:256], gates[:, i, :], ident[:]),
                 reads=[r_gates[i], r_ident], writes=[rB[4]])
            c.op("act", lambda: nc.scalar.copy(gT[:, i * 128:(i + 1) * 128], B[4][0:32, 128:256]), reads=[rB[4]], writes=[r_gT[i]])
            for h in range(2):
                c.op("pe", lambda: nc.tensor.matmul(B[5 + h][:], gT[:, i * 128:(i + 1) * 128], bdn32[:, h * 512:(h + 1) * 512],
                                                    start=True, stop=True), reads=[r_gT[i], r_bdn], writes=[rB[5 + h]])
                c.op("dve", lambda: nc.vector.scalar_tensor_tensor(yacc[:, i, h * 512:(h + 1) * 512], yacc[:, i, h * 512:(h + 1) * 512],
                                                                   ALPHA, B[5 + h][:], ALU.mult, ALU.add),
                     reads=[r_yacc[i], rB[5 + h]], writes=[r_yacc[i]])
        for e in range(n_exp):
            q = blk * n_exp + e
            ab = q % 2
            if q + 1 < n_blk * n_exp:
                issue_wd(q + 1)
            for fc in range(8):
                gi = q * 8 + fc
                if gi + 2 < n_blk * n_exp * 8:
                    issue_wu(gi + 2)
                ws = gi % 4
                row = e * 8 + fc
                for g in range(2):
                    pg = (pg_i % 2); pl = 2 + (pg_i % 2); pg_i += 1
                    for two, bank in ((0, pg), (1, pl)):
                        for k in range(8):
                            c.op("pe", lambda: nc.tensor.matmul(B[bank][:], wuc[ws][:, k, two:256:2], x1Tb[:, k, g * 512:(g + 1) * 512],
                                                                start=(k == 0), stop=(k == 7)),
                                 reads=[r_wuc[ws]] + r_x1Tb[g * 4:(g + 1) * 4], writes=[rB[bank]], acc=(k > 0))
                    ti = tmp_i % NTMP; tmp_i += 1
                    c.op("dve", lambda: nc.vector.tensor_scalar(tg[ti][:], B[pg][:], bupT[:, 0, row:row + 1], 7.0, ALU.add, ALU.min),
                         reads=[rB[pg], r_bupT], writes=[r_tg[ti]])
                    c.op("act", lambda: nc.scalar.activation(tsg[ti][:], tg[ti][:], AF.Sigmoid, scale=1.702),
                         reads=[r_tg[ti]], writes=[r_tsg[ti]])
                    c.op("dve", lambda: nc.vector.tensor_scalar(tl[ti][:], B[pl][:], bupT[:, 1, row:row + 1], -6.0, ALU.add, ALU.max),
                         reads=[rB[pl], r_bupT], writes=[r_tl[ti]])
                    c.op("pool", lambda: nc.gpsimd.tensor_tensor(tg[ti][:], tg[ti][:], tsg[ti][:], ALU.mult),
                         reads=[r_tg[ti], r_tsg[ti]], writes=[r_tg[ti]])
                    c.op("dve", lambda: nc.vector.scalar_tensor_tensor(act[ab][:, fc, g * 512:(g + 1) * 512], tl[ti][:], 8.0, tg[ti][:],
                                                                       ALU.min, ALU.mult),
                         reads=[r_tg[ti], r_tl[ti]], writes=[r_act[ab]], acc=(fc + g > 0))
            for g in range(2):
                for j in range(4):
                    tI = g * 4 + j
                    for h in range(2):
                        po = 4 + (po_i % 4); po_i += 1
                        for fc in range(8):
                            c.op("pe", lambda: nc.tensor.matmul(B[po][:], act[ab][:, fc, tI * 128:(tI + 1) * 128],
                                                                wd[ab][:, fc, h * 512:(h + 1) * 512], start=(fc == 0), stop=(fc == 7)),
                                 reads=[r_act[ab], r_wd[ab]], writes=[rB[po]], acc=(fc > 0))
                        c.op("dve", lambda: nc.vector.scalar_tensor_tensor(yacc[:, tI, h * 512:(h + 1) * 512], B[po][:],
                                                                           gates[:, tI, e:e + 1], yacc[:, tI, h * 512:(h + 1) * 512],
                                                                           ALU.mult, ALU.add),
                             reads=[rB[po], r_gates[tI], r_yacc[tI]], writes=[r_yacc[tI]])
        for i in range(NTL):
            b2 = i % 2
            layer_norm(yacc[:, i, :], r_yacc[i], ot[b2][:], r_ot[b2], sm[b2], r_sm[b2], 2, 3)
            c.dma("sp", xo[t0 + i * 128:t0 + (i + 1) * 128, :], ot[b2][:], reads=[r_ot[b2]], writes=[r_out[b2]])
    c.finish("sp", r_out)
    print("F: ins", c.n_ins, "waits", c.n_wait, "sbuf left", nc.sbuf_bytes_remaining)


GN_EPS = 1e-6


def me_host_consts(T):
    pos = np.arange(T, dtype=np.float32)
    inv = (10000.0 ** (-np.arange(0, 64, 2, dtype=np.float32) / 64)).astype(np.float32)
    ang = pos[:, None] * inv[None, :]
    cos = np.cos(ang).astype(np.float32).T
    sin = np.sin(ang).astype(np.float32).T
    cosF = np.concatenate([cos, cos], 0)
    sinS = np.concatenate([-sin, sin], 0)
    s8 = np.float32(0.125)
    cq = np.concatenate([cosF, cosF * s8], 0)
    sq = np.concatenate([sinS, sinS * s8], 0)
    ck = np.concatenate([cosF * s8, cosF], 0)
    sk = np.concatenate([sinS * s8, sinS], 0)
    tab = np.ascontiguousarray(np.stack([cq, sq, ck, sk], 0)).astype(np.float32)
    i = np.arange(128, dtype=np.float32)
    m = i[:, None]; c = i[None, :]
    cst = np.zeros((128, 6, 128), np.float32)
    cst[:, 0, :] = np.maximum(c - m, 0)
    cst[:, 1, :] = (c >= m)
    cst[:, 2, :] = np.maximum(m - c, 0)
    cst[:, 3, :] = (m > c)
    cst[:, 4, :] = c + 1.0
    cst[:, 5, :] = 128.0 - c
    col = np.zeros((128, 4), np.float32)
    col[:, 0] = 127.0 - i
    col[:, 1] = i
    col[:, 2] = 128.0
    o = np.arange(17)[None, :, None]
    delta = (o - 8) * 128 + m[:, None, :].astype(np.int64) - c[None, :, :].astype(np.int64)
    delta = delta.astype(np.int64)
    ad = np.abs(delta)
    mm = (ad <= 64).astype(np.float32) + ((delta % 4 == 0) & (ad <= 256)) + ((delta % 16 == 0) & (ad <= 1024))
    mm = mm.astype(np.float32).reshape(128, 17 * 128)
    return dict(tab=tab, cst=cst.reshape(128, 768), col=col, mm=mm, idn=np.eye(128, dtype=np.float32))


def me_weight_cols(head):
    H = 64
    base = lambda blk: blk * 512 + head * H
    rng = lambda b: list(range(base(b), base(b) + H))
    sw = lambda b: list(range(base(b) + 32, base(b) + 64)) + list(range(base(b), base(b) + 32))
    cols = []
    cols += rng(0) + rng(5)
    cols += sw(0) + sw(5)
    cols += rng(1) + rng(6)
    cols += sw(1) + sw(6)
    cols += rng(2) + rng(7)
    cols += rng(3) + rng(4)
    return np.array(cols)


def build_ME(T=16384):
    nc = bass.Bass("TRN2", target_bir_lowering=False)
    c = Ctx(nc)
    xT = nc.dram_tensor("xT", [1024, T], F32, kind="ExternalInput").ap()
    wsel = nc.dram_tensor("wsel", [1024, 768], F32, kind="ExternalInput").ap()
    dec = nc.dram_tensor("dec", [1, 2], F32, kind="ExternalInput").ap()
    tab = nc.dram_tensor("tab", [4, 128, T], F32, kind="ExternalInput").ap()
    cst = nc.dram_tensor("cst", [128, 768], F32, kind="ExternalInput").ap()
    col = nc.dram_tensor("col", [128, 4], F32, kind="ExternalInput").ap()
    mm = nc.dram_tensor("mm", [128, 17 * 128], F32, kind="ExternalInput").ap()
    idn = nc.dram_tensor("idn", [128, 128], F32, kind="ExternalInput").ap()
    y = nc.dram_tensor("y", [T, 128], F32, kind="ExternalOutput").ap()
    emit_ME(nc, c, xT, wsel, dec, tab, cst, col, mm, idn, y, T)
    return nc


def emit_ME(nc, c, xT, wsel, dec, tab, cst, col, mm, idn, y, T, pfx="E"):
    NCH = T // 128
    sb = lambda n, s, d: c.sb(pfx + n, s, d)
    V = nc.vector
    A = nc.scalar
    identb = sb("identb", [128, 128], BF16); r_identb = Res("identb")
    c.dma("pool", identb[:], idn, writes=[r_identb])
    wb = sb("wb", [128, 8, 768], BF16); r_wb = Res("wb")
    c.dma("pool", wb[:], wsel.rearrange("(c p) e -> p c e", p=128), writes=[r_wb])
    mmb = sb("mmb", [128, 17 * 128], BF16); r_mm = Res("mm")
    c.dma("pool", mmb[:], mm, writes=[r_mm])
    cs = sb("cs", [128, 6, 128], F32); r_cs = Res("cs")
    c.dma("sp", cs[:].rearrange("p a b -> p (a b)"), cst, writes=[r_cs])
    cl = sb("cl", [128, 4], F32); r_cl = Res("cl")
    c.dma("sp", cl[:], col, writes=[r_cl])
    dl = sb("dl", [128, 16], F32); r_dl = Res("dl")
    c.dma("sp", dl[:, 0:2], dec.partition_broadcast(128), writes=[r_dl])
    ops = [
        lambda: V.tensor_scalar(dl[:, 2:4], dl[:, 0:2], -1.0, None, ALU.mult),
        lambda: V.tensor_tensor(dl[:, 2:4], dl[:, 2:4], dl[:, 0:2], ALU.max),
    ]
    for f in ops:
        c.op("dve", f, reads=[r_dl], writes=[r_dl])
    c.op("act", lambda: A.activation(dl[:, 2:4], dl[:, 2:4], AF.Exp, scale=-1.0), reads=[r_dl], writes=[r_dl])
    ops = [
        lambda: V.tensor_scalar(dl[:, 4:6], dl[:, 2:4], 2.0, None, ALU.add),
        lambda: V.reciprocal(dl[:, 4:6], dl[:, 4:6]),
        lambda: V.tensor_tensor(dl[:, 4:6], dl[:, 4:6], dl[:, 2:4], ALU.mult),
        lambda: V.tensor_tensor(dl[:, 6:8], dl[:, 4:6], dl[:, 4:6], ALU.mult),
        lambda: V.tensor_scalar(dl[:, 8:10], dl[:, 6:8], 1.0 / 11, 1.0 / 9, ALU.mult, ALU.add),
        lambda: V.tensor_tensor(dl[:, 8:10], dl[:, 8:10], dl[:, 6:8], ALU.mult),
        lambda: V.tensor_scalar(dl[:, 8:10], dl[:, 8:10], 1.0 / 7, None, ALU.add),
        lambda: V.tensor_tensor(dl[:, 8:10], dl[:, 8:10], dl[:, 6:8], ALU.mult),
        lambda: V.tensor_scalar(dl[:, 8:10], dl[:, 8:10], 1.0 / 5, None, ALU.add),
        lambda: V.tensor_tensor(dl[:, 8:10], dl[:, 8:10], dl[:, 6:8], ALU.mult),
        lambda: V.tensor_scalar(dl[:, 8:10], dl[:, 8:10], 1.0 / 3, None, ALU.add),
        lambda: V.tensor_tensor(dl[:, 8:10], dl[:, 8:10], dl[:, 6:8], ALU.mult),
        lambda: V.tensor_scalar(dl[:, 8:10], dl[:, 8:10], 1.0, None, ALU.add),
        lambda: V.tensor_tensor(dl[:, 8:10], dl[:, 8:10], dl[:, 4:6], ALU.mult),
        lambda: V.tensor_scalar(dl[:, 10:12], dl[:, 0:2], 0.0, None, ALU.min),
        lambda: V.scalar_tensor_tensor(dl[:, 10:12], dl[:, 8:10], -2.0, dl[:, 10:12], ALU.mult, ALU.add),
    ]
    for f in ops:
        c.op("dve", f, reads=[r_dl], writes=[r_dl])
    lgf = dl[:, 10:11]
    lgb = dl[:, 11:12]
    decT = sb("decT", [128, 2, 128], F32); r_dec = Res("decT")
    xi = sb("xi", [128, 2, 128], F32); r_xi = Res("xi")
    zc = sb("zc", [128, 4], F32); r_zc = Res("zc")
    c.op("act", lambda: A.activation(decT[:, 0, :], cs[:, 0, :], AF.Exp, scale=lgf), reads=[r_cs, r_dl], writes=[r_dec])
    c.op("act", lambda: A.activation(decT[:, 1, :], cs[:, 2, :], AF.Exp, scale=lgb), reads=[r_cs, r_dl], writes=[r_dec])
    c.op("dve", lambda: V.tensor_tensor(decT[:, 0, :], decT[:, 0, :], cs[:, 1, :], ALU.mult), reads=[r_dec, r_cs], writes=[r_dec])
    c.op("dve", lambda: V.tensor_tensor(decT[:, 1, :], decT[:, 1, :], cs[:, 3, :], ALU.mult), reads=[r_dec, r_cs], writes=[r_dec])
    c.op("act", lambda: A.activation(xi[:, 0, :], cs[:, 4, :], AF.Exp, scale=lgf), reads=[r_cs, r_dl], writes=[r_xi])
    c.op("act", lambda: A.activation(xi[:, 1, :], cs[:, 5, :], AF.Exp, scale=lgb), reads=[r_cs, r_dl], writes=[r_xi])
    c.op("act", lambda: A.activation(zc[:, 0:1], cl[:, 0:1], AF.Exp, scale=lgf), reads=[r_cl, r_dl], writes=[r_zc])
    c.op("act", lambda: A.activation(zc[:, 1:2], cl[:, 1:2], AF.Exp, scale=lgb), reads=[r_cl, r_dl], writes=[r_zc])
    c.op("act", lambda: A.activation(zc[:, 2:3], cl[:, 2:3], AF.Exp, scale=lgf), reads=[r_cl, r_dl], writes=[r_zc])
    c.op("act", lambda: A.activation(zc[:, 3:4], cl[:, 2:3], AF.Exp, scale=lgb), reads=[r_cl, r_dl], writes=[r_zc])
    epsT = sb("epsT", [128, 1], F32); r_eps = Res("eps")
    c.op("dve", lambda: V.memset(epsT[:], GN_EPS), writes=[r_eps])

    qkT = sb("qkT", [128, 2, T], BF16)
    r_qk = [Res("qk%d" % n) for n in range(NCH)]
    vall = sb("vall", [128, NCH, 64], BF16); r_v = [Res("v%d" % n) for n in range(NCH)]
    dvall = sb("dvall", [128, NCH, 66], BF16); r_dv = [Res("dv%d" % n) for n in range(NCH)]
    Rf = sb("Rf", [64, NCH + 1, 64], BF16); r_Rf = [Res("Rf%d" % n) for n in range(NCH + 1)]
    Rrun = sb("Rrun", [64, 2, 64], F32); r_Rrun = [Res("Rrun0"), Res("Rrun1")]
    Rbb = [sb("Rbb%d" % i, [64, 64], BF16) for i in range(2)]; r_Rbb = [Res("Rbb0"), Res("Rbb1")]
    ones_dst = Res("dvones")
    c.op("pool", lambda: nc.gpsimd.memset(dvall[:, :, 64:66], 1.0), writes=[ones_dst])
    c.op("dve", lambda: V.memset(Rrun[:], 0.0), writes=r_Rrun)
    c.op("dve", lambda: V.memset(Rf[:, 0, :], 0.0), writes=[r_Rf[0]])
    c.op("dve", lambda: V.memset(Rbb[0][:], 0.0), writes=[r_Rbb[0]])

    NB = 2
    xTt = [sb("xTt%d" % i, [128, 8, 128], BF16) for i in range(NB)]; r_xTt = [Res("xTt%d" % i) for i in range(NB)]
    tbt = [sb("tbt%d" % i, [128, 4, 128], F32) for i in range(NB)]; r_tbt = [Res("tbt%d" % i) for i in range(NB)]
    prod = [sb("prod%d" % i, [128, 4, 128], F32) for i in range(NB)]; r_prod = [Res("prod%d" % i) for i in range(NB)]
    kz = [sb("kz%d" % i, [128, 64], BF16) for i in range(NB)]; r_kz = [Res("kz%d" % i) for i in range(NB)]
    scT = [sb("scT%d" % i, [128, 2, 128], BF16) for i in range(NB)]; r_scT = [Res("scT%d" % i) for i in range(NB)]
    qxi = [sb("qxi%d" % i, [64, 2, 128], BF16) for i in range(NB)]; r_qxi = [Res("qxi%d" % i) for i in range(NB)]
    st = [sb("st%d" % i, [128, 32], F32) for i in range(NB)]; r_st = [Res("st%d" % i) for i in range(NB)]
    xn = [sb("xn%d" % i, [128, 128], F32) for i in range(NB)]; r_xn = [Res("xn%d" % i) for i in range(NB)]
    sg = [sb("sg%d" % i, [128, 128], F32) for i in range(NB)]; r_sg = [Res("sg%d" % i) for i in range(NB)]
    yt = [sb("yt%d" % i, [128, 128], F32) for i in range(NB)]; r_yt = [Res("yt%d" % i) for i in range(NB)]
    pd = [sb("pd%d" % i, [128, 512], BF16) for i in range(3)]; r_pd = [Res("pd%d" % i) for i in range(3)]
    r_y = [Res("y0"), Res("y1")]
    PA = c.ps(pfx + "PA", [128, 512], F32); r_PA = Res("PA")
    PV = c.ps(pfx + "PV", [128, 512], F32); r_PV = Res("PV")
    PT = c.ps(pfx + "PT", [128, 1024], BF16); r_PT = Res("PT")
    PS = c.ps(pfx + "PS", [128, 512], F32); r_PS = Res("PS")
    PO = c.ps(pfx + "PO", [128, 512], F32); r_PO = Res("PO")
    PD = [c.ps(pfx + "PD%d" % i, [128, 512], F32) for i in range(3)]; r_PD = [Res("PD%d" % i) for i in range(3)]

    def load_x(n, want_tab):
        b = n % NB
        c.dma("pool", xTt[b][:], xT[:, n * 128:(n + 1) * 128].rearrange("(c p) t -> p c t", p=128), writes=[r_xTt[b]])
        if want_tab:
            c.dma("sp", tbt[b][:], tab[:, :, n * 128:(n + 1) * 128].rearrange("a p t -> p a t"), writes=[r_tbt[b]])

    def ktrans_state(n, direction, b):
        c.op("pe", lambda: nc.tensor.transpose(PT[:, 0:64], qkT[0:64, 1, n * 128:(n + 1) * 128], identb[0:64, 0:64]),
             reads=[r_qk[n], r_identb], writes=[r_PT])
        c.op("dve", lambda: V.tensor_scalar(kz[b][:], PT[:, 0:64], zc[:, direction:direction + 1], None, ALU.mult),
             reads=[r_PT, r_zc], writes=[r_kz[b]])
        c.op("pe", lambda: nc.tensor.matmul(PS[0:64, 0:64], kz[b][:], vall[:, n, :], start=True, stop=True),
             reads=[r_kz[b], r_v[n]], writes=[r_PS])

    load_x(0, True)
    for n in range(NCH):
        b = n % NB
        if n + 1 < NCH:
            load_x(n + 1, True)
        for g in range(4):
            for k in range(8):
                c.op("pe", lambda: nc.tensor.matmul(PA[:, g * 128:(g + 1) * 128], wb[:, k, g * 128:(g + 1) * 128], xTt[b][:, k, :],
                                                    start=(k == 0), stop=(k == 7)),
                     reads=[r_wb, r_xTt[b]], writes=[r_PA], acc=(g + k > 0))
        for k in range(8):
            c.op("pe", lambda: nc.tensor.matmul(PV[:, 0:128], xTt[b][:, k, :], wb[:, k, 512:640], start=(k == 0), stop=(k == 7)),
                 reads=[r_wb, r_xTt[b]], writes=[r_PV], acc=(k > 0))
        c.op("dve", lambda: V.tensor_tensor(prod[b][:], PA[:].rearrange("p (a t) -> p a t", a=4), tbt[b][:], ALU.mult),
             reads=[r_PA, r_tbt[b]], writes=[r_prod[b]])
        c.op("dve", lambda: V.tensor_tensor(qkT[:, :, n * 128:(n + 1) * 128], prod[b][:, 0:4:2, :], prod[b][:, 1:4:2, :], ALU.add),
             reads=[r_prod[b]], writes=[r_qk[n]])
        c.op("act", lambda: A.copy(vall[:, n, :], PV[:, 0:64]), reads=[r_PV], writes=[r_v[n]])
        c.op("act", lambda: A.copy(dvall[:, n, 0:64], PV[:, 64:128]), reads=[r_PV, ones_dst], writes=[r_dv[n]])
        ktrans_state(n, 0, b)
        c.op("dve", lambda: V.scalar_tensor_tensor(Rrun[:, 0, :], Rrun[:, 0, :], zc[0:64, 2:3], PS[0:64, 0:64], ALU.mult, ALU.add),
             reads=[r_Rrun[0], r_zc, r_PS], writes=[r_Rrun[0]])
        c.op("act", lambda: A.copy(Rf[:, n + 1, :], Rrun[:, 0, :]), reads=[r_Rrun[0]], writes=[r_Rf[n + 1]])

    load_x(NCH - 1, False)
    pd_i = 0
    for n in range(NCH - 1, -1, -1):
        b = n % NB
        rb_cur = (NCH - 1 - n) % 2
        if n - 1 >= 0:
            load_x(n - 1, False)
        sl = slice(n * 128, (n + 1) * 128)
        for k in range(8):
            c.op("pe", lambda: nc.tensor.matmul(PV[:, 0:128], xTt[b][:, k, :], wb[:, k, 640:768], start=(k == 0), stop=(k == 7)),
                 reads=[r_wb, r_xTt[b]], writes=[r_PV], acc=(k > 0))
        c.op("pe", lambda: nc.tensor.matmul(PS[:, 128:256], qkT[0:64, 1, sl], qkT[0:64, 0, sl], start=True, stop=True),
             reads=[r_qk[n]], writes=[r_PS])
        c.op("dve", lambda: V.tensor_tensor(scT[b][:], PS[:, 128:256].unsqueeze(1).broadcast_to([128, 2, 128]), decT[:], ALU.mult),
             reads=[r_PS, r_dec], writes=[r_scT[b]])
        c.op("pool", lambda: nc.gpsimd.tensor_tensor(qxi[b][:], qkT[0:64, 0, sl].unsqueeze(1).broadcast_to([64, 2, 128]), xi[0:64], ALU.mult),
             reads=[r_qk[n], r_xi], writes=[r_qxi[b]])
        c.op("pe", lambda: nc.tensor.matmul(PO[:, 0:64], scT[b][:, 0, :], vall[:, n, :], start=True, stop=False),
             reads=[r_scT[b], r_v[n]], writes=[r_PO])
        c.op("pe", lambda: nc.tensor.matmul(PO[:, 0:64], qxi[b][:, 0, :], Rf[:, n, :], start=False, stop=True),
             reads=[r_qxi[b], r_Rf[n]], writes=[r_PO], acc=True)
        c.op("pe", lambda: nc.tensor.matmul(PO[:, 64:128], scT[b][:, 1, :], vall[:, n, :], start=True, stop=False),
             reads=[r_scT[b], r_v[n]], writes=[r_PO], acc=True)
        c.op("pe", lambda: nc.tensor.matmul(PO[:, 64:128], qxi[b][:, 1, :], Rbb[rb_cur][:], start=False, stop=True),
             reads=[r_qxi[b], r_Rbb[rb_cur]], writes=[r_PO], acc=True)
        if n > 0:
            ktrans_state(n, 1, b)
            c.op("dve", lambda: V.scalar_tensor_tensor(Rrun[:, 1, :], Rrun[:, 1, :], zc[0:64, 3:4], PS[0:64, 0:64], ALU.mult, ALU.add),
                 reads=[r_Rrun[1], r_zc, r_PS], writes=[r_Rrun[1]])
            c.op("act", lambda: A.copy(Rbb[1 - rb_cur][:], Rrun[:, 1, :]), reads=[r_Rrun[1]], writes=[r_Rbb[1 - rb_cur]])
        s = st[b]; rs = r_st[b]
        c.op("dve", lambda: V.bn_stats(s[:, 0:6], PO[:, 0:64]), reads=[r_PO], writes=[rs])
        c.op("dve", lambda: V.bn_stats(s[:, 6:12], PO[:, 64:128]), reads=[r_PO], writes=[rs])
        c.op("dve", lambda: V.bn_aggr(s[:, 12:14], s[:, 0:6]), reads=[rs], writes=[rs])
        c.op("dve", lambda: V.bn_aggr(s[:, 14:16], s[:, 6:12]), reads=[rs], writes=[rs])
        c.op("act", lambda: A.activation(s[:, 16:18], s[:, 13:16:2], AF.Ln, bias=epsT[:, 0:1], scale=1.0), reads=[rs, r_eps], writes=[rs])
        c.op("act", lambda: A.activation(s[:, 16:18], s[:, 16:18], AF.Exp, scale=-0.5), reads=[rs], writes=[rs])
        c.op("dve", lambda: V.tensor_scalar(xn[b][:, 0:64], PO[:, 0:64], s[:, 12:13], s[:, 16:17], ALU.subtract, ALU.mult),
             reads=[r_PO, rs], writes=[r_xn[b]])
        c.op("dve", lambda: V.tensor_scalar(xn[b][:, 64:128], PO[:, 64:128], s[:, 14:15], s[:, 17:18], ALU.subtract, ALU.mult),
             reads=[r_PO, rs], writes=[r_xn[b]])
        c.op("act", lambda: A.activation(sg[b][:], PV[:, 0:128], AF.Exp, scale=-1.0), reads=[r_PV], writes=[r_sg[b]])
        c.op("pool", lambda: nc.gpsimd.tensor_scalar(sg[b][:], sg[b][:], 1.0, None, ALU.add), reads=[r_sg[b]], writes=[r_sg[b]])
        c.op("dve", lambda: V.reciprocal(sg[b][:], sg[b][:]), reads=[r_sg[b]], writes=[r_sg[b]])
        c.op("dve", lambda: V.tensor_tensor(sg[b][:], sg[b][:], PV[:, 0:128], ALU.mult), reads=[r_sg[b], r_PV], writes=[r_sg[b]])
        c.op("pool", lambda: nc.gpsimd.tensor_tensor(xn[b][:], xn[b][:], sg[b][:], ALU.mult), reads=[r_xn[b], r_sg[b]], writes=[r_xn[b]])
        c.op("pool", lambda: nc.gpsimd.tensor_tensor(yt[b][:, 0:64], xn[b][:, 0:64], xn[b][:, 64:128], ALU.add),
             reads=[r_xn[b]], writes=[r_yt[b]])
        kts = [kt for kt in range(n - 8, n + 9) if 0 <= kt < NCH]
        groups = [kts[i:i + 4] for i in range(0, len(kts), 4)]
        first = True
        for gi, grp in enumerate(groups):
            pb = pd_i % 3; pd_i += 1
            for j, kt in enumerate(grp):
                c.op("pe", lambda: nc.tensor.matmul(PD[pb][:, j * 128:(j + 1) * 128], qkT[64:128, 1, kt * 128:(kt + 1) * 128],
                                                    qkT[64:128, 0, sl], start=True, stop=True),
                     reads=[r_qk[kt], r_qk[n]], writes=[r_PD[pb]], acc=(j > 0))
            w = len(grp) * 128
            o0 = (grp[0] - n + 8) * 128
            c.op("act", lambda: A.activation(pd[pb][:, 0:w], PD[pb][:, 0:w], AF.Exp), reads=[r_PD[pb]], writes=[r_pd[pb]])
            c.op("pool", lambda: nc.gpsimd.tensor_tensor(pd[pb][:, 0:w], pd[pb][:, 0:w], mmb[:, o0:o0 + w], ALU.mult),
                 reads=[r_pd[pb], r_mm], writes=[r_pd[pb]])
            for j, kt in enumerate(grp):
                last = (gi == len(groups) - 1 and j == len(grp) - 1)
                c.op("pe", lambda: nc.tensor.matmul(PO[:, 256:321], pd[pb][:, j * 128:(j + 1) * 128], dvall[:, kt, 0:65],
                                                    start=first, stop=last),
                     reads=[r_pd[pb], r_dv[kt]], writes=[r_PO], acc=True)
                first = False
        c.op("dve", lambda: V.reciprocal(s[:, 20:21], PO[:, 320:321]), reads=[r_PO, rs], writes=[rs])
        c.op("dve", lambda: V.tensor_scalar(yt[b][:, 64:128], PO[:, 256:320], s[:, 20:21], None, ALU.mult),
             reads=[r_PO, rs], writes=[r_yt[b]])
        c.dma("sp", y[sl, :], yt[b][:], reads=[r_yt[b]], writes=[r_y[b]])
    c.finish("sp", r_y)
    print("ME: ins", c.n_ins, "waits", c.n_wait, "sbuf left", nc.sbuf_bytes_remaining)


NEG = -30000.0


def mo_patterns(NCH):
    pats = []
    for o in range(5):
        pats.append((4, 4 + o - 2))
    for n in (0, 1, NCH - 2, NCH - 1):
        kts = range(0, 4) if n < 2 else range(NCH - 4, NCH)
        for kt in kts:
            pats.append((n, kt))
    return pats


def mo_keys(n, NCH):
    if 2 <= n <= NCH - 3:
        return [(n + o - 2, o) for o in range(5)]
    idx = {0: 0, 1: 1, NCH - 2: 2, NCH - 1: 3}[n]
    kts = range(0, 4) if n < 2 else range(NCH - 4, NCH)
    return [(kt, 5 + idx * 4 + j) for j, kt in enumerate(kts)]


def mo_host_bias(rpb2, T):
    NCH = T // 128
    rows = T // 64
    pats = mo_patterns(NCH)
    m = np.arange(128)[:, None]
    c = np.arange(128)[None, :]
    out = np.full((128, 2, len(pats), 128), NEG, np.float32)
    for pi, (n, kt) in enumerate(pats):
        rm = 2 * kt + m // 64; wm = m % 64
        rc = 2 * n + c // 64; wc = c % 64
        r0 = np.clip(rc - 4, 0, rows - 8)
        c0 = np.clip(wc - 8, 0, 64 - 16)
        valid = (rm >= r0) & (rm < r0 + 8) & (wm >= c0) & (wm < c0 + 16)
        ro = np.clip(rm - rc + 7, 0, 14)
        co = np.clip(wm - wc + 15, 0, 30)
        for h in range(2):
            g = rpb2[h][ro, co]
            out[:, h, pi, :] = np.where(valid, g, np.float32(NEG))
    return out.reshape(128, 2 * len(pats) * 128)


def mo_weight_cols(core):
    h0, h1 = 2 * core, 2 * core + 1
    rng = lambda blk, h: list(range(blk * 1024 + h * 64, blk * 1024 + (h + 1) * 64))
    return np.array(rng(0, h0) + rng(0, h1) + rng(1, h0) + rng(1, h1) + rng(2, h0) + rng(2, h1))


def build_MO(T=16384):
    nc = bass.Bass("TRN2", target_bir_lowering=False)
    c = Ctx(nc)
    xT = nc.dram_tensor("xT", [1024, T], F32, kind="ExternalInput").ap()
    wsel = nc.dram_tensor("wsel", [1024, 384], F32, kind="ExternalInput").ap()
    bias = nc.dram_tensor("bias", [128, 2 * 21 * 128], F32, kind="ExternalInput").ap()
    idn = nc.dram_tensor("idn", [128, 128], F32, kind="ExternalInput").ap()
    y = nc.dram_tensor("y", [T, 128], F32, kind="ExternalOutput").ap()
    emit_MO(nc, c, xT, wsel, bias, idn, y, T)
    return nc


def emit_MO(nc, c, xT, wsel, bias, idn, y, T, pfx="O"):
    NCH = T // 128
    sb = lambda n, s, d: c.sb(pfx + n, s, d)
    V = nc.vector
    A = nc.scalar
    identb = sb("identb", [128, 128], BF16); r_identb = Res("identb")
    c.dma("pool", identb[:], idn, writes=[r_identb])
    wb = sb("wb", [128, 8, 384], BF16); r_wb = Res("wb")
    c.dma("pool", wb[:], wsel.rearrange("(c p) e -> p c e", p=128), writes=[r_wb])
    biasb = sb("biasb", [128, 2, 21, 128], BF16); r_bias = Res("bias")
    c.dma("pool", biasb[:].rearrange("p a b c -> p (a b c)"), bias, writes=[r_bias])

    qkT = sb("qkT", [128, 2, T], BF16); r_qk = [Res("qk%d" % n) for n in range(NCH)]
    vall = sb("vall", [128, NCH, 2, 66], BF16); r_v = [Res("v%d" % n) for n in range(NCH)]
    ones_dst = Res("ones")
    c.op("pool", lambda: nc.gpsimd.memset(vall[:, :, :, 64:66], 1.0), writes=[ones_dst])
    NB = 2
    xTt = [sb("xTt%d" % i, [128, 8, 128], BF16) for i in range(NB)]; r_xTt = [Res("xTt%d" % i) for i in range(NB)]
    pd = [sb("pd%d" % i, [128, 640], BF16) for i in range(3)]; r_pd = [Res("pd%d" % i) for i in range(3)]
    yt = [sb("yt%d" % i, [128, 128], F32) for i in range(NB)]; r_yt = [Res("yt%d" % i) for i in range(NB)]
    st = [sb("st%d" % i, [128, 8], F32) for i in range(NB)]; r_st = [Res("st%d" % i) for i in range(NB)]
    r_y = [Res("y0"), Res("y1")]
    PA = c.ps(pfx + "PA", [128, 512], F32); r_PA = Res("PA")
    PV = c.ps(pfx + "PV", [128, 512], F32); r_PV = Res("PV")
    PO = c.ps(pfx + "PO", [128, 512], F32); r_PO = Res("PO")
    PD = [c.ps(pfx + "PD%d" % i, [128, 1024], F32) for i in range(2)]; r_PD = [Res("PD%d" % i) for i in range(2)]

    def load_x(n):
        b = n % NB
        c.dma("pool", xTt[b][:], xT[:, n * 128:(n + 1) * 128].rearrange("(c p) t -> p c t", p=128), writes=[r_xTt[b]])

    load_x(0)
    for n in range(NCH):
        b = n % NB
        if n + 1 < NCH:
            load_x(n + 1)
        for g in range(2):
            for k in range(8):
                c.op("pe", lambda: nc.tensor.matmul(PA[:, g * 128:(g + 1) * 128], wb[:, k, g * 128:(g + 1) * 128], xTt[b][:, k, :],
                                                    start=(k == 0), stop=(k == 7)),
                     reads=[r_wb, r_xTt[b]], writes=[r_PA], acc=(g + k > 0))
        for k in range(8):
            c.op("pe", lambda: nc.tensor.matmul(PV[:, 0:128], xTt[b][:, k, :], wb[:, k, 256:384], start=(k == 0), stop=(k == 7)),
                 reads=[r_wb, r_xTt[b]], writes=[r_PV], acc=(k > 0))
        c.op("act", lambda: A.activation(qkT[:, 0, n * 128:(n + 1) * 128], PA[:, 0:128], AF.Copy, scale=0.125),
             reads=[r_PA], writes=[r_qk[n]])
        c.op("dve", lambda: V.tensor_copy(qkT[:, 1, n * 128:(n + 1) * 128], PA[:, 128:256]), reads=[r_PA], writes=[r_qk[n]])
        c.op("dve", lambda: V.tensor_copy(vall[:, n, :, 0:64], PV[:, 0:128].rearrange("p (h d) -> p h d", h=2)),
             reads=[r_PV, ones_dst], writes=[r_v[n]])

    pd_i = 0
    for n in range(NCH):
        b = n % NB
        sl = slice(n * 128, (n + 1) * 128)
        keys = mo_keys(n, NCH)
        for hh in range(2):
            hp = slice(hh * 64, (hh + 1) * 64)
            pb = pd_i % 2; sbi = pd_i % 3; pd_i += 1
            for j, (kt, pat) in enumerate(keys):
                c.op("pe", lambda: nc.tensor.matmul(PD[pb][:, j * 128:(j + 1) * 128], qkT[hp, 1, kt * 128:(kt + 1) * 128],
                                                    qkT[hp, 0, sl], start=True, stop=False),
                     reads=[r_qk[kt], r_qk[n]], writes=[r_PD[pb]], acc=(j > 0))
                c.op("pe", lambda: nc.tensor.matmul(PD[pb][:, j * 128:(j + 1) * 128], identb[:], biasb[:, hh, pat, :],
                                                    start=False, stop=True),
                     reads=[r_identb, r_bias], writes=[r_PD[pb]], acc=True)
            nk = len(keys)
            w0 = min(nk, 4) * 128
            c.op("act", lambda: A.activation(pd[sbi][:, 0:w0], PD[pb][:, 0:w0], AF.Exp), reads=[r_PD[pb]], writes=[r_pd[sbi]])
            if nk > 4:
                c.op("act", lambda: A.activation(pd[sbi][:, 512:640], PD[pb][:, 512:640], AF.Exp), reads=[r_PD[pb]], writes=[r_pd[sbi]], acc=True)
            for j, (kt, pat) in enumerate(keys):
                c.op("pe", lambda: nc.tensor.matmul(PO[:, hh * 128:hh * 128 + 65], pd[sbi][:, j * 128:(j + 1) * 128], vall[:, kt, hh, 0:65],
                                                    start=(j == 0), stop=(j == nk - 1)),
                     reads=[r_pd[sbi], r_v[kt]], writes=[r_PO], acc=(hh + j > 0))
        s = st[b]; rs = r_st[b]
        for hh in range(2):
            c.op("dve", lambda: V.reciprocal(s[:, hh:hh + 1], PO[:, hh * 128 + 64:hh * 128 + 65]), reads=[r_PO, rs], writes=[rs])
            c.op("dve", lambda: V.tensor_scalar(yt[b][:, hh * 64:(hh + 1) * 64], PO[:, hh * 128:hh * 128 + 64], s[:, hh:hh + 1], None, ALU.mult),
                 reads=[r_PO, rs], writes=[r_yt[b]])
        c.dma("sp", y[sl, :], yt[b][:], reads=[r_yt[b]], writes=[r_y[b]])
    c.finish("sp", r_y)
    print("MO: ins", c.n_ins, "waits", c.n_wait, "sbuf left", nc.sbuf_bytes_remaining)


N_CORES = 8
T_SEQ = 16384
_PROGS = {}


def _prog(name):
    if name not in _PROGS:
        _PROGS[name] = {"ME": lambda: build_ME(T_SEQ), "MO": lambda: build_MO(T_SEQ), "F": lambda: build_F(32, 2)}[name]()
    return _PROGS[name]


def _launch(nc, in_maps):
    res = run_bass_kernel_spmd(nc, in_maps, core_ids=list(range(N_CORES)))
    return res.results


def kernel(x, ab_w_in, ab_w_out, ret_decay, c_w_in, c_w_out, c_rpb, ln_g, ln_b,
           router_w, router_b, exp_w_up, exp_b_up, exp_w_down, exp_b_down):
    f32 = np.float32
    xs = np.ascontiguousarray(np.asarray(x, f32)[0])
    T = xs.shape[0]
    idn = np.eye(128, dtype=f32)
    me_c = me_host_consts(T)
    per = T // N_CORES
    for layer in range(4):
        j = layer // 2
        xT = np.ascontiguousarray(xs.T)
        ycat = np.empty((T, 1024), f32)
        if layer % 2 == 0:
            w_in = np.asarray(ab_w_in[j], f32)
            dec = np.asarray(ret_decay[j], f32)
            in_maps = [dict(xT=xT, wsel=np.ascontiguousarray(w_in[:, me_weight_cols(h)]),
                            dec=np.ascontiguousarray(dec[:, h][None, :]), **me_c) for h in range(N_CORES)]
            outs = _launch(_prog("ME"), in_maps)
            for h in range(N_CORES):
                yh = outs[h]["y"]
                ycat[:, h * 64:(h + 1) * 64] = yh[:, 0:64]
                ycat[:, 512 + h * 64:512 + (h + 1) * 64] = yh[:, 64:128]
            w_out = np.asarray(ab_w_out[j], f32)
        else:
            w_in = np.asarray(c_w_in[j], f32)
            rpb = np.asarray(c_rpb[j], f32)
            in_maps = [dict(xT=xT, wsel=np.ascontiguousarray(w_in[:, mo_weight_cols(cc)]),
                            bias=mo_host_bias(rpb[2 * cc:2 * cc + 2], T), idn=idn) for cc in range(N_CORES)]
            outs = _launch(_prog("MO"), in_maps)
            for cc in range(N_CORES):
                ycat[:, cc * 128:(cc + 1) * 128] = outs[cc]["y"]
            w_out = np.asarray(c_w_out[j], f32)
        lnp = np.ascontiguousarray(np.stack([ln_g[layer, 0], ln_b[layer, 0], ln_g[layer, 1], ln_b[layer, 1]], 0).astype(f32))
        common = dict(w_out=np.ascontiguousarray(w_out), lnp=lnp,
                      rw=np.ascontiguousarray(np.asarray(router_w[layer], f32)),
                      rb=np.ascontiguousarray(np.asarray(router_b[layer], f32)[None, :]),
                      wup=np.ascontiguousarray(np.asarray(exp_w_up[layer], f32)),
                      bup=np.ascontiguousarray(np.asarray(exp_b_up[layer], f32).reshape(256, 256)),
                      wdn=np.ascontiguousarray(np.asarray(exp_w_down[layer], f32)),
                      bdn=np.ascontiguousarray(np.asarray(exp_b_down[layer], f32)), idn=idn)
        in_maps = []
        for cc in range(N_CORES):
            sl = slice(cc * per, (cc + 1) * per)
            in_maps.append(dict(common, x=np.ascontiguousarray(xs[sl]), yT=np.ascontiguousarray(ycat[sl].T)))
        outs = _launch(_prog("F"), in_maps)
        xs = np.concatenate([outs[cc]["xo"] for cc in range(N_CORES)], 0)
    return xs[None].astype(f32)
```

```python
import numpy as np
import concourse.bass as bass
import concourse.mybir as mybir
from concourse.bass_utils import run_bass_kernel_spmd

F32 = mybir.dt.float32
BF16 = mybir.dt.bfloat16
AF = mybir.ActivationFunctionType
ALU = mybir.AluOpType
AX = mybir.AxisListType


class Res:
    __slots__ = ("name", "w", "r", "dsem", "dcnt")

    def __init__(self, name):
        self.name = name
        self.w = None
        self.r = []
        self.dsem = None
        self.dcnt = 0


class Ctx:
    def __init__(self, nc):
        self.nc = nc
        self.eng = {"pe": nc.tensor, "act": nc.scalar, "dve": nc.vector,
                    "pool": nc.gpsimd, "sp": nc.sync}
        self.sem = {k: nc.alloc_semaphore("c_" + k) for k in ("pe", "act", "dve", "pool")}
        self.cnt = {k: 0 for k in self.sem}
        self.seen = {k: {} for k in self.eng}
        self.semobj = dict(self.sem)
        self.n_dsem = 0
        self.n_wait = 0
        self.n_ins = 0

    def sb(self, name, shape, dt):
        return self.nc.alloc_sbuf_tensor(name, list(shape), dt)

    def ps(self, name, shape, dt=F32):
        return self.nc.alloc_psum_tensor(name, list(shape), dt)

    def _dsem(self, res):
        if res.dsem is None:
            key = "d%d" % self.n_dsem
            self.n_dsem += 1
            res.dsem = key
            self.semobj[key] = self.nc.alloc_semaphore(key)
        return res.dsem

    def _wait(self, e, deps):
        seen = self.seen[e]
        eng = self.eng[e]
        best = {}
        for d in deps:
            if d is None:
                continue
            k, v = d
            if best.get(k, 0) < v:
                best[k] = v
        for k, v in best.items():
            if seen.get(k, 0) < v:
                eng.wait_ge(self.semobj[k], v)
                seen[k] = v
                self.n_wait += 1

    def op(self, e, fn, reads=(), writes=(), acc=False):
        deps = [r.w for r in reads]
        if not acc:
            for w in writes:
                deps.append(w.w)
                deps.extend(w.r)
        self._wait(e, deps)
        ins = fn()
        self.cnt[e] += 1
        ins.then_inc(self.sem[e], 1)
        ev = (e, self.cnt[e])
        self.seen[e][e] = max(self.seen[e].get(e, 0), 0)
        for r in reads:
            r.r.append(ev)
        for w in writes:
            w.w = ev
            if not acc:
                w.r = []
        self.n_ins += 1
        return ins

    def dma(self, q, out, in_, reads=(), writes=(), **kw):
        assert len(writes) == 1
        wres = writes[0]
        deps = [r.w for r in reads]
        deps.append(wres.w)
        deps.extend(wres.r)
        self._wait(q, deps)
        key = self._dsem(wres)
        ins = self.eng[q].dma_start(out=out, in_=in_, **kw)
        wres.dcnt += 16
        ins.then_inc(self.semobj[key], 16)
        ev = (key, wres.dcnt)
        for r in reads:
            r.r.append(ev)
        wres.w = ev
        wres.r = []
        self.n_ins += 1
        return ins

    def dma_fn(self, q, fn, reads=(), writes=()):
        wres = writes[0]
        deps = [r.w for r in reads]
        deps.append(wres.w)
        deps.extend(wres.r)
        self._wait(q, deps)
        key = self._dsem(wres)
        ins = fn()
        wres.dcnt += 16
        ins.then_inc(self.semobj[key], 16)
        ev = (key, wres.dcnt)
        for r in reads:
            r.r.append(ev)
        wres.w = ev
        wres.r = []
        self.n_ins += 1
        return ins

    def dma_more(self, q, out, in_, reads=(), writes=(), **kw):
        wres = writes[0]
        deps = [r.w for r in reads]
        self._wait(q, deps)
        key = self._dsem(wres)
        ins = self.eng[q].dma_start(out=out, in_=in_, **kw)
        wres.dcnt += 16
        ins.then_inc(self.semobj[key], 16)
        ev = (key, wres.dcnt)
        for r in reads:
            r.r.append(ev)
        wres.w = ev
        self.n_ins += 1
        return ins

    def finish(self, q, resources):
        self._wait(q, [r.w for r in resources])


ALPHA = (2.0 * 4) ** 0.25
LN_EPS = 1e-5


def build_F2(n_exp=32, n_blk=2):
    nc = bass.Bass("TRN2", target_bir_lowering=False)
    c = Ctx(nc)
    TB = 1024
    NT = TB * n_blk
    D = 1024
    x = nc.dram_tensor("x", [NT, D], F32, kind="ExternalInput").ap()
    yT = nc.dram_tensor("yT", [D, NT], F32, kind="ExternalInput").ap()
    w_out = nc.dram_tensor("w_out", [D, D], F32, kind="ExternalInput").ap()
    lnp = nc.dram_tensor("lnp", [4, D], F32, kind="ExternalInput").ap()
    rw = nc.dram_tensor("rw", [D, 32], F32, kind="ExternalInput").ap()
    rb = nc.dram_tensor("rb", [1, 32], F32, kind="ExternalInput").ap()
    wup = nc.dram_tensor("wup", [n_exp, D, 2 * D], F32, kind="ExternalInput").ap()
    bup = nc.dram_tensor("bup", [256, 256], F32, kind="ExternalInput").ap()
    wdn = nc.dram_tensor("wdn", [n_exp, D, D], F32, kind="ExternalInput").ap()
    bdn = nc.dram_tensor("bdn", [32, D], F32, kind="ExternalInput").ap()
    idn = nc.dram_tensor("idn", [128, 128], F32, kind="ExternalInput").ap()
    fcst = nc.dram_tensor("fcst", [128, 512], F32, kind="ExternalInput").ap()
    xo = nc.dram_tensor("xo", [NT, D], F32, kind="ExternalOutput").ap()

    emit_F2(nc, c, x, yT, w_out, lnp, rw, rb, wup, bup, wdn, bdn, idn, fcst, xo, n_exp=n_exp, n_blk=n_blk)
    return nc


def emit_F2(nc, c, x, yT, w_out, lnp, rw, rb, wup, bup, wdn, bdn, idn, fcst, xo, n_exp=32, n_blk=2, pfx="F"):
    CAP = 256
    TB = 1024
    D = 1024
    NTL = TB // 128
    sb = lambda n, s, d: c.sb(pfx + n, s, d)
    ident = sb("ident", [128, 128], F32); r_ident = Res("ident")
    c.dma("sp", ident[:], idn, writes=[r_ident])
    rw32 = sb("rw32", [128, 8, 32], F32); r_rw = Res("rw")
    c.dma("sp", rw32[:], rw.rearrange("(c p) e -> p c e", p=128), writes=[r_rw])
    rbB = sb("rbB", [128, 32], F32); r_rb = Res("rb")
    c.dma("sp", rbB[:], rb.partition_broadcast(128), writes=[r_rb])
    bdn32 = sb("bdn32", [32, D], F32); r_bdn = Res("bdn")
    c.dma("sp", bdn32[:], bdn, writes=[r_bdn])
    lnB = [sb("lnB%d" % i, [128, D], F32) for i in range(4)]
    r_ln = [Res("ln%d" % i) for i in range(4)]
    for i in range(4):
        c.dma("sp", lnB[i][:], lnp[i:i + 1, :].partition_broadcast(128), writes=[r_ln[i]])
    iota = sb("iota", [128, 256], F32); r_iota = Res("iota")
    c.dma("sp", iota[:], fcst[:, 0:256], writes=[r_iota])
    UO = sb("UO", [128, 256], BF16); r_UO = Res("UO")
    c.dma("pool", UO[:], fcst[:, 256:512], writes=[r_UO])
    identb = sb("identb", [128, 128], BF16); r_identb = Res("identb")
    c.dma("pool", identb[:], idn, writes=[r_identb])
    B = [c.ps(pfx + "bank%d" % i, [128, 512], F32) for i in range(7)]
    rB = [Res("bank%d" % i) for i in range(7)]
    TP = c.ps(pfx + "TP", [128, 1024], BF16); r_TP = Res("TP")
    bupraw = sb("bupraw", [128, 2, 256], F32); r_bupraw = Res("bupraw")
    c.dma("sp", bupraw[:], bup.rearrange("(g p) k -> p g k", p=128), writes=[r_bupraw])
    bupT = sb("bupT", [128, 2, 256], F32); r_bupT = Res("bupT")
    for two in range(2):
        for g in range(2):
            c.op("pe", lambda: nc.tensor.transpose(B[0][:, (two * 2 + g) * 128:(two * 2 + g + 1) * 128],
                                                   bupraw[:, g, two:256:2], ident[:]),
                 reads=[r_bupraw, r_ident], writes=[rB[0]], acc=(two + g > 0))
    c.op("act", lambda: nc.scalar.copy(bupT[:].rearrange("p a b -> p (a b)"), B[0][:]), reads=[rB[0]], writes=[r_bupT])

    c.op("dve", lambda: nc.vector.tensor_scalar(bupT[:, 1, :], bupT[:, 1, :], 1.0, None, ALU.add), reads=[r_bupT], writes=[r_bupT])

    yacc = sb("yacc", [128, NTL, D], F32); r_yacc = [Res("yacc%d" % i) for i in range(NTL)]
    x1b = sb("x1b", [128, NTL, D], BF16); r_x1b = [Res("x1b%d" % i) for i in range(NTL)]
    maskf = sb("maskf", [128, NTL, 32], F32); r_mask = [Res("mask%d" % i) for i in range(NTL)]
    maskb = sb("maskb", [128, NTL, 32], BF16)
    rank = sb("rank", [128, NTL, 32], F32); r_rank = [Res("rank%d" % i) for i in range(NTL)]
    carry = sb("carry", [128, 32], F32); r_carry = Res("carry")
    gates = sb("gates", [128, NTL, 32], F32); r_gates = [Res("gates%d" % i) for i in range(NTL)]
    gT = sb("gT", [32, TB], F32); r_gT = [Res("gT%d" % i) for i in range(NTL)]
    x1T32 = [sb("x1T32_%d" % i, [128, 8, 128], F32) for i in range(2)]; r_x1T32 = [Res("x1T32_%d" % i) for i in range(2)]
    sm = [sb("sm%d" % i, [128, 128], F32) for i in range(2)]; r_sm = [Res("sm%d" % i) for i in range(2)]
    wuc = [sb("wuc%d" % i, [128, 8, 256], BF16) for i in range(4)]; r_wuc = [Res("wuc%d" % i) for i in range(4)]
    wd = [sb("wd%d" % i, [128, 8, D], BF16) for i in range(2)]; r_wd = [Res("wd%d" % i) for i in range(2)]
    yTb, r_yTb = wd[0], r_wd[0]
    woutb, r_wout = wd[1], r_wd[1]
    Pm = [sb("Pm%d" % i, [128, NTL, CAP], BF16) for i in range(2)]; r_Pm = [Res("Pm%d" % i) for i in range(2)]
    Gm = [sb("Gm%d" % i, [128, NTL, CAP], BF16) for i in range(2)]; r_Gm = [Res("Gm%d" % i) for i in range(2)]
    GT = [sb("GT%d" % i, [128, 2, TB], BF16) for i in range(2)]; r_GT = [Res("GT%d" % i) for i in range(2)]
    XgT = [sb("XgT%d" % i, [128, 8, CAP], BF16) for i in range(2)]; r_XgT = [Res("XgT%d" % i) for i in range(2)]
    act = [sb("act%d" % i, [128, 8, CAP], BF16) for i in range(2)]; r_act = [Res("act%d" % i) for i in range(2)]
    osb = [sb("osb%d" % i, [128, 2, D], BF16) for i in range(2)]; r_osb = [Res("osb%d" % i) for i in range(2)]
    NTMP = 2
    tg = [sb("tg%d" % i, [128, CAP], F32) for i in range(NTMP)]; r_tg = [Res("tg%d" % i) for i in range(NTMP)]
    tsg = [sb("tsg%d" % i, [128, CAP], F32) for i in range(NTMP)]; r_tsg = [Res("tsg%d" % i) for i in range(NTMP)]
    tl = [sb("tl%d" % i, [128, CAP], F32) for i in range(NTMP)]; r_tl = [Res("tl%d" % i) for i in range(NTMP)]
    ot = [sb("ot%d" % i, [128, D], F32) for i in range(2)]; r_ot = [Res("ot%d" % i) for i in range(2)]
    zt, r_zt = ot, r_ot
    xt, r_xt = ot, r_ot
    r_out = [Res("xo0"), Res("xo1")]

    def layer_norm(src, r_src, dst, r_dst, s, r_s, gi, bi):
        for h in range(2):
            c.op("dve", lambda: nc.vector.bn_stats(s[:, h * 6:(h + 1) * 6], src[:, h * 512:(h + 1) * 512]),
                 reads=[r_src], writes=[r_s], acc=(h > 0))
        c.op("dve", lambda: nc.vector.bn_aggr(s[:, 12:14], s[:, 0:12]),
             reads=[r_s], writes=[r_s])
        c.op("act", lambda: nc.scalar.activation(s[:, 14:15], s[:, 13:14], AF.Sqrt, bias=epsT[:, 0:1], scale=1.0),
             reads=[r_s, r_eps], writes=[r_s])
        c.op("dve", lambda: nc.vector.reciprocal(s[:, 15:16], s[:, 14:15]), reads=[r_s], writes=[r_s])
        c.op("dve", lambda: nc.vector.tensor_scalar(dst, src, s[:, 12:13], s[:, 15:16], ALU.subtract, ALU.mult),
             reads=[r_src, r_s], writes=[r_dst])
        c.op("dve", lambda: nc.vector.tensor_tensor(dst, dst, lnB[gi][:], ALU.mult), reads=[r_dst, r_ln[gi]], writes=[r_dst])
        c.op("dve", lambda: nc.vector.tensor_tensor(dst, dst, lnB[bi][:], ALU.add), reads=[r_dst, r_ln[bi]], writes=[r_dst])

    epsT = sb("epsT", [128, 1], F32); r_eps = Res("eps")
    c.op("dve", lambda: nc.vector.memset(epsT[:], LN_EPS), writes=[r_eps])

    def issue_wd(q):
        e_ = q % n_exp
        c.dma("pool", wd[q % 2][:], wdn[e_].rearrange("(c p) d -> p c d", p=128), writes=[r_wd[q % 2]])

    def issue_wu(gi):
        e_ = (gi // 8) % n_exp
        fc_ = gi % 8
        c.dma("pool", wuc[gi % 4][:], wup[e_][:, fc_ * 256:(fc_ + 1) * 256].rearrange("(c p) f -> p c f", p=128), writes=[r_wuc[gi % 4]])

    issue_wu(0)
    issue_wu(1)
    tmp_i = 0
    pg_i = 0
    po_i = 0
    sc_i = 0
    GXB = (3, 6)
    for blk in range(n_blk):
        t0 = blk * TB
        c.dma("pool", yTb[:], yT[:, t0:t0 + TB].rearrange("(c p) t -> p c t", p=128), writes=[r_yTb])
        c.dma("pool", woutb[:], w_out.rearrange("(c p) d -> p c d", p=128), writes=[r_wout])
        for i in range(NTL):
            b2 = i % 2
            c.dma("sp", xt[b2][:], x[t0 + i * 128:t0 + (i + 1) * 128, :], writes=[r_xt[b2]])
            for h in range(2):
                for k in range(8):
                    c.op("pe", lambda: nc.tensor.matmul(B[h][:], yTb[:, k, i * 128:(i + 1) * 128],
                                                        woutb[:, k, h * 512:(h + 1) * 512], start=(k == 0), stop=(k == 7)),
                         reads=[r_yTb, r_wout], writes=[rB[h]], acc=(k > 0))
                c.op("dve", lambda: nc.vector.scalar_tensor_tensor(zt[b2][:, h * 512:(h + 1) * 512], xt[b2][:, h * 512:(h + 1) * 512],
                                                                   ALPHA, B[h][:], ALU.mult, ALU.add),
                     reads=[r_xt[b2], rB[h]], writes=[r_zt[b2]], acc=(h > 0))
            layer_norm(zt[b2][:], r_zt[b2], yacc[:, i, :], r_yacc[i], sm[b2], r_sm[b2], 0, 1)
            for k in range(8):
                bk = 2 + k // 4
                c.op("pe", lambda: nc.tensor.transpose(B[bk][:, (k % 4) * 128:(k % 4 + 1) * 128],
                                                       yacc[:, i, k * 128:(k + 1) * 128], ident[:]),
                     reads=[r_yacc[i], r_ident], writes=[rB[bk]], acc=(k % 4 > 0))
            for hh in range(2):
                c.op("act", lambda: nc.scalar.copy(x1T32[b2][:, hh * 4:(hh + 1) * 4, :].rearrange("p a b -> p (a b)"), B[2 + hh][:]),
                     reads=[rB[2 + hh]], writes=[r_x1T32[b2]], acc=(hh > 0))
            c.op("act", lambda: nc.scalar.copy(x1b[:, i, :], yacc[:, i, :]), reads=[r_yacc[i]], writes=[r_x1b[i]])
            for k in range(8):
                c.op("pe", lambda: nc.tensor.matmul(B[4][:, 0:32], x1T32[b2][:, k, :], rw32[:, k, :], start=(k == 0), stop=(k == 7)),
                     reads=[r_x1T32[b2], r_rw], writes=[rB[4]], acc=(k > 0))
            s = sm[b2]; r_s = r_sm[b2]
            c.op("dve", lambda: nc.vector.tensor_tensor(s[:, 32:64], B[4][:, 0:32], rbB[:], ALU.add), reads=[rB[4], r_rb], writes=[r_s])
            c.op("dve", lambda: nc.vector.max(s[:, 16:24], s[:, 32:64]), reads=[r_s], writes=[r_s])
            c.op("dve", lambda: nc.vector.tensor_scalar(s[:, 64:96], s[:, 32:64], s[:, 19:20], None, ALU.is_ge), reads=[r_s], writes=[r_s])
            c.op("dve", lambda: nc.vector.tensor_copy(maskf[:, i, :], s[:, 64:96]), reads=[r_s], writes=[r_mask[i]])
            c.op("dve", lambda: nc.vector.tensor_copy(maskb[:, i, :], s[:, 64:96]), reads=[r_s], writes=[r_mask[i]])
            c.op("pe", lambda: nc.tensor.matmul(B[4][:, 256:288], UO[:, 0:128], maskb[:, i, :], start=True, stop=True),
                 reads=[r_UO, r_mask[i]], writes=[rB[4]])
            c.op("pe", lambda: nc.tensor.matmul(B[4][:, 288:320], UO[:, 128:256], maskb[:, i, :], start=True, stop=True),
                 reads=[r_UO, r_mask[i]], writes=[rB[4]], acc=True)
            if i == 0:
                c.op("dve", lambda: nc.vector.tensor_copy(rank[:, i, :], B[4][:, 256:288]), reads=[rB[4]], writes=[r_rank[i]])
                c.op("dve", lambda: nc.vector.tensor_copy(carry[:], B[4][:, 288:320]), reads=[rB[4]], writes=[r_carry])
            else:
                c.op("dve", lambda: nc.vector.tensor_tensor(rank[:, i, :], B[4][:, 256:288], carry[:], ALU.add),
                     reads=[rB[4], r_carry], writes=[r_rank[i]])
                c.op("dve", lambda: nc.vector.tensor_tensor(carry[:], carry[:], B[4][:, 288:320], ALU.add),
                     reads=[rB[4], r_carry], writes=[r_carry])
            c.op("dve", lambda: nc.vector.tensor_scalar(s[:, 24:25], s[:, 16:17], -1.0, None, ALU.mult), reads=[r_s], writes=[r_s])
            c.op("act", lambda: nc.scalar.activation(s[:, 96:128], s[:, 32:64], AF.Exp, bias=s[:, 24:25], scale=1.0), reads=[r_s], writes=[r_s])
            c.op("dve", lambda: nc.vector.tensor_tensor(s[:, 96:128], s[:, 96:128], s[:, 64:96], ALU.mult), reads=[r_s], writes=[r_s])
            c.op("dve", lambda: nc.vector.reduce_sum(s[:, 25:26], s[:, 96:128], AX.X), reads=[r_s], writes=[r_s])
            c.op("dve", lambda: nc.vector.reciprocal(s[:, 26:27], s[:, 25:26]), reads=[r_s], writes=[r_s])
            c.op("dve", lambda: nc.vector.tensor_scalar(gates[:, i, :], s[:, 96:128], s[:, 26:27], None, ALU.mult), reads=[r_s], writes=[r_gates[i]])
            c.op("pe", lambda: nc.tensor.transpose(B[4][0:32, 128:256], gates[:, i, :], ident[:]),
                 reads=[r_gates[i], r_ident], writes=[rB[4]])
            c.op("act", lambda: nc.scalar.copy(gT[:, i * 128:(i + 1) * 128], B[4][0:32, 128:256]), reads=[rB[4]], writes=[r_gT[i]])
            for h in range(2):
                c.op("pe", lambda: nc.tensor.matmul(B[2 + h][:], gT[:, i * 128:(i + 1) * 128], bdn32[:, h * 512:(h + 1) * 512],
                                                    start=True, stop=True), reads=[r_gT[i], r_bdn], writes=[rB[2 + h]])
                c.op("dve", lambda: nc.vector.scalar_tensor_tensor(yacc[:, i, h * 512:(h + 1) * 512], yacc[:, i, h * 512:(h + 1) * 512],
                                                                   ALPHA, B[2 + h][:], ALU.mult, ALU.add),
                     reads=[r_yacc[i], rB[2 + h]], writes=[r_yacc[i]])
        issue_wd(blk * n_exp)
        def build(e):
            ab = e % 2
            for i in range(NTL):
                c.op("dve", lambda: nc.vector.tensor_scalar(Pm[ab][:, i, :], iota[:], rank[:, i, e:e + 1], maskf[:, i, e:e + 1],
                                                            ALU.is_equal, ALU.mult),
                     reads=[r_iota, r_rank[i], r_mask[i]], writes=[r_Pm[ab]], acc=(i > 0))
                c.op("dve", lambda: nc.vector.tensor_scalar(Gm[ab][:, i, :], iota[:], rank[:, i, e:e + 1], gates[:, i, e:e + 1],
                                                            ALU.is_equal, ALU.mult),
                     reads=[r_iota, r_rank[i], r_gates[i]], writes=[r_Gm[ab]], acc=(i > 0))

        def transposes(e):
            ab = e % 2
            for st_ in range(2):
                for i in range(NTL):
                    c.op("pe", lambda: nc.tensor.transpose(TP[:, i * 128:(i + 1) * 128], Gm[ab][:, i, st_ * 128:(st_ + 1) * 128], identb[:]),
                         reads=[r_Gm[ab], r_identb], writes=[r_TP], acc=(i > 0))
                c.op("act", lambda: nc.scalar.copy(GT[ab][:, st_, :], TP[:]), reads=[r_TP], writes=[r_GT[ab]], acc=(st_ > 0))

        def gather(e):
            ab = e % 2
            for k in range(8):
                gh = k % 2
                for i in range(NTL):
                    c.op("pe", lambda: nc.tensor.matmul(B[GXB[gh]][:, 0:256], x1b[:, i, k * 128:(k + 1) * 128], Pm[ab][:, i, :],
                                                        start=(i == 0), stop=(i == NTL - 1)),
                         reads=[r_x1b[i], r_Pm[ab]], writes=[rB[GXB[gh]]], acc=(i > 0))
                c.op("act", lambda: nc.scalar.copy(XgT[ab][:, k, :], B[GXB[gh]][:, 0:256]), reads=[rB[GXB[gh]]], writes=[r_XgT[ab]], acc=(k > 0))

        def up(e):
            nonlocal pg_i, tmp_i
            q = blk * n_exp + e
            ab = e % 2
            for fc in range(8):
                gi = q * 8 + fc
                if gi + 2 < n_blk * n_exp * 8:
                    issue_wu(gi + 2)
                ws = gi % 4
                row = e * 8 + fc
                ub = pg_i % 2; pg_i += 1
                for two in range(2):
                    for k in range(8):
                        c.op("pe", lambda: nc.tensor.matmul(B[ub][:, two * 256:(two + 1) * 256], wuc[ws][:, k, two:256:2], XgT[ab][:, k, :],
                                                            start=(k == 0), stop=(k == 7)),
                             reads=[r_wuc[ws], r_XgT[ab]], writes=[rB[ub]], acc=(two + k > 0))
                ti = tmp_i % NTMP; tmp_i += 1
                c.op("dve", lambda: nc.vector.tensor_scalar(tg[ti][:], B[ub][:, 0:256], bupT[:, 0, row:row + 1], 7.0, ALU.add, ALU.min),
                     reads=[rB[ub], r_bupT], writes=[r_tg[ti]])
                c.op("act", lambda: nc.scalar.activation(tsg[ti][:], tg[ti][:], AF.Sigmoid, scale=1.702),
                     reads=[r_tg[ti]], writes=[r_tsg[ti]])
                c.op("dve", lambda: nc.vector.tensor_scalar(tl[ti][:], B[ub][:, 256:512], bupT[:, 1, row:row + 1], -6.0, ALU.add, ALU.max),
                     reads=[rB[ub], r_bupT], writes=[r_tl[ti]])
                c.op("dve", lambda: nc.vector.scalar_tensor_tensor(tl[ti][:], tl[ti][:], 8.0, tg[ti][:], ALU.min, ALU.mult),
                     reads=[r_tg[ti], r_tl[ti]], writes=[r_tl[ti]])
                c.op("dve", lambda: nc.vector.tensor_tensor(act[ab][:, fc, :], tl[ti][:], tsg[ti][:], ALU.mult),
                     reads=[r_tl[ti], r_tsg[ti]], writes=[r_act[ab]], acc=(fc > 0))

        def down(e):
            ab = e % 2
            for st_ in range(2):
                for h in range(2):
                    po = 2
                    for fc in range(8):
                        c.op("pe", lambda: nc.tensor.matmul(B[po][:], act[ab][:, fc, st_ * 128:(st_ + 1) * 128],
                                                            wd[ab][:, fc, h * 512:(h + 1) * 512], start=(fc == 0), stop=(fc == 7)),
                             reads=[r_act[ab], r_wd[ab]], writes=[rB[po]], acc=(fc > 0))
                    c.op("act", lambda: nc.scalar.copy(osb[ab][:, st_, h * 512:(h + 1) * 512], B[po][:]), reads=[rB[po]], writes=[r_osb[ab]],
                         acc=(st_ + h > 0))

        def scatter(e):
            nonlocal sc_i
            ab = e % 2
            for i in range(NTL):
                for h in range(2):
                    sc = 4 + (sc_i % 2); sc_i += 1
                    for st_ in range(2):
                        c.op("pe", lambda: nc.tensor.matmul(B[sc][:], GT[ab][:, st_, i * 128:(i + 1) * 128], osb[ab][:, st_, h * 512:(h + 1) * 512],
                                                            start=(st_ == 0), stop=(st_ == 1)),
                             reads=[r_GT[ab], r_osb[ab]], writes=[rB[sc]], acc=(st_ > 0))
                    c.op("dve", lambda: nc.vector.tensor_tensor(yacc[:, i, h * 512:(h + 1) * 512], yacc[:, i, h * 512:(h + 1) * 512], B[sc][:], ALU.add),
                         reads=[rB[sc], r_yacc[i]], writes=[r_yacc[i]])

        build(0)
        transposes(0)
        gather(0)
        for e in range(n_exp):
            q = blk * n_exp + e
            if e + 1 < n_exp:
                issue_wd(q + 1)
                build(e + 1)
            up(e)
            if e + 1 < n_exp:
                gather(e + 1)
            down(e)
            if e + 1 < n_exp:
                transposes(e + 1)
            scatter(e)
        for i in range(NTL):
            b2 = i % 2
            layer_norm(yacc[:, i, :], r_yacc[i], ot[b2][:], r_ot[b2], sm[b2], r_sm[b2], 2, 3)
            c.dma("sp", xo[t0 + i * 128:t0 + (i + 1) * 128, :], ot[b2][:], reads=[r_ot[b2]], writes=[r_out[b2]])
    c.finish("sp", r_out)
    print("F: ins", c.n_ins, "waits", c.n_wait, "sbuf left", nc.sbuf_bytes_remaining)


GN_EPS = 1e-6


def me_host_consts(T):
    pos = np.arange(T, dtype=np.float32)
    inv = (10000.0 ** (-np.arange(0, 64, 2, dtype=np.float32) / 64)).astype(np.float32)
    ang = pos[:, None] * inv[None, :]
    cos = np.cos(ang).astype(np.float32).T
    sin = np.sin(ang).astype(np.float32).T
    cosF = np.concatenate([cos, cos], 0)
    sinS = np.concatenate([-sin, sin], 0)
    s8 = np.float32(0.125)
    cq = np.concatenate([cosF, cosF * s8], 0)
    sq = np.concatenate([sinS, sinS * s8], 0)
    ck = np.concatenate([cosF * s8, cosF], 0)
    sk = np.concatenate([sinS * s8, sinS], 0)
    tab = np.ascontiguousarray(np.stack([cq, sq, ck, sk], 0)).astype(np.float32)
    i = np.arange(128, dtype=np.float32)
    m = i[:, None]; c = i[None, :]
    cst = np.zeros((128, 6, 128), np.float32)
    cst[:, 0, :] = np.maximum(c - m, 0)
    cst[:, 1, :] = (c >= m)
    cst[:, 2, :] = np.maximum(m - c, 0)
    cst[:, 3, :] = (m > c)
    cst[:, 4, :] = c + 1.0
    cst[:, 5, :] = 128.0 - c
    col = np.zeros((128, 4), np.float32)
    col[:, 0] = 127.0 - i
    col[:, 1] = i
    col[:, 2] = 128.0
    o = np.arange(17)[None, :, None]
    delta = (o - 8) * 128 + m[:, None, :].astype(np.int64) - c[None, :, :].astype(np.int64)
    delta = delta.astype(np.int64)
    ad = np.abs(delta)
    mm = (ad <= 64).astype(np.float32) + ((delta % 4 == 0) & (ad <= 256)) + ((delta % 16 == 0) & (ad <= 1024))
    mm = mm.astype(np.float32).reshape(128, 17 * 128)
    return dict(tab=tab, cst=cst.reshape(128, 768), col=col, mm=mm, idn=np.eye(128, dtype=np.float32))


def me_weight_cols(head):
    H = 64
    base = lambda blk: blk * 512 + head * H
    rng = lambda b: list(range(base(b), base(b) + H))
    sw = lambda b: list(range(base(b) + 32, base(b) + 64)) + list(range(base(b), base(b) + 32))
    cols = []
    cols += rng(0) + rng(5)
    cols += sw(0) + sw(5)
    cols += rng(1) + rng(6)
    cols += sw(1) + sw(6)
    cols += rng(2) + rng(7)
    cols += rng(3) + rng(4)
    return np.array(cols)


def build_ME(T=16384):
    nc = bass.Bass("TRN2", target_bir_lowering=False)
    c = Ctx(nc)
    xT = nc.dram_tensor("xT", [1024, T], F32, kind="ExternalInput").ap()
    wsel = nc.dram_tensor("wsel", [1024, 768], F32, kind="ExternalInput").ap()
    dec = nc.dram_tensor("dec", [1, 2], F32, kind="ExternalInput").ap()
    tab = nc.dram_tensor("tab", [4, 128, T], F32, kind="ExternalInput").ap()
    cst = nc.dram_tensor("cst", [128, 768], F32, kind="ExternalInput").ap()
    col = nc.dram_tensor("col", [128, 4], F32, kind="ExternalInput").ap()
    mm = nc.dram_tensor("mm", [128, 17 * 128], F32, kind="ExternalInput").ap()
    idn = nc.dram_tensor("idn", [128, 128], F32, kind="ExternalInput").ap()
    y = nc.dram_tensor("y", [T, 128], F32, kind="ExternalOutput").ap()
    emit_ME(nc, c, xT, wsel, dec, tab, cst, col, mm, idn, y, T)
    return nc


def emit_ME(nc, c, xT, wsel, dec, tab, cst, col, mm, idn, y, T, pfx="E"):
    NCH = T // 128
    sb = lambda n, s, d: c.sb(pfx + n, s, d)
    V = nc.vector
    A = nc.scalar
    identb = sb("identb", [128, 128], BF16); r_identb = Res("identb")
    c.dma("pool", identb[:], idn, writes=[r_identb])
    wb = sb("wb", [128, 8, 768], BF16); r_wb = Res("wb")
    c.dma("pool", wb[:], wsel.rearrange("(c p) e -> p c e", p=128), writes=[r_wb])
    mmb = sb("mmb", [128, 17 * 128], BF16); r_mm = Res("mm")
    c.dma("pool", mmb[:], mm, writes=[r_mm])
    cs = sb("cs", [128, 6, 128], F32); r_cs = Res("cs")
    c.dma("sp", cs[:].rearrange("p a b -> p (a b)"), cst, writes=[r_cs])
    cl = sb("cl", [128, 4], F32); r_cl = Res("cl")
    c.dma("sp", cl[:], col, writes=[r_cl])
    dl = sb("dl", [128, 16], F32); r_dl = Res("dl")
    c.dma("sp", dl[:, 0:2], dec.partition_broadcast(128), writes=[r_dl])
    ops = [
        lambda: V.tensor_scalar(dl[:, 2:4], dl[:, 0:2], -1.0, None, ALU.mult),
        lambda: V.tensor_tensor(dl[:, 2:4], dl[:, 2:4], dl[:, 0:2], ALU.max),
    ]
    for f in ops:
        c.op("dve", f, reads=[r_dl], writes=[r_dl])
    c.op("act", lambda: A.activation(dl[:, 2:4], dl[:, 2:4], AF.Exp, scale=-1.0), reads=[r_dl], writes=[r_dl])
    ops = [
        lambda: V.tensor_scalar(dl[:, 4:6], dl[:, 2:4], 2.0, None, ALU.add),
        lambda: V.reciprocal(dl[:, 4:6], dl[:, 4:6]),
        lambda: V.tensor_tensor(dl[:, 4:6], dl[:, 4:6], dl[:, 2:4], ALU.mult),
        lambda: V.tensor_tensor(dl[:, 6:8], dl[:, 4:6], dl[:, 4:6], ALU.mult),
        lambda: V.tensor_scalar(dl[:, 8:10], dl[:, 6:8], 1.0 / 11, 1.0 / 9, ALU.mult, ALU.add),
        lambda: V.tensor_tensor(dl[:, 8:10], dl[:, 8:10], dl[:, 6:8], ALU.mult),
        lambda: V.tensor_scalar(dl[:, 8:10], dl[:, 8:10], 1.0 / 7, None, ALU.add),
        lambda: V.tensor_tensor(dl[:, 8:10], dl[:, 8:10], dl[:, 6:8], ALU.mult),
        lambda: V.tensor_scalar(dl[:, 8:10], dl[:, 8:10], 1.0 / 5, None, ALU.add),
        lambda: V.tensor_tensor(dl[:, 8:10], dl[:, 8:10], dl[:, 6:8], ALU.mult),
        lambda: V.tensor_scalar(dl[:, 8:10], dl[:, 8:10], 1.0 / 3, None, ALU.add),
        lambda: V.tensor_tensor(dl[:, 8:10], dl[:, 8:10], dl[:, 6:8], ALU.mult),
        lambda: V.tensor_scalar(dl[:, 8:10], dl[:, 8:10], 1.0, None, ALU.add),
        lambda: V.tensor_tensor(dl[:, 8:10], dl[:, 8:10], dl[:, 4:6], ALU.mult),
        lambda: V.tensor_scalar(dl[:, 10:12], dl[:, 0:2], 0.0, None, ALU.min),
        lambda: V.scalar_tensor_tensor(dl[:, 10:12], dl[:, 8:10], -2.0, dl[:, 10:12], ALU.mult, ALU.add),
    ]
    for f in ops:
        c.op("dve", f, reads=[r_dl], writes=[r_dl])
    lgf = dl[:, 10:11]
    lgb = dl[:, 11:12]
    decT = sb("decT", [128, 2, 128], F32); r_dec = Res("decT")
    xi = sb("xi", [128, 2, 128], F32); r_xi = Res("xi")
    zc = sb("zc", [128, 4], F32); r_zc = Res("zc")
    c.op("act", lambda: A.activation(decT[:, 0, :], cs[:, 0, :], AF.Exp, scale=lgf), reads=[r_cs, r_dl], writes=[r_dec])
    c.op("act", lambda: A.activation(decT[:, 1, :], cs[:, 2, :], AF.Exp, scale=lgb), reads=[r_cs, r_dl], writes=[r_dec])
    c.op("dve", lambda: V.tensor_tensor(decT[:, 0, :], decT[:, 0, :], cs[:, 1, :], ALU.mult), reads=[r_dec, r_cs], writes=[r_dec])
    c.op("dve", lambda: V.tensor_tensor(decT[:, 1, :], decT[:, 1, :], cs[:, 3, :], ALU.mult), reads=[r_dec, r_cs], writes=[r_dec])
    c.op("act", lambda: A.activation(xi[:, 0, :], cs[:, 4, :], AF.Exp, scale=lgf), reads=[r_cs, r_dl], writes=[r_xi])
    c.op("act", lambda: A.activation(xi[:, 1, :], cs[:, 5, :], AF.Exp, scale=lgb), reads=[r_cs, r_dl], writes=[r_xi])
    c.op("act", lambda: A.activation(zc[:, 0:1], cl[:, 0:1], AF.Exp, scale=lgf), reads=[r_cl, r_dl], writes=[r_zc])
    c.op("act", lambda: A.activation(zc[:, 1:2], cl[:, 1:2], AF.Exp, scale=lgb), reads=[r_cl, r_dl], writes=[r_zc])
    c.op("act", lambda: A.activation(zc[:, 2:3], cl[:, 2:3], AF.Exp, scale=lgf), reads=[r_cl, r_dl], writes=[r_zc])
    c.op("act", lambda: A.activation(zc[:, 3:4], cl[:, 2:3], AF.Exp, scale=lgb), reads=[r_cl, r_dl], writes=[r_zc])
    epsT = sb("epsT", [128, 1], F32); r_eps = Res("eps")
    c.op("dve", lambda: V.memset(epsT[:], GN_EPS), writes=[r_eps])

    qkT = sb("qkT", [128, 2, T], BF16)
    r_qk = [Res("qk%d" % n) for n in range(NCH)]
    vall = sb("vall", [128, NCH, 64], BF16); r_v = [Res("v%d" % n) for n in range(NCH)]
    dvall = sb("dvall", [128, NCH, 66], BF16); r_dv = [Res("dv%d" % n) for n in range(NCH)]
    Rf = sb("Rf", [64, NCH + 1, 64], BF16); r_Rf = [Res("Rf%d" % n) for n in range(NCH + 1)]
    Rrun = sb("Rrun", [64, 2, 64], F32); r_Rrun = [Res("Rrun0"), Res("Rrun1")]
    Rbb = [sb("Rbb%d" % i, [64, 64], BF16) for i in range(2)]; r_Rbb = [Res("Rbb0"), Res("Rbb1")]
    ones_dst = Res("dvones")
    c.op("pool", lambda: nc.gpsimd.memset(dvall[:, :, 64:66], 1.0), writes=[ones_dst])
    c.op("dve", lambda: V.memset(Rrun[:], 0.0), writes=r_Rrun)
    c.op("dve", lambda: V.memset(Rf[:, 0, :], 0.0), writes=[r_Rf[0]])
    c.op("dve", lambda: V.memset(Rbb[0][:], 0.0), writes=[r_Rbb[0]])

    NB = 2
    xTt = [sb("xTt%d" % i, [128, 8, 128], BF16) for i in range(NB)]; r_xTt = [Res("xTt%d" % i) for i in range(NB)]
    tbt = [sb("tbt%d" % i, [128, 4, 128], F32) for i in range(NB)]; r_tbt = [Res("tbt%d" % i) for i in range(NB)]
    prod = [sb("prod%d" % i, [128, 4, 128], F32) for i in range(NB)]; r_prod = [Res("prod%d" % i) for i in range(NB)]
    kz = [sb("kz%d" % i, [128, 64], BF16) for i in range(NB)]; r_kz = [Res("kz%d" % i) for i in range(NB)]
    scT = [sb("scT%d" % i, [128, 2, 128], BF16) for i in range(NB)]; r_scT = [Res("scT%d" % i) for i in range(NB)]
    qxi = [sb("qxi%d" % i, [64, 2, 128], BF16) for i in range(NB)]; r_qxi = [Res("qxi%d" % i) for i in range(NB)]
    st = [sb("st%d" % i, [128, 32], F32) for i in range(NB)]; r_st = [Res("st%d" % i) for i in range(NB)]
    xn = [sb("xn%d" % i, [128, 128], F32) for i in range(NB)]; r_xn = [Res("xn%d" % i) for i in range(NB)]
    sg = [sb("sg%d" % i, [128, 128], F32) for i in range(NB)]; r_sg = [Res("sg%d" % i) for i in range(NB)]
    yt = [sb("yt%d" % i, [128, 128], F32) for i in range(NB)]; r_yt = [Res("yt%d" % i) for i in range(NB)]
    pd = [sb("pd%d" % i, [128, 512], BF16) for i in range(3)]; r_pd = [Res("pd%d" % i) for i in range(3)]
    r_y = [Res("y0"), Res("y1")]
    PA = c.ps(pfx + "PA", [128, 512], F32); r_PA = Res("PA")
    PV = c.ps(pfx + "PV", [128, 512], F32); r_PV = Res("PV")
    PT = c.ps(pfx + "PT", [128, 1024], BF16); r_PT = Res("PT")
    PS = c.ps(pfx + "PS", [128, 512], F32); r_PS = Res("PS")
    PO = c.ps(pfx + "PO", [128, 512], F32); r_PO = Res("PO")
    PD = [c.ps(pfx + "PD%d" % i, [128, 512], F32) for i in range(3)]; r_PD = [Res("PD%d" % i) for i in range(3)]

    def load_x(n, want_tab):
        b = n % NB
        c.dma("pool", xTt[b][:], xT[:, n * 128:(n + 1) * 128].rearrange("(c p) t -> p c t", p=128), writes=[r_xTt[b]])
        if want_tab:
            c.dma("sp", tbt[b][:], tab[:, :, n * 128:(n + 1) * 128].rearrange("a p t -> p a t"), writes=[r_tbt[b]])

    def ktrans_state(n, direction, b):
        c.op("pe", lambda: nc.tensor.transpose(PT[:, 0:64], qkT[0:64, 1, n * 128:(n + 1) * 128], identb[0:64, 0:64]),
             reads=[r_qk[n], r_identb], writes=[r_PT])
        c.op("dve", lambda: V.tensor_scalar(kz[b][:], PT[:, 0:64], zc[:, direction:direction + 1], None, ALU.mult),
             reads=[r_PT, r_zc], writes=[r_kz[b]])
        c.op("pe", lambda: nc.tensor.matmul(PS[0:64, 0:64], kz[b][:], vall[:, n, :], start=True, stop=True),
             reads=[r_kz[b], r_v[n]], writes=[r_PS])

    load_x(0, True)
    for n in range(NCH):
        b = n % NB
        if n + 1 < NCH:
            load_x(n + 1, True)
        for g in range(4):
            for k in range(8):
                c.op("pe", lambda: nc.tensor.matmul(PA[:, g * 128:(g + 1) * 128], wb[:, k, g * 128:(g + 1) * 128], xTt[b][:, k, :],
                                                    start=(k == 0), stop=(k == 7)),
                     reads=[r_wb, r_xTt[b]], writes=[r_PA], acc=(g + k > 0))
        for k in range(8):
            c.op("pe", lambda: nc.tensor.matmul(PV[:, 0:128], xTt[b][:, k, :], wb[:, k, 512:640], start=(k == 0), stop=(k == 7)),
                 reads=[r_wb, r_xTt[b]], writes=[r_PV], acc=(k > 0))
        c.op("dve", lambda: V.tensor_tensor(prod[b][:], PA[:].rearrange("p (a t) -> p a t", a=4), tbt[b][:], ALU.mult),
             reads=[r_PA, r_tbt[b]], writes=[r_prod[b]])
        c.op("dve", lambda: V.tensor_tensor(qkT[:, :, n * 128:(n + 1) * 128], prod[b][:, 0:4:2, :], prod[b][:, 1:4:2, :], ALU.add),
             reads=[r_prod[b]], writes=[r_qk[n]])
        c.op("act", lambda: A.copy(vall[:, n, :], PV[:, 0:64]), reads=[r_PV], writes=[r_v[n]])
        c.op("act", lambda: A.copy(dvall[:, n, 0:64], PV[:, 64:128]), reads=[r_PV, ones_dst], writes=[r_dv[n]])
        ktrans_state(n, 0, b)
        c.op("dve", lambda: V.scalar_tensor_tensor(Rrun[:, 0, :], Rrun[:, 0, :], zc[0:64, 2:3], PS[0:64, 0:64], ALU.mult, ALU.add),
             reads=[r_Rrun[0], r_zc, r_PS], writes=[r_Rrun[0]])
        c.op("act", lambda: A.copy(Rf[:, n + 1, :], Rrun[:, 0, :]), reads=[r_Rrun[0]], writes=[r_Rf[n + 1]])

    load_x(NCH - 1, False)
    pd_i = 0
    for n in range(NCH - 1, -1, -1):
        b = n % NB
        rb_cur = (NCH - 1 - n) % 2
        if n - 1 >= 0:
            load_x(n - 1, False)
        sl = slice(n * 128, (n + 1) * 128)
        for k in range(8):
            c.op("pe", lambda: nc.tensor.matmul(PV[:, 0:128], xTt[b][:, k, :], wb[:, k, 640:768], start=(k == 0), stop=(k == 7)),
                 reads=[r_wb, r_xTt[b]], writes=[r_PV], acc=(k > 0))
        c.op("pe", lambda: nc.tensor.matmul(PS[:, 128:256], qkT[0:64, 1, sl], qkT[0:64, 0, sl], start=True, stop=True),
             reads=[r_qk[n]], writes=[r_PS])
        c.op("dve", lambda: V.tensor_tensor(scT[b][:], PS[:, 128:256].unsqueeze(1).broadcast_to([128, 2, 128]), decT[:], ALU.mult),
             reads=[r_PS, r_dec], writes=[r_scT[b]])
        c.op("pool", lambda: nc.gpsimd.tensor_tensor(qxi[b][:], qkT[0:64, 0, sl].unsqueeze(1).broadcast_to([64, 2, 128]), xi[0:64], ALU.mult),
             reads=[r_qk[n], r_xi], writes=[r_qxi[b]])
        c.op("pe", lambda: nc.tensor.matmul(PO[:, 0:64], scT[b][:, 0, :], vall[:, n, :], start=True, stop=False),
             reads=[r_scT[b], r_v[n]], writes=[r_PO])
        c.op("pe", lambda: nc.tensor.matmul(PO[:, 0:64], qxi[b][:, 0, :], Rf[:, n, :], start=False, stop=True),
             reads=[r_qxi[b], r_Rf[n]], writes=[r_PO], acc=True)
        c.op("pe", lambda: nc.tensor.matmul(PO[:, 64:128], scT[b][:, 1, :], vall[:, n, :], start=True, stop=False),
             reads=[r_scT[b], r_v[n]], writes=[r_PO], acc=True)
        c.op("pe", lambda: nc.tensor.matmul(PO[:, 64:128], qxi[b][:, 1, :], Rbb[rb_cur][:], start=False, stop=True),
             reads=[r_qxi[b], r_Rbb[rb_cur]], writes=[r_PO], acc=True)
        if n > 0:
            ktrans_state(n, 1, b)
            c.op("dve", lambda: V.scalar_tensor_tensor(Rrun[:, 1, :], Rrun[:, 1, :], zc[0:64, 3:4], PS[0:64, 0:64], ALU.mult, ALU.add),
                 reads=[r_Rrun[1], r_zc, r_PS], writes=[r_Rrun[1]])
            c.op("act", lambda: A.copy(Rbb[1 - rb_cur][:], Rrun[:, 1, :]), reads=[r_Rrun[1]], writes=[r_Rbb[1 - rb_cur]])
        s = st[b]; rs = r_st[b]
        c.op("dve", lambda: V.bn_stats(s[:, 0:6], PO[:, 0:64]), reads=[r_PO], writes=[rs])
        c.op("dve", lambda: V.bn_stats(s[:, 6:12], PO[:, 64:128]), reads=[r_PO], writes=[rs])
        c.op("dve", lambda: V.bn_aggr(s[:, 12:14], s[:, 0:6]), reads=[rs], writes=[rs])
        c.op("dve", lambda: V.bn_aggr(s[:, 14:16], s[:, 6:12]), reads=[rs], writes=[rs])
        c.op("act", lambda: A.activation(s[:, 16:18], s[:, 13:16:2], AF.Ln, bias=epsT[:, 0:1], scale=1.0), reads=[rs, r_eps], writes=[rs])
        c.op("act", lambda: A.activation(s[:, 16:18], s[:, 16:18], AF.Exp, scale=-0.5), reads=[rs], writes=[rs])
        c.op("dve", lambda: V.tensor_scalar(xn[b][:, 0:64], PO[:, 0:64], s[:, 12:13], s[:, 16:17], ALU.subtract, ALU.mult),
             reads=[r_PO, rs], writes=[r_xn[b]])
        c.op("dve", lambda: V.tensor_scalar(xn[b][:, 64:128], PO[:, 64:128], s[:, 14:15], s[:, 17:18], ALU.subtract, ALU.mult),
             reads=[r_PO, rs], writes=[r_xn[b]])
        c.op("act", lambda: A.activation(sg[b][:], PV[:, 0:128], AF.Exp, scale=-1.0), reads=[r_PV], writes=[r_sg[b]])
        c.op("pool", lambda: nc.gpsimd.tensor_scalar(sg[b][:], sg[b][:], 1.0, None, ALU.add), reads=[r_sg[b]], writes=[r_sg[b]])
        c.op("dve", lambda: V.reciprocal(sg[b][:], sg[b][:]), reads=[r_sg[b]], writes=[r_sg[b]])
        c.op("dve", lambda: V.tensor_tensor(sg[b][:], sg[b][:], PV[:, 0:128], ALU.mult), reads=[r_sg[b], r_PV], writes=[r_sg[b]])
        c.op("pool", lambda: nc.gpsimd.tensor_tensor(xn[b][:], xn[b][:], sg[b][:], ALU.mult), reads=[r_xn[b], r_sg[b]], writes=[r_xn[b]])
        c.op("pool", lambda: nc.gpsimd.tensor_tensor(yt[b][:, 0:64], xn[b][:, 0:64], xn[b][:, 64:128], ALU.add),
             reads=[r_xn[b]], writes=[r_yt[b]])
        kts = [kt for kt in range(n - 8, n + 9) if 0 <= kt < NCH]
        groups = [kts[i:i + 4] for i in range(0, len(kts), 4)]
        first = True
        for gi, grp in enumerate(groups):
            pb = pd_i % 3; pd_i += 1
            for j, kt in enumerate(grp):
                c.op("pe", lambda: nc.tensor.matmul(PD[pb][:, j * 128:(j + 1) * 128], qkT[64:128, 1, kt * 128:(kt + 1) * 128],
                                                    qkT[64:128, 0, sl], start=True, stop=True),
                     reads=[r_qk[kt], r_qk[n]], writes=[r_PD[pb]], acc=(j > 0))
            w = len(grp) * 128
            o0 = (grp[0] - n + 8) * 128
            c.op("act", lambda: A.activation(pd[pb][:, 0:w], PD[pb][:, 0:w], AF.Exp), reads=[r_PD[pb]], writes=[r_pd[pb]])
            c.op("pool", lambda: nc.gpsimd.tensor_tensor(pd[pb][:, 0:w], pd[pb][:, 0:w], mmb[:, o0:o0 + w], ALU.mult),
                 reads=[r_pd[pb], r_mm], writes=[r_pd[pb]])
            for j, kt in enumerate(grp):
                last = (gi == len(groups) - 1 and j == len(grp) - 1)
                c.op("pe", lambda: nc.tensor.matmul(PO[:, 256:321], pd[pb][:, j * 128:(j + 1) * 128], dvall[:, kt, 0:65],
                                                    start=first, stop=last),
                     reads=[r_pd[pb], r_dv[kt]], writes=[r_PO], acc=True)
                first = False
        c.op("dve", lambda: V.reciprocal(s[:, 20:21], PO[:, 320:321]), reads=[r_PO, rs], writes=[rs])
        c.op("dve", lambda: V.tensor_scalar(yt[b][:, 64:128], PO[:, 256:320], s[:, 20:21], None, ALU.mult),
             reads=[r_PO, rs], writes=[r_yt[b]])
        c.dma("sp", y[sl, :], yt[b][:], reads=[r_yt[b]], writes=[r_y[b]])
    c.finish("sp", r_y)
    print("ME: ins", c.n_ins, "waits", c.n_wait, "sbuf left", nc.sbuf_bytes_remaining)


NEG = -30000.0


def mo_patterns(NCH):
    pats = []
    for o in range(5):
        pats.append((4, 4 + o - 2))
    for n in (0, 1, NCH - 2, NCH - 1):
        kts = range(0, 4) if n < 2 else range(NCH - 4, NCH)
        for kt in kts:
            pats.append((n, kt))
    return pats


def mo_keys(n, NCH):
    if 2 <= n <= NCH - 3:
        return [(n + o - 2, o) for o in range(5)]
    idx = {0: 0, 1: 1, NCH - 2: 2, NCH - 1: 3}[n]
    kts = range(0, 4) if n < 2 else range(NCH - 4, NCH)
    return [(kt, 5 + idx * 4 + j) for j, kt in enumerate(kts)]


def mo_host_bias(rpb2, T):
    NCH = T // 128
    rows = T // 64
    pats = mo_patterns(NCH)
    m = np.arange(128)[:, None]
    c = np.arange(128)[None, :]
    out = np.full((128, 2, len(pats), 128), NEG, np.float32)
    for pi, (n, kt) in enumerate(pats):
        rm = 2 * kt + m // 64; wm = m % 64
        rc = 2 * n + c // 64; wc = c % 64
        r0 = np.clip(rc - 4, 0, rows - 8)
        c0 = np.clip(wc - 8, 0, 64 - 16)
        valid = (rm >= r0) & (rm < r0 + 8) & (wm >= c0) & (wm < c0 + 16)
        ro = np.clip(rm - rc + 7, 0, 14)
        co = np.clip(wm - wc + 15, 0, 30)
        for h in range(2):
            g = rpb2[h][ro, co]
            out[:, h, pi, :] = np.where(valid, g, np.float32(NEG))
    return out.reshape(128, 2 * len(pats) * 128)


def mo_weight_cols(core):
    h0, h1 = 2 * core, 2 * core + 1
    rng = lambda blk, h: list(range(blk * 1024 + h * 64, blk * 1024 + (h + 1) * 64))
    return np.array(rng(0, h0) + rng(0, h1) + rng(1, h0) + rng(1, h1) + rng(2, h0) + rng(2, h1))


def build_MO(T=16384):
    nc = bass.Bass("TRN2", target_bir_lowering=False)
    c = Ctx(nc)
    xT = nc.dram_tensor("xT", [1024, T], F32, kind="ExternalInput").ap()
    wsel = nc.dram_tensor("wsel", [1024, 384], F32, kind="ExternalInput").ap()
    bias = nc.dram_tensor("bias", [128, 2 * 21 * 128], F32, kind="ExternalInput").ap()
    idn = nc.dram_tensor("idn", [128, 128], F32, kind="ExternalInput").ap()
    y = nc.dram_tensor("y", [T, 128], F32, kind="ExternalOutput").ap()
    emit_MO(nc, c, xT, wsel, bias, idn, y, T)
    return nc


def emit_MO(nc, c, xT, wsel, bias, idn, y, T, pfx="O"):
    NCH = T // 128
    sb = lambda n, s, d: c.sb(pfx + n, s, d)
    V = nc.vector
    A = nc.scalar
    identb = sb("identb", [128, 128], BF16); r_identb = Res("identb")
    c.dma("pool", identb[:], idn, writes=[r_identb])
    wb = sb("wb", [128, 8, 384], BF16); r_wb = Res("wb")
    c.dma("pool", wb[:], wsel.rearrange("(c p) e -> p c e", p=128), writes=[r_wb])
    biasb = sb("biasb", [128, 2, 21, 128], BF16); r_bias = Res("bias")
    c.dma("pool", biasb[:].rearrange("p a b c -> p (a b c)"), bias, writes=[r_bias])

    qkT = sb("qkT", [128, 2, T], BF16); r_qk = [Res("qk%d" % n) for n in range(NCH)]
    vall = sb("vall", [128, NCH, 2, 66], BF16); r_v = [Res("v%d" % n) for n in range(NCH)]
    ones_dst = Res("ones")
    c.op("pool", lambda: nc.gpsimd.memset(vall[:, :, :, 64:66], 1.0), writes=[ones_dst])
    NB = 2
    xTt = [sb("xTt%d" % i, [128, 8, 128], BF16) for i in range(NB)]; r_xTt = [Res("xTt%d" % i) for i in range(NB)]
    pd = [sb("pd%d" % i, [128, 640], BF16) for i in range(3)]; r_pd = [Res("pd%d" % i) for i in range(3)]
    yt = [sb("yt%d" % i, [128, 128], F32) for i in range(NB)]; r_yt = [Res("yt%d" % i) for i in range(NB)]
    st = [sb("st%d" % i, [128, 8], F32) for i in range(NB)]; r_st = [Res("st%d" % i) for i in range(NB)]
    r_y = [Res("y0"), Res("y1")]
    PA = c.ps(pfx + "PA", [128, 512], F32); r_PA = Res("PA")
    PV = c.ps(pfx + "PV", [128, 512], F32); r_PV = Res("PV")
    PO = c.ps(pfx + "PO", [128, 512], F32); r_PO = Res("PO")
    PD = [c.ps(pfx + "PD%d" % i, [128, 1024], F32) for i in range(2)]; r_PD = [Res("PD%d" % i) for i in range(2)]

    def load_x(n):
        b = n % NB
        c.dma("pool", xTt[b][:], xT[:, n * 128:(n + 1) * 128].rearrange("(c p) t -> p c t", p=128), writes=[r_xTt[b]])

    load_x(0)
    for n in range(NCH):
        b = n % NB
        if n + 1 < NCH:
            load_x(n + 1)
        for g in range(2):
            for k in range(8):
                c.op("pe", lambda: nc.tensor.matmul(PA[:, g * 128:(g + 1) * 128], wb[:, k, g * 128:(g + 1) * 128], xTt[b][:, k, :],
                                                    start=(k == 0), stop=(k == 7)),
                     reads=[r_wb, r_xTt[b]], writes=[r_PA], acc=(g + k > 0))
        for k in range(8):
            c.op("pe", lambda: nc.tensor.matmul(PV[:, 0:128], xTt[b][:, k, :], wb[:, k, 256:384], start=(k == 0), stop=(k == 7)),
                 reads=[r_wb, r_xTt[b]], writes=[r_PV], acc=(k > 0))
        c.op("act", lambda: A.activation(qkT[:, 0, n * 128:(n + 1) * 128], PA[:, 0:128], AF.Copy, scale=0.125),
             reads=[r_PA], writes=[r_qk[n]])
        c.op("dve", lambda: V.tensor_copy(qkT[:, 1, n * 128:(n + 1) * 128], PA[:, 128:256]), reads=[r_PA], writes=[r_qk[n]])
        c.op("dve", lambda: V.tensor_copy(vall[:, n, :, 0:64], PV[:, 0:128].rearrange("p (h d) -> p h d", h=2)),
             reads=[r_PV, ones_dst], writes=[r_v[n]])

    pd_i = 0
    for n in range(NCH):
        b = n % NB
        sl = slice(n * 128, (n + 1) * 128)
        keys = mo_keys(n, NCH)
        for hh in range(2):
            hp = slice(hh * 64, (hh + 1) * 64)
            pb = pd_i % 2; sbi = pd_i % 3; pd_i += 1
            for j, (kt, pat) in enumerate(keys):
                c.op("pe", lambda: nc.tensor.matmul(PD[pb][:, j * 128:(j + 1) * 128], qkT[hp, 1, kt * 128:(kt + 1) * 128],
                                                    qkT[hp, 0, sl], start=True, stop=False),
                     reads=[r_qk[kt], r_qk[n]], writes=[r_PD[pb]], acc=(j > 0))
                c.op("pe", lambda: nc.tensor.matmul(PD[pb][:, j * 128:(j + 1) * 128], identb[:], biasb[:, hh, pat, :],
                                                    start=False, stop=True),
                     reads=[r_identb, r_bias], writes=[r_PD[pb]], acc=True)
            nk = len(keys)
            w0 = min(nk, 4) * 128
            c.op("act", lambda: A.activation(pd[sbi][:, 0:w0], PD[pb][:, 0:w0], AF.Exp), reads=[r_PD[pb]], writes=[r_pd[sbi]])
            if nk > 4:
                c.op("act", lambda: A.activation(pd[sbi][:, 512:640], PD[pb][:, 512:640], AF.Exp), reads=[r_PD[pb]], writes=[r_pd[sbi]], acc=True)
            for j, (kt, pat) in enumerate(keys):
                c.op("pe", lambda: nc.tensor.matmul(PO[:, hh * 128:hh * 128 + 65], pd[sbi][:, j * 128:(j + 1) * 128], vall[:, kt, hh, 0:65],
                                                    start=(j == 0), stop=(j == nk - 1)),
                     reads=[r_pd[sbi], r_v[kt]], writes=[r_PO], acc=(hh + j > 0))
        s = st[b]; rs = r_st[b]
        for hh in range(2):
            c.op("dve", lambda: V.reciprocal(s[:, hh:hh + 1], PO[:, hh * 128 + 64:hh * 128 + 65]), reads=[r_PO, rs], writes=[rs])
            c.op("dve", lambda: V.tensor_scalar(yt[b][:, hh * 64:(hh + 1) * 64], PO[:, hh * 128:hh * 128 + 64], s[:, hh:hh + 1], None, ALU.mult),
                 reads=[r_PO, rs], writes=[r_yt[b]])
        c.dma("sp", y[sl, :], yt[b][:], reads=[r_yt[b]], writes=[r_y[b]])
    c.finish("sp", r_y)
    print("MO: ins", c.n_ins, "waits", c.n_wait, "sbuf left", nc.sbuf_bytes_remaining)


N_CORES = 8
T_SEQ = 16384
_PROGS = {}


def _prog(name):
    if name not in _PROGS:
        _PROGS[name] = {"ME": lambda: build_ME(T_SEQ), "MO": lambda: build_MO(T_SEQ), "F": lambda: build_F2(32, 2)}[name]()
    return _PROGS[name]


def _launch(nc, in_maps):
    res = run_bass_kernel_spmd(nc, in_maps, core_ids=list(range(N_CORES)))
    return res.results


def kernel(x, ab_w_in, ab_w_out, ret_decay, c_w_in, c_w_out, c_rpb, ln_g, ln_b,
           router_w, router_b, exp_w_up, exp_b_up, exp_w_down, exp_b_down):
    f32 = np.float32
    xs = np.ascontiguousarray(np.asarray(x, f32)[0])
    T = xs.shape[0]
    idn = np.eye(128, dtype=f32)
    ii = np.arange(128)
    fcst = np.ascontiguousarray(np.concatenate([np.tile(np.arange(256, dtype=f32)[None, :], (128, 1)),
                                                (ii[:, None] < ii[None, :]).astype(f32), np.ones((128, 128), f32)], 1))
    me_c = me_host_consts(T)
    per = T // N_CORES
    for layer in range(4):
        j = layer // 2
        xT = np.ascontiguousarray(xs.T)
        ycat = np.empty((T, 1024), f32)
        if layer % 2 == 0:
            w_in = np.asarray(ab_w_in[j], f32)
            dec = np.asarray(ret_decay[j], f32)
            in_maps = [dict(xT=xT, wsel=np.ascontiguousarray(w_in[:, me_weight_cols(h)]),
                            dec=np.ascontiguousarray(dec[:, h][None, :]), **me_c) for h in range(N_CORES)]
            outs = _launch(_prog("ME"), in_maps)
            for h in range(N_CORES):
                yh = outs[h]["y"]
                ycat[:, h * 64:(h + 1) * 64] = yh[:, 0:64]
                ycat[:, 512 + h * 64:512 + (h + 1) * 64] = yh[:, 64:128]
            w_out = np.asarray(ab_w_out[j], f32)
        else:
            w_in = np.asarray(c_w_in[j], f32)
            rpb = np.asarray(c_rpb[j], f32)
            in_maps = [dict(xT=xT, wsel=np.ascontiguousarray(w_in[:, mo_weight_cols(cc)]),
                            bias=mo_host_bias(rpb[2 * cc:2 * cc + 2], T), idn=idn) for cc in range(N_CORES)]
            outs = _launch(_prog("MO"), in_maps)
            for cc in range(N_CORES):
                ycat[:, cc * 128:(cc + 1) * 128] = outs[cc]["y"]
            w_out = np.asarray(c_w_out[j], f32)
        lnp = np.ascontiguousarray(np.stack([ln_g[layer, 0], ln_b[layer, 0], ln_g[layer, 1], ln_b[layer, 1]], 0).astype(f32))
        common = dict(w_out=np.ascontiguousarray(w_out), lnp=lnp,
                      rw=np.ascontiguousarray(np.asarray(router_w[layer], f32)),
                      rb=np.ascontiguousarray(np.asarray(router_b[layer], f32)[None, :]),
                      wup=np.ascontiguousarray(np.asarray(exp_w_up[layer], f32)),
                      bup=np.ascontiguousarray(np.asarray(exp_b_up[layer], f32).reshape(256, 256)),
                      wdn=np.ascontiguousarray(np.asarray(exp_w_down[layer], f32)),
                      bdn=np.ascontiguousarray(np.asarray(exp_b_down[layer], f32)), idn=idn, fcst=fcst)
        in_maps = []
        for cc in range(N_CORES):
            sl = slice(cc * per, (cc + 1) * per)
            in_maps.append(dict(common, x=np.ascontiguousarray(xs[sl]), yT=np.ascontiguousarray(ycat[sl].T)))
        outs = _launch(_prog("F"), in_maps)
        xs = np.concatenate([outs[cc]["xo"] for cc in range(N_CORES)], 0)
    return xs[None].astype(f32)
```

```python
import numpy as np
import concourse.bass as bass
import concourse.mybir as mybir
from concourse.bass_utils import run_bass_kernel_spmd

F32 = mybir.dt.float32
BF16 = mybir.dt.bfloat16
AF = mybir.ActivationFunctionType
ALU = mybir.AluOpType
AX = mybir.AxisListType


class Res:
    __slots__ = ("name", "w", "r", "dsem", "dcnt")

    def __init__(self, name):
        self.name = name
        self.w = None
        self.r = []
        self.dsem = None
        self.dcnt = 0


class Ctx:
    def __init__(self, nc):
        self.nc = nc
        self.eng = {"pe": nc.tensor, "act": nc.scalar, "dve": nc.vector,
                    "pool": nc.gpsimd, "sp": nc.sync}
        self.sem = {k: nc.alloc_semaphore("c_" + k) for k in ("pe", "act", "dve", "pool")}
        self.cnt = {k: 0 for k in self.sem}
        self.seen = {k: {} for k in self.eng}
        self.semobj = dict(self.sem)
        self.n_dsem = 0
        self.n_wait = 0
        self.n_ins = 0

    def sb(self, name, shape, dt):
        return self.nc.alloc_sbuf_tensor(name, list(shape), dt)

    def ps(self, name, shape, dt=F32):
        return self.nc.alloc_psum_tensor(name, list(shape), dt)

    def _dsem(self, res):
        if res.dsem is None:
            key = "d%d" % self.n_dsem
            self.n_dsem += 1
            res.dsem = key
            self.semobj[key] = self.nc.alloc_semaphore(key)
        return res.dsem

    def _wait(self, e, deps):
        seen = self.seen[e]
        eng = self.eng[e]
        best = {}
        for d in deps:
            if d is None:
                continue
            k, v = d
            if best.get(k, 0) < v:
                best[k] = v
        for k, v in best.items():
            if seen.get(k, 0) < v:
                eng.wait_ge(self.semobj[k], v)
                seen[k] = v
                self.n_wait += 1

    def op(self, e, fn, reads=(), writes=(), acc=False):
        deps = [r.w for r in reads]
        if not acc:
            for w in writes:
                deps.append(w.w)
                deps.extend(w.r)
        self._wait(e, deps)
        ins = fn()
        self.cnt[e] += 1
        ins.then_inc(self.sem[e], 1)
        ev = (e, self.cnt[e])
        self.seen[e][e] = max(self.seen[e].get(e, 0), 0)
        for r in reads:
            r.r.append(ev)
        for w in writes:
            w.w = ev
            if not acc:
                w.r = []
        self.n_ins += 1
        return ins

    def dma(self, q, out, in_, reads=(), writes=(), **kw):
        assert len(writes) == 1
        wres = writes[0]
        deps = [r.w for r in reads]
        deps.append(wres.w)
        deps.extend(wres.r)
        self._wait(q, deps)
        key = self._dsem(wres)
        ins = self.eng[q].dma_start(out=out, in_=in_, **kw)
        wres.dcnt += 16
        ins.then_inc(self.semobj[key], 16)
        ev = (key, wres.dcnt)
        for r in reads:
            r.r.append(ev)
        wres.w = ev
        wres.r = []
        self.n_ins += 1
        return ins

    def dma_fn(self, q, fn, reads=(), writes=()):
        wres = writes[0]
        deps = [r.w for r in reads]
        deps.append(wres.w)
        deps.extend(wres.r)
        self._wait(q, deps)
        key = self._dsem(wres)
        ins = fn()
        wres.dcnt += 16
        ins.then_inc(self.semobj[key], 16)
        ev = (key, wres.dcnt)
        for r in reads:
            r.r.append(ev)
        wres.w = ev
        wres.r = []
        self.n_ins += 1
        return ins

    def dma_more(self, q, out, in_, reads=(), writes=(), **kw):
        wres = writes[0]
        deps = [r.w for r in reads]
        self._wait(q, deps)
        key = self._dsem(wres)
        ins = self.eng[q].dma_start(out=out, in_=in_, **kw)
        wres.dcnt += 16
        ins.then_inc(self.semobj[key], 16)
        ev = (key, wres.dcnt)
        for r in reads:
            r.r.append(ev)
        wres.w = ev
        self.n_ins += 1
        return ins

    def finish(self, q, resources):
        self._wait(q, [r.w for r in resources])


ALPHA = (2.0 * 4) ** 0.25
LN_EPS = 1e-5


def build_F2(n_exp=32, n_blk=2):
    nc = bass.Bass("TRN2", target_bir_lowering=False)
    c = Ctx(nc)
    TB = 1024
    NT = TB * n_blk
    D = 1024
    x = nc.dram_tensor("x", [NT, D], F32, kind="ExternalInput").ap()
    yT = nc.dram_tensor("yT", [D, NT], F32, kind="ExternalInput").ap()
    w_out = nc.dram_tensor("w_out", [D, D], F32, kind="ExternalInput").ap()
    lnp = nc.dram_tensor("lnp", [4, D], F32, kind="ExternalInput").ap()
    rw = nc.dram_tensor("rw", [D, 32], F32, kind="ExternalInput").ap()
    rb = nc.dram_tensor("rb", [1, 32], F32, kind="ExternalInput").ap()
    wup = nc.dram_tensor("wup", [n_exp, D, 2 * D], F32, kind="ExternalInput").ap()
    bup = nc.dram_tensor("bup", [256, 256], F32, kind="ExternalInput").ap()
    wdn = nc.dram_tensor("wdn", [n_exp, D, D], F32, kind="ExternalInput").ap()
    bdn = nc.dram_tensor("bdn", [32, D], F32, kind="ExternalInput").ap()
    idn = nc.dram_tensor("idn", [128, 128], F32, kind="ExternalInput").ap()
    fcst = nc.dram_tensor("fcst", [128, 512], F32, kind="ExternalInput").ap()
    xo = nc.dram_tensor("xo", [NT, D], F32, kind="ExternalOutput").ap()

    emit_F2(nc, c, x, yT, w_out, lnp, rw, rb, wup, bup, wdn, bdn, idn, fcst, xo, n_exp=n_exp, n_blk=n_blk)
    return nc


def emit_F2(nc, c, x, yT, w_out, lnp, rw, rb, wup, bup, wdn, bdn, idn, fcst, xo, n_exp=32, n_blk=2, pfx="F"):
    CAP = 256
    TB = 1024
    D = 1024
    NTL = TB // 128
    sb = lambda n, s, d: c.sb(pfx + n, s, d)
    ident = sb("ident", [128, 128], F32); r_ident = Res("ident")
    c.dma("sp", ident[:], idn, writes=[r_ident])
    rw32 = sb("rw32", [128, 8, 32], F32); r_rw = Res("rw")
    c.dma("sp", rw32[:], rw.rearrange("(c p) e -> p c e", p=128), writes=[r_rw])
    rbB = sb("rbB", [128, 32], F32); r_rb = Res("rb")
    c.dma("sp", rbB[:], rb.partition_broadcast(128), writes=[r_rb])
    bdn32 = sb("bdn32", [32, D], F32); r_bdn = Res("bdn")
    c.dma("sp", bdn32[:], bdn, writes=[r_bdn])
    lnB = [sb("lnB%d" % i, [128, D], F32) for i in range(4)]
    r_ln = [Res("ln%d" % i) for i in range(4)]
    for i in range(4):
        c.dma("sp", lnB[i][:], lnp[i:i + 1, :].partition_broadcast(128), writes=[r_ln[i]])
    iota = sb("iota", [128, 256], F32); r_iota = Res("iota")
    c.dma("sp", iota[:], fcst[:, 0:256], writes=[r_iota])
    UO = sb("UO", [128, 256], BF16); r_UO = Res("UO")
    c.dma("pool", UO[:], fcst[:, 256:512], writes=[r_UO])
    identb = sb("identb", [128, 128], BF16); r_identb = Res("identb")
    c.dma("pool", identb[:], idn, writes=[r_identb])
    B = [c.ps(pfx + "bank%d" % i, [128, 512], F32) for i in range(7)]
    rB = [Res("bank%d" % i) for i in range(7)]
    TP = c.ps(pfx + "TP", [128, 1024], BF16); r_TP = Res("TP")
    bupraw = sb("bupraw", [128, 2, 256], F32); r_bupraw = Res("bupraw")
    c.dma("sp", bupraw[:], bup.rearrange("(g p) k -> p g k", p=128), writes=[r_bupraw])
    bupT = sb("bupT", [128, 2, 256], F32); r_bupT = Res("bupT")
    for two in range(2):
        for g in range(2):
            c.op("pe", lambda: nc.tensor.transpose(B[0][:, (two * 2 + g) * 128:(two * 2 + g + 1) * 128],
                                                   bupraw[:, g, two:256:2], ident[:]),
                 reads=[r_bupraw, r_ident], writes=[rB[0]], acc=(two + g > 0))
    c.op("act", lambda: nc.scalar.copy(bupT[:].rearrange("p a b -> p (a b)"), B[0][:]), reads=[rB[0]], writes=[r_bupT])

    c.op("dve", lambda: nc.vector.tensor_scalar(bupT[:, 1, :], bupT[:, 1, :], 1.0, None, ALU.add), reads=[r_bupT], writes=[r_bupT])

    yacc = sb("yacc", [128, NTL, D], F32); r_yacc = [Res("yacc%d" % i) for i in range(NTL)]
    x1b = sb("x1b", [128, NTL, D], BF16); r_x1b = [Res("x1b%d" % i) for i in range(NTL)]
    maskf = sb("maskf", [128, NTL, 32], F32); r_mask = [Res("mask%d" % i) for i in range(NTL)]
    maskb = sb("maskb", [128, NTL, 32], BF16)
    rank = sb("rank", [128, NTL, 32], F32); r_rank = [Res("rank%d" % i) for i in range(NTL)]
    carry = sb("carry", [128, 32], F32); r_carry = Res("carry")
    gates = sb("gates", [128, NTL, 32], F32); r_gates = [Res("gates%d" % i) for i in range(NTL)]
    gT = sb("gT", [32, TB], F32); r_gT = [Res("gT%d" % i) for i in range(NTL)]
    x1T32 = [sb("x1T32_%d" % i, [128, 8, 128], F32) for i in range(2)]; r_x1T32 = [Res("x1T32_%d" % i) for i in range(2)]
    sm = [sb("sm%d" % i, [128, 128], F32) for i in range(2)]; r_sm = [Res("sm%d" % i) for i in range(2)]
    wuc = [sb("wuc%d" % i, [128, 8, 256], BF16) for i in range(4)]; r_wuc = [Res("wuc%d" % i) for i in range(4)]
    wd = [sb("wd%d" % i, [128, 8, D], BF16) for i in range(2)]; r_wd = [Res("wd%d" % i) for i in range(2)]
    yTb, r_yTb = wd[0], r_wd[0]
    woutb, r_wout = wd[1], r_wd[1]
    Pm = [sb("Pm%d" % i, [128, NTL, CAP], BF16) for i in range(2)]; r_Pm = [Res("Pm%d" % i) for i in range(2)]
    Gm = [sb("Gm%d" % i, [128, NTL, CAP], BF16) for i in range(2)]; r_Gm = [Res("Gm%d" % i) for i in range(2)]
    GT = [sb("GT%d" % i, [128, 2, TB], BF16) for i in range(2)]; r_GT = [Res("GT%d" % i) for i in range(2)]
    XgT = [sb("XgT%d" % i, [128, 8, CAP], BF16) for i in range(2)]; r_XgT = [Res("XgT%d" % i) for i in range(2)]
    act = [sb("act%d" % i, [128, 8, CAP], BF16) for i in range(2)]; r_act = [Res("act%d" % i) for i in range(2)]
    osb = [sb("osb%d" % i, [128, 2, D], BF16) for i in range(2)]; r_osb = [Res("osb%d" % i) for i in range(2)]
    NTMP = 2
    tg = [sb("tg%d" % i, [128, CAP], F32) for i in range(NTMP)]; r_tg = [Res("tg%d" % i) for i in range(NTMP)]
    tsg = [sb("tsg%d" % i, [128, CAP], F32) for i in range(NTMP)]; r_tsg = [Res("tsg%d" % i) for i in range(NTMP)]
    tl = [sb("tl%d" % i, [128, CAP], F32) for i in range(NTMP)]; r_tl = [Res("tl%d" % i) for i in range(NTMP)]
    ot = [sb("ot%d" % i, [128, D], F32) for i in range(2)]; r_ot = [Res("ot%d" % i) for i in range(2)]
    zt, r_zt = ot, r_ot
    xt, r_xt = ot, r_ot
    r_out = [Res("xo0"), Res("xo1")]

    def layer_norm(src, r_src, dst, r_dst, s, r_s, gi, bi):
        for h in range(2):
            c.op("dve", lambda: nc.vector.bn_stats(s[:, h * 6:(h + 1) * 6], src[:, h * 512:(h + 1) * 512]),
                 reads=[r_src], writes=[r_s], acc=(h > 0))
        c.op("dve", lambda: nc.vector.bn_aggr(s[:, 12:14], s[:, 0:12]),
             reads=[r_s], writes=[r_s])
        c.op("act", lambda: nc.scalar.activation(s[:, 14:15], s[:, 13:14], AF.Sqrt, bias=epsT[:, 0:1], scale=1.0),
             reads=[r_s, r_eps], writes=[r_s])
        c.op("dve", lambda: nc.vector.reciprocal(s[:, 15:16], s[:, 14:15]), reads=[r_s], writes=[r_s])
        c.op("dve", lambda: nc.vector.tensor_scalar(dst, src, s[:, 12:13], s[:, 15:16], ALU.subtract, ALU.mult),
             reads=[r_src, r_s], writes=[r_dst])
        c.op("dve", lambda: nc.vector.tensor_tensor(dst, dst, lnB[gi][:], ALU.mult), reads=[r_dst, r_ln[gi]], writes=[r_dst])
        c.op("dve", lambda: nc.vector.tensor_tensor(dst, dst, lnB[bi][:], ALU.add), reads=[r_dst, r_ln[bi]], writes=[r_dst])

    epsT = sb("epsT", [128, 1], F32); r_eps = Res("eps")
    c.op("dve", lambda: nc.vector.memset(epsT[:], LN_EPS), writes=[r_eps])

    def issue_wd(q):
        e_ = q % n_exp
        c.dma("pool", wd[q % 2][:], wdn[e_].rearrange("(c p) d -> p c d", p=128), writes=[r_wd[q % 2]])

    def issue_wu(gi):
        e_ = (gi // 8) % n_exp
        fc_ = gi % 8
        c.dma("pool", wuc[gi % 4][:], wup[e_][:, fc_ * 256:(fc_ + 1) * 256].rearrange("(c p) f -> p c f", p=128), writes=[r_wuc[gi % 4]])

    issue_wu(0)
    issue_wu(1)
    tmp_i = 0
    pg_i = 0
    po_i = 0
    sc_i = 0
    GXB = (3, 6)
    for blk in range(n_blk):
        t0 = blk * TB
        c.dma("pool", yTb[:], yT[:, t0:t0 + TB].rearrange("(c p) t -> p c t", p=128), writes=[r_yTb])
        c.dma("pool", woutb[:], w_out.rearrange("(c p) d -> p c d", p=128), writes=[r_wout])
        for i in range(NTL):
            b2 = i % 2
            c.dma("sp", xt[b2][:], x[t0 + i * 128:t0 + (i + 1) * 128, :], writes=[r_xt[b2]])
            for h in range(2):
                for k in range(8):
                    c.op("pe", lambda: nc.tensor.matmul(B[h][:], yTb[:, k, i * 128:(i + 1) * 128],
                                                        woutb[:, k, h * 512:(h + 1) * 512], start=(k == 0), stop=(k == 7)),
                         reads=[r_yTb, r_wout], writes=[rB[h]], acc=(k > 0))
                c.op("dve", lambda: nc.vector.scalar_tensor_tensor(zt[b2][:, h * 512:(h + 1) * 512], xt[b2][:, h * 512:(h + 1) * 512],
                                                                   ALPHA, B[h][:], ALU.mult, ALU.add),
                     reads=[r_xt[b2], rB[h]], writes=[r_zt[b2]], acc=(h > 0))
            layer_norm(zt[b2][:], r_zt[b2], yacc[:, i, :], r_yacc[i], sm[b2], r_sm[b2], 0, 1)
            for k in range(8):
                bk = 2 + k // 4
                c.op("pe", lambda: nc.tensor.transpose(B[bk][:, (k % 4) * 128:(k % 4 + 1) * 128],
                                                       yacc[:, i, k * 128:(k + 1) * 128], ident[:]),
                     reads=[r_yacc[i], r_ident], writes=[rB[bk]], acc=(k % 4 > 0))
            for hh in range(2):
                c.op("act", lambda: nc.scalar.copy(x1T32[b2][:, hh * 4:(hh + 1) * 4, :].rearrange("p a b -> p (a b)"), B[2 + hh][:]),
                     reads=[rB[2 + hh]], writes=[r_x1T32[b2]], acc=(hh > 0))
            c.op("act", lambda: nc.scalar.copy(x1b[:, i, :], yacc[:, i, :]), reads=[r_yacc[i]], writes=[r_x1b[i]])
            for k in range(8):
                c.op("pe", lambda: nc.tensor.matmul(B[4][:, 0:32], x1T32[b2][:, k, :], rw32[:, k, :], start=(k == 0), stop=(k == 7)),
                     reads=[r_x1T32[b2], r_rw], writes=[rB[4]], acc=(k > 0))
            s = sm[b2]; r_s = r_sm[b2]
            c.op("dve", lambda: nc.vector.tensor_tensor(s[:, 32:64], B[4][:, 0:32], rbB[:], ALU.add), reads=[rB[4], r_rb], writes=[r_s])
            c.op("dve", lambda: nc.vector.max(s[:, 16:24], s[:, 32:64]), reads=[r_s], writes=[r_s])
            c.op("dve", lambda: nc.vector.tensor_scalar(s[:, 64:96], s[:, 32:64], s[:, 19:20], None, ALU.is_ge), reads=[r_s], writes=[r_s])
            c.op("dve", lambda: nc.vector.tensor_copy(maskf[:, i, :], s[:, 64:96]), reads=[r_s], writes=[r_mask[i]])
            c.op("dve", lambda: nc.vector.tensor_copy(maskb[:, i, :], s[:, 64:96]), reads=[r_s], writes=[r_mask[i]])
            c.op("pe", lambda: nc.tensor.matmul(B[4][:, 256:288], UO[:, 0:128], maskb[:, i, :], start=True, stop=True),
                 reads=[r_UO, r_mask[i]], writes=[rB[4]])
            c.op("pe", lambda: nc.tensor.matmul(B[4][:, 288:320], UO[:, 128:256], maskb[:, i, :], start=True, stop=True),
                 reads=[r_UO, r_mask[i]], writes=[rB[4]], acc=True)
            if i == 0:
                c.op("dve", lambda: nc.vector.tensor_copy(rank[:, i, :], B[4][:, 256:288]), reads=[rB[4]], writes=[r_rank[i]])
                c.op("dve", lambda: nc.vector.tensor_copy(carry[:], B[4][:, 288:320]), reads=[rB[4]], writes=[r_carry])
            else:
                c.op("dve", lambda: nc.vector.tensor_tensor(rank[:, i, :], B[4][:, 256:288], carry[:], ALU.add),
                     reads=[rB[4], r_carry], writes=[r_rank[i]])
                c.op("dve", lambda: nc.vector.tensor_tensor(carry[:], carry[:], B[4][:, 288:320], ALU.add),
                     reads=[rB[4], r_carry], writes=[r_carry])
            c.op("dve", lambda: nc.vector.tensor_scalar(s[:, 24:25], s[:, 16:17], -1.0, None, ALU.mult), reads=[r_s], writes=[r_s])
            c.op("act", lambda: nc.scalar.activation(s[:, 96:128], s[:, 32:64], AF.Exp, bias=s[:, 24:25], scale=1.0), reads=[r_s], writes=[r_s])
            c.op("dve", lambda: nc.vector.tensor_tensor(s[:, 96:128], s[:, 96:128], s[:, 64:96], ALU.mult), reads=[r_s], writes=[r_s])
            c.op("dve", lambda: nc.vector.reduce_sum(s[:, 25:26], s[:, 96:128], AX.X), reads=[r_s], writes=[r_s])
            c.op("dve", lambda: nc.vector.reciprocal(s[:, 26:27], s[:, 25:26]), reads=[r_s], writes=[r_s])
            c.op("dve", lambda: nc.vector.tensor_scalar(gates[:, i, :], s[:, 96:128], s[:, 26:27], None, ALU.mult), reads=[r_s], writes=[r_gates[i]])
            c.op("pe", lambda: nc.tensor.transpose(B[4][0:32, 128:256], gates[:, i, :], ident[:]),
                 reads=[r_gates[i], r_ident], writes=[rB[4]])
            c.op("act", lambda: nc.scalar.copy(gT[:, i * 128:(i + 1) * 128], B[4][0:32, 128:256]), reads=[rB[4]], writes=[r_gT[i]])
            for h in range(2):
                c.op("pe", lambda: nc.tensor.matmul(B[2 + h][:], gT[:, i * 128:(i + 1) * 128], bdn32[:, h * 512:(h + 1) * 512],
                                                    start=True, stop=True), reads=[r_gT[i], r_bdn], writes=[rB[2 + h]])
                c.op("dve", lambda: nc.vector.scalar_tensor_tensor(yacc[:, i, h * 512:(h + 1) * 512], yacc[:, i, h * 512:(h + 1) * 512],
                                                                   ALPHA, B[2 + h][:], ALU.mult, ALU.add),
                     reads=[r_yacc[i], rB[2 + h]], writes=[r_yacc[i]])
        issue_wd(blk * n_exp)
        def build(e):
            ab = e % 2
            for i in range(NTL):
                c.op("dve", lambda: nc.vector.tensor_scalar(Pm[ab][:, i, :], iota[:], rank[:, i, e:e + 1], maskf[:, i, e:e + 1],
                                                            ALU.is_equal, ALU.mult),
                     reads=[r_iota, r_rank[i], r_mask[i]], writes=[r_Pm[ab]], acc=(i > 0))
                c.op("dve", lambda: nc.vector.tensor_scalar(Gm[ab][:, i, :], iota[:], rank[:, i, e:e + 1], gates[:, i, e:e + 1],
                                                            ALU.is_equal, ALU.mult),
                     reads=[r_iota, r_rank[i], r_gates[i]], writes=[r_Gm[ab]], acc=(i > 0))

        def transposes(e):
            ab = e % 2
            for st_ in range(2):
                for i in range(NTL):
                    c.op("pe", lambda: nc.tensor.transpose(TP[:, i * 128:(i + 1) * 128], Gm[ab][:, i, st_ * 128:(st_ + 1) * 128], identb[:]),
                         reads=[r_Gm[ab], r_identb], writes=[r_TP], acc=(i > 0))
                c.op("act", lambda: nc.scalar.copy(GT[ab][:, st_, :], TP[:]), reads=[r_TP], writes=[r_GT[ab]], acc=(st_ > 0))

        def gather(e):
            ab = e % 2
            for k in range(8):
                gh = k % 2
                for i in range(NTL):
                    c.op("pe", lambda: nc.tensor.matmul(B[GXB[gh]][:, 0:256], x1b[:, i, k * 128:(k + 1) * 128], Pm[ab][:, i, :],
                                                        start=(i == 0), stop=(i == NTL - 1)),
                         reads=[r_x1b[i], r_Pm[ab]], writes=[rB[GXB[gh]]], acc=(i > 0))
                c.op("act", lambda: nc.scalar.copy(XgT[ab][:, k, :], B[GXB[gh]][:, 0:256]), reads=[rB[GXB[gh]]], writes=[r_XgT[ab]], acc=(k > 0))

        def up(e):
            nonlocal pg_i, tmp_i
            q = blk * n_exp + e
            ab = e % 2
            for fc in range(8):
                gi = q * 8 + fc
                if gi + 2 < n_blk * n_exp * 8:
                    issue_wu(gi + 2)
                ws = gi % 4
                row = e * 8 + fc
                ub = pg_i % 2; pg_i += 1
                for two in range(2):
                    for k in range(8):
                        c.op("pe", lambda: nc.tensor.matmul(B[ub][:, two * 256:(two + 1) * 256], wuc[ws][:, k, two:256:2], XgT[ab][:, k, :],
                                                            start=(k == 0), stop=(k == 7)),
                             reads=[r_wuc[ws], r_XgT[ab]], writes=[rB[ub]], acc=(two + k > 0))
                ti = tmp_i % NTMP; tmp_i += 1
                c.op("dve", lambda: nc.vector.tensor_scalar(tg[ti][:], B[ub][:, 0:256], bupT[:, 0, row:row + 1], 7.0, ALU.add, ALU.min),
                     reads=[rB[ub], r_bupT], writes=[r_tg[ti]])
                c.op("act", lambda: nc.scalar.activation(tsg[ti][:], tg[ti][:], AF.Sigmoid, scale=1.702),
                     reads=[r_tg[ti]], writes=[r_tsg[ti]])
                c.op("dve", lambda: nc.vector.tensor_scalar(tl[ti][:], B[ub][:, 256:512], bupT[:, 1, row:row + 1], -6.0, ALU.add, ALU.max),
                     reads=[rB[ub], r_bupT], writes=[r_tl[ti]])
                c.op("dve", lambda: nc.vector.scalar_tensor_tensor(tl[ti][:], tl[ti][:], 8.0, tg[ti][:], ALU.min, ALU.mult),
                     reads=[r_tg[ti], r_tl[ti]], writes=[r_tl[ti]])
                c.op("dve", lambda: nc.vector.tensor_tensor(act[ab][:, fc, :], tl[ti][:], tsg[ti][:], ALU.mult),
                     reads=[r_tl[ti], r_tsg[ti]], writes=[r_act[ab]], acc=(fc > 0))

        def down(e):
            ab = e % 2
            for st_ in range(2):
                for h in range(2):
                    po = 2
                    for fc in range(8):
                        c.op("pe", lambda: nc.tensor.matmul(B[po][:], act[ab][:, fc, st_ * 128:(st_ + 1) * 128],
                                                            wd[ab][:, fc, h * 512:(h + 1) * 512], start=(fc == 0), stop=(fc == 7)),
                             reads=[r_act[ab], r_wd[ab]], writes=[rB[po]], acc=(fc > 0))
                    c.op("act", lambda: nc.scalar.copy(osb[ab][:, st_, h * 512:(h + 1) * 512], B[po][:]), reads=[rB[po]], writes=[r_osb[ab]],
                         acc=(st_ + h > 0))

        def scatter(e):
            nonlocal sc_i
            ab = e % 2
            for i in range(NTL):
                for h in range(2):
                    sc = 4 + (sc_i % 2); sc_i += 1
                    for st_ in range(2):
                        c.op("pe", lambda: nc.tensor.matmul(B[sc][:], GT[ab][:, st_, i * 128:(i + 1) * 128], osb[ab][:, st_, h * 512:(h + 1) * 512],
                                                            start=(st_ == 0), stop=(st_ == 1)),
                             reads=[r_GT[ab], r_osb[ab]], writes=[rB[sc]], acc=(st_ > 0))
                    c.op("dve", lambda: nc.vector.tensor_tensor(yacc[:, i, h * 512:(h + 1) * 512], yacc[:, i, h * 512:(h + 1) * 512], B[sc][:], ALU.add),
                         reads=[rB[sc], r_yacc[i]], writes=[r_yacc[i]])

        build(0)
        transposes(0)
        gather(0)
        for e in range(n_exp):
            q = blk * n_exp + e
            if e + 1 < n_exp:
                issue_wd(q + 1)
                build(e + 1)
            up(e)
            if e + 1 < n_exp:
                gather(e + 1)
            down(e)
            if e + 1 < n_exp:
                transposes(e + 1)
            scatter(e)
        for i in range(NTL):
            b2 = i % 2
            layer_norm(yacc[:, i, :], r_yacc[i], ot[b2][:], r_ot[b2], sm[b2], r_sm[b2], 2, 3)
            c.dma("sp", xo[t0 + i * 128:t0 + (i + 1) * 128, :], ot[b2][:], reads=[r_ot[b2]], writes=[r_out[b2]])
    c.finish("sp", r_out)
    print("F: ins", c.n_ins, "waits", c.n_wait, "sbuf left", nc.sbuf_bytes_remaining)


GN_EPS = 1e-6


def me_host_consts(T):
    pos = np.arange(T, dtype=np.float32)
    inv = (10000.0 ** (-np.arange(0, 64, 2, dtype=np.float32) / 64)).astype(np.float32)
    ang = pos[:, None] * inv[None, :]
    cos = np.cos(ang).astype(np.float32).T
    sin = np.sin(ang).astype(np.float32).T
    cosF = np.concatenate([cos, cos], 0)
    sinS = np.concatenate([-sin, sin], 0)
    s8 = np.float32(0.125)
    cq = np.concatenate([cosF, cosF * s8], 0)
    sq = np.concatenate([sinS, sinS * s8], 0)
    ck = np.concatenate([cosF * s8, cosF], 0)
    sk = np.concatenate([sinS * s8, sinS], 0)
    tab = np.ascontiguousarray(np.stack([cq, sq, ck, sk], 0)).astype(np.float32)
    i = np.arange(128, dtype=np.float32)
    m = i[:, None]; c = i[None, :]
    cst = np.zeros((128, 6, 128), np.float32)
    cst[:, 0, :] = np.maximum(c - m, 0)
    cst[:, 1, :] = (c >= m)
    cst[:, 2, :] = np.maximum(m - c, 0)
    cst[:, 3, :] = (m > c)
    cst[:, 4, :] = c + 1.0
    cst[:, 5, :] = 128.0 - c
    col = np.zeros((128, 4), np.float32)
    col[:, 0] = 127.0 - i
    col[:, 1] = i
    col[:, 2] = 128.0
    o = np.arange(17)[None, :, None]
    delta = (o - 8) * 128 + m[:, None, :].astype(np.int64) - c[None, :, :].astype(np.int64)
    delta = delta.astype(np.int64)
    ad = np.abs(delta)
    mm = (ad <= 64).astype(np.float32) + ((delta % 4 == 0) & (ad <= 256)) + ((delta % 16 == 0) & (ad <= 1024))
    mm = mm.astype(np.float32).reshape(128, 17 * 128)
    return dict(tab=tab, cst=cst.reshape(128, 768), col=col, mm=mm, idn=np.eye(128, dtype=np.float32))


def me_weight_cols(head):
    H = 64
    base = lambda blk: blk * 512 + head * H
    rng = lambda b: list(range(base(b), base(b) + H))
    sw = lambda b: list(range(base(b) + 32, base(b) + 64)) + list(range(base(b), base(b) + 32))
    cols = []
    cols += rng(0) + rng(5)
    cols += sw(0) + sw(5)
    cols += rng(1) + rng(6)
    cols += sw(1) + sw(6)
    cols += rng(2) + rng(7)
    cols += rng(3) + rng(4)
    return np.array(cols)


def build_ME(T=16384):
    nc = bass.Bass("TRN2", target_bir_lowering=False)
    c = Ctx(nc)
    xT = nc.dram_tensor("xT", [1024, T], F32, kind="ExternalInput").ap()
    wsel = nc.dram_tensor("wsel", [1024, 768], F32, kind="ExternalInput").ap()
    dec = nc.dram_tensor("dec", [1, 2], F32, kind="ExternalInput").ap()
    tab = nc.dram_tensor("tab", [4, 128, T], F32, kind="ExternalInput").ap()
    cst = nc.dram_tensor("cst", [128, 768], F32, kind="ExternalInput").ap()
    col = nc.dram_tensor("col", [128, 4], F32, kind="ExternalInput").ap()
    mm = nc.dram_tensor("mm", [128, 17 * 128], F32, kind="ExternalInput").ap()
    idn = nc.dram_tensor("idn", [128, 128], F32, kind="ExternalInput").ap()
    y = nc.dram_tensor("y", [T, 128], F32, kind="ExternalOutput").ap()
    emit_ME(nc, c, xT, wsel, dec, tab, cst, col, mm, idn, y, T)
    return nc


def emit_ME(nc, c, xT, wsel, dec, tab, cst, col, mm, idn, y, T, pfx="E"):
    NCH = T // 128
    sb = lambda n, s, d: c.sb(pfx + n, s, d)
    V = nc.vector
    A = nc.scalar
    identb = sb("identb", [128, 128], BF16); r_identb = Res("identb")
    c.dma("pool", identb[:], idn, writes=[r_identb])
    wb = sb("wb", [128, 8, 768], BF16); r_wb = Res("wb")
    c.dma("pool", wb[:], wsel.rearrange("(c p) e -> p c e", p=128), writes=[r_wb])
    mmb = sb("mmb", [128, 17 * 128], BF16); r_mm = Res("mm")
    c.dma("pool", mmb[:], mm, writes=[r_mm])
    cs = sb("cs", [128, 6, 128], F32); r_cs = Res("cs")
    c.dma("sp", cs[:].rearrange("p a b -> p (a b)"), cst, writes=[r_cs])
    cl = sb("cl", [128, 4], F32); r_cl = Res("cl")
    c.dma("sp", cl[:], col, writes=[r_cl])
    dl = sb("dl", [128, 16], F32); r_dl = Res("dl")
    c.dma("sp", dl[:, 0:2], dec.partition_broadcast(128), writes=[r_dl])
    ops = [
        lambda: V.tensor_scalar(dl[:, 2:4], dl[:, 0:2], -1.0, None, ALU.mult),
        lambda: V.tensor_tensor(dl[:, 2:4], dl[:, 2:4], dl[:, 0:2], ALU.max),
    ]
    for f in ops:
        c.op("dve", f, reads=[r_dl], writes=[r_dl])
    c.op("act", lambda: A.activation(dl[:, 2:4], dl[:, 2:4], AF.Exp, scale=-1.0), reads=[r_dl], writes=[r_dl])
    ops = [
        lambda: V.tensor_scalar(dl[:, 4:6], dl[:, 2:4], 2.0, None, ALU.add),
        lambda: V.reciprocal(dl[:, 4:6], dl[:, 4:6]),
        lambda: V.tensor_tensor(dl[:, 4:6], dl[:, 4:6], dl[:, 2:4], ALU.mult),
        lambda: V.tensor_tensor(dl[:, 6:8], dl[:, 4:6], dl[:, 4:6], ALU.mult),
        lambda: V.tensor_scalar(dl[:, 8:10], dl[:, 6:8], 1.0 / 11, 1.0 / 9, ALU.mult, ALU.add),
        lambda: V.tensor_tensor(dl[:, 8:10], dl[:, 8:10], dl[:, 6:8], ALU.mult),
        lambda: V.tensor_scalar(dl[:, 8:10], dl[:, 8:10], 1.0 / 7, None, ALU.add),
        lambda: V.tensor_tensor(dl[:, 8:10], dl[:, 8:10], dl[:, 6:8], ALU.mult),
        lambda: V.tensor_scalar(dl[:, 8:10], dl[:, 8:10], 1.0 / 5, None, ALU.add),
        lambda: V.tensor_tensor(dl[:, 8:10], dl[:, 8:10], dl[:, 6:8], ALU.mult),
        lambda: V.tensor_scalar(dl[:, 8:10], dl[:, 8:10], 1.0 / 3, None, ALU.add),
        lambda: V.tensor_tensor(dl[:, 8:10], dl[:, 8:10], dl[:, 6:8], ALU.mult),
        lambda: V.tensor_scalar(dl[:, 8:10], dl[:, 8:10], 1.0, None, ALU.add),
        lambda: V.tensor_tensor(dl[:, 8:10], dl[:, 8:10], dl[:, 4:6], ALU.mult),
        lambda: V.tensor_scalar(dl[:, 10:12], dl[:, 0:2], 0.0, None, ALU.min),
        lambda: V.scalar_tensor_tensor(dl[:, 10:12], dl[:, 8:10], -2.0, dl[:, 10:12], ALU.mult, ALU.add),
    ]
    for f in ops:
        c.op("dve", f, reads=[r_dl], writes=[r_dl])
    lgf = dl[:, 10:11]
    lgb = dl[:, 11:12]
    decT = sb("decT", [128, 2, 128], F32); r_dec = Res("decT")
    xi = sb("xi", [128, 2, 128], F32); r_xi = Res("xi")
    zc = sb("zc", [128, 4], F32); r_zc = Res("zc")
    c.op("act", lambda: A.activation(decT[:, 0, :], cs[:, 0, :], AF.Exp, scale=lgf), reads=[r_cs, r_dl], writes=[r_dec])
    c.op("act", lambda: A.activation(decT[:, 1, :], cs[:, 2, :], AF.Exp, scale=lgb), reads=[r_cs, r_dl], writes=[r_dec])
    c.op("dve", lambda: V.tensor_tensor(decT[:, 0, :], decT[:, 0, :], cs[:, 1, :], ALU.mult), reads=[r_dec, r_cs], writes=[r_dec])
    c.op("dve", lambda: V.tensor_tensor(decT[:, 1, :], decT[:, 1, :], cs[:, 3, :], ALU.mult), reads=[r_dec, r_cs], writes=[r_dec])
    c.op("act", lambda: A.activation(xi[:, 0, :], cs[:, 4, :], AF.Exp, scale=lgf), reads=[r_cs, r_dl], writes=[r_xi])
    c.op("act", lambda: A.activation(xi[:, 1, :], cs[:, 5, :], AF.Exp, scale=lgb), reads=[r_cs, r_dl], writes=[r_xi])
    c.op("act", lambda: A.activation(zc[:, 0:1], cl[:, 0:1], AF.Exp, scale=lgf), reads=[r_cl, r_dl], writes=[r_zc])
    c.op("act", lambda: A.activation(zc[:, 1:2], cl[:, 1:2], AF.Exp, scale=lgb), reads=[r_cl, r_dl], writes=[r_zc])
    c.op("act", lambda: A.activation(zc[:, 2:3], cl[:, 2:3], AF.Exp, scale=lgf), reads=[r_cl, r_dl], writes=[r_zc])
    c.op("act", lambda: A.activation(zc[:, 3:4], cl[:, 2:3], AF.Exp, scale=lgb), reads=[r_cl, r_dl], writes=[r_zc])
    epsT = sb("epsT", [128, 1], F32); r_eps = Res("eps")
    c.op("dve", lambda: V.memset(epsT[:], GN_EPS), writes=[r_eps])

    qkT = sb("qkT", [128, 2, T], BF16)
    r_qk = [Res("qk%d" % n) for n in range(NCH)]
    vall = sb("vall", [128, NCH, 64], BF16); r_v = [Res("v%d" % n) for n in range(NCH)]
    dvall = sb("dvall", [128, NCH, 66], BF16); r_dv = [Res("dv%d" % n) for n in range(NCH)]
    Rf = sb("Rf", [64, NCH + 1, 64], BF16); r_Rf = [Res("Rf%d" % n) for n in range(NCH + 1)]
    Rrun = sb("Rrun", [64, 2, 64], F32); r_Rrun = [Res("Rrun0"), Res("Rrun1")]
    Rbb = [sb("Rbb%d" % i, [64, 64], BF16) for i in range(2)]; r_Rbb = [Res("Rbb0"), Res("Rbb1")]
    ones_dst = Res("dvones")
    c.op("pool", lambda: nc.gpsimd.memset(dvall[:, :, 64:66], 1.0), writes=[ones_dst])
    c.op("dve", lambda: V.memset(Rrun[:], 0.0), writes=r_Rrun)
    c.op("dve", lambda: V.memset(Rf[:, 0, :], 0.0), writes=[r_Rf[0]])
    c.op("dve", lambda: V.memset(Rbb[0][:], 0.0), writes=[r_Rbb[0]])

    NB = 2
    xTt = [sb("xTt%d" % i, [128, 8, 128], BF16) for i in range(NB)]; r_xTt = [Res("xTt%d" % i) for i in range(NB)]
    tbt = [sb("tbt%d" % i, [128, 4, 128], F32) for i in range(NB)]; r_tbt = [Res("tbt%d" % i) for i in range(NB)]
    prod = [sb("prod%d" % i, [128, 4, 128], F32) for i in range(NB)]; r_prod = [Res("prod%d" % i) for i in range(NB)]
    kz = [sb("kz%d" % i, [128, 64], BF16) for i in range(NB)]; r_kz = [Res("kz%d" % i) for i in range(NB)]
    scT = [sb("scT%d" % i, [128, 2, 128], BF16) for i in range(NB)]; r_scT = [Res("scT%d" % i) for i in range(NB)]
    qxi = [sb("qxi%d" % i, [64, 2, 128], BF16) for i in range(NB)]; r_qxi = [Res("qxi%d" % i) for i in range(NB)]
    st = [sb("st%d" % i, [128, 32], F32) for i in range(NB)]; r_st = [Res("st%d" % i) for i in range(NB)]
    xn = [sb("xn%d" % i, [128, 128], F32) for i in range(NB)]; r_xn = [Res("xn%d" % i) for i in range(NB)]
    sg = [sb("sg%d" % i, [128, 128], F32) for i in range(NB)]; r_sg = [Res("sg%d" % i) for i in range(NB)]
    yt = [sb("yt%d" % i, [128, 128], F32) for i in range(NB)]; r_yt = [Res("yt%d" % i) for i in range(NB)]
    pd = [sb("pd%d" % i, [128, 512], BF16) for i in range(3)]; r_pd = [Res("pd%d" % i) for i in range(3)]
    r_y = [Res("y0"), Res("y1")]
    PA = c.ps(pfx + "PA", [128, 512], F32); r_PA = Res("PA")
    PV = c.ps(pfx + "PV", [128, 512], F32); r_PV = Res("PV")
    PT = c.ps(pfx + "PT", [128, 1024], BF16); r_PT = Res("PT")
    PS = c.ps(pfx + "PS", [128, 512], F32); r_PS = Res("PS")
    PO = c.ps(pfx + "PO", [128, 512], F32); r_PO = Res("PO")
    PD = [c.ps(pfx + "PD%d" % i, [128, 512], F32) for i in range(3)]; r_PD = [Res("PD%d" % i) for i in range(3)]

    def load_x(n, want_tab):
        b = n % NB
        c.dma("pool", xTt[b][:], xT[:, n * 128:(n + 1) * 128].rearrange("(c p) t -> p c t", p=128), writes=[r_xTt[b]])
        if want_tab:
            c.dma("sp", tbt[b][:], tab[:, :, n * 128:(n + 1) * 128].rearrange("a p t -> p a t"), writes=[r_tbt[b]])

    def ktrans_state(n, direction, b):
        c.op("pe", lambda: nc.tensor.transpose(PT[:, 0:64], qkT[0:64, 1, n * 128:(n + 1) * 128], identb[0:64, 0:64]),
             reads=[r_qk[n], r_identb], writes=[r_PT])
        c.op("dve", lambda: V.tensor_scalar(kz[b][:], PT[:, 0:64], zc[:, direction:direction + 1], None, ALU.mult),
             reads=[r_PT, r_zc], writes=[r_kz[b]])
        c.op("pe", lambda: nc.tensor.matmul(PS[0:64, 0:64], kz[b][:], vall[:, n, :], start=True, stop=True),
             reads=[r_kz[b], r_v[n]], writes=[r_PS])

    load_x(0, True)
    for n in range(NCH):
        b = n % NB
        if n + 1 < NCH:
            load_x(n + 1, True)
        for g in range(4):
            for k in range(8):
                c.op("pe", lambda: nc.tensor.matmul(PA[:, g * 128:(g + 1) * 128], wb[:, k, g * 128:(g + 1) * 128], xTt[b][:, k, :],
                                                    start=(k == 0), stop=(k == 7)),
                     reads=[r_wb, r_xTt[b]], writes=[r_PA], acc=(g + k > 0))
        for k in range(8):
            c.op("pe", lambda: nc.tensor.matmul(PV[:, 0:128], xTt[b][:, k, :], wb[:, k, 512:640], start=(k == 0), stop=(k == 7)),
                 reads=[r_wb, r_xTt[b]], writes=[r_PV], acc=(k > 0))
        c.op("dve", lambda: V.tensor_tensor(prod[b][:], PA[:].rearrange("p (a t) -> p a t", a=4), tbt[b][:], ALU.mult),
             reads=[r_PA, r_tbt[b]], writes=[r_prod[b]])
        c.op("dve", lambda: V.tensor_tensor(qkT[:, :, n * 128:(n + 1) * 128], prod[b][:, 0:4:2, :], prod[b][:, 1:4:2, :], ALU.add),
             reads=[r_prod[b]], writes=[r_qk[n]])
        c.op("act", lambda: A.copy(vall[:, n, :], PV[:, 0:64]), reads=[r_PV], writes=[r_v[n]])
        c.op("act", lambda: A.copy(dvall[:, n, 0:64], PV[:, 64:128]), reads=[r_PV, ones_dst], writes=[r_dv[n]])
        ktrans_state(n, 0, b)
        c.op("dve", lambda: V.scalar_tensor_tensor(Rrun[:, 0, :], Rrun[:, 0, :], zc[0:64, 2:3], PS[0:64, 0:64], ALU.mult, ALU.add),
             reads=[r_Rrun[0], r_zc, r_PS], writes=[r_Rrun[0]])
        c.op("act", lambda: A.copy(Rf[:, n + 1, :], Rrun[:, 0, :]), reads=[r_Rrun[0]], writes=[r_Rf[n + 1]])

    load_x(NCH - 1, False)

    def ret_steps(n):
        b = n % NB
        rb_cur = (NCH - 1 - n) % 2
        sl = slice(n * 128, (n + 1) * 128)
        s = st[b]; rs = r_st[b]

        def r1():
            for k in range(8):
                c.op("pe", lambda: nc.tensor.matmul(PV[:, 0:128], xTt[b][:, k, :], wb[:, k, 640:768], start=(k == 0), stop=(k == 7)),
                     reads=[r_wb, r_xTt[b]], writes=[r_PV], acc=(k > 0))
            c.op("pe", lambda: nc.tensor.matmul(PS[:, 128:256], qkT[0:64, 1, sl], qkT[0:64, 0, sl], start=True, stop=True),
                 reads=[r_qk[n]], writes=[r_PS])

        def r2():
            c.op("dve", lambda: V.tensor_tensor(scT[b][:], PS[:, 128:256].unsqueeze(1).broadcast_to([128, 2, 128]), decT[:], ALU.mult),
                 reads=[r_PS, r_dec], writes=[r_scT[b]])
            c.op("dve", lambda: V.tensor_tensor(qxi[b][:], qkT[0:64, 0, sl].unsqueeze(1).broadcast_to([64, 2, 128]), xi[0:64], ALU.mult),
                 reads=[r_qk[n], r_xi], writes=[r_qxi[b]])
            c.op("act", lambda: A.activation(sg[b][:], PV[:, 0:128], AF.Exp, scale=-1.0), reads=[r_PV], writes=[r_sg[b]])

        def r3():
            c.op("pe", lambda: nc.tensor.matmul(PO[:, 0:64], scT[b][:, 0, :], vall[:, n, :], start=True, stop=False),
                 reads=[r_scT[b], r_v[n]], writes=[r_PO])
            c.op("pe", lambda: nc.tensor.matmul(PO[:, 0:64], qxi[b][:, 0, :], Rf[:, n, :], start=False, stop=True),
                 reads=[r_qxi[b], r_Rf[n]], writes=[r_PO], acc=True)
            c.op("pe", lambda: nc.tensor.matmul(PO[:, 64:128], scT[b][:, 1, :], vall[:, n, :], start=True, stop=False),
                 reads=[r_scT[b], r_v[n]], writes=[r_PO], acc=True)
            c.op("pe", lambda: nc.tensor.matmul(PO[:, 64:128], qxi[b][:, 1, :], Rbb[rb_cur][:], start=False, stop=True),
                 reads=[r_qxi[b], r_Rbb[rb_cur]], writes=[r_PO], acc=True)

        def r4():
            if n > 0:
                ktrans_state(n, 1, b)
                c.op("dve", lambda: V.scalar_tensor_tensor(Rrun[:, 1, :], Rrun[:, 1, :], zc[0:64, 3:4], PS[0:64, 0:64], ALU.mult, ALU.add),
                     reads=[r_Rrun[1], r_zc, r_PS], writes=[r_Rrun[1]])
                c.op("act", lambda: A.copy(Rbb[1 - rb_cur][:], Rrun[:, 1, :]), reads=[r_Rrun[1]], writes=[r_Rbb[1 - rb_cur]])

        def r5():
            c.op("dve", lambda: V.bn_stats(s[:, 0:6], PO[:, 0:64]), reads=[r_PO], writes=[rs])
            c.op("dve", lambda: V.bn_stats(s[:, 6:12], PO[:, 64:128]), reads=[r_PO], writes=[rs])
            c.op("dve", lambda: V.bn_aggr(s[:, 12:14], s[:, 0:6]), reads=[rs], writes=[rs])
            c.op("dve", lambda: V.bn_aggr(s[:, 14:16], s[:, 6:12]), reads=[rs], writes=[rs])
            c.op("act", lambda: A.activation(s[:, 16:18], s[:, 13:16:2], AF.Ln, bias=epsT[:, 0:1], scale=1.0), reads=[rs, r_eps], writes=[rs])
            c.op("act", lambda: A.activation(s[:, 16:18], s[:, 16:18], AF.Exp, scale=-0.5), reads=[rs], writes=[rs])
            c.op("dve", lambda: V.tensor_scalar(sg[b][:], sg[b][:], 1.0, None, ALU.add), reads=[r_sg[b]], writes=[r_sg[b]])
            c.op("dve", lambda: V.reciprocal(sg[b][:], sg[b][:]), reads=[r_sg[b]], writes=[r_sg[b]])
            c.op("dve", lambda: V.tensor_tensor(sg[b][:], sg[b][:], PV[:, 0:128], ALU.mult), reads=[r_sg[b], r_PV], writes=[r_sg[b]])

        def r6():
            c.op("dve", lambda: V.tensor_scalar(xn[b][:, 0:64], PO[:, 0:64], s[:, 12:13], s[:, 16:17], ALU.subtract, ALU.mult),
                 reads=[r_PO, rs], writes=[r_xn[b]])
            c.op("dve", lambda: V.tensor_scalar(xn[b][:, 64:128], PO[:, 64:128], s[:, 14:15], s[:, 17:18], ALU.subtract, ALU.mult),
                 reads=[r_PO, rs], writes=[r_xn[b]])
            c.op("pool", lambda: nc.gpsimd.tensor_tensor(xn[b][:], xn[b][:], sg[b][:], ALU.mult), reads=[r_xn[b], r_sg[b]], writes=[r_xn[b]])
            c.op("pool", lambda: nc.gpsimd.tensor_tensor(yt[b][:, 0:64], xn[b][:, 0:64], xn[b][:, 64:128], ALU.add),
                 reads=[r_xn[b]], writes=[r_yt[b]])
        return [r1, r2, r3, r4, r5, r6]

    def dil_steps(n):
        b = n % NB
        sl = slice(n * 128, (n + 1) * 128)
        s2 = st[b]; rs = r_st[b]
        kts = [kt for kt in range(n - 8, n + 9) if 0 <= kt < NCH]
        groups = [kts[i:i + 4] for i in range(0, len(kts), 4)]
        G = len(groups)

        def qk(gi):
            grp = groups[gi]; pb = gi % 3
            for j, kt in enumerate(grp):
                c.op("pe", lambda: nc.tensor.matmul(PD[pb][:, j * 128:(j + 1) * 128], qkT[64:128, 1, kt * 128:(kt + 1) * 128],
                                                    qkT[64:128, 0, sl], start=True, stop=True),
                     reads=[r_qk[kt], r_qk[n]], writes=[r_PD[pb]], acc=(j > 0))
            w = len(grp) * 128
            o0 = (grp[0] - n + 8) * 128
            c.op("act", lambda: A.activation(pd[pb][:, 0:w], PD[pb][:, 0:w], AF.Exp), reads=[r_PD[pb]], writes=[r_pd[pb]])
            c.op("pool", lambda: nc.gpsimd.tensor_tensor(pd[pb][:, 0:w], pd[pb][:, 0:w], mmb[:, o0:o0 + w], ALU.mult),
                 reads=[r_pd[pb], r_mm], writes=[r_pd[pb]])

        def pv(gi):
            grp = groups[gi]; pb = gi % 3
            for j, kt in enumerate(grp):
                first = (gi == 0 and j == 0)
                last = (gi == G - 1 and j == len(grp) - 1)
                c.op("pe", lambda: nc.tensor.matmul(PA[:, 0:65], pd[pb][:, j * 128:(j + 1) * 128], dvall[:, kt, 0:65],
                                                    start=first, stop=last),
                     reads=[r_pd[pb], r_dv[kt]], writes=[r_PA], acc=(not first))

        def fin():
            c.op("dve", lambda: V.reciprocal(s2[:, 20:21], PA[:, 64:65]), reads=[r_PA, rs], writes=[rs])
            c.op("dve", lambda: V.tensor_scalar(yt[b][:, 64:128], PA[:, 0:64], s2[:, 20:21], None, ALU.mult),
                 reads=[r_PA, rs], writes=[r_yt[b]])
        steps = []
        from functools import partial
        steps.append(partial(qk, 0))
        if G > 1:
            steps.append(partial(qk, 1))
        for k in range(G):
            if k + 2 < G:
                steps.append(partial(qk, k + 2))
            steps.append(partial(pv, k))
        steps.append(fin)
        return steps

    for n in range(NCH - 1, -1, -1):
        b = n % NB
        if n - 1 >= 0:
            load_x(n - 1, False)
        rs_ = ret_steps(n)
        ds_ = dil_steps(n)
        i = j = 0
        while i < len(ds_) or j < len(rs_):
            if i < len(ds_):
                ds_[i](); i += 1
            if j < len(rs_):
                rs_[j](); j += 1
        c.dma("sp", y[n * 128:(n + 1) * 128, :], yt[b][:], reads=[r_yt[b]], writes=[r_y[b]])
    c.finish("sp", r_y)
    print("ME: ins", c.n_ins, "waits", c.n_wait, "sbuf left", nc.sbuf_bytes_remaining)


NEG = -30000.0


def mo_patterns(NCH):
    pats = []
    for o in range(5):
        pats.append((4, 4 + o - 2))
    for n in (0, 1, NCH - 2, NCH - 1):
        kts = range(0, 4) if n < 2 else range(NCH - 4, NCH)
        for kt in kts:
            pats.append((n, kt))
    return pats


def mo_keys(n, NCH):
    if 2 <= n <= NCH - 3:
        return [(n + o - 2, o) for o in range(5)]
    idx = {0: 0, 1: 1, NCH - 2: 2, NCH - 1: 3}[n]
    kts = range(0, 4) if n < 2 else range(NCH - 4, NCH)
    return [(kt, 5 + idx * 4 + j) for j, kt in enumerate(kts)]


def mo_host_bias(rpb2, T):
    NCH = T // 128
    rows = T // 64
    pats = mo_patterns(NCH)
    m = np.arange(128)[:, None]
    c = np.arange(128)[None, :]
    out = np.full((128, 2, len(pats), 128), NEG, np.float32)
    for pi, (n, kt) in enumerate(pats):
        rm = 2 * kt + m // 64; wm = m % 64
        rc = 2 * n + c // 64; wc = c % 64
        r0 = np.clip(rc - 4, 0, rows - 8)
        c0 = np.clip(wc - 8, 0, 64 - 16)
        valid = (rm >= r0) & (rm < r0 + 8) & (wm >= c0) & (wm < c0 + 16)
        ro = np.clip(rm - rc + 7, 0, 14)
        co = np.clip(wm - wc + 15, 0, 30)
        for h in range(2):
            g = rpb2[h][ro, co]
            out[:, h, pi, :] = np.where(valid, g, np.float32(NEG))
    return out.reshape(128, 2 * len(pats) * 128)


def mo_weight_cols(core):
    h0, h1 = 2 * core, 2 * core + 1
    rng = lambda blk, h: list(range(blk * 1024 + h * 64, blk * 1024 + (h + 1) * 64))
    return np.array(rng(0, h0) + rng(0, h1) + rng(1, h0) + rng(1, h1) + rng(2, h0) + rng(2, h1))


def build_MO(T=16384):
    nc = bass.Bass("TRN2", target_bir_lowering=False)
    c = Ctx(nc)
    xT = nc.dram_tensor("xT", [1024, T], F32, kind="ExternalInput").ap()
    wsel = nc.dram_tensor("wsel", [1024, 384], F32, kind="ExternalInput").ap()
    bias = nc.dram_tensor("bias", [128, 2 * 21 * 128], F32, kind="ExternalInput").ap()
    idn = nc.dram_tensor("idn", [128, 128], F32, kind="ExternalInput").ap()
    y = nc.dram_tensor("y", [T, 128], F32, kind="ExternalOutput").ap()
    emit_MO(nc, c, xT, wsel, bias, idn, y, T)
    return nc


def emit_MO(nc, c, xT, wsel, bias, idn, y, T, pfx="O"):
    NCH = T // 128
    sb = lambda n, s, d: c.sb(pfx + n, s, d)
    V = nc.vector
    A = nc.scalar
    identb = sb("identb", [128, 128], BF16); r_identb = Res("identb")
    c.dma("pool", identb[:], idn, writes=[r_identb])
    wb = sb("wb", [128, 8, 384], BF16); r_wb = Res("wb")
    c.dma("pool", wb[:], wsel.rearrange("(c p) e -> p c e", p=128), writes=[r_wb])
    biasb = sb("biasb", [128, 2, 21, 128], BF16); r_bias = Res("bias")
    c.dma("pool", biasb[:].rearrange("p a b c -> p (a b c)"), bias, writes=[r_bias])

    qkT = sb("qkT", [128, 2, T], BF16); r_qk = [Res("qk%d" % n) for n in range(NCH)]
    vall = sb("vall", [128, NCH, 2, 66], BF16); r_v = [Res("v%d" % n) for n in range(NCH)]
    ones_dst = Res("ones")
    c.op("pool", lambda: nc.gpsimd.memset(vall[:, :, :, 64:66], 1.0), writes=[ones_dst])
    NB = 2
    xTt = [sb("xTt%d" % i, [128, 8, 128], BF16) for i in range(NB)]; r_xTt = [Res("xTt%d" % i) for i in range(NB)]
    pd = [sb("pd%d" % i, [128, 640], BF16) for i in range(3)]; r_pd = [Res("pd%d" % i) for i in range(3)]
    yt = [sb("yt%d" % i, [128, 128], F32) for i in range(NB)]; r_yt = [Res("yt%d" % i) for i in range(NB)]
    st = [sb("st%d" % i, [128, 8], F32) for i in range(NB)]; r_st = [Res("st%d" % i) for i in range(NB)]
    r_y = [Res("y0"), Res("y1")]
    PA = c.ps(pfx + "PA", [128, 512], F32); r_PA = Res("PA")
    PV = c.ps(pfx + "PV", [128, 512], F32); r_PV = Res("PV")
    PO = c.ps(pfx + "PO", [128, 512], F32); r_PO = Res("PO")
    PD = [c.ps(pfx + "PD%d" % i, [128, 1024], F32) for i in range(2)]; r_PD = [Res("PD%d" % i) for i in range(2)]

    def load_x(n):
        b = n % NB
        c.dma("pool", xTt[b][:], xT[:, n * 128:(n + 1) * 128].rearrange("(c p) t -> p c t", p=128), writes=[r_xTt[b]])

    load_x(0)
    for n in range(NCH):
        b = n % NB
        if n + 1 < NCH:
            load_x(n + 1)
        for g in range(2):
            for k in range(8):
                c.op("pe", lambda: nc.tensor.matmul(PA[:, g * 128:(g + 1) * 128], wb[:, k, g * 128:(g + 1) * 128], xTt[b][:, k, :],
                                                    start=(k == 0), stop=(k == 7)),
                     reads=[r_wb, r_xTt[b]], writes=[r_PA], acc=(g + k > 0))
        for k in range(8):
            c.op("pe", lambda: nc.tensor.matmul(PV[:, 0:128], xTt[b][:, k, :], wb[:, k, 256:384], start=(k == 0), stop=(k == 7)),
                 reads=[r_wb, r_xTt[b]], writes=[r_PV], acc=(k > 0))
        c.op("act", lambda: A.activation(qkT[:, 0, n * 128:(n + 1) * 128], PA[:, 0:128], AF.Copy, scale=0.125),
             reads=[r_PA], writes=[r_qk[n]])
        c.op("dve", lambda: V.tensor_copy(qkT[:, 1, n * 128:(n + 1) * 128], PA[:, 128:256]), reads=[r_PA], writes=[r_qk[n]])
        c.op("dve", lambda: V.tensor_copy(vall[:, n, :, 0:64], PV[:, 0:128].rearrange("p (h d) -> p h d", h=2)),
             reads=[r_PV, ones_dst], writes=[r_v[n]])

    POs = [PO, PA]; r_POs = [r_PO, r_PA]
    units = [(n, hh) for n in range(NCH) for hh in range(2)]

    def qk(ui):
        n, hh = units[ui]
        sl = slice(n * 128, (n + 1) * 128)
        keys = mo_keys(n, NCH)
        hp = slice(hh * 64, (hh + 1) * 64)
        pb = ui % 2; sbi = ui % 3
        for j, (kt, pat) in enumerate(keys):
            c.op("pe", lambda: nc.tensor.matmul(PD[pb][:, j * 128:(j + 1) * 128], qkT[hp, 1, kt * 128:(kt + 1) * 128],
                                                qkT[hp, 0, sl], start=True, stop=False),
                 reads=[r_qk[kt], r_qk[n]], writes=[r_PD[pb]], acc=(j > 0))
            c.op("pe", lambda: nc.tensor.matmul(PD[pb][:, j * 128:(j + 1) * 128], identb[:], biasb[:, hh, pat, :],
                                                start=False, stop=True),
                 reads=[r_identb, r_bias], writes=[r_PD[pb]], acc=True)
        nk = len(keys)
        w0 = min(nk, 4) * 128
        c.op("act", lambda: A.activation(pd[sbi][:, 0:w0], PD[pb][:, 0:w0], AF.Exp), reads=[r_PD[pb]], writes=[r_pd[sbi]])
        if nk > 4:
            c.op("act", lambda: A.activation(pd[sbi][:, 512:640], PD[pb][:, 512:640], AF.Exp), reads=[r_PD[pb]], writes=[r_pd[sbi]], acc=True)

    def pv(ui):
        n, hh = units[ui]
        keys = mo_keys(n, NCH)
        nk = len(keys)
        sbi = ui % 3
        po = POs[n % 2]; r_po = r_POs[n % 2]
        for j, (kt, pat) in enumerate(keys):
            c.op("pe", lambda: nc.tensor.matmul(po[:, hh * 128:hh * 128 + 65], pd[sbi][:, j * 128:(j + 1) * 128], vall[:, kt, hh, 0:65],
                                                start=(j == 0), stop=(j == nk - 1)),
                 reads=[r_pd[sbi], r_v[kt]], writes=[r_po], acc=(hh + j > 0))

    def fin(n):
        b = n % NB
        s = st[b]; rs = r_st[b]
        po = POs[n % 2]; r_po = r_POs[n % 2]
        for hh in range(2):
            c.op("dve", lambda: V.reciprocal(s[:, hh:hh + 1], po[:, hh * 128 + 64:hh * 128 + 65]), reads=[r_po, rs], writes=[rs])
            c.op("dve", lambda: V.tensor_scalar(yt[b][:, hh * 64:(hh + 1) * 64], po[:, hh * 128:hh * 128 + 64], s[:, hh:hh + 1], None, ALU.mult),
                 reads=[r_po, rs], writes=[r_yt[b]])
        c.dma("sp", y[n * 128:(n + 1) * 128, :], yt[b][:], reads=[r_yt[b]], writes=[r_y[b]])

    qk(0)
    for ui in range(len(units)):
        if ui + 1 < len(units):
            qk(ui + 1)
        pv(ui)
        if units[ui][1] == 1:
            fin(units[ui][0])
    c.finish("sp", r_y)
    print("MO: ins", c.n_ins, "waits", c.n_wait, "sbuf left", nc.sbuf_bytes_remaining)


N_CORES = 8
T_SEQ = 16384
_PROGS = {}


def _prog(name):
    if name not in _PROGS:
        _PROGS[name] = {"ME": lambda: build_ME(T_SEQ), "MO": lambda: build_MO(T_SEQ), "F": lambda: build_F2(32, 2)}[name]()
    return _PROGS[name]


def _launch(nc, in_maps):
    res = run_bass_kernel_spmd(nc, in_maps, core_ids=list(range(N_CORES)))
    return res.results


def kernel(x, ab_w_in, ab_w_out, ret_decay, c_w_in, c_w_out, c_rpb, ln_g, ln_b,
           router_w, router_b, exp_w_up, exp_b_up, exp_w_down, exp_b_down):
    f32 = np.float32
    xs = np.ascontiguousarray(np.asarray(x, f32)[0])
    T = xs.shape[0]
    idn = np.eye(128, dtype=f32)
    ii = np.arange(128)
    fcst = np.ascontiguousarray(np.concatenate([np.tile(np.arange(256, dtype=f32)[None, :], (128, 1)),
                                                (ii[:, None] < ii[None, :]).astype(f32), np.ones((128, 128), f32)], 1))
    me_c = me_host_consts(T)
    per = T // N_CORES
    for layer in range(4):
        j = layer // 2
        xT = np.ascontiguousarray(xs.T)
        ycat = np.empty((T, 1024), f32)
        if layer % 2 == 0:
            w_in = np.asarray(ab_w_in[j], f32)
            dec = np.asarray(ret_decay[j], f32)
            in_maps = [dict(xT=xT, wsel=np.ascontiguousarray(w_in[:, me_weight_cols(h)]),
                            dec=np.ascontiguousarray(dec[:, h][None, :]), **me_c) for h in range(N_CORES)]
            outs = _launch(_prog("ME"), in_maps)
            for h in range(N_CORES):
                yh = outs[h]["y"]
                ycat[:, h * 64:(h + 1) * 64] = yh[:, 0:64]
                ycat[:, 512 + h * 64:512 + (h + 1) * 64] = yh[:, 64:128]
            w_out = np.asarray(ab_w_out[j], f32)
        else:
            w_in = np.asarray(c_w_in[j], f32)
            rpb = np.asarray(c_rpb[j], f32)
            in_maps = [dict(xT=xT, wsel=np.ascontiguousarray(w_in[:, mo_weight_cols(cc)]),
                            bias=mo_host_bias(rpb[2 * cc:2 * cc + 2], T), idn=idn) for cc in range(N_CORES)]
            outs = _launch(_prog("MO"), in_maps)
            for cc in range(N_CORES):
                ycat[:, cc * 128:(cc + 1) * 128] = outs[cc]["y"]
            w_out = np.asarray(c_w_out[j], f32)
        lnp = np.ascontiguousarray(np.stack([ln_g[layer, 0], ln_b[layer, 0], ln_g[layer, 1], ln_b[layer, 1]], 0).astype(f32))
        common = dict(w_out=np.ascontiguousarray(w_out), lnp=lnp,
                      rw=np.ascontiguousarray(np.asarray(router_w[layer], f32)),
                      rb=np.ascontiguousarray(np.asarray(router_b[layer], f32)[None, :]),
                      wup=np.ascontiguousarray(np.asarray(exp_w_up[layer], f32)),
                      bup=np.ascontiguousarray(np.asarray(exp_b_up[layer], f32).reshape(256, 256)),
                      wdn=np.ascontiguousarray(np.asarray(exp_w_down[layer], f32)),
                      bdn=np.ascontiguousarray(np.asarray(exp_b_down[layer], f32)), idn=idn, fcst=fcst)
        in_maps = []
        for cc in range(N_CORES):
            sl = slice(cc * per, (cc + 1) * per)
            in_maps.append(dict(common, x=np.ascontiguousarray(xs[sl]), yT=np.ascontiguousarray(ycat[sl].T)))
        outs = _launch(_prog("F"), in_maps)
        xs = np.concatenate([outs[cc]["xo"] for cc in range(N_CORES)], 0)
    return xs[None].astype(f32)
```

```python
import numpy as np
import concourse.bass as bass
import concourse.mybir as mybir
from concourse.bass_utils import run_bass_kernel_spmd

F32 = mybir.dt.float32
BF16 = mybir.dt.bfloat16
AF = mybir.ActivationFunctionType
ALU = mybir.AluOpType
AX = mybir.AxisListType


class Res:
    __slots__ = ("name", "w", "r", "dsem", "dcnt")

    def __init__(self, name):
        self.name = name
        self.w = None
        self.r = []
        self.dsem = None
        self.dcnt = 0


class Ctx:
    def __init__(self, nc):
        self.nc = nc
        self.eng = {"pe": nc.tensor, "act": nc.scalar, "dve": nc.vector,
                    "pool": nc.gpsimd, "sp": nc.sync}
        self.sem = {k: nc.alloc_semaphore("c_" + k) for k in ("pe", "act", "dve", "pool")}
        self.cnt = {k: 0 for k in self.sem}
        self.seen = {k: {} for k in self.eng}
        self.semobj = dict(self.sem)
        self.n_dsem = 0
        self.n_wait = 0
        self.n_ins = 0

    def sb(self, name, shape, dt):
        return self.nc.alloc_sbuf_tensor(name, list(shape), dt)

    def ps(self, name, shape, dt=F32):
        return self.nc.alloc_psum_tensor(name, list(shape), dt)

    def _dsem(self, res):
        if res.dsem is None:
            key = "d%d" % self.n_dsem
            self.n_dsem += 1
            res.dsem = key
            self.semobj[key] = self.nc.alloc_semaphore(key)
        return res.dsem

    def _wait(self, e, deps):
        seen = self.seen[e]
        eng = self.eng[e]
        best = {}
        for d in deps:
            if d is None:
                continue
            k, v = d
            if best.get(k, 0) < v:
                best[k] = v
        for k, v in best.items():
            if seen.get(k, 0) < v:
                eng.wait_ge(self.semobj[k], v)
                seen[k] = v
                self.n_wait += 1

    def op(self, e, fn, reads=(), writes=(), acc=False):
        deps = [r.w for r in reads]
        if not acc:
            for w in writes:
                deps.append(w.w)
                deps.extend(w.r)
        self._wait(e, deps)
        ins = fn()
        self.cnt[e] += 1
        ins.then_inc(self.sem[e], 1)
        ev = (e, self.cnt[e])
        self.seen[e][e] = max(self.seen[e].get(e, 0), 0)
        for r in reads:
            r.r.append(ev)
        for w in writes:
            w.w = ev
            if not acc:
                w.r = []
        self.n_ins += 1
        return ins

    def dma(self, q, out, in_, reads=(), writes=(), **kw):
        assert len(writes) == 1
        wres = writes[0]
        deps = [r.w for r in reads]
        deps.append(wres.w)
        deps.extend(wres.r)
        self._wait(q, deps)
        key = self._dsem(wres)
        ins = self.eng[q].dma_start(out=out, in_=in_, **kw)
        wres.dcnt += 16
        ins.then_inc(self.semobj[key], 16)
        ev = (key, wres.dcnt)
        for r in reads:
            r.r.append(ev)
        wres.w = ev
        wres.r = []
        self.n_ins += 1
        return ins

    def dma_fn(self, q, fn, reads=(), writes=()):
        wres = writes[0]
        deps = [r.w for r in reads]
        deps.append(wres.w)
        deps.extend(wres.r)
        self._wait(q, deps)
        key = self._dsem(wres)
        ins = fn()
        wres.dcnt += 16
        ins.then_inc(self.semobj[key], 16)
        ev = (key, wres.dcnt)
        for r in reads:
            r.r.append(ev)
        wres.w = ev
        wres.r = []
        self.n_ins += 1
        return ins

    def dma_more(self, q, out, in_, reads=(), writes=(), **kw):
        wres = writes[0]
        deps = [r.w for r in reads]
        self._wait(q, deps)
        key = self._dsem(wres)
        ins = self.eng[q].dma_start(out=out, in_=in_, **kw)
        wres.dcnt += 16
        ins.then_inc(self.semobj[key], 16)
        ev = (key, wres.dcnt)
        for r in reads:
            r.r.append(ev)
        wres.w = ev
        self.n_ins += 1
        return ins

    def finish(self, q, resources):
        self._wait(q, [r.w for r in resources])


ALPHA = (2.0 * 4) ** 0.25
LN_EPS = 1e-5


def build_F2(n_exp=32, n_blk=2):
    nc = bass.Bass("TRN2", target_bir_lowering=False)
    c = Ctx(nc)
    TB = 1024
    NT = TB * n_blk
    D = 1024
    x = nc.dram_tensor("x", [NT, D], F32, kind="ExternalInput").ap()
    yT = nc.dram_tensor("yT", [D, NT], F32, kind="ExternalInput").ap()
    w_out = nc.dram_tensor("w_out", [D, D], F32, kind="ExternalInput").ap()
    lnp = nc.dram_tensor("lnp", [4, D], F32, kind="ExternalInput").ap()
    rw = nc.dram_tensor("rw", [D, 32], F32, kind="ExternalInput").ap()
    rb = nc.dram_tensor("rb", [1, 32], F32, kind="ExternalInput").ap()
    wup = nc.dram_tensor("wup", [n_exp, D, 2 * D], F32, kind="ExternalInput").ap()
    bup = nc.dram_tensor("bup", [256, 256], F32, kind="ExternalInput").ap()
    wdn = nc.dram_tensor("wdn", [n_exp, D, D], F32, kind="ExternalInput").ap()
    bdn = nc.dram_tensor("bdn", [32, D], F32, kind="ExternalInput").ap()
    idn = nc.dram_tensor("idn", [128, 128], F32, kind="ExternalInput").ap()
    fcst = nc.dram_tensor("fcst", [128, 512], F32, kind="ExternalInput").ap()
    xo = nc.dram_tensor("xo", [NT, D], F32, kind="ExternalOutput").ap()

    emit_F2(nc, c, x, yT, w_out, lnp, rw, rb, wup, bup, wdn, bdn, idn, fcst, xo, n_exp=n_exp, n_blk=n_blk)
    return nc


def emit_F2(nc, c, x, yT, w_out, lnp, rw, rb, wup, bup, wdn, bdn, idn, fcst, xo, n_exp=32, n_blk=2, pfx="F"):
    CAP = 256
    TB = 1024
    D = 1024
    NTL = TB // 128
    sb = lambda n, s, d: c.sb(pfx + n, s, d)
    ident = sb("ident", [128, 128], F32); r_ident = Res("ident")
    c.dma("sp", ident[:], idn, writes=[r_ident])
    rw32 = sb("rw32", [128, 8, 32], F32); r_rw = Res("rw")
    c.dma("sp", rw32[:], rw.rearrange("(c p) e -> p c e", p=128), writes=[r_rw])
    rbB = sb("rbB", [128, 32], F32); r_rb = Res("rb")
    c.dma("sp", rbB[:], rb.partition_broadcast(128), writes=[r_rb])
    bdn32 = sb("bdn32", [32, D], F32); r_bdn = Res("bdn")
    c.dma("sp", bdn32[:], bdn, writes=[r_bdn])
    lnB = [sb("lnB%d" % i, [128, D], F32) for i in range(4)]
    r_ln = [Res("ln%d" % i) for i in range(4)]
    for i in range(4):
        c.dma("sp", lnB[i][:], lnp[i:i + 1, :].partition_broadcast(128), writes=[r_ln[i]])
    iota = sb("iota", [128, 256], F32); r_iota = Res("iota")
    c.dma("sp", iota[:], fcst[:, 0:256], writes=[r_iota])
    UO = sb("UO", [128, 256], BF16); r_UO = Res("UO")
    c.dma("pool", UO[:], fcst[:, 256:512], writes=[r_UO])
    identb = sb("identb", [128, 128], BF16); r_identb = Res("identb")
    c.dma("pool", identb[:], idn, writes=[r_identb])
    B = [c.ps(pfx + "bank%d" % i, [128, 512], F32) for i in range(7)]
    rB = [Res("bank%d" % i) for i in range(7)]
    TP = c.ps(pfx + "TP", [128, 1024], BF16); r_TP = Res("TP")
    bupraw = sb("bupraw", [128, 2, 256], F32); r_bupraw = Res("bupraw")
    c.dma("sp", bupraw[:], bup.rearrange("(g p) k -> p g k", p=128), writes=[r_bupraw])
    bupT = sb("bupT", [128, 2, 256], F32); r_bupT = Res("bupT")
    for two in range(2):
        for g in range(2):
            c.op("pe", lambda: nc.tensor.transpose(B[0][:, (two * 2 + g) * 128:(two * 2 + g + 1) * 128],
                                                   bupraw[:, g, two:256:2], ident[:]),
                 reads=[r_bupraw, r_ident], writes=[rB[0]], acc=(two + g > 0))
    c.op("act", lambda: nc.scalar.copy(bupT[:].rearrange("p a b -> p (a b)"), B[0][:]), reads=[rB[0]], writes=[r_bupT])

    c.op("dve", lambda: nc.vector.tensor_scalar(bupT[:, 1, :], bupT[:, 1, :], 1.0, None, ALU.add), reads=[r_bupT], writes=[r_bupT])

    yacc = sb("yacc", [128, NTL, D], F32); r_yacc = [Res("yacc%d" % i) for i in range(NTL)]
    x1b = sb("x1b", [128, NTL, D], BF16); r_x1b = [Res("x1b%d" % i) for i in range(NTL)]
    maskf = sb("maskf", [128, NTL, 32], F32); r_mask = [Res("mask%d" % i) for i in range(NTL)]
    maskb = sb("maskb", [128, NTL, 32], BF16)
    rank = sb("rank", [128, NTL, 32], F32); r_rank = [Res("rank%d" % i) for i in range(NTL)]
    carry = sb("carry", [128, 32], F32); r_carry = Res("carry")
    gates = sb("gates", [128, NTL, 32], F32); r_gates = [Res("gates%d" % i) for i in range(NTL)]
    gT = sb("gT", [32, TB], F32); r_gT = [Res("gT%d" % i) for i in range(NTL)]
    x1T32 = [sb("x1T32_%d" % i, [128, 8, 128], F32) for i in range(2)]; r_x1T32 = [Res("x1T32_%d" % i) for i in range(2)]
    sm = [sb("sm%d" % i, [128, 128], F32) for i in range(2)]; r_sm = [Res("sm%d" % i) for i in range(2)]
    wuc = [sb("wuc%d" % i, [128, 8, 256], BF16) for i in range(4)]; r_wuc = [Res("wuc%d" % i) for i in range(4)]
    wd = [sb("wd%d" % i, [128, 8, D], BF16) for i in range(2)]; r_wd = [Res("wd%d" % i) for i in range(2)]
    yTb, r_yTb = wd[0], r_wd[0]
    woutb, r_wout = wd[1], r_wd[1]
    Pm = [sb("Pm%d" % i, [128, NTL, CAP], BF16) for i in range(2)]; r_Pm = [Res("Pm%d" % i) for i in range(2)]
    Gm = [sb("Gm%d" % i, [128, NTL, CAP], BF16) for i in range(2)]; r_Gm = [Res("Gm%d" % i) for i in range(2)]
    GT = [sb("GT%d" % i, [128, 2, TB], BF16) for i in range(2)]; r_GT = [Res("GT%d" % i) for i in range(2)]
    XgT = [sb("XgT%d" % i, [128, 8, CAP], BF16) for i in range(2)]; r_XgT = [Res("XgT%d" % i) for i in range(2)]
    act = [sb("act%d" % i, [128, 8, CAP], BF16) for i in range(2)]; r_act = [Res("act%d" % i) for i in range(2)]
    osb = [sb("osb%d" % i, [128, 2, D], BF16) for i in range(2)]; r_osb = [Res("osb%d" % i) for i in range(2)]
    NTMP = 2
    tg = [sb("tg%d" % i, [128, CAP], F32) for i in range(NTMP)]; r_tg = [Res("tg%d" % i) for i in range(NTMP)]
    tsg = [sb("tsg%d" % i, [128, CAP], F32) for i in range(NTMP)]; r_tsg = [Res("tsg%d" % i) for i in range(NTMP)]
    tl = [sb("tl%d" % i, [128, CAP], F32) for i in range(NTMP)]; r_tl = [Res("tl%d" % i) for i in range(NTMP)]
    ot = [sb("ot%d" % i, [128, D], F32) for i in range(2)]; r_ot = [Res("ot%d" % i) for i in range(2)]
    zt, r_zt = ot, r_ot
    xt, r_xt = ot, r_ot
    r_out = [Res("xo0"), Res("xo1")]

    def layer_norm(src, r_src, dst, r_dst, s, r_s, gi, bi):
        for h in range(2):
            c.op("dve", lambda: nc.vector.bn_stats(s[:, h * 6:(h + 1) * 6], src[:, h * 512:(h + 1) * 512]),
                 reads=[r_src], writes=[r_s], acc=(h > 0))
        c.op("dve", lambda: nc.vector.bn_aggr(s[:, 12:14], s[:, 0:12]),
             reads=[r_s], writes=[r_s])
        c.op("act", lambda: nc.scalar.activation(s[:, 14:15], s[:, 13:14], AF.Sqrt, bias=epsT[:, 0:1], scale=1.0),
             reads=[r_s, r_eps], writes=[r_s])
        c.op("dve", lambda: nc.vector.reciprocal(s[:, 15:16], s[:, 14:15]), reads=[r_s], writes=[r_s])
        c.op("dve", lambda: nc.vector.scalar_tensor_tensor(dst, src, s[:, 12:13], lnB[gi][:], ALU.subtract, ALU.mult),
             reads=[r_src, r_s, r_ln[gi]], writes=[r_dst])
        c.op("dve", lambda: nc.vector.scalar_tensor_tensor(dst, dst, s[:, 15:16], lnB[bi][:], ALU.mult, ALU.add),
             reads=[r_dst, r_s, r_ln[bi]], writes=[r_dst])

    epsT = sb("epsT", [128, 1], F32); r_eps = Res("eps")
    c.op("dve", lambda: nc.vector.memset(epsT[:], LN_EPS), writes=[r_eps])

    def issue_wd(q):
        e_ = q % n_exp
        c.dma("pool", wd[q % 2][:], wdn[e_].rearrange("(c p) d -> p c d", p=128), writes=[r_wd[q % 2]])

    def issue_wu(gi):
        e_ = (gi // 8) % n_exp
        fc_ = gi % 8
        c.dma("pool", wuc[gi % 4][:], wup[e_][:, fc_ * 256:(fc_ + 1) * 256].rearrange("(c p) f -> p c f", p=128), writes=[r_wuc[gi % 4]])

    issue_wu(0)
    issue_wu(1)
    tmp_i = 0
    pg_i = 0
    po_i = 0
    sc_i = 0
    GXB = (3, 6)
    for blk in range(n_blk):
        t0 = blk * TB
        c.dma("pool", yTb[:], yT[:, t0:t0 + TB].rearrange("(c p) t -> p c t", p=128), writes=[r_yTb])
        c.dma("pool", woutb[:], w_out.rearrange("(c p) d -> p c d", p=128), writes=[r_wout])
        def stageA(i):
            b2 = i % 2
            c.dma("sp", xt[b2][:], x[t0 + i * 128:t0 + (i + 1) * 128, :], writes=[r_xt[b2]])
            for h in range(2):
                for k in range(8):
                    c.op("pe", lambda: nc.tensor.matmul(B[h][:], yTb[:, k, i * 128:(i + 1) * 128],
                                                        woutb[:, k, h * 512:(h + 1) * 512], start=(k == 0), stop=(k == 7)),
                         reads=[r_yTb, r_wout], writes=[rB[h]], acc=(k > 0))
                c.op("dve", lambda: nc.vector.scalar_tensor_tensor(zt[b2][:, h * 512:(h + 1) * 512], xt[b2][:, h * 512:(h + 1) * 512],
                                                                   ALPHA, B[h][:], ALU.mult, ALU.add),
                     reads=[r_xt[b2], rB[h]], writes=[r_zt[b2]], acc=(h > 0))
            layer_norm(zt[b2][:], r_zt[b2], yacc[:, i, :], r_yacc[i], sm[b2], r_sm[b2], 0, 1)
            for k in range(8):
                bk = 2 + k // 4
                c.op("pe", lambda: nc.tensor.transpose(B[bk][:, (k % 4) * 128:(k % 4 + 1) * 128],
                                                       yacc[:, i, k * 128:(k + 1) * 128], ident[:]),
                     reads=[r_yacc[i], r_ident], writes=[rB[bk]], acc=(k % 4 > 0))
            for hh in range(2):
                c.op("act", lambda: nc.scalar.copy(x1T32[b2][:, hh * 4:(hh + 1) * 4, :].rearrange("p a b -> p (a b)"), B[2 + hh][:]),
                     reads=[rB[2 + hh]], writes=[r_x1T32[b2]], acc=(hh > 0))
            c.op("act", lambda: nc.scalar.copy(x1b[:, i, :], yacc[:, i, :]), reads=[r_yacc[i]], writes=[r_x1b[i]])
        def stageB(i):
            b2 = i % 2
            for k in range(8):
                c.op("pe", lambda: nc.tensor.matmul(B[4][:, 0:32], x1T32[b2][:, k, :], rw32[:, k, :], start=(k == 0), stop=(k == 7)),
                     reads=[r_x1T32[b2], r_rw], writes=[rB[4]], acc=(k > 0))
            s = sm[b2]; r_s = r_sm[b2]
            c.op("dve", lambda: nc.vector.tensor_tensor(s[:, 32:64], B[4][:, 0:32], rbB[:], ALU.add), reads=[rB[4], r_rb], writes=[r_s])
            c.op("dve", lambda: nc.vector.max(s[:, 16:24], s[:, 32:64]), reads=[r_s], writes=[r_s])
            c.op("dve", lambda: nc.vector.tensor_scalar(s[:, 64:96], s[:, 32:64], s[:, 19:20], None, ALU.is_ge), reads=[r_s], writes=[r_s])
            c.op("dve", lambda: nc.vector.tensor_copy(maskf[:, i, :], s[:, 64:96]), reads=[r_s], writes=[r_mask[i]])
            c.op("dve", lambda: nc.vector.tensor_copy(maskb[:, i, :], s[:, 64:96]), reads=[r_s], writes=[r_mask[i]])
            c.op("pe", lambda: nc.tensor.matmul(B[4][:, 256:288], UO[:, 0:128], maskb[:, i, :], start=True, stop=True),
                 reads=[r_UO, r_mask[i]], writes=[rB[4]])
            c.op("pe", lambda: nc.tensor.matmul(B[4][:, 288:320], UO[:, 128:256], maskb[:, i, :], start=True, stop=True),
                 reads=[r_UO, r_mask[i]], writes=[rB[4]], acc=True)
            if i == 0:
                c.op("dve", lambda: nc.vector.tensor_copy(rank[:, i, :], B[4][:, 256:288]), reads=[rB[4]], writes=[r_rank[i]])
                c.op("dve", lambda: nc.vector.tensor_copy(carry[:], B[4][:, 288:320]), reads=[rB[4]], writes=[r_carry])
            else:
                c.op("dve", lambda: nc.vector.tensor_tensor(rank[:, i, :], B[4][:, 256:288], carry[:], ALU.add),
                     reads=[rB[4], r_carry], writes=[r_rank[i]])
                c.op("dve", lambda: nc.vector.tensor_tensor(carry[:], carry[:], B[4][:, 288:320], ALU.add),
                     reads=[rB[4], r_carry], writes=[r_carry])
            c.op("dve", lambda: nc.vector.tensor_scalar(s[:, 24:25], s[:, 16:17], -1.0, None, ALU.mult), reads=[r_s], writes=[r_s])
            c.op("act", lambda: nc.scalar.activation(s[:, 96:128], s[:, 32:64], AF.Exp, bias=s[:, 24:25], scale=1.0), reads=[r_s], writes=[r_s])
            c.op("dve", lambda: nc.vector.tensor_tensor(s[:, 96:128], s[:, 96:128], s[:, 64:96], ALU.mult), reads=[r_s], writes=[r_s])
            c.op("dve", lambda: nc.vector.reduce_sum(s[:, 25:26], s[:, 96:128], AX.X), reads=[r_s], writes=[r_s])
            c.op("dve", lambda: nc.vector.reciprocal(s[:, 26:27], s[:, 25:26]), reads=[r_s], writes=[r_s])
            c.op("dve", lambda: nc.vector.tensor_scalar(gates[:, i, :], s[:, 96:128], s[:, 26:27], None, ALU.mult), reads=[r_s], writes=[r_gates[i]])
            c.op("pe", lambda: nc.tensor.transpose(B[4][0:32, 128:256], gates[:, i, :], ident[:]),
                 reads=[r_gates[i], r_ident], writes=[rB[4]])
            c.op("act", lambda: nc.scalar.copy(gT[:, i * 128:(i + 1) * 128], B[4][0:32, 128:256]), reads=[rB[4]], writes=[r_gT[i]])
            for h in range(2):
                c.op("pe", lambda: nc.tensor.matmul(B[5 + h][:], gT[:, i * 128:(i + 1) * 128], bdn32[:, h * 512:(h + 1) * 512],
                                                    start=True, stop=True), reads=[r_gT[i], r_bdn], writes=[rB[5 + h]])
                c.op("dve", lambda: nc.vector.scalar_tensor_tensor(yacc[:, i, h * 512:(h + 1) * 512], yacc[:, i, h * 512:(h + 1) * 512],
                                                                   ALPHA, B[5 + h][:], ALU.mult, ALU.add),
                     reads=[r_yacc[i], rB[5 + h]], writes=[r_yacc[i]])

        stageA(0)
        for i in range(NTL):
            if i + 1 < NTL:
                stageA(i + 1)
            stageB(i)
        issue_wd(blk * n_exp)
        def build(e):
            ab = e % 2
            for i in range(NTL):
                c.op("dve", lambda: nc.vector.tensor_scalar(Pm[ab][:, i, :], iota[:], rank[:, i, e:e + 1], maskf[:, i, e:e + 1],
                                                            ALU.is_equal, ALU.mult),
                     reads=[r_iota, r_rank[i], r_mask[i]], writes=[r_Pm[ab]], acc=(i > 0))
                c.op("dve", lambda: nc.vector.tensor_scalar(Gm[ab][:, i, :], iota[:], rank[:, i, e:e + 1], gates[:, i, e:e + 1],
                                                            ALU.is_equal, ALU.mult),
                     reads=[r_iota, r_rank[i], r_gates[i]], writes=[r_Gm[ab]], acc=(i > 0))

        def transposes(e):
            ab = e % 2
            for st_ in range(2):
                for i in range(NTL):
                    c.op("pe", lambda: nc.tensor.transpose(TP[:, i * 128:(i + 1) * 128], Gm[ab][:, i, st_ * 128:(st_ + 1) * 128], identb[:]),
                         reads=[r_Gm[ab], r_identb], writes=[r_TP], acc=(i > 0))
                c.op("act", lambda: nc.scalar.copy(GT[ab][:, st_, :], TP[:]), reads=[r_TP], writes=[r_GT[ab]], acc=(st_ > 0))

        def gather(e):
            ab = e % 2
            for k in range(8):
                gh = k % 2
                for i in range(NTL):
                    c.op("pe", lambda: nc.tensor.matmul(B[GXB[gh]][:, 0:256], x1b[:, i, k * 128:(k + 1) * 128], Pm[ab][:, i, :],
                                                        start=(i == 0), stop=(i == NTL - 1)),
                         reads=[r_x1b[i], r_Pm[ab]], writes=[rB[GXB[gh]]], acc=(i > 0))
                c.op("act", lambda: nc.scalar.copy(XgT[ab][:, k, :], B[GXB[gh]][:, 0:256]), reads=[rB[GXB[gh]]], writes=[r_XgT[ab]], acc=(k > 0))

        def up(e):
            nonlocal pg_i, tmp_i
            q = blk * n_exp + e
            ab = e % 2
            for fc in range(8):
                gi = q * 8 + fc
                if gi + 2 < n_blk * n_exp * 8:
                    issue_wu(gi + 2)
                ws = gi % 4
                row = e * 8 + fc
                ub = pg_i % 2; pg_i += 1
                for two in range(2):
                    for k in range(8):
                        c.op("pe", lambda: nc.tensor.matmul(B[ub][:, two * 256:(two + 1) * 256], wuc[ws][:, k, two:256:2], XgT[ab][:, k, :],
                                                            start=(k == 0), stop=(k == 7)),
                             reads=[r_wuc[ws], r_XgT[ab]], writes=[rB[ub]], acc=(two + k > 0))
                ti = tmp_i % NTMP; tmp_i += 1
                c.op("dve", lambda: nc.vector.tensor_scalar(tg[ti][:], B[ub][:, 0:256], bupT[:, 0, row:row + 1], 7.0, ALU.add, ALU.min),
                     reads=[rB[ub], r_bupT], writes=[r_tg[ti]])
                c.op("act", lambda: nc.scalar.activation(tsg[ti][:], tg[ti][:], AF.Sigmoid, scale=1.702),
                     reads=[r_tg[ti]], writes=[r_tsg[ti]])
                c.op("dve", lambda: nc.vector.tensor_scalar(tl[ti][:], B[ub][:, 256:512], bupT[:, 1, row:row + 1], -6.0, ALU.add, ALU.max),
                     reads=[rB[ub], r_bupT], writes=[r_tl[ti]])
                c.op("dve", lambda: nc.vector.scalar_tensor_tensor(tl[ti][:], tl[ti][:], 8.0, tg[ti][:], ALU.min, ALU.mult),
                     reads=[r_tg[ti], r_tl[ti]], writes=[r_tl[ti]])
                c.op("dve", lambda: nc.vector.tensor_tensor(act[ab][:, fc, :], tl[ti][:], tsg[ti][:], ALU.mult),
                     reads=[r_tl[ti], r_tsg[ti]], writes=[r_act[ab]], acc=(fc > 0))

        def down(e):
            ab = e % 2
            for st_ in range(2):
                for h in range(2):
                    po = 2
                    for fc in range(8):
                        c.op("pe", lambda: nc.tensor.matmul(B[po][:], act[ab][:, fc, st_ * 128:(st_ + 1) * 128],
                                                            wd[ab][:, fc, h * 512:(h + 1) * 512], start=(fc == 0), stop=(fc == 7)),
                             reads=[r_act[ab], r_wd[ab]], writes=[rB[po]], acc=(fc > 0))
                    c.op("act", lambda: nc.scalar.copy(osb[ab][:, st_, h * 512:(h + 1) * 512], B[po][:]), reads=[rB[po]], writes=[r_osb[ab]],
                         acc=(st_ + h > 0))

        def scatter(e):
            nonlocal sc_i
            ab = e % 2
            for i in range(NTL):
                for h in range(2):
                    sc = 4 + (sc_i % 2); sc_i += 1
                    for st_ in range(2):
                        c.op("pe", lambda: nc.tensor.matmul(B[sc][:], GT[ab][:, st_, i * 128:(i + 1) * 128], osb[ab][:, st_, h * 512:(h + 1) * 512],
                                                            start=(st_ == 0), stop=(st_ == 1)),
                             reads=[r_GT[ab], r_osb[ab]], writes=[rB[sc]], acc=(st_ > 0))
                    c.op("dve", lambda: nc.vector.tensor_tensor(yacc[:, i, h * 512:(h + 1) * 512], yacc[:, i, h * 512:(h + 1) * 512], B[sc][:], ALU.add),
                         reads=[rB[sc], r_yacc[i]], writes=[r_yacc[i]])

        build(0)
        transposes(0)
        gather(0)
        for e in range(n_exp):
            q = blk * n_exp + e
            if e + 1 < n_exp:
                issue_wd(q + 1)
                build(e + 1)
            up(e)
            if e + 1 < n_exp:
                gather(e + 1)
            down(e)
            if e + 1 < n_exp:
                transposes(e + 1)
            scatter(e)
        for i in range(NTL):
            b2 = i % 2
            layer_norm(yacc[:, i, :], r_yacc[i], ot[b2][:], r_ot[b2], sm[b2], r_sm[b2], 2, 3)
            c.dma("sp", xo[t0 + i * 128:t0 + (i + 1) * 128, :], ot[b2][:], reads=[r_ot[b2]], writes=[r_out[b2]])
    c.finish("sp", r_out)
    print("F: ins", c.n_ins, "waits", c.n_wait, "sbuf left", nc.sbuf_bytes_remaining)


GN_EPS = 1e-6


def me_host_consts(T):
    pos = np.arange(T, dtype=np.float32)
    inv = (10000.0 ** (-np.arange(0, 64, 2, dtype=np.float32) / 64)).astype(np.float32)
    ang = pos[:, None] * inv[None, :]
    cos = np.cos(ang).astype(np.float32).T
    sin = np.sin(ang).astype(np.float32).T
    cosF = np.concatenate([cos, cos], 0)
    sinS = np.concatenate([-sin, sin], 0)
    s8 = np.float32(0.125)
    cq = np.concatenate([cosF, cosF * s8], 0)
    sq = np.concatenate([sinS, sinS * s8], 0)
    ck = np.concatenate([cosF * s8, cosF], 0)
    sk = np.concatenate([sinS * s8, sinS], 0)
    tab = np.ascontiguousarray(np.stack([cq, sq, ck, sk], 0)).astype(np.float32)
    i = np.arange(128, dtype=np.float32)
    m = i[:, None]; c = i[None, :]
    cst = np.zeros((128, 6, 128), np.float32)
    cst[:, 0, :] = np.maximum(c - m, 0)
    cst[:, 1, :] = (c >= m)
    cst[:, 2, :] = np.maximum(m - c, 0)
    cst[:, 3, :] = (m > c)
    cst[:, 4, :] = c + 1.0
    cst[:, 5, :] = 128.0 - c
    col = np.zeros((128, 4), np.float32)
    col[:, 0] = 127.0 - i
    col[:, 1] = i
    col[:, 2] = 128.0
    o = np.arange(17)[None, :, None]
    delta = (o - 8) * 128 + m[:, None, :].astype(np.int64) - c[None, :, :].astype(np.int64)
    delta = delta.astype(np.int64)
    ad = np.abs(delta)
    mm = (ad <= 64).astype(np.float32) + ((delta % 4 == 0) & (ad <= 256)) + ((delta % 16 == 0) & (ad <= 1024))
    mm = mm.astype(np.float32).reshape(128, 17 * 128)
    return dict(tab=tab, cst=cst.reshape(128, 768), col=col, mm=mm, idn=np.eye(128, dtype=np.float32))


def me_weight_cols(head):
    H = 64
    base = lambda blk: blk * 512 + head * H
    rng = lambda b: list(range(base(b), base(b) + H))
    sw = lambda b: list(range(base(b) + 32, base(b) + 64)) + list(range(base(b), base(b) + 32))
    cols = []
    cols += rng(0) + rng(5)
    cols += sw(0) + sw(5)
    cols += rng(1) + rng(6)
    cols += sw(1) + sw(6)
    cols += rng(2) + rng(7)
    cols += rng(3) + rng(4)
    return np.array(cols)


def build_ME(T=16384):
    nc = bass.Bass("TRN2", target_bir_lowering=False)
    c = Ctx(nc)
    xT = nc.dram_tensor("xT", [1024, T], F32, kind="ExternalInput").ap()
    wsel = nc.dram_tensor("wsel", [1024, 768], F32, kind="ExternalInput").ap()
    dec = nc.dram_tensor("dec", [1, 2], F32, kind="ExternalInput").ap()
    tab = nc.dram_tensor("tab", [4, 128, T], F32, kind="ExternalInput").ap()
    cst = nc.dram_tensor("cst", [128, 768], F32, kind="ExternalInput").ap()
    col = nc.dram_tensor("col", [128, 4], F32, kind="ExternalInput").ap()
    mm = nc.dram_tensor("mm", [128, 17 * 128], F32, kind="ExternalInput").ap()
    idn = nc.dram_tensor("idn", [128, 128], F32, kind="ExternalInput").ap()
    y = nc.dram_tensor("y", [T, 128], F32, kind="ExternalOutput").ap()
    emit_ME(nc, c, xT, wsel, dec, tab, cst, col, mm, idn, y, T)
    return nc


def emit_ME(nc, c, xT, wsel, dec, tab, cst, col, mm, idn, y, T, pfx="E"):
    NCH = T // 128
    sb = lambda n, s, d: c.sb(pfx + n, s, d)
    V = nc.vector
    A = nc.scalar
    identb = sb("identb", [128, 128], BF16); r_identb = Res("identb")
    c.dma("pool", identb[:], idn, writes=[r_identb])
    wb = sb("wb", [128, 8, 768], BF16); r_wb = Res("wb")
    c.dma("pool", wb[:], wsel.rearrange("(c p) e -> p c e", p=128), writes=[r_wb])
    mmb = sb("mmb", [128, 17 * 128], BF16); r_mm = Res("mm")
    c.dma("pool", mmb[:], mm, writes=[r_mm])
    cs = sb("cs", [128, 6, 128], F32); r_cs = Res("cs")
    c.dma("sp", cs[:].rearrange("p a b -> p (a b)"), cst, writes=[r_cs])
    cl = sb("cl", [128, 4], F32); r_cl = Res("cl")
    c.dma("sp", cl[:], col, writes=[r_cl])
    dl = sb("dl", [128, 16], F32); r_dl = Res("dl")
    c.dma("sp", dl[:, 0:2], dec.partition_broadcast(128), writes=[r_dl])
    ops = [
        lambda: V.tensor_scalar(dl[:, 2:4], dl[:, 0:2], -1.0, None, ALU.mult),
        lambda: V.tensor_tensor(dl[:, 2:4], dl[:, 2:4], dl[:, 0:2], ALU.max),
    ]
    for f in ops:
        c.op("dve", f, reads=[r_dl], writes=[r_dl])
    c.op("act", lambda: A.activation(dl[:, 2:4], dl[:, 2:4], AF.Exp, scale=-1.0), reads=[r_dl], writes=[r_dl])
    ops = [
        lambda: V.tensor_scalar(dl[:, 4:6], dl[:, 2:4], 2.0, None, ALU.add),
        lambda: V.reciprocal(dl[:, 4:6], dl[:, 4:6]),
        lambda: V.tensor_tensor(dl[:, 4:6], dl[:, 4:6], dl[:, 2:4], ALU.mult),
        lambda: V.tensor_tensor(dl[:, 6:8], dl[:, 4:6], dl[:, 4:6], ALU.mult),
        lambda: V.tensor_scalar(dl[:, 8:10], dl[:, 6:8], 1.0 / 11, 1.0 / 9, ALU.mult, ALU.add),
        lambda: V.tensor_tensor(dl[:, 8:10], dl[:, 8:10], dl[:, 6:8], ALU.mult),
        lambda: V.tensor_scalar(dl[:, 8:10], dl[:, 8:10], 1.0 / 7, None, ALU.add),
        lambda: V.tensor_tensor(dl[:, 8:10], dl[:, 8:10], dl[:, 6:8], ALU.mult),
        lambda: V.tensor_scalar(dl[:, 8:10], dl[:, 8:10], 1.0 / 5, None, ALU.add),
        lambda: V.tensor_tensor(dl[:, 8:10], dl[:, 8:10], dl[:, 6:8], ALU.mult),
        lambda: V.tensor_scalar(dl[:, 8:10], dl[:, 8:10], 1.0 / 3, None, ALU.add),
        lambda: V.tensor_tensor(dl[:, 8:10], dl[:, 8:10], dl[:, 6:8], ALU.mult),
        lambda: V.tensor_scalar(dl[:, 8:10], dl[:, 8:10], 1.0, None, ALU.add),
        lambda: V.tensor_tensor(dl[:, 8:10], dl[:, 8:10], dl[:, 4:6], ALU.mult),
        lambda: V.tensor_scalar(dl[:, 10:12], dl[:, 0:2], 0.0, None, ALU.min),
        lambda: V.scalar_tensor_tensor(dl[:, 10:12], dl[:, 8:10], -2.0, dl[:, 10:12], ALU.mult, ALU.add),
    ]
    for f in ops:
        c.op("dve", f, reads=[r_dl], writes=[r_dl])
    lgf = dl[:, 10:11]
    lgb = dl[:, 11:12]
    decT = sb("decT", [128, 2, 128], F32); r_dec = Res("decT")
    xi = sb("xi", [128, 2, 128], F32); r_xi = Res("xi")
    zc = sb("zc", [128, 4], F32); r_zc = Res("zc")
    c.op("act", lambda: A.activation(decT[:, 0, :], cs[:, 0, :], AF.Exp, scale=lgf), reads=[r_cs, r_dl], writes=[r_dec])
    c.op("act", lambda: A.activation(decT[:, 1, :], cs[:, 2, :], AF.Exp, scale=lgb), reads=[r_cs, r_dl], writes=[r_dec])
    c.op("dve", lambda: V.tensor_tensor(decT[:, 0, :], decT[:, 0, :], cs[:, 1, :], ALU.mult), reads=[r_dec, r_cs], writes=[r_dec])
    c.op("dve", lambda: V.tensor_tensor(decT[:, 1, :], decT[:, 1, :], cs[:, 3, :], ALU.mult), reads=[r_dec, r_cs], writes=[r_dec])
    c.op("act", lambda: A.activation(xi[:, 0, :], cs[:, 4, :], AF.Exp, scale=lgf), reads=[r_cs, r_dl], writes=[r_xi])
    c.op("act", lambda: A.activation(xi[:, 1, :], cs[:, 5, :], AF.Exp, scale=lgb), reads=[r_cs, r_dl], writes=[r_xi])
    c.op("act", lambda: A.activation(zc[:, 0:1], cl[:, 0:1], AF.Exp, scale=lgf), reads=[r_cl, r_dl], writes=[r_zc])
    c.op("act", lambda: A.activation(zc[:, 1:2], cl[:, 1:2], AF.Exp, scale=lgb), reads=[r_cl, r_dl], writes=[r_zc])
    c.op("act", lambda: A.activation(zc[:, 2:3], cl[:, 2:3], AF.Exp, scale=lgf), reads=[r_cl, r_dl], writes=[r_zc])
    c.op("act", lambda: A.activation(zc[:, 3:4], cl[:, 2:3], AF.Exp, scale=lgb), reads=[r_cl, r_dl], writes=[r_zc])
    epsT = sb("epsT", [128, 1], F32); r_eps = Res("eps")
    c.op("dve", lambda: V.memset(epsT[:], GN_EPS), writes=[r_eps])

    qkT = sb("qkT", [128, 2, T], BF16)
    r_qk = [Res("qk%d" % n) for n in range(NCH)]
    vall = sb("vall", [128, NCH, 64], BF16); r_v = [Res("v%d" % n) for n in range(NCH)]
    dvall = sb("dvall", [128, NCH, 66], BF16); r_dv = [Res("dv%d" % n) for n in range(NCH)]
    Rf = sb("Rf", [64, NCH + 1, 64], BF16); r_Rf = [Res("Rf%d" % n) for n in range(NCH + 1)]
    Rrun = sb("Rrun", [64, 2, 64], F32); r_Rrun = [Res("Rrun0"), Res("Rrun1")]
    Rbb = [sb("Rbb%d" % i, [64, 64], BF16) for i in range(2)]; r_Rbb = [Res("Rbb0"), Res("Rbb1")]
    ones_dst = Res("dvones")
    c.op("pool", lambda: nc.gpsimd.memset(dvall[:, :, 64:66], 1.0), writes=[ones_dst])
    c.op("dve", lambda: V.memset(Rrun[:], 0.0), writes=r_Rrun)
    c.op("dve", lambda: V.memset(Rf[:, 0, :], 0.0), writes=[r_Rf[0]])
    c.op("dve", lambda: V.memset(Rbb[0][:], 0.0), writes=[r_Rbb[0]])

    NB = 2
    xTt = [sb("xTt%d" % i, [128, 8, 128], BF16) for i in range(NB)]; r_xTt = [Res("xTt%d" % i) for i in range(NB)]
    tbt = [sb("tbt%d" % i, [128, 4, 128], F32) for i in range(NB)]; r_tbt = [Res("tbt%d" % i) for i in range(NB)]
    prod = [sb("prod%d" % i, [128, 4, 128], F32) for i in range(NB)]; r_prod = [Res("prod%d" % i) for i in range(NB)]
    kz = [sb("kz%d" % i, [128, 64], BF16) for i in range(NB)]; r_kz = [Res("kz%d" % i) for i in range(NB)]
    scT = [sb("scT%d" % i, [128, 2, 128], BF16) for i in range(NB)]; r_scT = [Res("scT%d" % i) for i in range(NB)]
    qxi = [sb("qxi%d" % i, [64, 2, 128], BF16) for i in range(NB)]; r_qxi = [Res("qxi%d" % i) for i in range(NB)]
    st = [sb("st%d" % i, [128, 32], F32) for i in range(NB)]; r_st = [Res("st%d" % i) for i in range(NB)]
    xn = [sb("xn%d" % i, [128, 128], F32) for i in range(NB)]; r_xn = [Res("xn%d" % i) for i in range(NB)]
    sg = [sb("sg%d" % i, [128, 128], F32) for i in range(NB)]; r_sg = [Res("sg%d" % i) for i in range(NB)]
    yt = [sb("yt%d" % i, [128, 128], F32) for i in range(NB)]; r_yt = [Res("yt%d" % i) for i in range(NB)]
    pd = [sb("pd%d" % i, [128, 512], BF16) for i in range(3)]; r_pd = [Res("pd%d" % i) for i in range(3)]
    r_y = [Res("y0"), Res("y1")]
    PA = c.ps(pfx + "PA", [128, 512], F32); r_PA = Res("PA")
    PV = c.ps(pfx + "PV", [128, 512], F32); r_PV = Res("PV")
    PT = c.ps(pfx + "PT", [128, 1024], BF16); r_PT = Res("PT")
    PS = c.ps(pfx + "PS", [128, 512], F32); r_PS = Res("PS")
    PO = c.ps(pfx + "PO", [128, 512], F32); r_PO = Res("PO")
    PD = [c.ps(pfx + "PD%d" % i, [128, 512], F32) for i in range(3)]; r_PD = [Res("PD%d" % i) for i in range(3)]

    def load_x(n, want_tab):
        b = n % NB
        c.dma("pool", xTt[b][:], xT[:, n * 128:(n + 1) * 128].rearrange("(c p) t -> p c t", p=128), writes=[r_xTt[b]])
        if want_tab:
            c.dma("sp", tbt[b][:], tab[:, :, n * 128:(n + 1) * 128].rearrange("a p t -> p a t"), writes=[r_tbt[b]])

    def ktrans_state(n, direction, b):
        c.op("pe", lambda: nc.tensor.transpose(PT[:, 0:64], qkT[0:64, 1, n * 128:(n + 1) * 128], identb[0:64, 0:64]),
             reads=[r_qk[n], r_identb], writes=[r_PT])
        c.op("dve", lambda: V.tensor_scalar(kz[b][:], PT[:, 0:64], zc[:, direction:direction + 1], None, ALU.mult),
             reads=[r_PT, r_zc], writes=[r_kz[b]])
        c.op("pe", lambda: nc.tensor.matmul(PS[0:64, 0:64], kz[b][:], vall[:, n, :], start=True, stop=True),
             reads=[r_kz[b], r_v[n]], writes=[r_PS])

    load_x(0, True)
    for n in range(NCH):
        b = n % NB
        if n + 1 < NCH:
            load_x(n + 1, True)
        for g in range(4):
            for k in range(8):
                c.op("pe", lambda: nc.tensor.matmul(PA[:, g * 128:(g + 1) * 128], wb[:, k, g * 128:(g + 1) * 128], xTt[b][:, k, :],
                                                    start=(k == 0), stop=(k == 7)),
                     reads=[r_wb, r_xTt[b]], writes=[r_PA], acc=(g + k > 0))
        for k in range(8):
            c.op("pe", lambda: nc.tensor.matmul(PV[:, 0:128], xTt[b][:, k, :], wb[:, k, 512:640], start=(k == 0), stop=(k == 7)),
                 reads=[r_wb, r_xTt[b]], writes=[r_PV], acc=(k > 0))
        c.op("dve", lambda: V.tensor_tensor(prod[b][:], PA[:].rearrange("p (a t) -> p a t", a=4), tbt[b][:], ALU.mult),
             reads=[r_PA, r_tbt[b]], writes=[r_prod[b]])
        c.op("dve", lambda: V.tensor_tensor(qkT[:, :, n * 128:(n + 1) * 128], prod[b][:, 0:4:2, :], prod[b][:, 1:4:2, :], ALU.add),
             reads=[r_prod[b]], writes=[r_qk[n]])
        c.op("act", lambda: A.copy(vall[:, n, :], PV[:, 0:64]), reads=[r_PV], writes=[r_v[n]])
        c.op("act", lambda: A.copy(dvall[:, n, 0:64], PV[:, 64:128]), reads=[r_PV, ones_dst], writes=[r_dv[n]])
        ktrans_state(n, 0, b)
        c.op("dve", lambda: V.scalar_tensor_tensor(Rrun[:, 0, :], Rrun[:, 0, :], zc[0:64, 2:3], PS[0:64, 0:64], ALU.mult, ALU.add),
             reads=[r_Rrun[0], r_zc, r_PS], writes=[r_Rrun[0]])
        c.op("act", lambda: A.copy(Rf[:, n + 1, :], Rrun[:, 0, :]), reads=[r_Rrun[0]], writes=[r_Rf[n + 1]])

    load_x(NCH - 1, False)

    def ret_steps(n):
        b = n % NB
        rb_cur = (NCH - 1 - n) % 2
        sl = slice(n * 128, (n + 1) * 128)
        s = st[b]; rs = r_st[b]

        def r1():
            for k in range(8):
                c.op("pe", lambda: nc.tensor.matmul(PV[:, 0:128], xTt[b][:, k, :], wb[:, k, 640:768], start=(k == 0), stop=(k == 7)),
                     reads=[r_wb, r_xTt[b]], writes=[r_PV], acc=(k > 0))
            c.op("pe", lambda: nc.tensor.matmul(PS[:, 128:256], qkT[0:64, 1, sl], qkT[0:64, 0, sl], start=True, stop=True),
                 reads=[r_qk[n]], writes=[r_PS])

        def r2():
            c.op("dve", lambda: V.tensor_tensor(scT[b][:], PS[:, 128:256].unsqueeze(1).broadcast_to([128, 2, 128]), decT[:], ALU.mult),
                 reads=[r_PS, r_dec], writes=[r_scT[b]])
            c.op("dve", lambda: V.tensor_tensor(qxi[b][:], qkT[0:64, 0, sl].unsqueeze(1).broadcast_to([64, 2, 128]), xi[0:64], ALU.mult),
                 reads=[r_qk[n], r_xi], writes=[r_qxi[b]])
            c.op("act", lambda: A.activation(sg[b][:], PV[:, 0:128], AF.Exp, scale=-1.0), reads=[r_PV], writes=[r_sg[b]])

        def r3():
            c.op("pe", lambda: nc.tensor.matmul(PO[:, 0:64], scT[b][:, 0, :], vall[:, n, :], start=True, stop=False),
                 reads=[r_scT[b], r_v[n]], writes=[r_PO])
            c.op("pe", lambda: nc.tensor.matmul(PO[:, 0:64], qxi[b][:, 0, :], Rf[:, n, :], start=False, stop=True),
                 reads=[r_qxi[b], r_Rf[n]], writes=[r_PO], acc=True)
            c.op("pe", lambda: nc.tensor.matmul(PO[:, 64:128], scT[b][:, 1, :], vall[:, n, :], start=True, stop=False),
                 reads=[r_scT[b], r_v[n]], writes=[r_PO], acc=True)
            c.op("pe", lambda: nc.tensor.matmul(PO[:, 64:128], qxi[b][:, 1, :], Rbb[rb_cur][:], start=False, stop=True),
                 reads=[r_qxi[b], r_Rbb[rb_cur]], writes=[r_PO], acc=True)

        def r4():
            if n > 0:
                ktrans_state(n, 1, b)
                c.op("dve", lambda: V.scalar_tensor_tensor(Rrun[:, 1, :], Rrun[:, 1, :], zc[0:64, 3:4], PS[0:64, 0:64], ALU.mult, ALU.add),
                     reads=[r_Rrun[1], r_zc, r_PS], writes=[r_Rrun[1]])
                c.op("act", lambda: A.copy(Rbb[1 - rb_cur][:], Rrun[:, 1, :]), reads=[r_Rrun[1]], writes=[r_Rbb[1 - rb_cur]])

        def r5():
            c.op("dve", lambda: V.bn_stats(s[:, 0:6], PO[:, 0:64]), reads=[r_PO], writes=[rs])
            c.op("dve", lambda: V.bn_stats(s[:, 6:12], PO[:, 64:128]), reads=[r_PO], writes=[rs])
            c.op("dve", lambda: V.bn_aggr(s[:, 12:14], s[:, 0:6]), reads=[rs], writes=[rs])
            c.op("dve", lambda: V.bn_aggr(s[:, 14:16], s[:, 6:12]), reads=[rs], writes=[rs])
            c.op("act", lambda: A.activation(s[:, 16:18], s[:, 13:16:2], AF.Ln, bias=epsT[:, 0:1], scale=1.0), reads=[rs, r_eps], writes=[rs])
            c.op("act", lambda: A.activation(s[:, 16:18], s[:, 16:18], AF.Exp, scale=-0.5), reads=[rs], writes=[rs])
            c.op("dve", lambda: V.tensor_scalar(sg[b][:], sg[b][:], 1.0, None, ALU.add), reads=[r_sg[b]], writes=[r_sg[b]])
            c.op("dve", lambda: V.reciprocal(sg[b][:], sg[b][:]), reads=[r_sg[b]], writes=[r_sg[b]])
            c.op("dve", lambda: V.tensor_tensor(sg[b][:], sg[b][:], PV[:, 0:128], ALU.mult), reads=[r_sg[b], r_PV], writes=[r_sg[b]])

        def r6():
            c.op("dve", lambda: V.tensor_scalar(xn[b][:, 0:64], PO[:, 0:64], s[:, 12:13], s[:, 16:17], ALU.subtract, ALU.mult),
                 reads=[r_PO, rs], writes=[r_xn[b]])
            c.op("dve", lambda: V.tensor_scalar(xn[b][:, 64:128], PO[:, 64:128], s[:, 14:15], s[:, 17:18], ALU.subtract, ALU.mult),
                 reads=[r_PO, rs], writes=[r_xn[b]])
            c.op("pool", lambda: nc.gpsimd.tensor_tensor(xn[b][:], xn[b][:], sg[b][:], ALU.mult), reads=[r_xn[b], r_sg[b]], writes=[r_xn[b]])
            c.op("pool", lambda: nc.gpsimd.tensor_tensor(yt[b][:, 0:64], xn[b][:, 0:64], xn[b][:, 64:128], ALU.add),
                 reads=[r_xn[b]], writes=[r_yt[b]])
        return [r1, r2, r3, r4, r5, r6]

    def dil_steps(n):
        b = n % NB
        sl = slice(n * 128, (n + 1) * 128)
        s2 = st[b]; rs = r_st[b]
        kts = [kt for kt in range(n - 8, n + 9) if 0 <= kt < NCH]
        groups = [kts[i:i + 4] for i in range(0, len(kts), 4)]
        G = len(groups)

        def qk(gi):
            grp = groups[gi]; pb = gi % 3
            for j, kt in enumerate(grp):
                c.op("pe", lambda: nc.tensor.matmul(PD[pb][:, j * 128:(j + 1) * 128], qkT[64:128, 1, kt * 128:(kt + 1) * 128],
                                                    qkT[64:128, 0, sl], start=True, stop=True),
                     reads=[r_qk[kt], r_qk[n]], writes=[r_PD[pb]], acc=(j > 0))
            w = len(grp) * 128
            o0 = (grp[0] - n + 8) * 128
            c.op("act", lambda: A.activation(pd[pb][:, 0:w], PD[pb][:, 0:w], AF.Exp), reads=[r_PD[pb]], writes=[r_pd[pb]])
            c.op("pool", lambda: nc.gpsimd.tensor_tensor(pd[pb][:, 0:w], pd[pb][:, 0:w], mmb[:, o0:o0 + w], ALU.mult),
                 reads=[r_pd[pb], r_mm], writes=[r_pd[pb]])

        def pv(gi):
            grp = groups[gi]; pb = gi % 3
            for j, kt in enumerate(grp):
                first = (gi == 0 and j == 0)
                last = (gi == G - 1 and j == len(grp) - 1)
                c.op("pe", lambda: nc.tensor.matmul(PA[:, 0:65], pd[pb][:, j * 128:(j + 1) * 128], dvall[:, kt, 0:65],
                                                    start=first, stop=last),
                     reads=[r_pd[pb], r_dv[kt]], writes=[r_PA], acc=(not first))

        def fin():
            c.op("dve", lambda: V.reciprocal(s2[:, 20:21], PA[:, 64:65]), reads=[r_PA, rs], writes=[rs])
            c.op("dve", lambda: V.tensor_scalar(yt[b][:, 64:128], PA[:, 0:64], s2[:, 20:21], None, ALU.mult),
                 reads=[r_PA, rs], writes=[r_yt[b]])
        steps = []
        from functools import partial
        steps.append(partial(qk, 0))
        if G > 1:
            steps.append(partial(qk, 1))
        for k in range(G):
            if k + 2 < G:
                steps.append(partial(qk, k + 2))
            steps.append(partial(pv, k))
        steps.append(fin)
        return steps

    for n in range(NCH - 1, -1, -1):
        b = n % NB
        if n - 1 >= 0:
            load_x(n - 1, False)
        rs_ = ret_steps(n)
        ds_ = dil_steps(n)
        i = j = 0
        while i < len(ds_) or j < len(rs_):
            if i < len(ds_):
                ds_[i](); i += 1
            if j < len(rs_):
                rs_[j](); j += 1
        c.dma("sp", y[n * 128:(n + 1) * 128, :], yt[b][:], reads=[r_yt[b]], writes=[r_y[b]])
    c.finish("sp", r_y)
    print("ME: ins", c.n_ins, "waits", c.n_wait, "sbuf left", nc.sbuf_bytes_remaining)


NEG = -30000.0


def mo_patterns(NCH):
    pats = []
    for o in range(5):
        pats.append((4, 4 + o - 2))
    for n in (0, 1, NCH - 2, NCH - 1):
        kts = range(0, 4) if n < 2 else range(NCH - 4, NCH)
        for kt in kts:
            pats.append((n, kt))
    return pats


def mo_keys(n, NCH):
    if 2 <= n <= NCH - 3:
        return [(n + o - 2, o) for o in range(5)]
    idx = {0: 0, 1: 1, NCH - 2: 2, NCH - 1: 3}[n]
    kts = range(0, 4) if n < 2 else range(NCH - 4, NCH)
    return [(kt, 5 + idx * 4 + j) for j, kt in enumerate(kts)]


def mo_host_bias(rpb2, T):
    NCH = T // 128
    rows = T // 64
    pats = mo_patterns(NCH)
    m = np.arange(128)[:, None]
    c = np.arange(128)[None, :]
    out = np.full((128, 2, len(pats), 128), NEG, np.float32)
    for pi, (n, kt) in enumerate(pats):
        rm = 2 * kt + m // 64; wm = m % 64
        rc = 2 * n + c // 64; wc = c % 64
        r0 = np.clip(rc - 4, 0, rows - 8)
        c0 = np.clip(wc - 8, 0, 64 - 16)
        valid = (rm >= r0) & (rm < r0 + 8) & (wm >= c0) & (wm < c0 + 16)
        ro = np.clip(rm - rc + 7, 0, 14)
        co = np.clip(wm - wc + 15, 0, 30)
        for h in range(2):
            g = rpb2[h][ro, co]
            out[:, h, pi, :] = np.where(valid, g, np.float32(NEG))
    return out.reshape(128, 2 * len(pats) * 128)


def mo_weight_cols(core):
    h0, h1 = 2 * core, 2 * core + 1
    rng = lambda blk, h: list(range(blk * 1024 + h * 64, blk * 1024 + (h + 1) * 64))
    return np.array(rng(0, h0) + rng(0, h1) + rng(1, h0) + rng(1, h1) + rng(2, h0) + rng(2, h1))


def build_MO(T=16384):
    nc = bass.Bass("TRN2", target_bir_lowering=False)
    c = Ctx(nc)
    xT = nc.dram_tensor("xT", [1024, T], F32, kind="ExternalInput").ap()
    wsel = nc.dram_tensor("wsel", [1024, 384], F32, kind="ExternalInput").ap()
    bias = nc.dram_tensor("bias", [128, 2 * 21 * 128], F32, kind="ExternalInput").ap()
    idn = nc.dram_tensor("idn", [128, 128], F32, kind="ExternalInput").ap()
    y = nc.dram_tensor("y", [T, 128], F32, kind="ExternalOutput").ap()
    emit_MO(nc, c, xT, wsel, bias, idn, y, T)
    return nc


def emit_MO(nc, c, xT, wsel, bias, idn, y, T, pfx="O"):
    NCH = T // 128
    sb = lambda n, s, d: c.sb(pfx + n, s, d)
    V = nc.vector
    A = nc.scalar
    identb = sb("identb", [128, 128], BF16); r_identb = Res("identb")
    c.dma("pool", identb[:], idn, writes=[r_identb])
    wb = sb("wb", [128, 8, 384], BF16); r_wb = Res("wb")
    c.dma("pool", wb[:], wsel.rearrange("(c p) e -> p c e", p=128), writes=[r_wb])
    biasb = sb("biasb", [128, 2, 21, 128], BF16); r_bias = Res("bias")
    c.dma("pool", biasb[:].rearrange("p a b c -> p (a b c)"), bias, writes=[r_bias])

    qkT = sb("qkT", [128, 2, T], BF16); r_qk = [Res("qk%d" % n) for n in range(NCH)]
    vall = sb("vall", [128, NCH, 2, 66], BF16); r_v = [Res("v%d" % n) for n in range(NCH)]
    ones_dst = Res("ones")
    c.op("pool", lambda: nc.gpsimd.memset(vall[:, :, :, 64:66], 1.0), writes=[ones_dst])
    NB = 2
    xTt = [sb("xTt%d" % i, [128, 8, 128], BF16) for i in range(NB)]; r_xTt = [Res("xTt%d" % i) for i in range(NB)]
    pd = [sb("pd%d" % i, [128, 640], BF16) for i in range(3)]; r_pd = [Res("pd%d" % i) for i in range(3)]
    yt = [sb("yt%d" % i, [128, 128], F32) for i in range(NB)]; r_yt = [Res("yt%d" % i) for i in range(NB)]
    st = [sb("st%d" % i, [128, 8], F32) for i in range(NB)]; r_st = [Res("st%d" % i) for i in range(NB)]
    r_y = [Res("y0"), Res("y1")]
    PA = c.ps(pfx + "PA", [128, 512], F32); r_PA = Res("PA")
    PV = c.ps(pfx + "PV", [128, 512], F32); r_PV = Res("PV")
    PO = c.ps(pfx + "PO", [128, 512], F32); r_PO = Res("PO")
    PD = [c.ps(pfx + "PD%d" % i, [128, 1024], F32) for i in range(2)]; r_PD = [Res("PD%d" % i) for i in range(2)]

    def load_x(n):
        b = n % NB
        c.dma("pool", xTt[b][:], xT[:, n * 128:(n + 1) * 128].rearrange("(c p) t -> p c t", p=128), writes=[r_xTt[b]])

    load_x(0)
    for n in range(NCH):
        b = n % NB
        if n + 1 < NCH:
            load_x(n + 1)
        for g in range(2):
            for k in range(8):
                c.op("pe", lambda: nc.tensor.matmul(PA[:, g * 128:(g + 1) * 128], wb[:, k, g * 128:(g + 1) * 128], xTt[b][:, k, :],
                                                    start=(k == 0), stop=(k == 7)),
                     reads=[r_wb, r_xTt[b]], writes=[r_PA], acc=(g + k > 0))
        for k in range(8):
            c.op("pe", lambda: nc.tensor.matmul(PV[:, 0:128], xTt[b][:, k, :], wb[:, k, 256:384], start=(k == 0), stop=(k == 7)),
                 reads=[r_wb, r_xTt[b]], writes=[r_PV], acc=(k > 0))
        c.op("act", lambda: A.activation(qkT[:, 0, n * 128:(n + 1) * 128], PA[:, 0:128], AF.Copy, scale=0.125),
             reads=[r_PA], writes=[r_qk[n]])
        c.op("dve", lambda: V.tensor_copy(qkT[:, 1, n * 128:(n + 1) * 128], PA[:, 128:256]), reads=[r_PA], writes=[r_qk[n]])
        c.op("dve", lambda: V.tensor_copy(vall[:, n, :, 0:64], PV[:, 0:128].rearrange("p (h d) -> p h d", h=2)),
             reads=[r_PV, ones_dst], writes=[r_v[n]])

    POs = [PO, PA]; r_POs = [r_PO, r_PA]
    units = [(n, hh) for n in range(NCH) for hh in range(2)]

    def qk(ui):
        n, hh = units[ui]
        sl = slice(n * 128, (n + 1) * 128)
        keys = mo_keys(n, NCH)
        hp = slice(hh * 64, (hh + 1) * 64)
        pb = ui % 2; sbi = ui % 3
        for j, (kt, pat) in enumerate(keys):
            c.op("pe", lambda: nc.tensor.matmul(PD[pb][:, j * 128:(j + 1) * 128], qkT[hp, 1, kt * 128:(kt + 1) * 128],
                                                qkT[hp, 0, sl], start=True, stop=False),
                 reads=[r_qk[kt], r_qk[n]], writes=[r_PD[pb]], acc=(j > 0))
            c.op("pe", lambda: nc.tensor.matmul(PD[pb][:, j * 128:(j + 1) * 128], identb[:], biasb[:, hh, pat, :],
                                                start=False, stop=True),
                 reads=[r_identb, r_bias], writes=[r_PD[pb]], acc=True)
        nk = len(keys)
        w0 = min(nk, 4) * 128
        c.op("act", lambda: A.activation(pd[sbi][:, 0:w0], PD[pb][:, 0:w0], AF.Exp), reads=[r_PD[pb]], writes=[r_pd[sbi]])
        if nk > 4:
            c.op("act", lambda: A.activation(pd[sbi][:, 512:640], PD[pb][:, 512:640], AF.Exp), reads=[r_PD[pb]], writes=[r_pd[sbi]], acc=True)

    def pv(ui):
        n, hh = units[ui]
        keys = mo_keys(n, NCH)
        nk = len(keys)
        sbi = ui % 3
        po = POs[n % 2]; r_po = r_POs[n % 2]
        for j, (kt, pat) in enumerate(keys):
            c.op("pe", lambda: nc.tensor.matmul(po[:, hh * 128:hh * 128 + 65], pd[sbi][:, j * 128:(j + 1) * 128], vall[:, kt, hh, 0:65],
                                                start=(j == 0), stop=(j == nk - 1)),
                 reads=[r_pd[sbi], r_v[kt]], writes=[r_po], acc=(hh + j > 0))

    def fin(n):
        b = n % NB
        s = st[b]; rs = r_st[b]
        po = POs[n % 2]; r_po = r_POs[n % 2]
        for hh in range(2):
            c.op("dve", lambda: V.reciprocal(s[:, hh:hh + 1], po[:, hh * 128 + 64:hh * 128 + 65]), reads=[r_po, rs], writes=[rs])
            c.op("dve", lambda: V.tensor_scalar(yt[b][:, hh * 64:(hh + 1) * 64], po[:, hh * 128:hh * 128 + 64], s[:, hh:hh + 1], None, ALU.mult),
                 reads=[r_po, rs], writes=[r_yt[b]])
        c.dma("sp", y[n * 128:(n + 1) * 128, :], yt[b][:], reads=[r_yt[b]], writes=[r_y[b]])

    qk(0)
    for ui in range(len(units)):
        if ui + 1 < len(units):
            qk(ui + 1)
        pv(ui)
        if units[ui][1] == 1:
            fin(units[ui][0])
    c.finish("sp", r_y)
    print("MO: ins", c.n_ins, "waits", c.n_wait, "sbuf left", nc.sbuf_bytes_remaining)


N_CORES = 8
T_SEQ = 16384
_PROGS = {}


def _prog(name):
    if name not in _PROGS:
        _PROGS[name] = {"ME": lambda: build_ME(T_SEQ), "MO": lambda: build_MO(T_SEQ), "F": lambda: build_F2(32, 2)}[name]()
    return _PROGS[name]


def _launch(nc, in_maps):
    res = run_bass_kernel_spmd(nc, in_maps, core_ids=list(range(N_CORES)))
    return res.results


def kernel(x, ab_w_in, ab_w_out, ret_decay, c_w_in, c_w_out, c_rpb, ln_g, ln_b,
           router_w, router_b, exp_w_up, exp_b_up, exp_w_down, exp_b_down):
    f32 = np.float32
    xs = np.ascontiguousarray(np.asarray(x, f32)[0])
    T = xs.shape[0]
    idn = np.eye(128, dtype=f32)
    ii = np.arange(128)
    fcst = np.ascontiguousarray(np.concatenate([np.tile(np.arange(256, dtype=f32)[None, :], (128, 1)),
                                                (ii[:, None] < ii[None, :]).astype(f32), np.ones((128, 128), f32)], 1))
    me_c = me_host_consts(T)
    per = T // N_CORES
    for layer in range(4):
        j = layer // 2
        xT = np.ascontiguousarray(xs.T)
        ycat = np.empty((T, 1024), f32)
        if layer % 2 == 0:
            w_in = np.asarray(ab_w_in[j], f32)
            dec = np.asarray(ret_decay[j], f32)
            in_maps = [dict(xT=xT, wsel=np.ascontiguousarray(w_in[:, me_weight_cols(h)]),
                            dec=np.ascontiguousarray(dec[:, h][None, :]), **me_c) for h in range(N_CORES)]
            outs = _launch(_prog("ME"), in_maps)
            for h in range(N_CORES):
                yh = outs[h]["y"]
                ycat[:, h * 64:(h + 1) * 64] = yh[:, 0:64]
                ycat[:, 512 + h * 64:512 + (h + 1) * 64] = yh[:, 64:128]
            w_out = np.asarray(ab_w_out[j], f32)
        else:
            w_in = np.asarray(c_w_in[j], f32)
            rpb = np.asarray(c_rpb[j], f32)
            in_maps = [dict(xT=xT, wsel=np.ascontiguousarray(w_in[:, mo_weight_cols(cc)]),
                            bias=mo_host_bias(rpb[2 * cc:2 * cc + 2], T), idn=idn) for cc in range(N_CORES)]
            outs = _launch(_prog("MO"), in_maps)
            for cc in range(N_CORES):
                ycat[:, cc * 128:(cc + 1) * 128] = outs[cc]["y"]
            w_out = np.asarray(c_w_out[j], f32)
        lnp = np.ascontiguousarray(np.stack([ln_g[layer, 0], ln_b[layer, 0], ln_g[layer, 1], ln_b[layer, 1]], 0).astype(f32))
        common = dict(w_out=np.ascontiguousarray(w_out), lnp=lnp,
                      rw=np.ascontiguousarray(np.asarray(router_w[layer], f32)),
                      rb=np.ascontiguousarray(np.asarray(router_b[layer], f32)[None, :]),
                      wup=np.ascontiguousarray(np.asarray(exp_w_up[layer], f32)),
                      bup=np.ascontiguousarray(np.asarray(exp_b_up[layer], f32).reshape(256, 256)),
                      wdn=np.ascontiguousarray(np.asarray(exp_w_down[layer], f32)),
                      bdn=np.ascontiguousarray(np.asarray(exp_b_down[layer], f32)), idn=idn, fcst=fcst)
        in_maps = []
        for cc in range(N_CORES):
            sl = slice(cc * per, (cc + 1) * per)
            in_maps.append(dict(common, x=np.ascontiguousarray(xs[sl]), yT=np.ascontiguousarray(ycat[sl].T)))
        outs = _launch(_prog("F"), in_maps)
        xs = np.concatenate([outs[cc]["xo"] for cc in range(N_CORES)], 0)
    return xs[None].astype(f32)
```

```python
import numpy as np
import concourse.bass as bass
import concourse.mybir as mybir
from concourse.bass_utils import run_bass_kernel_spmd

F32 = mybir.dt.float32
BF16 = mybir.dt.bfloat16
AF = mybir.ActivationFunctionType
ALU = mybir.AluOpType
AX = mybir.AxisListType


class Res:
    __slots__ = ("name", "w", "r", "dsem", "dcnt")

    def __init__(self, name):
        self.name = name
        self.w = None
        self.r = []
        self.dsem = None
        self.dcnt = 0


class Ctx:
    def __init__(self, nc):
        self.nc = nc
        self.eng = {"pe": nc.tensor, "act": nc.scalar, "dve": nc.vector,
                    "pool": nc.gpsimd, "sp": nc.sync}
        self.sem = {k: nc.alloc_semaphore("c_" + k) for k in ("pe", "act", "dve", "pool")}
        self.cnt = {k: 0 for k in self.sem}
        self.seen = {k: {} for k in self.eng}
        self.semobj = dict(self.sem)
        self.n_dsem = 0
        self.n_wait = 0
        self.n_ins = 0

    def sb(self, name, shape, dt):
        return self.nc.alloc_sbuf_tensor(name, list(shape), dt)

    def ps(self, name, shape, dt=F32):
        return self.nc.alloc_psum_tensor(name, list(shape), dt)

    def _dsem(self, res):
        if res.dsem is None:
            key = "d%d" % self.n_dsem
            self.n_dsem += 1
            res.dsem = key
            self.semobj[key] = self.nc.alloc_semaphore(key)
        return res.dsem

    def _wait(self, e, deps):
        seen = self.seen[e]
        eng = self.eng[e]
        best = {}
        for d in deps:
            if d is None:
                continue
            k, v = d
            if best.get(k, 0) < v:
                best[k] = v
        for k, v in best.items():
            if seen.get(k, 0) < v:
                eng.wait_ge(self.semobj[k], v)
                seen[k] = v
                self.n_wait += 1

    def op(self, e, fn, reads=(), writes=(), acc=False):
        deps = [r.w for r in reads]
        if not acc:
            for w in writes:
                deps.append(w.w)
                deps.extend(w.r)
        self._wait(e, deps)
        ins = fn()
        self.cnt[e] += 1
        ins.then_inc(self.sem[e], 1)
        ev = (e, self.cnt[e])
        self.seen[e][e] = max(self.seen[e].get(e, 0), 0)
        for r in reads:
            r.r.append(ev)
        for w in writes:
            w.w = ev
            if not acc:
                w.r = []
        self.n_ins += 1
        return ins

    def dma(self, q, out, in_, reads=(), writes=(), **kw):
        assert len(writes) == 1
        wres = writes[0]
        deps = [r.w for r in reads]
        deps.append(wres.w)
        deps.extend(wres.r)
        self._wait(q, deps)
        key = self._dsem(wres)
        ins = self.eng[q].dma_start(out=out, in_=in_, **kw)
        wres.dcnt += 16
        ins.then_inc(self.semobj[key], 16)
        ev = (key, wres.dcnt)
        for r in reads:
            r.r.append(ev)
        wres.w = ev
        wres.r = []
        self.n_ins += 1
        return ins

    def dma_fn(self, q, fn, reads=(), writes=()):
        wres = writes[0]
        deps = [r.w for r in reads]
        deps.append(wres.w)
        deps.extend(wres.r)
        self._wait(q, deps)
        key = self._dsem(wres)
        ins = fn()
        wres.dcnt += 16
        ins.then_inc(self.semobj[key], 16)
        ev = (key, wres.dcnt)
        for r in reads:
            r.r.append(ev)
        wres.w = ev
        wres.r = []
        self.n_ins += 1
        return ins

    def dma_more(self, q, out, in_, reads=(), writes=(), **kw):
        wres = writes[0]
        deps = [r.w for r in reads]
        self._wait(q, deps)
        key = self._dsem(wres)
        ins = self.eng[q].dma_start(out=out, in_=in_, **kw)
        wres.dcnt += 16
        ins.then_inc(self.semobj[key], 16)
        ev = (key, wres.dcnt)
        for r in reads:
            r.r.append(ev)
        wres.w = ev
        self.n_ins += 1
        return ins

    def finish(self, q, resources):
        self._wait(q, [r.w for r in resources])


ALPHA = (2.0 * 4) ** 0.25
LN_EPS = 1e-5


def build_F2(n_exp=32, n_blk=2):
    nc = bass.Bass("TRN2", target_bir_lowering=False)
    c = Ctx(nc)
    TB = 1024
    NT = TB * n_blk
    D = 1024
    x = nc.dram_tensor("x", [NT, D], F32, kind="ExternalInput").ap()
    yT = nc.dram_tensor("yT", [D, NT], F32, kind="ExternalInput").ap()
    w_out = nc.dram_tensor("w_out", [D, D], F32, kind="ExternalInput").ap()
    lnp = nc.dram_tensor("lnp", [4, D], F32, kind="ExternalInput").ap()
    rw = nc.dram_tensor("rw", [D, 32], F32, kind="ExternalInput").ap()
    rb = nc.dram_tensor("rb", [1, 32], F32, kind="ExternalInput").ap()
    wup = nc.dram_tensor("wup", [n_exp, D, 2 * D], F32, kind="ExternalInput").ap()
    bup = nc.dram_tensor("bup", [256, 256], F32, kind="ExternalInput").ap()
    wdn = nc.dram_tensor("wdn", [n_exp, D, D], F32, kind="ExternalInput").ap()
    bdn = nc.dram_tensor("bdn", [32, D], F32, kind="ExternalInput").ap()
    idn = nc.dram_tensor("idn", [128, 128], F32, kind="ExternalInput").ap()
    fcst = nc.dram_tensor("fcst", [128, 512], F32, kind="ExternalInput").ap()
    xo = nc.dram_tensor("xo", [NT, D], F32, kind="ExternalOutput").ap()

    emit_F2(nc, c, x, yT, w_out, lnp, rw, rb, wup, bup, wdn, bdn, idn, fcst, xo, n_exp=n_exp, n_blk=n_blk)
    return nc


def emit_F2(nc, c, x, yT, w_out, lnp, rw, rb, wup, bup, wdn, bdn, idn, fcst, xo, n_exp=32, n_blk=2, pfx="F"):
    CAP = 256
    TB = 1024
    D = 1024
    NTL = TB // 128
    sb = lambda n, s, d: c.sb(pfx + n, s, d)
    ident = sb("ident", [128, 128], F32); r_ident = Res("ident")
    c.dma("sp", ident[:], idn, writes=[r_ident])
    rw32 = sb("rw32", [128, 8, 32], F32); r_rw = Res("rw")
    c.dma("sp", rw32[:], rw.rearrange("(c p) e -> p c e", p=128), writes=[r_rw])
    rbB = sb("rbB", [128, 32], F32); r_rb = Res("rb")
    c.dma("sp", rbB[:], rb.partition_broadcast(128), writes=[r_rb])
    bdn32 = sb("bdn32", [32, D], F32); r_bdn = Res("bdn")
    c.dma("sp", bdn32[:], bdn, writes=[r_bdn])
    lnB = [sb("lnB%d" % i, [128, D], F32) for i in range(4)]
    r_ln = [Res("ln%d" % i) for i in range(4)]
    for i in range(4):
        c.dma("sp", lnB[i][:], lnp[i:i + 1, :].partition_broadcast(128), writes=[r_ln[i]])
    iota = sb("iota", [128, 256], F32); r_iota = Res("iota")
    c.dma("sp", iota[:], fcst[:, 0:256], writes=[r_iota])
    UO = sb("UO", [128, 256], BF16); r_UO = Res("UO")
    c.dma("pool", UO[:], fcst[:, 256:512], writes=[r_UO])
    identb = sb("identb", [128, 128], BF16); r_identb = Res("identb")
    c.dma("pool", identb[:], idn, writes=[r_identb])
    B = [c.ps(pfx + "bank%d" % i, [128, 512], F32) for i in range(7)]
    rB = [Res("bank%d" % i) for i in range(7)]
    TP = c.ps(pfx + "TP", [128, 1024], BF16); r_TP = Res("TP")
    bupraw = sb("bupraw", [128, 2, 256], F32); r_bupraw = Res("bupraw")
    c.dma("sp", bupraw[:], bup.rearrange("(g p) k -> p g k", p=128), writes=[r_bupraw])
    bupT = sb("bupT", [128, 2, 256], F32); r_bupT = Res("bupT")
    for two in range(2):
        for g in range(2):
            c.op("pe", lambda: nc.tensor.transpose(B[0][:, (two * 2 + g) * 128:(two * 2 + g + 1) * 128],
                                                   bupraw[:, g, two:256:2], ident[:]),
                 reads=[r_bupraw, r_ident], writes=[rB[0]], acc=(two + g > 0))
    c.op("act", lambda: nc.scalar.copy(bupT[:].rearrange("p a b -> p (a b)"), B[0][:]), reads=[rB[0]], writes=[r_bupT])

    c.op("dve", lambda: nc.vector.tensor_scalar(bupT[:, 1, :], bupT[:, 1, :], 1.0, None, ALU.add), reads=[r_bupT], writes=[r_bupT])

    yacc = sb("yacc", [128, NTL, D], F32); r_yacc = [Res("yacc%d" % i) for i in range(NTL)]
    x1b = sb("x1b", [128, NTL, D], BF16); r_x1b = [Res("x1b%d" % i) for i in range(NTL)]
    maskf = sb("maskf", [128, NTL, 32], F32); r_mask = [Res("mask%d" % i) for i in range(NTL)]
    maskb = sb("maskb", [128, NTL, 32], BF16)
    rank = sb("rank", [128, NTL, 32], F32); r_rank = [Res("rank%d" % i) for i in range(NTL)]
    carry = sb("carry", [128, 32], F32); r_carry = Res("carry")
    gates = sb("gates", [128, NTL, 32], F32); r_gates = [Res("gates%d" % i) for i in range(NTL)]
    gT = sb("gT", [32, TB], F32); r_gT = [Res("gT%d" % i) for i in range(NTL)]
    x1T32 = [sb("x1T32_%d" % i, [128, 8, 128], F32) for i in range(2)]; r_x1T32 = [Res("x1T32_%d" % i) for i in range(2)]
    sm = [sb("sm%d" % i, [128, 128], F32) for i in range(2)]; r_sm = [Res("sm%d" % i) for i in range(2)]
    wuc = [sb("wuc%d" % i, [128, 8, 256], BF16) for i in range(4)]; r_wuc = [Res("wuc%d" % i) for i in range(4)]
    wd = [sb("wd%d" % i, [128, 8, D], BF16) for i in range(2)]; r_wd = [Res("wd%d" % i) for i in range(2)]
    yTb, r_yTb = wd[0], r_wd[0]
    woutb, r_wout = wd[1], r_wd[1]
    Pm = [sb("Pm%d" % i, [128, NTL, CAP], BF16) for i in range(2)]; r_Pm = [Res("Pm%d" % i) for i in range(2)]
    Gm = [sb("Gm%d" % i, [128, NTL, CAP], BF16) for i in range(2)]; r_Gm = [Res("Gm%d" % i) for i in range(2)]
    GT = [sb("GT%d" % i, [128, 2, TB], BF16) for i in range(2)]; r_GT = [Res("GT%d" % i) for i in range(2)]
    XgT = [sb("XgT%d" % i, [128, 8, CAP], BF16) for i in range(2)]; r_XgT = [Res("XgT%d" % i) for i in range(2)]
    act = [sb("act%d" % i, [128, 8, CAP], BF16) for i in range(2)]; r_act = [Res("act%d" % i) for i in range(2)]
    osb = [sb("osb%d" % i, [128, 2, D], BF16) for i in range(2)]; r_osb = [Res("osb%d" % i) for i in range(2)]
    NTMP = 2
    tg = [sb("tg%d" % i, [128, CAP], F32) for i in range(NTMP)]; r_tg = [Res("tg%d" % i) for i in range(NTMP)]
    tsg = [sb("tsg%d" % i, [128, CAP], F32) for i in range(NTMP)]; r_tsg = [Res("tsg%d" % i) for i in range(NTMP)]
    tl = [sb("tl%d" % i, [128, CAP], F32) for i in range(NTMP)]; r_tl = [Res("tl%d" % i) for i in range(NTMP)]
    ot = [sb("ot%d" % i, [128, D], F32) for i in range(2)]; r_ot = [Res("ot%d" % i) for i in range(2)]
    zt, r_zt = ot, r_ot
    xt, r_xt = ot, r_ot
    r_out = [Res("xo0"), Res("xo1")]

    def layer_norm(src, r_src, dst, r_dst, s, r_s, gi, bi):
        for h in range(2):
            c.op("dve", lambda: nc.vector.bn_stats(s[:, h * 6:(h + 1) * 6], src[:, h * 512:(h + 1) * 512]),
                 reads=[r_src], writes=[r_s], acc=(h > 0))
        c.op("dve", lambda: nc.vector.bn_aggr(s[:, 12:14], s[:, 0:12]),
             reads=[r_s], writes=[r_s])
        c.op("act", lambda: nc.scalar.activation(s[:, 14:15], s[:, 13:14], AF.Sqrt, bias=epsT[:, 0:1], scale=1.0),
             reads=[r_s, r_eps], writes=[r_s])
        c.op("dve", lambda: nc.vector.reciprocal(s[:, 15:16], s[:, 14:15]), reads=[r_s], writes=[r_s])
        c.op("dve", lambda: nc.vector.scalar_tensor_tensor(dst, src, s[:, 12:13], lnB[gi][:], ALU.subtract, ALU.mult),
             reads=[r_src, r_s, r_ln[gi]], writes=[r_dst])
        c.op("dve", lambda: nc.vector.scalar_tensor_tensor(dst, dst, s[:, 15:16], lnB[bi][:], ALU.mult, ALU.add),
             reads=[r_dst, r_s, r_ln[bi]], writes=[r_dst])

    epsT = sb("epsT", [128, 1], F32); r_eps = Res("eps")
    c.op("dve", lambda: nc.vector.memset(epsT[:], LN_EPS), writes=[r_eps])

    def issue_wd(q):
        e_ = q % n_exp
        c.dma("pool", wd[q % 2][:], wdn[e_].rearrange("(c p) d -> p c d", p=128), writes=[r_wd[q % 2]])

    def issue_wu(gi):
        e_ = (gi // 8) % n_exp
        fc_ = gi % 8
        c.dma("pool", wuc[gi % 4][:], wup[e_][:, fc_ * 256:(fc_ + 1) * 256].rearrange("(c p) f -> p c f", p=128), writes=[r_wuc[gi % 4]])

    issue_wu(0)
    issue_wu(1)
    tmp_i = 0
    pg_i = 0
    po_i = 0
    sc_i = 0
    GXB = (3, 6)
    for blk in range(n_blk):
        t0 = blk * TB
        c.dma("pool", yTb[:], yT[:, t0:t0 + TB].rearrange("(c p) t -> p c t", p=128), writes=[r_yTb])
        c.dma("pool", woutb[:], w_out.rearrange("(c p) d -> p c d", p=128), writes=[r_wout])
        def stageA(i):
            b2 = i % 2
            c.dma("sp", xt[b2][:], x[t0 + i * 128:t0 + (i + 1) * 128, :], writes=[r_xt[b2]])
            for h in range(2):
                for k in range(8):
                    c.op("pe", lambda: nc.tensor.matmul(B[h][:], yTb[:, k, i * 128:(i + 1) * 128],
                                                        woutb[:, k, h * 512:(h + 1) * 512], start=(k == 0), stop=(k == 7)),
                         reads=[r_yTb, r_wout], writes=[rB[h]], acc=(k > 0))
                c.op("dve", lambda: nc.vector.scalar_tensor_tensor(zt[b2][:, h * 512:(h + 1) * 512], xt[b2][:, h * 512:(h + 1) * 512],
                                                                   ALPHA, B[h][:], ALU.mult, ALU.add),
                     reads=[r_xt[b2], rB[h]], writes=[r_zt[b2]], acc=(h > 0))
            layer_norm(zt[b2][:], r_zt[b2], yacc[:, i, :], r_yacc[i], sm[b2], r_sm[b2], 0, 1)
            for k in range(8):
                bk = 2 + k // 4
                c.op("pe", lambda: nc.tensor.transpose(B[bk][:, (k % 4) * 128:(k % 4 + 1) * 128],
                                                       yacc[:, i, k * 128:(k + 1) * 128], ident[:]),
                     reads=[r_yacc[i], r_ident], writes=[rB[bk]], acc=(k % 4 > 0))
            for hh in range(2):
                c.op("act", lambda: nc.scalar.copy(x1T32[b2][:, hh * 4:(hh + 1) * 4, :].rearrange("p a b -> p (a b)"), B[2 + hh][:]),
                     reads=[rB[2 + hh]], writes=[r_x1T32[b2]], acc=(hh > 0))
            c.op("act", lambda: nc.scalar.copy(x1b[:, i, :], yacc[:, i, :]), reads=[r_yacc[i]], writes=[r_x1b[i]])
        def stageB(i):
            b2 = i % 2
            for k in range(8):
                c.op("pe", lambda: nc.tensor.matmul(B[4][:, 0:32], x1T32[b2][:, k, :], rw32[:, k, :], start=(k == 0), stop=(k == 7)),
                     reads=[r_x1T32[b2], r_rw], writes=[rB[4]], acc=(k > 0))
            s = sm[b2]; r_s = r_sm[b2]
            c.op("dve", lambda: nc.vector.tensor_tensor(s[:, 32:64], B[4][:, 0:32], rbB[:], ALU.add), reads=[rB[4], r_rb], writes=[r_s])
            c.op("dve", lambda: nc.vector.max(s[:, 16:24], s[:, 32:64]), reads=[r_s], writes=[r_s])
            c.op("dve", lambda: nc.vector.tensor_scalar(s[:, 64:96], s[:, 32:64], s[:, 19:20], None, ALU.is_ge), reads=[r_s], writes=[r_s])
            c.op("dve", lambda: nc.vector.tensor_copy(maskf[:, i, :], s[:, 64:96]), reads=[r_s], writes=[r_mask[i]])
            c.op("dve", lambda: nc.vector.tensor_copy(maskb[:, i, :], s[:, 64:96]), reads=[r_s], writes=[r_mask[i]])
            c.op("pe", lambda: nc.tensor.matmul(B[4][:, 256:288], UO[:, 0:128], maskb[:, i, :], start=True, stop=True),
                 reads=[r_UO, r_mask[i]], writes=[rB[4]])
            c.op("pe", lambda: nc.tensor.matmul(B[4][:, 288:320], UO[:, 128:256], maskb[:, i, :], start=True, stop=True),
                 reads=[r_UO, r_mask[i]], writes=[rB[4]], acc=True)
            if i == 0:
                c.op("dve", lambda: nc.vector.tensor_copy(rank[:, i, :], B[4][:, 256:288]), reads=[rB[4]], writes=[r_rank[i]])
                c.op("dve", lambda: nc.vector.tensor_copy(carry[:], B[4][:, 288:320]), reads=[rB[4]], writes=[r_carry])
            else:
                c.op("dve", lambda: nc.vector.tensor_tensor(rank[:, i, :], B[4][:, 256:288], carry[:], ALU.add),
                     reads=[rB[4], r_carry], writes=[r_rank[i]])
                c.op("dve", lambda: nc.vector.tensor_tensor(carry[:], carry[:], B[4][:, 288:320], ALU.add),
                     reads=[rB[4], r_carry], writes=[r_carry])
            c.op("dve", lambda: nc.vector.tensor_scalar(s[:, 24:25], s[:, 16:17], -1.0, None, ALU.mult), reads=[r_s], writes=[r_s])
            c.op("act", lambda: nc.scalar.activation(s[:, 96:128], s[:, 32:64], AF.Exp, bias=s[:, 24:25], scale=1.0), reads=[r_s], writes=[r_s])
            c.op("dve", lambda: nc.vector.tensor_tensor(s[:, 96:128], s[:, 96:128], s[:, 64:96], ALU.mult), reads=[r_s], writes=[r_s])
            c.op("dve", lambda: nc.vector.reduce_sum(s[:, 25:26], s[:, 96:128], AX.X), reads=[r_s], writes=[r_s])
            c.op("dve", lambda: nc.vector.reciprocal(s[:, 26:27], s[:, 25:26]), reads=[r_s], writes=[r_s])
            c.op("dve", lambda: nc.vector.tensor_scalar(gates[:, i, :], s[:, 96:128], s[:, 26:27], None, ALU.mult), reads=[r_s], writes=[r_gates[i]])
            c.op("pe", lambda: nc.tensor.transpose(B[4][0:32, 128:256], gates[:, i, :], ident[:]),
                 reads=[r_gates[i], r_ident], writes=[rB[4]])
            c.op("act", lambda: nc.scalar.copy(gT[:, i * 128:(i + 1) * 128], B[4][0:32, 128:256]), reads=[rB[4]], writes=[r_gT[i]])
            for h in range(2):
                c.op("pe", lambda: nc.tensor.matmul(B[5 + h][:], gT[:, i * 128:(i + 1) * 128], bdn32[:, h * 512:(h + 1) * 512],
                                                    start=True, stop=True), reads=[r_gT[i], r_bdn], writes=[rB[5 + h]])
                c.op("dve", lambda: nc.vector.scalar_tensor_tensor(yacc[:, i, h * 512:(h + 1) * 512], yacc[:, i, h * 512:(h + 1) * 512],
                                                                   ALPHA, B[5 + h][:], ALU.mult, ALU.add),
                     reads=[r_yacc[i], rB[5 + h]], writes=[r_yacc[i]])

        stageA(0)
        for i in range(NTL):
            if i + 1 < NTL:
                stageA(i + 1)
            stageB(i)
        issue_wd(blk * n_exp)
        def build(e):
            ab = e % 2
            for i in range(NTL):
                c.op("dve", lambda: nc.vector.tensor_scalar(Pm[ab][:, i, :], iota[:], rank[:, i, e:e + 1], maskf[:, i, e:e + 1],
                                                            ALU.is_equal, ALU.mult),
                     reads=[r_iota, r_rank[i], r_mask[i]], writes=[r_Pm[ab]], acc=(i > 0))
                c.op("dve", lambda: nc.vector.tensor_scalar(Gm[ab][:, i, :], iota[:], rank[:, i, e:e + 1], gates[:, i, e:e + 1],
                                                            ALU.is_equal, ALU.mult),
                     reads=[r_iota, r_rank[i], r_gates[i]], writes=[r_Gm[ab]], acc=(i > 0))

        def transposes(e):
            ab = e % 2
            for st_ in range(2):
                for i in range(NTL):
                    c.op("pe", lambda: nc.tensor.transpose(TP[:, i * 128:(i + 1) * 128], Gm[ab][:, i, st_ * 128:(st_ + 1) * 128], identb[:]),
                         reads=[r_Gm[ab], r_identb], writes=[r_TP], acc=(i > 0))
                c.op("act", lambda: nc.scalar.copy(GT[ab][:, st_, :], TP[:]), reads=[r_TP], writes=[r_GT[ab]], acc=(st_ > 0))

        def gather(e):
            ab = e % 2
            for k in range(8):
                gh = k % 2
                for i in range(NTL):
                    c.op("pe", lambda: nc.tensor.matmul(B[GXB[gh]][:, 0:256], x1b[:, i, k * 128:(k + 1) * 128], Pm[ab][:, i, :],
                                                        start=(i == 0), stop=(i == NTL - 1)),
                         reads=[r_x1b[i], r_Pm[ab]], writes=[rB[GXB[gh]]], acc=(i > 0))
                c.op("act", lambda: nc.scalar.copy(XgT[ab][:, k, :], B[GXB[gh]][:, 0:256]), reads=[rB[GXB[gh]]], writes=[r_XgT[ab]], acc=(k > 0))

        def up(e):
            nonlocal pg_i, tmp_i
            q = blk * n_exp + e
            ab = e % 2
            for fc in range(8):
                gi = q * 8 + fc
                if gi + 2 < n_blk * n_exp * 8:
                    issue_wu(gi + 2)
                ws = gi % 4
                row = e * 8 + fc
                ub = pg_i % 2; pg_i += 1
                for two in range(2):
                    for k in range(8):
                        c.op("pe", lambda: nc.tensor.matmul(B[ub][:, two * 256:(two + 1) * 256], wuc[ws][:, k, two:256:2], XgT[ab][:, k, :],
                                                            start=(k == 0), stop=(k == 7)),
                             reads=[r_wuc[ws], r_XgT[ab]], writes=[rB[ub]], acc=(two + k > 0))
                ti = tmp_i % NTMP; tmp_i += 1
                c.op("dve", lambda: nc.vector.tensor_scalar(tg[ti][:], B[ub][:, 0:256], bupT[:, 0, row:row + 1], 7.0, ALU.add, ALU.min),
                     reads=[rB[ub], r_bupT], writes=[r_tg[ti]])
                c.op("act", lambda: nc.scalar.activation(tsg[ti][:], tg[ti][:], AF.Sigmoid, scale=1.702),
                     reads=[r_tg[ti]], writes=[r_tsg[ti]])
                c.op("dve", lambda: nc.vector.tensor_scalar(tl[ti][:], B[ub][:, 256:512], bupT[:, 1, row:row + 1], -6.0, ALU.add, ALU.max),
                     reads=[rB[ub], r_bupT], writes=[r_tl[ti]])
                c.op("dve", lambda: nc.vector.scalar_tensor_tensor(tl[ti][:], tl[ti][:], 8.0, tg[ti][:], ALU.min, ALU.mult),
                     reads=[r_tg[ti], r_tl[ti]], writes=[r_tl[ti]])
                c.op("dve", lambda: nc.vector.tensor_tensor(act[ab][:, fc, :], tl[ti][:], tsg[ti][:], ALU.mult),
                     reads=[r_tl[ti], r_tsg[ti]], writes=[r_act[ab]], acc=(fc > 0))

        def down(e):
            ab = e % 2
            for st_ in range(2):
                for h in range(2):
                    po = 2
                    for fc in range(8):
                        c.op("pe", lambda: nc.tensor.matmul(B[po][:], act[ab][:, fc, st_ * 128:(st_ + 1) * 128],
                                                            wd[ab][:, fc, h * 512:(h + 1) * 512], start=(fc == 0), stop=(fc == 7)),
                             reads=[r_act[ab], r_wd[ab]], writes=[rB[po]], acc=(fc > 0))
                    c.op("act", lambda: nc.scalar.copy(osb[ab][:, st_, h * 512:(h + 1) * 512], B[po][:]), reads=[rB[po]], writes=[r_osb[ab]],
                         acc=(st_ + h > 0))

        def scatter2(e0, e1):
            nonlocal sc_i
            for i in range(NTL):
                for h in range(2):
                    sc = 4 + (sc_i % 2); sc_i += 1
                    idx = 0
                    for ee in (e0, e1):
                        ab = ee % 2
                        for st_ in range(2):
                            c.op("pe", lambda: nc.tensor.matmul(B[sc][:], GT[ab][:, st_, i * 128:(i + 1) * 128], osb[ab][:, st_, h * 512:(h + 1) * 512],
                                                                start=(idx == 0), stop=(idx == 3)),
                                 reads=[r_GT[ab], r_osb[ab]], writes=[rB[sc]], acc=(idx > 0))
                            idx += 1
                    c.op("dve", lambda: nc.vector.tensor_tensor(yacc[:, i, h * 512:(h + 1) * 512], yacc[:, i, h * 512:(h + 1) * 512], B[sc][:], ALU.add),
                         reads=[rB[sc], r_yacc[i]], writes=[r_yacc[i]])

        build(0)
        transposes(0)
        gather(0)
        for e in range(n_exp):
            q = blk * n_exp + e
            if e + 1 < n_exp:
                issue_wd(q + 1)
                build(e + 1)
            up(e)
            if e + 1 < n_exp:
                gather(e + 1)
            down(e)
            if e % 2 == 1:
                scatter2(e - 1, e)
            if e + 1 < n_exp:
                transposes(e + 1)
        for i in range(NTL):
            b2 = i % 2
            layer_norm(yacc[:, i, :], r_yacc[i], ot[b2][:], r_ot[b2], sm[b2], r_sm[b2], 2, 3)
            c.dma("sp", xo[t0 + i * 128:t0 + (i + 1) * 128, :], ot[b2][:], reads=[r_ot[b2]], writes=[r_out[b2]])
    c.finish("sp", r_out)
    print("F: ins", c.n_ins, "waits", c.n_wait, "sbuf left", nc.sbuf_bytes_remaining)


GN_EPS = 1e-6


def me_host_consts(T):
    pos = np.arange(T, dtype=np.float32)
    inv = (10000.0 ** (-np.arange(0, 64, 2, dtype=np.float32) / 64)).astype(np.float32)
    ang = pos[:, None] * inv[None, :]
    cos = np.cos(ang).astype(np.float32).T
    sin = np.sin(ang).astype(np.float32).T
    cosF = np.concatenate([cos, cos], 0)
    sinS = np.concatenate([-sin, sin], 0)
    s8 = np.float32(0.125)
    cq = np.concatenate([cosF, cosF * s8], 0)
    sq = np.concatenate([sinS, sinS * s8], 0)
    ck = np.concatenate([cosF * s8, cosF], 0)
    sk = np.concatenate([sinS * s8, sinS], 0)
    tab = np.ascontiguousarray(np.stack([cq, sq, ck, sk], 0)).astype(np.float32)
    i = np.arange(128, dtype=np.float32)
    m = i[:, None]; c = i[None, :]
    cst = np.zeros((128, 6, 128), np.float32)
    cst[:, 0, :] = np.maximum(c - m, 0)
    cst[:, 1, :] = (c >= m)
    cst[:, 2, :] = np.maximum(m - c, 0)
    cst[:, 3, :] = (m > c)
    cst[:, 4, :] = c + 1.0
    cst[:, 5, :] = 128.0 - c
    col = np.zeros((128, 4), np.float32)
    col[:, 0] = 127.0 - i
    col[:, 1] = i
    col[:, 2] = 128.0
    o = np.arange(17)[None, :, None]
    delta = (o - 8) * 128 + m[:, None, :].astype(np.int64) - c[None, :, :].astype(np.int64)
    delta = delta.astype(np.int64)
    ad = np.abs(delta)
    mm = (ad <= 64).astype(np.float32) + ((delta % 4 == 0) & (ad <= 256)) + ((delta % 16 == 0) & (ad <= 1024))
    mm = mm.astype(np.float32).reshape(128, 17 * 128)
    return dict(tab=tab, cst=cst.reshape(128, 768), col=col, mm=mm, idn=np.eye(128, dtype=np.float32))


def me_weight_cols(head):
    H = 64
    base = lambda blk: blk * 512 + head * H
    rng = lambda b: list(range(base(b), base(b) + H))
    sw = lambda b: list(range(base(b) + 32, base(b) + 64)) + list(range(base(b), base(b) + 32))
    cols = []
    cols += rng(0) + rng(5)
    cols += sw(0) + sw(5)
    cols += rng(1) + rng(6)
    cols += sw(1) + sw(6)
    cols += rng(2) + rng(7)
    cols += rng(3) + rng(4)
    return np.array(cols)


def build_ME(T=16384):
    nc = bass.Bass("TRN2", target_bir_lowering=False)
    c = Ctx(nc)
    xT = nc.dram_tensor("xT", [1024, T], F32, kind="ExternalInput").ap()
    wsel = nc.dram_tensor("wsel", [1024, 768], F32, kind="ExternalInput").ap()
    dec = nc.dram_tensor("dec", [1, 2], F32, kind="ExternalInput").ap()
    tab = nc.dram_tensor("tab", [4, 128, T], F32, kind="ExternalInput").ap()
    cst = nc.dram_tensor("cst", [128, 768], F32, kind="ExternalInput").ap()
    col = nc.dram_tensor("col", [128, 4], F32, kind="ExternalInput").ap()
    mm = nc.dram_tensor("mm", [128, 17 * 128], F32, kind="ExternalInput").ap()
    idn = nc.dram_tensor("idn", [128, 128], F32, kind="ExternalInput").ap()
    y = nc.dram_tensor("y", [T, 128], F32, kind="ExternalOutput").ap()
    emit_ME(nc, c, xT, wsel, dec, tab, cst, col, mm, idn, y, T)
    return nc


def emit_ME(nc, c, xT, wsel, dec, tab, cst, col, mm, idn, y, T, pfx="E"):
    NCH = T // 128
    sb = lambda n, s, d: c.sb(pfx + n, s, d)
    V = nc.vector
    A = nc.scalar
    identb = sb("identb", [128, 128], BF16); r_identb = Res("identb")
    c.dma("pool", identb[:], idn, writes=[r_identb])
    wb = sb("wb", [128, 8, 768], BF16); r_wb = Res("wb")
    c.dma("pool", wb[:], wsel.rearrange("(c p) e -> p c e", p=128), writes=[r_wb])
    mmb = sb("mmb", [128, 17 * 128], BF16); r_mm = Res("mm")
    c.dma("pool", mmb[:], mm, writes=[r_mm])
    cs = sb("cs", [128, 6, 128], F32); r_cs = Res("cs")
    c.dma("sp", cs[:].rearrange("p a b -> p (a b)"), cst, writes=[r_cs])
    cl = sb("cl", [128, 4], F32); r_cl = Res("cl")
    c.dma("sp", cl[:], col, writes=[r_cl])
    dl = sb("dl", [128, 16], F32); r_dl = Res("dl")
    c.dma("sp", dl[:, 0:2], dec.partition_broadcast(128), writes=[r_dl])
    ops = [
        lambda: V.tensor_scalar(dl[:, 2:4], dl[:, 0:2], -1.0, None, ALU.mult),
        lambda: V.tensor_tensor(dl[:, 2:4], dl[:, 2:4], dl[:, 0:2], ALU.max),
    ]
    for f in ops:
        c.op("dve", f, reads=[r_dl], writes=[r_dl])
    c.op("act", lambda: A.activation(dl[:, 2:4], dl[:, 2:4], AF.Exp, scale=-1.0), reads=[r_dl], writes=[r_dl])
    ops = [
        lambda: V.tensor_scalar(dl[:, 4:6], dl[:, 2:4], 2.0, None, ALU.add),
        lambda: V.reciprocal(dl[:, 4:6], dl[:, 4:6]),
        lambda: V.tensor_tensor(dl[:, 4:6], dl[:, 4:6], dl[:, 2:4], ALU.mult),
        lambda: V.tensor_tensor(dl[:, 6:8], dl[:, 4:6], dl[:, 4:6], ALU.mult),
        lambda: V.tensor_scalar(dl[:, 8:10], dl[:, 6:8], 1.0 / 11, 1.0 / 9, ALU.mult, ALU.add),
        lambda: V.tensor_tensor(dl[:, 8:10], dl[:, 8:10], dl[:, 6:8], ALU.mult),
        lambda: V.tensor_scalar(dl[:, 8:10], dl[:, 8:10], 1.0 / 7, None, ALU.add),
        lambda: V.tensor_tensor(dl[:, 8:10], dl[:, 8:10], dl[:, 6:8], ALU.mult),
        lambda: V.tensor_scalar(dl[:, 8:10], dl[:, 8:10], 1.0 / 5, None, ALU.add),
        lambda: V.tensor_tensor(dl[:, 8:10], dl[:, 8:10], dl[:, 6:8], ALU.mult),
        lambda: V.tensor_scalar(dl[:, 8:10], dl[:, 8:10], 1.0 / 3, None, ALU.add),
        lambda: V.tensor_tensor(dl[:, 8:10], dl[:, 8:10], dl[:, 6:8], ALU.mult),
        lambda: V.tensor_scalar(dl[:, 8:10], dl[:, 8:10], 1.0, None, ALU.add),
        lambda: V.tensor_tensor(dl[:, 8:10], dl[:, 8:10], dl[:, 4:6], ALU.mult),
        lambda: V.tensor_scalar(dl[:, 10:12], dl[:, 0:2], 0.0, None, ALU.min),
        lambda: V.scalar_tensor_tensor(dl[:, 10:12], dl[:, 8:10], -2.0, dl[:, 10:12], ALU.mult, ALU.add),
    ]
    for f in ops:
        c.op("dve", f, reads=[r_dl], writes=[r_dl])
    lgf = dl[:, 10:11]
    lgb = dl[:, 11:12]
    decT = sb("decT", [128, 2, 128], F32); r_dec = Res("decT")
    xi = sb("xi", [128, 2, 128], F32); r_xi = Res("xi")
    zc = sb("zc", [128, 4], F32); r_zc = Res("zc")
    c.op("act", lambda: A.activation(decT[:, 0, :], cs[:, 0, :], AF.Exp, scale=lgf), reads=[r_cs, r_dl], writes=[r_dec])
    c.op("act", lambda: A.activation(decT[:, 1, :], cs[:, 2, :], AF.Exp, scale=lgb), reads=[r_cs, r_dl], writes=[r_dec])
    c.op("dve", lambda: V.tensor_tensor(decT[:, 0, :], decT[:, 0, :], cs[:, 1, :], ALU.mult), reads=[r_dec, r_cs], writes=[r_dec])
    c.op("dve", lambda: V.tensor_tensor(decT[:, 1, :], decT[:, 1, :], cs[:, 3, :], ALU.mult), reads=[r_dec, r_cs], writes=[r_dec])
    c.op("act", lambda: A.activation(xi[:, 0, :], cs[:, 4, :], AF.Exp, scale=lgf), reads=[r_cs, r_dl], writes=[r_xi])
    c.op("act", lambda: A.activation(xi[:, 1, :], cs[:, 5, :], AF.Exp, scale=lgb), reads=[r_cs, r_dl], writes=[r_xi])
    c.op("act", lambda: A.activation(zc[:, 0:1], cl[:, 0:1], AF.Exp, scale=lgf), reads=[r_cl, r_dl], writes=[r_zc])
    c.op("act", lambda: A.activation(zc[:, 1:2], cl[:, 1:2], AF.Exp, scale=lgb), reads=[r_cl, r_dl], writes=[r_zc])
    c.op("act", lambda: A.activation(zc[:, 2:3], cl[:, 2:3], AF.Exp, scale=lgf), reads=[r_cl, r_dl], writes=[r_zc])
    c.op("act", lambda: A.activation(zc[:, 3:4], cl[:, 2:3], AF.Exp, scale=lgb), reads=[r_cl, r_dl], writes=[r_zc])
    epsT = sb("epsT", [128, 1], F32); r_eps = Res("eps")
    c.op("dve", lambda: V.memset(epsT[:], GN_EPS), writes=[r_eps])

    qkT = sb("qkT", [128, 2, T], BF16)
    r_qk = [Res("qk%d" % n) for n in range(NCH)]
    vall = sb("vall", [128, NCH, 64], BF16); r_v = [Res("v%d" % n) for n in range(NCH)]
    dvall = sb("dvall", [128, NCH, 66], BF16); r_dv = [Res("dv%d" % n) for n in range(NCH)]
    Rf = sb("Rf", [64, NCH + 1, 64], BF16); r_Rf = [Res("Rf%d" % n) for n in range(NCH + 1)]
    Rrun = sb("Rrun", [64, 2, 64], F32); r_Rrun = [Res("Rrun0"), Res("Rrun1")]
    Rbb = [sb("Rbb%d" % i, [64, 64], BF16) for i in range(2)]; r_Rbb = [Res("Rbb0"), Res("Rbb1")]
    ones_dst = Res("dvones")
    c.op("pool", lambda: nc.gpsimd.memset(dvall[:, :, 64:66], 1.0), writes=[ones_dst])
    c.op("dve", lambda: V.memset(Rrun[:], 0.0), writes=r_Rrun)
    c.op("dve", lambda: V.memset(Rf[:, 0, :], 0.0), writes=[r_Rf[0]])
    c.op("dve", lambda: V.memset(Rbb[0][:], 0.0), writes=[r_Rbb[0]])

    NB = 2
    xTt = [sb("xTt%d" % i, [128, 8, 128], BF16) for i in range(NB)]; r_xTt = [Res("xTt%d" % i) for i in range(NB)]
    tbt = [sb("tbt%d" % i, [128, 4, 128], F32) for i in range(NB)]; r_tbt = [Res("tbt%d" % i) for i in range(NB)]
    prod = [sb("prod%d" % i, [128, 4, 128], F32) for i in range(NB)]; r_prod = [Res("prod%d" % i) for i in range(NB)]
    kz = [sb("kz%d" % i, [128, 64], BF16) for i in range(NB)]; r_kz = [Res("kz%d" % i) for i in range(NB)]
    scT = [sb("scT%d" % i, [128, 2, 128], BF16) for i in range(NB)]; r_scT = [Res("scT%d" % i) for i in range(NB)]
    qxi = [sb("qxi%d" % i, [64, 2, 128], BF16) for i in range(NB)]; r_qxi = [Res("qxi%d" % i) for i in range(NB)]
    st = [sb("st%d" % i, [128, 32], F32) for i in range(NB)]; r_st = [Res("st%d" % i) for i in range(NB)]
    xn = [sb("xn%d" % i, [128, 128], F32) for i in range(NB)]; r_xn = [Res("xn%d" % i) for i in range(NB)]
    sg = [sb("sg%d" % i, [128, 128], F32) for i in range(NB)]; r_sg = [Res("sg%d" % i) for i in range(NB)]
    yt = [sb("yt%d" % i, [128, 128], F32) for i in range(NB)]; r_yt = [Res("yt%d" % i) for i in range(NB)]
    pd = [sb("pd%d" % i, [128, 512], BF16) for i in range(3)]; r_pd = [Res("pd%d" % i) for i in range(3)]
    r_y = [Res("y0"), Res("y1")]
    PA = c.ps(pfx + "PA", [128, 512], F32); r_PA = Res("PA")
    PV = c.ps(pfx + "PV", [128, 512], F32); r_PV = Res("PV")
    PT = c.ps(pfx + "PT", [128, 1024], BF16); r_PT = Res("PT")
    PS = c.ps(pfx + "PS", [128, 512], F32); r_PS = Res("PS")
    PO = c.ps(pfx + "PO", [128, 512], F32); r_PO = Res("PO")
    PD = [c.ps(pfx + "PD%d" % i, [128, 512], F32) for i in range(3)]; r_PD = [Res("PD%d" % i) for i in range(3)]

    def load_x(n, want_tab):
        b = n % NB
        c.dma("pool", xTt[b][:], xT[:, n * 128:(n + 1) * 128].rearrange("(c p) t -> p c t", p=128), writes=[r_xTt[b]])
        if want_tab:
            c.dma("sp", tbt[b][:], tab[:, :, n * 128:(n + 1) * 128].rearrange("a p t -> p a t"), writes=[r_tbt[b]])

    def ktrans_state(n, direction, b):
        c.op("pe", lambda: nc.tensor.transpose(PT[:, 0:64], qkT[0:64, 1, n * 128:(n + 1) * 128], identb[0:64, 0:64]),
             reads=[r_qk[n], r_identb], writes=[r_PT])
        c.op("dve", lambda: V.tensor_scalar(kz[b][:], PT[:, 0:64], zc[:, direction:direction + 1], None, ALU.mult),
             reads=[r_PT, r_zc], writes=[r_kz[b]])
        c.op("pe", lambda: nc.tensor.matmul(PS[0:64, 0:64], kz[b][:], vall[:, n, :], start=True, stop=True),
             reads=[r_kz[b], r_v[n]], writes=[r_PS])

    load_x(0, True)
    for n in range(NCH):
        b = n % NB
        if n + 1 < NCH:
            load_x(n + 1, True)
        for g in range(4):
            for k in range(8):
                c.op("pe", lambda: nc.tensor.matmul(PA[:, g * 128:(g + 1) * 128], wb[:, k, g * 128:(g + 1) * 128], xTt[b][:, k, :],
                                                    start=(k == 0), stop=(k == 7)),
                     reads=[r_wb, r_xTt[b]], writes=[r_PA], acc=(g + k > 0))
        for k in range(8):
            c.op("pe", lambda: nc.tensor.matmul(PV[:, 0:128], xTt[b][:, k, :], wb[:, k, 512:640], start=(k == 0), stop=(k == 7)),
                 reads=[r_wb, r_xTt[b]], writes=[r_PV], acc=(k > 0))
        c.op("dve", lambda: V.tensor_tensor(prod[b][:], PA[:].rearrange("p (a t) -> p a t", a=4), tbt[b][:], ALU.mult),
             reads=[r_PA, r_tbt[b]], writes=[r_prod[b]])
        c.op("dve", lambda: V.tensor_tensor(qkT[:, :, n * 128:(n + 1) * 128], prod[b][:, 0:4:2, :], prod[b][:, 1:4:2, :], ALU.add),
             reads=[r_prod[b]], writes=[r_qk[n]])
        c.op("act", lambda: A.copy(vall[:, n, :], PV[:, 0:64]), reads=[r_PV], writes=[r_v[n]])
        c.op("act", lambda: A.copy(dvall[:, n, 0:64], PV[:, 64:128]), reads=[r_PV, ones_dst], writes=[r_dv[n]])
        ktrans_state(n, 0, b)
        c.op("dve", lambda: V.scalar_tensor_tensor(Rrun[:, 0, :], Rrun[:, 0, :], zc[0:64, 2:3], PS[0:64, 0:64], ALU.mult, ALU.add),
             reads=[r_Rrun[0], r_zc, r_PS], writes=[r_Rrun[0]])
        c.op("act", lambda: A.copy(Rf[:, n + 1, :], Rrun[:, 0, :]), reads=[r_Rrun[0]], writes=[r_Rf[n + 1]])

    load_x(NCH - 1, False)

    def ret_steps(n):
        b = n % NB
        rb_cur = (NCH - 1 - n) % 2
        sl = slice(n * 128, (n + 1) * 128)
        s = st[b]; rs = r_st[b]

        def r1():
            for k in range(8):
                c.op("pe", lambda: nc.tensor.matmul(PV[:, 0:128], xTt[b][:, k, :], wb[:, k, 640:768], start=(k == 0), stop=(k == 7)),
                     reads=[r_wb, r_xTt[b]], writes=[r_PV], acc=(k > 0))
            c.op("pe", lambda: nc.tensor.matmul(PS[:, 128:256], qkT[0:64, 1, sl], qkT[0:64, 0, sl], start=True, stop=True),
                 reads=[r_qk[n]], writes=[r_PS])

        def r2():
            c.op("dve", lambda: V.tensor_tensor(scT[b][:], PS[:, 128:256].unsqueeze(1).broadcast_to([128, 2, 128]), decT[:], ALU.mult),
                 reads=[r_PS, r_dec], writes=[r_scT[b]])
            c.op("dve", lambda: V.tensor_tensor(qxi[b][:], qkT[0:64, 0, sl].unsqueeze(1).broadcast_to([64, 2, 128]), xi[0:64], ALU.mult),
                 reads=[r_qk[n], r_xi], writes=[r_qxi[b]])
            c.op("act", lambda: A.activation(sg[b][:], PV[:, 0:128], AF.Exp, scale=-1.0), reads=[r_PV], writes=[r_sg[b]])

        def r3():
            c.op("pe", lambda: nc.tensor.matmul(PO[:, 0:64], scT[b][:, 0, :], vall[:, n, :], start=True, stop=False),
                 reads=[r_scT[b], r_v[n]], writes=[r_PO])
            c.op("pe", lambda: nc.tensor.matmul(PO[:, 0:64], qxi[b][:, 0, :], Rf[:, n, :], start=False, stop=True),
                 reads=[r_qxi[b], r_Rf[n]], writes=[r_PO], acc=True)
            c.op("pe", lambda: nc.tensor.matmul(PO[:, 64:128], scT[b][:, 1, :], vall[:, n, :], start=True, stop=False),
                 reads=[r_scT[b], r_v[n]], writes=[r_PO], acc=True)
            c.op("pe", lambda: nc.tensor.matmul(PO[:, 64:128], qxi[b][:, 1, :], Rbb[rb_cur][:], start=False, stop=True),
                 reads=[r_qxi[b], r_Rbb[rb_cur]], writes=[r_PO], acc=True)

        def r4():
            if n > 0:
                ktrans_state(n, 1, b)
                c.op("dve", lambda: V.scalar_tensor_tensor(Rrun[:, 1, :], Rrun[:, 1, :], zc[0:64, 3:4], PS[0:64, 0:64], ALU.mult, ALU.add),
                     reads=[r_Rrun[1], r_zc, r_PS], writes=[r_Rrun[1]])
                c.op("act", lambda: A.copy(Rbb[1 - rb_cur][:], Rrun[:, 1, :]), reads=[r_Rrun[1]], writes=[r_Rbb[1 - rb_cur]])

        def r5():
            c.op("dve", lambda: V.bn_stats(s[:, 0:6], PO[:, 0:64]), reads=[r_PO], writes=[rs])
            c.op("dve", lambda: V.bn_stats(s[:, 6:12], PO[:, 64:128]), reads=[r_PO], writes=[rs])
            c.op("dve", lambda: V.bn_aggr(s[:, 12:14], s[:, 0:6]), reads=[rs], writes=[rs])
            c.op("dve", lambda: V.bn_aggr(s[:, 14:16], s[:, 6:12]), reads=[rs], writes=[rs])
            c.op("act", lambda: A.activation(s[:, 16:18], s[:, 13:16:2], AF.Ln, bias=epsT[:, 0:1], scale=1.0), reads=[rs, r_eps], writes=[rs])
            c.op("act", lambda: A.activation(s[:, 16:18], s[:, 16:18], AF.Exp, scale=-0.5), reads=[rs], writes=[rs])
            c.op("dve", lambda: V.tensor_scalar(sg[b][:], sg[b][:], 1.0, None, ALU.add), reads=[r_sg[b]], writes=[r_sg[b]])
            c.op("dve", lambda: V.reciprocal(sg[b][:], sg[b][:]), reads=[r_sg[b]], writes=[r_sg[b]])
            c.op("dve", lambda: V.tensor_tensor(sg[b][:], sg[b][:], PV[:, 0:128], ALU.mult), reads=[r_sg[b], r_PV], writes=[r_sg[b]])

        def r6():
            c.op("dve", lambda: V.tensor_scalar(xn[b][:, 0:64], PO[:, 0:64], s[:, 12:13], s[:, 16:17], ALU.subtract, ALU.mult),
                 reads=[r_PO, rs], writes=[r_xn[b]])
            c.op("dve", lambda: V.tensor_scalar(xn[b][:, 64:128], PO[:, 64:128], s[:, 14:15], s[:, 17:18], ALU.subtract, ALU.mult),
                 reads=[r_PO, rs], writes=[r_xn[b]])
            c.op("pool", lambda: nc.gpsimd.tensor_tensor(xn[b][:], xn[b][:], sg[b][:], ALU.mult), reads=[r_xn[b], r_sg[b]], writes=[r_xn[b]])
            c.op("pool", lambda: nc.gpsimd.tensor_tensor(yt[b][:, 0:64], xn[b][:, 0:64], xn[b][:, 64:128], ALU.add),
                 reads=[r_xn[b]], writes=[r_yt[b]])
        return [r1, r2, r3, r4, r5, r6]

    def dil_steps(n):
        b = n % NB
        sl = slice(n * 128, (n + 1) * 128)
        s2 = st[b]; rs = r_st[b]
        kts = [kt for kt in range(n - 8, n + 9) if 0 <= kt < NCH]
        groups = [kts[i:i + 4] for i in range(0, len(kts), 4)]
        G = len(groups)

        def qk(gi):
            grp = groups[gi]; pb = gi % 3
            for j, kt in enumerate(grp):
                c.op("pe", lambda: nc.tensor.matmul(PD[pb][:, j * 128:(j + 1) * 128], qkT[64:128, 1, kt * 128:(kt + 1) * 128],
                                                    qkT[64:128, 0, sl], start=True, stop=True),
                     reads=[r_qk[kt], r_qk[n]], writes=[r_PD[pb]], acc=(j > 0))
            w = len(grp) * 128
            o0 = (grp[0] - n + 8) * 128
            c.op("act", lambda: A.activation(pd[pb][:, 0:w], PD[pb][:, 0:w], AF.Exp), reads=[r_PD[pb]], writes=[r_pd[pb]])
            c.op("pool", lambda: nc.gpsimd.tensor_tensor(pd[pb][:, 0:w], pd[pb][:, 0:w], mmb[:, o0:o0 + w], ALU.mult),
                 reads=[r_pd[pb], r_mm], writes=[r_pd[pb]])

        def pv(gi):
            grp = groups[gi]; pb = gi % 3
            for j, kt in enumerate(grp):
                first = (gi == 0 and j == 0)
                last = (gi == G - 1 and j == len(grp) - 1)
                c.op("pe", lambda: nc.tensor.matmul(PA[:, 0:65], pd[pb][:, j * 128:(j + 1) * 128], dvall[:, kt, 0:65],
                                                    start=first, stop=last),
                     reads=[r_pd[pb], r_dv[kt]], writes=[r_PA], acc=(not first))

        def fin():
            c.op("dve", lambda: V.reciprocal(s2[:, 20:21], PA[:, 64:65]), reads=[r_PA, rs], writes=[rs])
            c.op("dve", lambda: V.tensor_scalar(yt[b][:, 64:128], PA[:, 0:64], s2[:, 20:21], None, ALU.mult),
                 reads=[r_PA, rs], writes=[r_yt[b]])
        steps = []
        from functools import partial
        steps.append(partial(qk, 0))
        if G > 1:
            steps.append(partial(qk, 1))
        for k in range(G):
            if k + 2 < G:
                steps.append(partial(qk, k + 2))
            steps.append(partial(pv, k))
        steps.append(fin)
        return steps

    for n in range(NCH - 1, -1, -1):
        b = n % NB
        if n - 1 >= 0:
            load_x(n - 1, False)
        rs_ = ret_steps(n)
        ds_ = dil_steps(n)
        i = j = 0
        while i < len(ds_) or j < len(rs_):
            if i < len(ds_):
                ds_[i](); i += 1
            if j < len(rs_):
                rs_[j](); j += 1
        c.dma("sp", y[n * 128:(n + 1) * 128, :], yt[b][:], reads=[r_yt[b]], writes=[r_y[b]])
    c.finish("sp", r_y)
    print("ME: ins", c.n_ins, "waits", c.n_wait, "sbuf left", nc.sbuf_bytes_remaining)


NEG = -30000.0


def mo_patterns(NCH):
    pats = []
    for o in range(5):
        pats.append((4, 4 + o - 2))
    for n in (0, 1, NCH - 2, NCH - 1):
        kts = range(0, 4) if n < 2 else range(NCH - 4, NCH)
        for kt in kts:
            pats.append((n, kt))
    return pats


def mo_keys(n, NCH):
    if 2 <= n <= NCH - 3:
        return [(n + o - 2, o) for o in range(5)]
    idx = {0: 0, 1: 1, NCH - 2: 2, NCH - 1: 3}[n]
    kts = range(0, 4) if n < 2 else range(NCH - 4, NCH)
    return [(kt, 5 + idx * 4 + j) for j, kt in enumerate(kts)]


def mo_host_bias(rpb2, T):
    NCH = T // 128
    rows = T // 64
    pats = mo_patterns(NCH)
    m = np.arange(128)[:, None]
    c = np.arange(128)[None, :]
    out = np.full((128, 2, len(pats), 128), NEG, np.float32)
    for pi, (n, kt) in enumerate(pats):
        rm = 2 * kt + m // 64; wm = m % 64
        rc = 2 * n + c // 64; wc = c % 64
        r0 = np.clip(rc - 4, 0, rows - 8)
        c0 = np.clip(wc - 8, 0, 64 - 16)
        valid = (rm >= r0) & (rm < r0 + 8) & (wm >= c0) & (wm < c0 + 16)
        ro = np.clip(rm - rc + 7, 0, 14)
        co = np.clip(wm - wc + 15, 0, 30)
        for h in range(2):
            g = rpb2[h][ro, co]
            out[:, h, pi, :] = np.where(valid, g, np.float32(NEG))
    return out.reshape(128, 2 * len(pats) * 128)


def mo_weight_cols(core):
    h0, h1 = 2 * core, 2 * core + 1
    rng = lambda blk, h: list(range(blk * 1024 + h * 64, blk * 1024 + (h + 1) * 64))
    return np.array(rng(0, h0) + rng(0, h1) + rng(1, h0) + rng(1, h1) + rng(2, h0) + rng(2, h1))


def build_MO(T=16384):
    nc = bass.Bass("TRN2", target_bir_lowering=False)
    c = Ctx(nc)
    xT = nc.dram_tensor("xT", [1024, T], F32, kind="ExternalInput").ap()
    wsel = nc.dram_tensor("wsel", [1024, 384], F32, kind="ExternalInput").ap()
    bias = nc.dram_tensor("bias", [128, 2 * 21 * 128], F32, kind="ExternalInput").ap()
    idn = nc.dram_tensor("idn", [128, 128], F32, kind="ExternalInput").ap()
    y = nc.dram_tensor("y", [T, 128], F32, kind="ExternalOutput").ap()
    emit_MO(nc, c, xT, wsel, bias, idn, y, T)
    return nc


def emit_MO(nc, c, xT, wsel, bias, idn, y, T, pfx="O"):
    NCH = T // 128
    sb = lambda n, s, d: c.sb(pfx + n, s, d)
    V = nc.vector
    A = nc.scalar
    identb = sb("identb", [128, 128], BF16); r_identb = Res("identb")
    c.dma("pool", identb[:], idn, writes=[r_identb])
    wb = sb("wb", [128, 8, 384], BF16); r_wb = Res("wb")
    c.dma("pool", wb[:], wsel.rearrange("(c p) e -> p c e", p=128), writes=[r_wb])
    biasb = sb("biasb", [128, 2, 21, 128], BF16); r_bias = Res("bias")
    c.dma("pool", biasb[:].rearrange("p a b c -> p (a b c)"), bias, writes=[r_bias])

    qkT = sb("qkT", [128, 2, T], BF16); r_qk = [Res("qk%d" % n) for n in range(NCH)]
    vall = sb("vall", [128, NCH, 2, 66], BF16); r_v = [Res("v%d" % n) for n in range(NCH)]
    ones_dst = Res("ones")
    c.op("pool", lambda: nc.gpsimd.memset(vall[:, :, :, 64:66], 1.0), writes=[ones_dst])
    NB = 2
    xTt = [sb("xTt%d" % i, [128, 8, 128], BF16) for i in range(NB)]; r_xTt = [Res("xTt%d" % i) for i in range(NB)]
    pd = [sb("pd%d" % i, [128, 640], BF16) for i in range(3)]; r_pd = [Res("pd%d" % i) for i in range(3)]
    yt = [sb("yt%d" % i, [128, 128], F32) for i in range(NB)]; r_yt = [Res("yt%d" % i) for i in range(NB)]
    st = [sb("st%d" % i, [128, 8], F32) for i in range(NB)]; r_st = [Res("st%d" % i) for i in range(NB)]
    r_y = [Res("y0"), Res("y1")]
    PA = c.ps(pfx + "PA", [128, 512], F32); r_PA = Res("PA")
    PV = c.ps(pfx + "PV", [128, 512], F32); r_PV = Res("PV")
    PO = c.ps(pfx + "PO", [128, 512], F32); r_PO = Res("PO")
    PD = [c.ps(pfx + "PD%d" % i, [128, 1024], F32) for i in range(2)]; r_PD = [Res("PD%d" % i) for i in range(2)]

    def load_x(n):
        b = n % NB
        c.dma("pool", xTt[b][:], xT[:, n * 128:(n + 1) * 128].rearrange("(c p) t -> p c t", p=128), writes=[r_xTt[b]])

    load_x(0)
    for n in range(NCH):
        b = n % NB
        if n + 1 < NCH:
            load_x(n + 1)
        for g in range(2):
            for k in range(8):
                c.op("pe", lambda: nc.tensor.matmul(PA[:, g * 128:(g + 1) * 128], wb[:, k, g * 128:(g + 1) * 128], xTt[b][:, k, :],
                                                    start=(k == 0), stop=(k == 7)),
                     reads=[r_wb, r_xTt[b]], writes=[r_PA], acc=(g + k > 0))
        for k in range(8):
            c.op("pe", lambda: nc.tensor.matmul(PV[:, 0:128], xTt[b][:, k, :], wb[:, k, 256:384], start=(k == 0), stop=(k == 7)),
                 reads=[r_wb, r_xTt[b]], writes=[r_PV], acc=(k > 0))
        c.op("act", lambda: A.activation(qkT[:, 0, n * 128:(n + 1) * 128], PA[:, 0:128], AF.Copy, scale=0.125),
             reads=[r_PA], writes=[r_qk[n]])
        c.op("dve", lambda: V.tensor_copy(qkT[:, 1, n * 128:(n + 1) * 128], PA[:, 128:256]), reads=[r_PA], writes=[r_qk[n]])
        c.op("dve", lambda: V.tensor_copy(vall[:, n, :, 0:64], PV[:, 0:128].rearrange("p (h d) -> p h d", h=2)),
             reads=[r_PV, ones_dst], writes=[r_v[n]])

    POs = [PO, PA]; r_POs = [r_PO, r_PA]
    units = [(n, hh) for n in range(NCH) for hh in range(2)]

    def qk(ui):
        n, hh = units[ui]
        sl = slice(n * 128, (n + 1) * 128)
        keys = mo_keys(n, NCH)
        hp = slice(hh * 64, (hh + 1) * 64)
        pb = ui % 2; sbi = ui % 3
        for j, (kt, pat) in enumerate(keys):
            c.op("pe", lambda: nc.tensor.matmul(PD[pb][:, j * 128:(j + 1) * 128], qkT[hp, 1, kt * 128:(kt + 1) * 128],
                                                qkT[hp, 0, sl], start=True, stop=False),
                 reads=[r_qk[kt], r_qk[n]], writes=[r_PD[pb]], acc=(j > 0))
            c.op("pe", lambda: nc.tensor.matmul(PD[pb][:, j * 128:(j + 1) * 128], identb[:], biasb[:, hh, pat, :],
                                                start=False, stop=True),
                 reads=[r_identb, r_bias], writes=[r_PD[pb]], acc=True)
        nk = len(keys)
        w0 = min(nk, 4) * 128
        c.op("act", lambda: A.activation(pd[sbi][:, 0:w0], PD[pb][:, 0:w0], AF.Exp), reads=[r_PD[pb]], writes=[r_pd[sbi]])
        if nk > 4:
            c.op("act", lambda: A.activation(pd[sbi][:, 512:640], PD[pb][:, 512:640], AF.Exp), reads=[r_PD[pb]], writes=[r_pd[sbi]], acc=True)

    def pv(ui):
        n, hh = units[ui]
        keys = mo_keys(n, NCH)
        nk = len(keys)
        sbi = ui % 3
        po = POs[n % 2]; r_po = r_POs[n % 2]
        for j, (kt, pat) in enumerate(keys):
            c.op("pe", lambda: nc.tensor.matmul(po[:, hh * 128:hh * 128 + 65], pd[sbi][:, j * 128:(j + 1) * 128], vall[:, kt, hh, 0:65],
                                                start=(j == 0), stop=(j == nk - 1)),
                 reads=[r_pd[sbi], r_v[kt]], writes=[r_po], acc=(hh + j > 0))

    def fin(n):
        b = n % NB
        s = st[b]; rs = r_st[b]
        po = POs[n % 2]; r_po = r_POs[n % 2]
        for hh in range(2):
            c.op("dve", lambda: V.reciprocal(s[:, hh:hh + 1], po[:, hh * 128 + 64:hh * 128 + 65]), reads=[r_po, rs], writes=[rs])
            c.op("dve", lambda: V.tensor_scalar(yt[b][:, hh * 64:(hh + 1) * 64], po[:, hh * 128:hh * 128 + 64], s[:, hh:hh + 1], None, ALU.mult),
                 reads=[r_po, rs], writes=[r_yt[b]])
        c.dma("sp", y[n * 128:(n + 1) * 128, :], yt[b][:], reads=[r_yt[b]], writes=[r_y[b]])

    qk(0)
    for ui in range(len(units)):
        if ui + 1 < len(units):
            qk(ui + 1)
        pv(ui)
        if units[ui][1] == 1:
            fin(units[ui][0])
    c.finish("sp", r_y)
    print("MO: ins", c.n_ins, "waits", c.n_wait, "sbuf left", nc.sbuf_bytes_remaining)


N_CORES = 8
T_SEQ = 16384
_PROGS = {}


def _prog(name):
    if name not in _PROGS:
        _PROGS[name] = {"ME": lambda: build_ME(T_SEQ), "MO": lambda: build_MO(T_SEQ), "F": lambda: build_F2(32, 2)}[name]()
    return _PROGS[name]


def _launch(nc, in_maps):
    res = run_bass_kernel_spmd(nc, in_maps, core_ids=list(range(N_CORES)))
    return res.results


def kernel(x, ab_w_in, ab_w_out, ret_decay, c_w_in, c_w_out, c_rpb, ln_g, ln_b,
           router_w, router_b, exp_w_up, exp_b_up, exp_w_down, exp_b_down):
    f32 = np.float32
    xs = np.ascontiguousarray(np.asarray(x, f32)[0])
    T = xs.shape[0]
    idn = np.eye(128, dtype=f32)
    ii = np.arange(128)
    fcst = np.ascontiguousarray(np.concatenate([np.tile(np.arange(256, dtype=f32)[None, :], (128, 1)),
                                                (ii[:, None] < ii[None, :]).astype(f32), np.ones((128, 128), f32)], 1))
    me_c = me_host_consts(T)
    per = T // N_CORES
    for layer in range(4):
        j = layer // 2
        xT = np.ascontiguousarray(xs.T)
        ycat = np.empty((T, 1024), f32)
        if layer % 2 == 0:
            w_in = np.asarray(ab_w_in[j], f32)
            dec = np.asarray(ret_decay[j], f32)
            in_maps = [dict(xT=xT, wsel=np.ascontiguousarray(w_in[:, me_weight_cols(h)]),
                            dec=np.ascontiguousarray(dec[:, h][None, :]), **me_c) for h in range(N_CORES)]
            outs = _launch(_prog("ME"), in_maps)
            for h in range(N_CORES):
                yh = outs[h]["y"]
                ycat[:, h * 64:(h + 1) * 64] = yh[:, 0:64]
                ycat[:, 512 + h * 64:512 + (h + 1) * 64] = yh[:, 64:128]
            w_out = np.asarray(ab_w_out[j], f32)
        else:
            w_in = np.asarray(c_w_in[j], f32)
            rpb = np.asarray(c_rpb[j], f32)
            in_maps = [dict(xT=xT, wsel=np.ascontiguousarray(w_in[:, mo_weight_cols(cc)]),
                            bias=mo_host_bias(rpb[2 * cc:2 * cc + 2], T), idn=idn) for cc in range(N_CORES)]
            outs = _launch(_prog("MO"), in_maps)
            for cc in range(N_CORES):
                ycat[:, cc * 128:(cc + 1) * 128] = outs[cc]["y"]
            w_out = np.asarray(c_w_out[j], f32)
        lnp = np.ascontiguousarray(np.stack([ln_g[layer, 0], ln_b[layer, 0], ln_g[layer, 1], ln_b[layer, 1]], 0).astype(f32))
        common = dict(w_out=np.ascontiguousarray(w_out), lnp=lnp,
                      rw=np.ascontiguousarray(np.asarray(router_w[layer], f32)),
                      rb=np.ascontiguousarray(np.asarray(router_b[layer], f32)[None, :]),
                      wup=np.ascontiguousarray(np.asarray(exp_w_up[layer], f32)),
                      bup=np.ascontiguousarray(np.asarray(exp_b_up[layer], f32).reshape(256, 256)),
                      wdn=np.ascontiguousarray(np.asarray(exp_w_down[layer], f32)),
                      bdn=np.ascontiguousarray(np.asarray(exp_b_down[layer], f32)), idn=idn, fcst=fcst)
        in_maps = []
        for cc in range(N_CORES):
            sl = slice(cc * per, (cc + 1) * per)
            in_maps.append(dict(common, x=np.ascontiguousarray(xs[sl]), yT=np.ascontiguousarray(ycat[sl].T)))
        outs = _launch(_prog("F"), in_maps)
        xs = np.concatenate([outs[cc]["xo"] for cc in range(N_CORES)], 0)
    return xs[None].astype(f32)
```

```python
import numpy as np
import concourse.bass as bass
import concourse.mybir as mybir
from concourse.bass_utils import run_bass_kernel_spmd

F32 = mybir.dt.float32
BF16 = mybir.dt.bfloat16
AF = mybir.ActivationFunctionType
ALU = mybir.AluOpType
AX = mybir.AxisListType


class Res:
    __slots__ = ("name", "w", "r", "dsem", "dcnt")

    def __init__(self, name):
        self.name = name
        self.w = None
        self.r = []
        self.dsem = None
        self.dcnt = 0


class Ctx:
    def __init__(self, nc):
        self.nc = nc
        self.eng = {"pe": nc.tensor, "act": nc.scalar, "dve": nc.vector,
                    "pool": nc.gpsimd, "sp": nc.sync}
        self.sem = {k: nc.alloc_semaphore("c_" + k) for k in ("pe", "act", "dve", "pool")}
        self.cnt = {k: 0 for k in self.sem}
        self.seen = {k: {} for k in self.eng}
        self.semobj = dict(self.sem)
        self.n_dsem = 0
        self.n_wait = 0
        self.n_ins = 0

    def sb(self, name, shape, dt):
        return self.nc.alloc_sbuf_tensor(name, list(shape), dt)

    def ps(self, name, shape, dt=F32):
        return self.nc.alloc_psum_tensor(name, list(shape), dt)

    def _dsem(self, res):
        if res.dsem is None:
            key = "d%d" % self.n_dsem
            self.n_dsem += 1
            res.dsem = key
            self.semobj[key] = self.nc.alloc_semaphore(key)
        return res.dsem

    def _wait(self, e, deps):
        seen = self.seen[e]
        eng = self.eng[e]
        best = {}
        for d in deps:
            if d is None:
                continue
            k, v = d
            if best.get(k, 0) < v:
                best[k] = v
        for k, v in best.items():
            if seen.get(k, 0) < v:
                eng.wait_ge(self.semobj[k], v)
                seen[k] = v
                self.n_wait += 1

    def op(self, e, fn, reads=(), writes=(), acc=False):
        deps = [r.w for r in reads]
        if not acc:
            for w in writes:
                deps.append(w.w)
                deps.extend(w.r)
        self._wait(e, deps)
        ins = fn()
        self.cnt[e] += 1
        ins.then_inc(self.sem[e], 1)
        ev = (e, self.cnt[e])
        self.seen[e][e] = max(self.seen[e].get(e, 0), 0)
        for r in reads:
            r.r.append(ev)
        for w in writes:
            w.w = ev
            if not acc:
                w.r = []
        self.n_ins += 1
        return ins

    def dma(self, q, out, in_, reads=(), writes=(), **kw):
        assert len(writes) == 1
        wres = writes[0]
        deps = [r.w for r in reads]
        deps.append(wres.w)
        deps.extend(wres.r)
        self._wait(q, deps)
        key = self._dsem(wres)
        ins = self.eng[q].dma_start(out=out, in_=in_, **kw)
        wres.dcnt += 16
        ins.then_inc(self.semobj[key], 16)
        ev = (key, wres.dcnt)
        for r in reads:
            r.r.append(ev)
        wres.w = ev
        wres.r = []
        self.n_ins += 1
        return ins

    def dma_fn(self, q, fn, reads=(), writes=()):
        wres = writes[0]
        deps = [r.w for r in reads]
        deps.append(wres.w)
        deps.extend(wres.r)
        self._wait(q, deps)
        key = self._dsem(wres)
        ins = fn()
        wres.dcnt += 16
        ins.then_inc(self.semobj[key], 16)
        ev = (key, wres.dcnt)
        for r in reads:
            r.r.append(ev)
        wres.w = ev
        wres.r = []
        self.n_ins += 1
        return ins

    def dma_more(self, q, out, in_, reads=(), writes=(), **kw):
        wres = writes[0]
        deps = [r.w for r in reads]
        self._wait(q, deps)
        key = self._dsem(wres)
        ins = self.eng[q].dma_start(out=out, in_=in_, **kw)
        wres.dcnt += 16
        ins.then_inc(self.semobj[key], 16)
        ev = (key, wres.dcnt)
        for r in reads:
            r.r.append(ev)
        wres.w = ev
        self.n_ins += 1
        return ins

    def finish(self, q, resources):
        self._wait(q, [r.w for r in resources])


ALPHA = (2.0 * 4) ** 0.25
LN_EPS = 1e-5


def build_F2(n_exp=32, n_blk=2):
    nc = bass.Bass("TRN2", target_bir_lowering=False)
    c = Ctx(nc)
    TB = 1024
    NT = TB * n_blk
    D = 1024
    x = nc.dram_tensor("x", [NT, D], F32, kind="ExternalInput").ap()
    yT = nc.dram_tensor("yT", [D, NT], F32, kind="ExternalInput").ap()
    w_out = nc.dram_tensor("w_out", [D, D], F32, kind="ExternalInput").ap()
    lnp = nc.dram_tensor("lnp", [4, D], F32, kind="ExternalInput").ap()
    rw = nc.dram_tensor("rw", [D, 32], F32, kind="ExternalInput").ap()
    rb = nc.dram_tensor("rb", [1, 32], F32, kind="ExternalInput").ap()
    wup = nc.dram_tensor("wup", [n_exp, D, 2 * D], F32, kind="ExternalInput").ap()
    bup = nc.dram_tensor("bup", [256, 256], F32, kind="ExternalInput").ap()
    wdn = nc.dram_tensor("wdn", [n_exp, D, D], F32, kind="ExternalInput").ap()
    bdn = nc.dram_tensor("bdn", [32, D], F32, kind="ExternalInput").ap()
    idn = nc.dram_tensor("idn", [128, 128], F32, kind="ExternalInput").ap()
    fcst = nc.dram_tensor("fcst", [128, 512], F32, kind="ExternalInput").ap()
    xo = nc.dram_tensor("xo", [NT, D], F32, kind="ExternalOutput").ap()

    emit_F2(nc, c, x, yT, w_out, lnp, rw, rb, wup, bup, wdn, bdn, idn, fcst, xo, n_exp=n_exp, n_blk=n_blk)
    return nc


def emit_F2(nc, c, x, yT, w_out, lnp, rw, rb, wup, bup, wdn, bdn, idn, fcst, xo, n_exp=32, n_blk=2, pfx="F"):
    CAP = 256
    TB = 1024
    D = 1024
    NTL = TB // 128
    sb = lambda n, s, d: c.sb(pfx + n, s, d)
    ident = sb("ident", [128, 128], F32); r_ident = Res("ident")
    c.dma("sp", ident[:], idn, writes=[r_ident])
    rw32 = sb("rw32", [128, 8, 32], F32); r_rw = Res("rw")
    c.dma("sp", rw32[:], rw.rearrange("(c p) e -> p c e", p=128), writes=[r_rw])
    rbB = sb("rbB", [128, 32], F32); r_rb = Res("rb")
    c.dma("sp", rbB[:], rb.partition_broadcast(128), writes=[r_rb])
    bdn32 = sb("bdn32", [32, D], F32); r_bdn = Res("bdn")
    c.dma("sp", bdn32[:], bdn, writes=[r_bdn])
    lnB = [sb("lnB%d" % i, [128, D], F32) for i in range(4)]
    r_ln = [Res("ln%d" % i) for i in range(4)]
    for i in range(4):
        c.dma("sp", lnB[i][:], lnp[i:i + 1, :].partition_broadcast(128), writes=[r_ln[i]])
    iota = sb("iota", [128, 256], F32); r_iota = Res("iota")
    c.dma("sp", iota[:], fcst[:, 0:256], writes=[r_iota])
    UO = sb("UO", [128, 256], BF16); r_UO = Res("UO")
    c.dma("pool", UO[:], fcst[:, 256:512], writes=[r_UO])
    identb = sb("identb", [128, 128], BF16); r_identb = Res("identb")
    c.dma("pool", identb[:], idn, writes=[r_identb])
    B = [c.ps(pfx + "bank%d" % i, [128, 512], F32) for i in range(7)]
    rB = [Res("bank%d" % i) for i in range(7)]
    TP = c.ps(pfx + "TP", [128, 1024], BF16); r_TP = Res("TP")
    bupraw = sb("bupraw", [128, 2, 256], F32); r_bupraw = Res("bupraw")
    c.dma("sp", bupraw[:], bup.rearrange("(g p) k -> p g k", p=128), writes=[r_bupraw])
    bupT = sb("bupT", [128, 2, 256], F32); r_bupT = Res("bupT")
    for two in range(2):
        for g in range(2):
            c.op("pe", lambda: nc.tensor.transpose(B[0][:, (two * 2 + g) * 128:(two * 2 + g + 1) * 128],
                                                   bupraw[:, g, two:256:2], ident[:]),
                 reads=[r_bupraw, r_ident], writes=[rB[0]], acc=(two + g > 0))
    c.op("act", lambda: nc.scalar.copy(bupT[:].rearrange("p a b -> p (a b)"), B[0][:]), reads=[rB[0]], writes=[r_bupT])

    c.op("dve", lambda: nc.vector.tensor_scalar(bupT[:, 1, :], bupT[:, 1, :], 1.0, None, ALU.add), reads=[r_bupT], writes=[r_bupT])

    yacc = sb("yacc", [128, NTL, D], F32); r_yacc = [Res("yacc%d" % i) for i in range(NTL)]
    x1b = sb("x1b", [128, NTL, D], BF16); r_x1b = [Res("x1b%d" % i) for i in range(NTL)]
    maskf = sb("maskf", [128, NTL, 32], F32); r_mask = [Res("mask%d" % i) for i in range(NTL)]
    maskb = sb("maskb", [128, NTL, 32], BF16)
    rank = sb("rank", [128, NTL, 32], F32); r_rank = [Res("rank%d" % i) for i in range(NTL)]
    carry = sb("carry", [128, 32], F32); r_carry = Res("carry")
    gates = sb("gates", [128, NTL, 32], F32); r_gates = [Res("gates%d" % i) for i in range(NTL)]
    gT = sb("gT", [32, TB], F32); r_gT = [Res("gT%d" % i) for i in range(NTL)]
    x1T32 = [sb("x1T32_%d" % i, [128, 8, 128], F32) for i in range(2)]; r_x1T32 = [Res("x1T32_%d" % i) for i in range(2)]
    sm = [sb("sm%d" % i, [128, 128], F32) for i in range(2)]; r_sm = [Res("sm%d" % i) for i in range(2)]
    wuc = [sb("wuc%d" % i, [128, 8, 256], BF16) for i in range(4)]; r_wuc = [Res("wuc%d" % i) for i in range(4)]
    wd = [sb("wd%d" % i, [128, 8, D], BF16) for i in range(2)]; r_wd = [Res("wd%d" % i) for i in range(2)]
    yTb, r_yTb = wd[0], r_wd[0]
    woutb, r_wout = wd[1], r_wd[1]
    Pm = [sb("Pm%d" % i, [128, NTL, CAP], BF16) for i in range(2)]; r_Pm = [Res("Pm%d" % i) for i in range(2)]
    Gm = [sb("Gm%d" % i, [128, NTL, CAP], BF16) for i in range(2)]; r_Gm = [Res("Gm%d" % i) for i in range(2)]
    GT = [sb("GT%d" % i, [128, 2, TB], BF16) for i in range(2)]; r_GT = [Res("GT%d" % i) for i in range(2)]
    XgT = [sb("XgT%d" % i, [128, 8, CAP], BF16) for i in range(2)]; r_XgT = [Res("XgT%d" % i) for i in range(2)]
    act = [sb("act%d" % i, [128, 8, CAP], BF16) for i in range(2)]; r_act = [Res("act%d" % i) for i in range(2)]
    osb = [sb("osb%d" % i, [128, 2, D], BF16) for i in range(2)]; r_osb = [Res("osb%d" % i) for i in range(2)]
    NTMP = 2
    tg = [sb("tg%d" % i, [128, CAP], F32) for i in range(NTMP)]; r_tg = [Res("tg%d" % i) for i in range(NTMP)]
    tsg = [sb("tsg%d" % i, [128, CAP], F32) for i in range(NTMP)]; r_tsg = [Res("tsg%d" % i) for i in range(NTMP)]
    tl = [sb("tl%d" % i, [128, CAP], F32) for i in range(NTMP)]; r_tl = [Res("tl%d" % i) for i in range(NTMP)]
    ot = [sb("ot%d" % i, [128, D], F32) for i in range(2)]; r_ot = [Res("ot%d" % i) for i in range(2)]
    zt, r_zt = ot, r_ot
    xt, r_xt = ot, r_ot
    r_out = [Res("xo0"), Res("xo1")]

    def layer_norm(src, r_src, dst, r_dst, s, r_s, gi, bi):
        for h in range(2):
            c.op("dve", lambda: nc.vector.bn_stats(s[:, h * 6:(h + 1) * 6], src[:, h * 512:(h + 1) * 512]),
                 reads=[r_src], writes=[r_s], acc=(h > 0))
        c.op("dve", lambda: nc.vector.bn_aggr(s[:, 12:14], s[:, 0:12]),
             reads=[r_s], writes=[r_s])
        c.op("act", lambda: nc.scalar.activation(s[:, 14:15], s[:, 13:14], AF.Sqrt, bias=epsT[:, 0:1], scale=1.0),
             reads=[r_s, r_eps], writes=[r_s])
        c.op("dve", lambda: nc.vector.reciprocal(s[:, 15:16], s[:, 14:15]), reads=[r_s], writes=[r_s])
        c.op("dve", lambda: nc.vector.scalar_tensor_tensor(dst, src, s[:, 12:13], lnB[gi][:], ALU.subtract, ALU.mult),
             reads=[r_src, r_s, r_ln[gi]], writes=[r_dst])
        c.op("dve", lambda: nc.vector.scalar_tensor_tensor(dst, dst, s[:, 15:16], lnB[bi][:], ALU.mult, ALU.add),
             reads=[r_dst, r_s, r_ln[bi]], writes=[r_dst])

    epsT = sb("epsT", [128, 1], F32); r_eps = Res("eps")
    c.op("dve", lambda: nc.vector.memset(epsT[:], LN_EPS), writes=[r_eps])

    def issue_wd(q):
        e_ = q % n_exp
        c.dma("pool", wd[q % 2][:], wdn[e_].rearrange("(c p) d -> p c d", p=128), writes=[r_wd[q % 2]])

    def issue_wu(gi):
        e_ = (gi // 8) % n_exp
        fc_ = gi % 8
        c.dma("pool", wuc[gi % 4][:], wup[e_][:, fc_ * 256:(fc_ + 1) * 256].rearrange("(c p) f -> p c f", p=128), writes=[r_wuc[gi % 4]])

    issue_wu(0)
    issue_wu(1)
    tmp_i = 0
    pg_i = 0
    po_i = 0
    sc_i = 0
    GXB = (3, 6)
    for blk in range(n_blk):
        t0 = blk * TB
        c.dma("pool", yTb[:], yT[:, t0:t0 + TB].rearrange("(c p) t -> p c t", p=128), writes=[r_yTb])
        c.dma("pool", woutb[:], w_out.rearrange("(c p) d -> p c d", p=128), writes=[r_wout])
        def stageA(i):
            b2 = i % 2
            c.dma("sp", xt[b2][:], x[t0 + i * 128:t0 + (i + 1) * 128, :], writes=[r_xt[b2]])
            for h in range(2):
                for k in range(8):
                    c.op("pe", lambda: nc.tensor.matmul(B[h][:], yTb[:, k, i * 128:(i + 1) * 128],
                                                        woutb[:, k, h * 512:(h + 1) * 512], start=(k == 0), stop=(k == 7)),
                         reads=[r_yTb, r_wout], writes=[rB[h]], acc=(k > 0))
                c.op("dve", lambda: nc.vector.scalar_tensor_tensor(zt[b2][:, h * 512:(h + 1) * 512], xt[b2][:, h * 512:(h + 1) * 512],
                                                                   ALPHA, B[h][:], ALU.mult, ALU.add),
                     reads=[r_xt[b2], rB[h]], writes=[r_zt[b2]], acc=(h > 0))
            layer_norm(zt[b2][:], r_zt[b2], yacc[:, i, :], r_yacc[i], sm[b2], r_sm[b2], 0, 1)
            for k in range(8):
                bk = 2 + k // 4
                c.op("pe", lambda: nc.tensor.transpose(B[bk][:, (k % 4) * 128:(k % 4 + 1) * 128],
                                                       yacc[:, i, k * 128:(k + 1) * 128], ident[:]),
                     reads=[r_yacc[i], r_ident], writes=[rB[bk]], acc=(k % 4 > 0))
            for hh in range(2):
                c.op("act", lambda: nc.scalar.copy(x1T32[b2][:, hh * 4:(hh + 1) * 4, :].rearrange("p a b -> p (a b)"), B[2 + hh][:]),
                     reads=[rB[2 + hh]], writes=[r_x1T32[b2]], acc=(hh > 0))
            c.op("act", lambda: nc.scalar.copy(x1b[:, i, :], yacc[:, i, :]), reads=[r_yacc[i]], writes=[r_x1b[i]])
        def stageB(i):
            b2 = i % 2
            for k in range(8):
                c.op("pe", lambda: nc.tensor.matmul(B[4][:, 0:32], x1T32[b2][:, k, :], rw32[:, k, :], start=(k == 0), stop=(k == 7)),
                     reads=[r_x1T32[b2], r_rw], writes=[rB[4]], acc=(k > 0))
            s = sm[b2]; r_s = r_sm[b2]
            c.op("dve", lambda: nc.vector.tensor_tensor(s[:, 32:64], B[4][:, 0:32], rbB[:], ALU.add), reads=[rB[4], r_rb], writes=[r_s])
            c.op("dve", lambda: nc.vector.max(s[:, 16:24], s[:, 32:64]), reads=[r_s], writes=[r_s])
            c.op("dve", lambda: nc.vector.tensor_scalar(s[:, 64:96], s[:, 32:64], s[:, 19:20], None, ALU.is_ge), reads=[r_s], writes=[r_s])
            c.op("dve", lambda: nc.vector.tensor_copy(maskf[:, i, :], s[:, 64:96]), reads=[r_s], writes=[r_mask[i]])
            c.op("dve", lambda: nc.vector.tensor_copy(maskb[:, i, :], s[:, 64:96]), reads=[r_s], writes=[r_mask[i]])
            c.op("pe", lambda: nc.tensor.matmul(B[4][:, 256:288], UO[:, 0:128], maskb[:, i, :], start=True, stop=True),
                 reads=[r_UO, r_mask[i]], writes=[rB[4]])
            c.op("pe", lambda: nc.tensor.matmul(B[4][:, 288:320], UO[:, 128:256], maskb[:, i, :], start=True, stop=True),
                 reads=[r_UO, r_mask[i]], writes=[rB[4]], acc=True)
            if i == 0:
                c.op("dve", lambda: nc.vector.tensor_copy(rank[:, i, :], B[4][:, 256:288]), reads=[rB[4]], writes=[r_rank[i]])
                c.op("dve", lambda: nc.vector.tensor_copy(carry[:], B[4][:, 288:320]), reads=[rB[4]], writes=[r_carry])
            else:
                c.op("dve", lambda: nc.vector.tensor_tensor(rank[:, i, :], B[4][:, 256:288], carry[:], ALU.add),
                     reads=[rB[4], r_carry], writes=[r_rank[i]])
                c.op("dve", lambda: nc.vector.tensor_tensor(carry[:], carry[:], B[4][:, 288:320], ALU.add),
                     reads=[rB[4], r_carry], writes=[r_carry])
            c.op("dve", lambda: nc.vector.tensor_scalar(s[:, 24:25], s[:, 16:17], -1.0, None, ALU.mult), reads=[r_s], writes=[r_s])
            c.op("act", lambda: nc.scalar.activation(s[:, 96:128], s[:, 32:64], AF.Exp, bias=s[:, 24:25], scale=1.0), reads=[r_s], writes=[r_s])
            c.op("dve", lambda: nc.vector.tensor_tensor(s[:, 96:128], s[:, 96:128], s[:, 64:96], ALU.mult), reads=[r_s], writes=[r_s])
            c.op("dve", lambda: nc.vector.reduce_sum(s[:, 25:26], s[:, 96:128], AX.X), reads=[r_s], writes=[r_s])
            c.op("dve", lambda: nc.vector.reciprocal(s[:, 26:27], s[:, 25:26]), reads=[r_s], writes=[r_s])
            c.op("dve", lambda: nc.vector.tensor_scalar(gates[:, i, :], s[:, 96:128], s[:, 26:27], None, ALU.mult), reads=[r_s], writes=[r_gates[i]])
            c.op("pe", lambda: nc.tensor.transpose(B[4][0:32, 128:256], gates[:, i, :], ident[:]),
                 reads=[r_gates[i], r_ident], writes=[rB[4]])
            c.op("act", lambda: nc.scalar.copy(gT[:, i * 128:(i + 1) * 128], B[4][0:32, 128:256]), reads=[rB[4]], writes=[r_gT[i]])
            for h in range(2):
                c.op("pe", lambda: nc.tensor.matmul(B[5 + h][:], gT[:, i * 128:(i + 1) * 128], bdn32[:, h * 512:(h + 1) * 512],
                                                    start=True, stop=True), reads=[r_gT[i], r_bdn], writes=[rB[5 + h]])
                c.op("dve", lambda: nc.vector.scalar_tensor_tensor(yacc[:, i, h * 512:(h + 1) * 512], yacc[:, i, h * 512:(h + 1) * 512],
                                                                   ALPHA, B[5 + h][:], ALU.mult, ALU.add),
                     reads=[r_yacc[i], rB[5 + h]], writes=[r_yacc[i]])

        stageA(0)
        for i in range(NTL):
            if i + 1 < NTL:
                stageA(i + 1)
            stageB(i)
        issue_wd(blk * n_exp)
        def build(e):
            ab = e % 2
            for i in range(NTL):
                c.op("dve", lambda: nc.vector.tensor_scalar(Pm[ab][:, i, :], iota[:], rank[:, i, e:e + 1], maskf[:, i, e:e + 1],
                                                            ALU.is_equal, ALU.mult),
                     reads=[r_iota, r_rank[i], r_mask[i]], writes=[r_Pm[ab]], acc=(i > 0))
                c.op("dve", lambda: nc.vector.tensor_scalar(Gm[ab][:, i, :], iota[:], rank[:, i, e:e + 1], gates[:, i, e:e + 1],
                                                            ALU.is_equal, ALU.mult),
                     reads=[r_iota, r_rank[i], r_gates[i]], writes=[r_Gm[ab]], acc=(i > 0))

        def transposes(e):
            ab = e % 2
            for st_ in range(2):
                for i in range(NTL):
                    c.op("pe", lambda: nc.tensor.transpose(TP[:, i * 128:(i + 1) * 128], Gm[ab][:, i, st_ * 128:(st_ + 1) * 128], identb[:]),
                         reads=[r_Gm[ab], r_identb], writes=[r_TP], acc=(i > 0))
                c.op("act", lambda: nc.scalar.copy(GT[ab][:, st_, :], TP[:]), reads=[r_TP], writes=[r_GT[ab]], acc=(st_ > 0))

        def gather(e):
            ab = e % 2
            for k in range(8):
                gh = k % 2
                for i in range(NTL):
                    c.op("pe", lambda: nc.tensor.matmul(B[GXB[gh]][:, 0:256], x1b[:, i, k * 128:(k + 1) * 128], Pm[ab][:, i, :],
                                                        start=(i == 0), stop=(i == NTL - 1)),
                         reads=[r_x1b[i], r_Pm[ab]], writes=[rB[GXB[gh]]], acc=(i > 0))
                c.op("act", lambda: nc.scalar.copy(XgT[ab][:, k, :], B[GXB[gh]][:, 0:256]), reads=[rB[GXB[gh]]], writes=[r_XgT[ab]], acc=(k > 0))

        def up(e):
            nonlocal pg_i, tmp_i
            q = blk * n_exp + e
            ab = e % 2
            for fc in range(8):
                gi = q * 8 + fc
                if gi + 2 < n_blk * n_exp * 8:
                    issue_wu(gi + 2)
                ws = gi % 4
                row = e * 8 + fc
                ub = pg_i % 2; pg_i += 1
                for two in range(2):
                    for k in range(8):
                        c.op("pe", lambda: nc.tensor.matmul(B[ub][:, two * 256:(two + 1) * 256], wuc[ws][:, k, two:256:2], XgT[ab][:, k, :],
                                                            start=(k == 0), stop=(k == 7)),
                             reads=[r_wuc[ws], r_XgT[ab]], writes=[rB[ub]], acc=(two + k > 0))
                ti = tmp_i % NTMP; tmp_i += 1
                c.op("dve", lambda: nc.vector.tensor_scalar(tg[ti][:], B[ub][:, 0:256], bupT[:, 0, row:row + 1], 7.0, ALU.add, ALU.min),
                     reads=[rB[ub], r_bupT], writes=[r_tg[ti]])
                c.op("act", lambda: nc.scalar.activation(tsg[ti][:], tg[ti][:], AF.Sigmoid, scale=1.702),
                     reads=[r_tg[ti]], writes=[r_tsg[ti]])
                c.op("dve", lambda: nc.vector.tensor_scalar(tl[ti][:], B[ub][:, 256:512], bupT[:, 1, row:row + 1], -6.0, ALU.add, ALU.max),
                     reads=[rB[ub], r_bupT], writes=[r_tl[ti]])
                c.op("dve", lambda: nc.vector.scalar_tensor_tensor(tl[ti][:], tl[ti][:], 8.0, tg[ti][:], ALU.min, ALU.mult),
                     reads=[r_tg[ti], r_tl[ti]], writes=[r_tl[ti]])
                c.op("dve", lambda: nc.vector.tensor_tensor(act[ab][:, fc, :], tl[ti][:], tsg[ti][:], ALU.mult),
                     reads=[r_tl[ti], r_tsg[ti]], writes=[r_act[ab]], acc=(fc > 0))

        def down(e):
            ab = e % 2
            for st_ in range(2):
                for h in range(2):
                    po = 2
                    for fc in range(8):
                        c.op("pe", lambda: nc.tensor.matmul(B[po][:], act[ab][:, fc, st_ * 128:(st_ + 1) * 128],
                                                            wd[ab][:, fc, h * 512:(h + 1) * 512], start=(fc == 0), stop=(fc == 7)),
                             reads=[r_act[ab], r_wd[ab]], writes=[rB[po]], acc=(fc > 0))
                    c.op("act", lambda: nc.scalar.copy(osb[ab][:, st_, h * 512:(h + 1) * 512], B[po][:]), reads=[rB[po]], writes=[r_osb[ab]],
                         acc=(st_ + h > 0))

        def scatter2(e0, e1):
            nonlocal sc_i
            for i in range(NTL):
                for h in range(2):
                    sc = 4 + (sc_i % 2); sc_i += 1
                    idx = 0
                    for ee in (e0, e1):
                        ab = ee % 2
                        for st_ in range(2):
                            c.op("pe", lambda: nc.tensor.matmul(B[sc][:], GT[ab][:, st_, i * 128:(i + 1) * 128], osb[ab][:, st_, h * 512:(h + 1) * 512],
                                                                start=(idx == 0), stop=(idx == 3)),
                                 reads=[r_GT[ab], r_osb[ab]], writes=[rB[sc]], acc=(idx > 0))
                            idx += 1
                    c.op("dve", lambda: nc.vector.tensor_tensor(yacc[:, i, h * 512:(h + 1) * 512], yacc[:, i, h * 512:(h + 1) * 512], B[sc][:], ALU.add),
                         reads=[rB[sc], r_yacc[i]], writes=[r_yacc[i]])

        build(0)
        transposes(0)
        gather(0)
        for e in range(n_exp):
            q = blk * n_exp + e
            if e + 1 < n_exp:
                issue_wd(q + 1)
                build(e + 1)
            up(e)
            if e + 1 < n_exp:
                gather(e + 1)
            down(e)
            if e % 2 == 1:
                scatter2(e - 1, e)
            if e + 1 < n_exp:
                transposes(e + 1)
        for i in range(NTL):
            b2 = i % 2
            layer_norm(yacc[:, i, :], r_yacc[i], ot[b2][:], r_ot[b2], sm[b2], r_sm[b2], 2, 3)
            c.dma("sp", xo[t0 + i * 128:t0 + (i + 1) * 128, :], ot[b2][:], reads=[r_ot[b2]], writes=[r_out[b2]])
    c.finish("sp", r_out)
    print("F: ins", c.n_ins, "waits", c.n_wait, "sbuf left", nc.sbuf_bytes_remaining)


GN_EPS = 1e-6


def me_host_consts(T):
    pos = np.arange(T, dtype=np.float32)
    inv = (10000.0 ** (-np.arange(0, 64, 2, dtype=np.float32) / 64)).astype(np.float32)
    ang = pos[:, None] * inv[None, :]
    cos = np.cos(ang).astype(np.float32).T
    sin = np.sin(ang).astype(np.float32).T
    cosF = np.concatenate([cos, cos], 0)
    sinS = np.concatenate([-sin, sin], 0)
    s8 = np.float32(0.125)
    cq = np.concatenate([cosF, cosF * s8], 0)
    sq = np.concatenate([sinS, sinS * s8], 0)
    ck = np.concatenate([cosF * s8, cosF], 0)
    sk = np.concatenate([sinS * s8, sinS], 0)
    tab = np.ascontiguousarray(np.stack([cq, sq, ck, sk], 0)).astype(np.float32)
    i = np.arange(128, dtype=np.float32)
    m = i[:, None]; c = i[None, :]
    cst = np.zeros((128, 6, 128), np.float32)
    cst[:, 0, :] = np.maximum(c - m, 0)
    cst[:, 1, :] = (c >= m)
    cst[:, 2, :] = np.maximum(m - c, 0)
    cst[:, 3, :] = (m > c)
    cst[:, 4, :] = c + 1.0
    cst[:, 5, :] = 128.0 - c
    col = np.zeros((128, 4), np.float32)
    col[:, 0] = 127.0 - i
    col[:, 1] = i
    col[:, 2] = 128.0
    o = np.arange(17)[None, :, None]
    delta = (o - 8) * 128 + m[:, None, :].astype(np.int64) - c[None, :, :].astype(np.int64)
    delta = delta.astype(np.int64)
    ad = np.abs(delta)
    mm = (ad <= 64).astype(np.float32) + ((delta % 4 == 0) & (ad <= 256)) + ((delta % 16 == 0) & (ad <= 1024))
    mm = mm.astype(np.float32).reshape(128, 17 * 128)
    return dict(tab=tab, cst=cst.reshape(128, 768), col=col, mm=mm, idn=np.eye(128, dtype=np.float32))


def me_weight_cols(head):
    H = 64
    base = lambda blk: blk * 512 + head * H
    rng = lambda b: list(range(base(b), base(b) + H))
    sw = lambda b: list(range(base(b) + 32, base(b) + 64)) + list(range(base(b), base(b) + 32))
    cols = []
    cols += rng(0) + rng(5)
    cols += sw(0) + sw(5)
    cols += rng(1) + rng(6)
    cols += sw(1) + sw(6)
    cols += rng(2) + rng(7)
    cols += rng(3) + rng(4)
    return np.array(cols)


def build_ME(T=16384):
    nc = bass.Bass("TRN2", target_bir_lowering=False)
    c = Ctx(nc)
    xT = nc.dram_tensor("xT", [1024, T], F32, kind="ExternalInput").ap()
    wsel = nc.dram_tensor("wsel", [1024, 768], F32, kind="ExternalInput").ap()
    dec = nc.dram_tensor("dec", [1, 2], F32, kind="ExternalInput").ap()
    tab = nc.dram_tensor("tab", [4, 128, T], F32, kind="ExternalInput").ap()
    cst = nc.dram_tensor("cst", [128, 768], F32, kind="ExternalInput").ap()
    col = nc.dram_tensor("col", [128, 4], F32, kind="ExternalInput").ap()
    mm = nc.dram_tensor("mm", [128, 17 * 128], F32, kind="ExternalInput").ap()
    idn = nc.dram_tensor("idn", [128, 128], F32, kind="ExternalInput").ap()
    y = nc.dram_tensor("y", [T, 128], F32, kind="ExternalOutput").ap()
    emit_ME(nc, c, xT, wsel, dec, tab, cst, col, mm, idn, y, T)
    return nc


def emit_ME(nc, c, xT, wsel, dec, tab, cst, col, mm, idn, y, T, pfx="E"):
    NCH = T // 128
    sb = lambda n, s, d: c.sb(pfx + n, s, d)
    V = nc.vector
    A = nc.scalar
    identb = sb("identb", [128, 128], BF16); r_identb = Res("identb")
    c.dma("pool", identb[:], idn, writes=[r_identb])
    wb = sb("wb", [128, 8, 768], BF16); r_wb = Res("wb")
    c.dma("pool", wb[:], wsel.rearrange("(c p) e -> p c e", p=128), writes=[r_wb])
    mmb = sb("mmb", [128, 17 * 128], BF16); r_mm = Res("mm")
    c.dma("pool", mmb[:], mm, writes=[r_mm])
    cs = sb("cs", [128, 6, 128], F32); r_cs = Res("cs")
    c.dma("sp", cs[:].rearrange("p a b -> p (a b)"), cst, writes=[r_cs])
    cl = sb("cl", [128, 4], F32); r_cl = Res("cl")
    c.dma("sp", cl[:], col, writes=[r_cl])
    dl = sb("dl", [128, 16], F32); r_dl = Res("dl")
    c.dma("sp", dl[:, 0:2], dec.partition_broadcast(128), writes=[r_dl])
    ops = [
        lambda: V.tensor_scalar(dl[:, 2:4], dl[:, 0:2], -1.0, None, ALU.mult),
        lambda: V.tensor_tensor(dl[:, 2:4], dl[:, 2:4], dl[:, 0:2], ALU.max),
    ]
    for f in ops:
        c.op("dve", f, reads=[r_dl], writes=[r_dl])
    c.op("act", lambda: A.activation(dl[:, 2:4], dl[:, 2:4], AF.Exp, scale=-1.0), reads=[r_dl], writes=[r_dl])
    ops = [
        lambda: V.tensor_scalar(dl[:, 4:6], dl[:, 2:4], 2.0, None, ALU.add),
        lambda: V.reciprocal(dl[:, 4:6], dl[:, 4:6]),
        lambda: V.tensor_tensor(dl[:, 4:6], dl[:, 4:6], dl[:, 2:4], ALU.mult),
        lambda: V.tensor_tensor(dl[:, 6:8], dl[:, 4:6], dl[:, 4:6], ALU.mult),
        lambda: V.tensor_scalar(dl[:, 8:10], dl[:, 6:8], 1.0 / 11, 1.0 / 9, ALU.mult, ALU.add),
        lambda: V.tensor_tensor(dl[:, 8:10], dl[:, 8:10], dl[:, 6:8], ALU.mult),
        lambda: V.tensor_scalar(dl[:, 8:10], dl[:, 8:10], 1.0 / 7, None, ALU.add),
        lambda: V.tensor_tensor(dl[:, 8:10], dl[:, 8:10], dl[:, 6:8], ALU.mult),
        lambda: V.tensor_scalar(dl[:, 8:10], dl[:, 8:10], 1.0 / 5, None, ALU.add),
        lambda: V.tensor_tensor(dl[:, 8:10], dl[:, 8:10], dl[:, 6:8], ALU.mult),
        lambda: V.tensor_scalar(dl[:, 8:10], dl[:, 8:10], 1.0 / 3, None, ALU.add),
        lambda: V.tensor_tensor(dl[:, 8:10], dl[:, 8:10], dl[:, 6:8], ALU.mult),
        lambda: V.tensor_scalar(dl[:, 8:10], dl[:, 8:10], 1.0, None, ALU.add),
        lambda: V.tensor_tensor(dl[:, 8:10], dl[:, 8:10], dl[:, 4:6], ALU.mult),
        lambda: V.tensor_scalar(dl[:, 10:12], dl[:, 0:2], 0.0, None, ALU.min),
        lambda: V.scalar_tensor_tensor(dl[:, 10:12], dl[:, 8:10], -2.0, dl[:, 10:12], ALU.mult, ALU.add),
    ]
    for f in ops:
        c.op("dve", f, reads=[r_dl], writes=[r_dl])
    lgf = dl[:, 10:11]
    lgb = dl[:, 11:12]
    decT = sb("decT", [128, 2, 128], F32); r_dec = Res("decT")
    xi = sb("xi", [128, 2, 128], F32); r_xi = Res("xi")
    zc = sb("zc", [128, 4], F32); r_zc = Res("zc")
    c.op("act", lambda: A.activation(decT[:, 0, :], cs[:, 0, :], AF.Exp, scale=lgf), reads=[r_cs, r_dl], writes=[r_dec])
    c.op("act", lambda: A.activation(decT[:, 1, :], cs[:, 2, :], AF.Exp, scale=lgb), reads=[r_cs, r_dl], writes=[r_dec])
    c.op("dve", lambda: V.tensor_tensor(decT[:, 0, :], decT[:, 0, :], cs[:, 1, :], ALU.mult), reads=[r_dec, r_cs], writes=[r_dec])
    c.op("dve", lambda: V.tensor_tensor(decT[:, 1, :], decT[:, 1, :], cs[:, 3, :], ALU.mult), reads=[r_dec, r_cs], writes=[r_dec])
    c.op("act", lambda: A.activation(xi[:, 0, :], cs[:, 4, :], AF.Exp, scale=lgf), reads=[r_cs, r_dl], writes=[r_xi])
    c.op("act", lambda: A.activation(xi[:, 1, :], cs[:, 5, :], AF.Exp, scale=lgb), reads=[r_cs, r_dl], writes=[r_xi])
    c.op("act", lambda: A.activation(zc[:, 0:1], cl[:, 0:1], AF.Exp, scale=lgf), reads=[r_cl, r_dl], writes=[r_zc])
    c.op("act", lambda: A.activation(zc[:, 1:2], cl[:, 1:2], AF.Exp, scale=lgb), reads=[r_cl, r_dl], writes=[r_zc])
    c.op("act", lambda: A.activation(zc[:, 2:3], cl[:, 2:3], AF.Exp, scale=lgf), reads=[r_cl, r_dl], writes=[r_zc])
    c.op("act", lambda: A.activation(zc[:, 3:4], cl[:, 2:3], AF.Exp, scale=lgb), reads=[r_cl, r_dl], writes=[r_zc])
    epsT = sb("epsT", [128, 1], F32); r_eps = Res("eps")
    c.op("dve", lambda: V.memset(epsT[:], GN_EPS), writes=[r_eps])

    qkT = sb("qkT", [128, 2, T], BF16)
    r_qk = [Res("qk%d" % n) for n in range(NCH)]
    vall = sb("vall", [128, NCH, 64], BF16); r_v = [Res("v%d" % n) for n in range(NCH)]
    dvall = sb("dvall", [128, NCH, 66], BF16); r_dv = [Res("dv%d" % n) for n in range(NCH)]
    Rf = sb("Rf", [64, NCH + 1, 64], BF16); r_Rf = [Res("Rf%d" % n) for n in range(NCH + 1)]
    Rrun = sb("Rrun", [64, 2, 64], F32); r_Rrun = [Res("Rrun0"), Res("Rrun1")]
    Rbb = [sb("Rbb%d" % i, [64, 64], BF16) for i in range(2)]; r_Rbb = [Res("Rbb0"), Res("Rbb1")]
    ones_dst = Res("dvones")
    c.op("pool", lambda: nc.gpsimd.memset(dvall[:, :, 64:66], 1.0), writes=[ones_dst])
    c.op("dve", lambda: V.memset(Rrun[:], 0.0), writes=r_Rrun)
    c.op("dve", lambda: V.memset(Rf[:, 0, :], 0.0), writes=[r_Rf[0]])
    c.op("dve", lambda: V.memset(Rbb[0][:], 0.0), writes=[r_Rbb[0]])

    NB = 2
    xS = [sb("xS%d" % i, [128, 8, 512], BF16) for i in range(2)]; r_xS = [Res("xS%d" % i) for i in range(2)]
    resident = {}
    NS = NCH // 4
    tbt = [sb("tbt%d" % i, [128, 4, 128], F32) for i in range(NB)]; r_tbt = [Res("tbt%d" % i) for i in range(NB)]
    prod = [sb("prod%d" % i, [128, 4, 128], F32) for i in range(NB)]; r_prod = [Res("prod%d" % i) for i in range(NB)]
    kz = [sb("kz%d" % i, [128, 64], BF16) for i in range(NB)]; r_kz = [Res("kz%d" % i) for i in range(NB)]
    scT = [sb("scT%d" % i, [128, 2, 128], BF16) for i in range(NB)]; r_scT = [Res("scT%d" % i) for i in range(NB)]
    qxi = [sb("qxi%d" % i, [64, 2, 128], BF16) for i in range(NB)]; r_qxi = [Res("qxi%d" % i) for i in range(NB)]
    st = [sb("st%d" % i, [128, 32], F32) for i in range(NB)]; r_st = [Res("st%d" % i) for i in range(NB)]
    xn = [sb("xn%d" % i, [128, 128], F32) for i in range(NB)]; r_xn = [Res("xn%d" % i) for i in range(NB)]
    sg = [sb("sg%d" % i, [128, 128], F32) for i in range(NB)]; r_sg = [Res("sg%d" % i) for i in range(NB)]
    yt = [sb("yt%d" % i, [128, 128], F32) for i in range(NB)]; r_yt = [Res("yt%d" % i) for i in range(NB)]
    pd = [sb("pd%d" % i, [128, 512], BF16) for i in range(3)]; r_pd = [Res("pd%d" % i) for i in range(3)]
    r_y = [Res("y0"), Res("y1")]
    PA = c.ps(pfx + "PA", [128, 512], F32); r_PA = Res("PA")
    PV = c.ps(pfx + "PV", [128, 512], F32); r_PV = Res("PV")
    PT = c.ps(pfx + "PT", [128, 1024], BF16); r_PT = Res("PT")
    PS = c.ps(pfx + "PS", [128, 512], F32); r_PS = Res("PS")
    PO = c.ps(pfx + "PO", [128, 512], F32); r_PO = Res("PO")
    PD = [c.ps(pfx + "PD%d" % i, [128, 512], F32) for i in range(3)]; r_PD = [Res("PD%d" % i) for i in range(3)]

    def load_slab(sidx):
        bi = sidx % 2
        if resident.get(bi) == sidx:
            return
        c.dma("pool", xS[bi][:], xT[:, sidx * 512:(sidx + 1) * 512].rearrange("(c p) t -> p c t", p=128), writes=[r_xS[bi]])
        resident[bi] = sidx

    def load_tab(n):
        b = n % NB
        c.dma("sp", tbt[b][:], tab[:, :, n * 128:(n + 1) * 128].rearrange("a p t -> p a t"), writes=[r_tbt[b]])

    def xv(n, k):
        return xS[(n // 4) % 2][:, k, (n % 4) * 128:(n % 4 + 1) * 128]

    def rxv(n):
        return r_xS[(n // 4) % 2]

    def ktrans_state(n, direction, b):
        c.op("pe", lambda: nc.tensor.transpose(PT[:, 0:64], qkT[0:64, 1, n * 128:(n + 1) * 128], identb[0:64, 0:64]),
             reads=[r_qk[n], r_identb], writes=[r_PT])
        c.op("dve", lambda: V.tensor_scalar(kz[b][:], PT[:, 0:64], zc[:, direction:direction + 1], None, ALU.mult),
             reads=[r_PT, r_zc], writes=[r_kz[b]])
        c.op("pe", lambda: nc.tensor.matmul(PS[0:64, 0:64], kz[b][:], vall[:, n, :], start=True, stop=True),
             reads=[r_kz[b], r_v[n]], writes=[r_PS])

    load_slab(0)
    load_tab(0)
    for n in range(NCH):
        b = n % NB
        if n % 4 == 0 and n // 4 + 1 < NS:
            load_slab(n // 4 + 1)
        if n + 1 < NCH:
            load_tab(n + 1)
        for g in range(4):
            for k in range(8):
                c.op("pe", lambda: nc.tensor.matmul(PA[:, g * 128:(g + 1) * 128], wb[:, k, g * 128:(g + 1) * 128], xv(n, k),
                                                    start=(k == 0), stop=(k == 7)),
                     reads=[r_wb, rxv(n)], writes=[r_PA], acc=(g + k > 0))
        for k in range(8):
            c.op("pe", lambda: nc.tensor.matmul(PV[:, 0:128], xv(n, k), wb[:, k, 512:640], start=(k == 0), stop=(k == 7)),
                 reads=[r_wb, rxv(n)], writes=[r_PV], acc=(k > 0))
        c.op("dve", lambda: V.tensor_tensor(prod[b][:], PA[:].rearrange("p (a t) -> p a t", a=4), tbt[b][:], ALU.mult),
             reads=[r_PA, r_tbt[b]], writes=[r_prod[b]])
        c.op("dve", lambda: V.tensor_tensor(qkT[:, :, n * 128:(n + 1) * 128], prod[b][:, 0:4:2, :], prod[b][:, 1:4:2, :], ALU.add),
             reads=[r_prod[b]], writes=[r_qk[n]])
        c.op("act", lambda: A.copy(vall[:, n, :], PV[:, 0:64]), reads=[r_PV], writes=[r_v[n]])
        c.op("act", lambda: A.copy(dvall[:, n, 0:64], PV[:, 64:128]), reads=[r_PV, ones_dst], writes=[r_dv[n]])
        ktrans_state(n, 0, b)
        c.op("dve", lambda: V.scalar_tensor_tensor(Rrun[:, 0, :], Rrun[:, 0, :], zc[0:64, 2:3], PS[0:64, 0:64], ALU.mult, ALU.add),
             reads=[r_Rrun[0], r_zc, r_PS], writes=[r_Rrun[0]])
        c.op("act", lambda: A.copy(Rf[:, n + 1, :], Rrun[:, 0, :]), reads=[r_Rrun[0]], writes=[r_Rf[n + 1]])

    load_slab(NS - 1)

    def ret_steps(n):
        b = n % NB
        rb_cur = (NCH - 1 - n) % 2
        sl = slice(n * 128, (n + 1) * 128)
        s = st[b]; rs = r_st[b]

        def r1():
            for k in range(8):
                c.op("pe", lambda: nc.tensor.matmul(PV[:, 0:128], xv(n, k), wb[:, k, 640:768], start=(k == 0), stop=(k == 7)),
                     reads=[r_wb, rxv(n)], writes=[r_PV], acc=(k > 0))
            c.op("pe", lambda: nc.tensor.matmul(PS[:, 128:256], qkT[0:64, 1, sl], qkT[0:64, 0, sl], start=True, stop=True),
                 reads=[r_qk[n]], writes=[r_PS])

        def r2():
            c.op("dve", lambda: V.tensor_tensor(scT[b][:], PS[:, 128:256].unsqueeze(1).broadcast_to([128, 2, 128]), decT[:], ALU.mult),
                 reads=[r_PS, r_dec], writes=[r_scT[b]])
            c.op("dve", lambda: V.tensor_tensor(qxi[b][:], qkT[0:64, 0, sl].unsqueeze(1).broadcast_to([64, 2, 128]), xi[0:64], ALU.mult),
                 reads=[r_qk[n], r_xi], writes=[r_qxi[b]])
            c.op("act", lambda: A.activation(sg[b][:], PV[:, 0:128], AF.Exp, scale=-1.0), reads=[r_PV], writes=[r_sg[b]])

        def r3():
            c.op("pe", lambda: nc.tensor.matmul(PO[:, 0:64], scT[b][:, 0, :], vall[:, n, :], start=True, stop=False),
                 reads=[r_scT[b], r_v[n]], writes=[r_PO])
            c.op("pe", lambda: nc.tensor.matmul(PO[:, 0:64], qxi[b][:, 0, :], Rf[:, n, :], start=False, stop=True),
                 reads=[r_qxi[b], r_Rf[n]], writes=[r_PO], acc=True)
            c.op("pe", lambda: nc.tensor.matmul(PO[:, 64:128], scT[b][:, 1, :], vall[:, n, :], start=True, stop=False),
                 reads=[r_scT[b], r_v[n]], writes=[r_PO], acc=True)
            c.op("pe", lambda: nc.tensor.matmul(PO[:, 64:128], qxi[b][:, 1, :], Rbb[rb_cur][:], start=False, stop=True),
                 reads=[r_qxi[b], r_Rbb[rb_cur]], writes=[r_PO], acc=True)

        def r4():
            if n > 0:
                ktrans_state(n, 1, b)
                c.op("dve", lambda: V.scalar_tensor_tensor(Rrun[:, 1, :], Rrun[:, 1, :], zc[0:64, 3:4], PS[0:64, 0:64], ALU.mult, ALU.add),
                     reads=[r_Rrun[1], r_zc, r_PS], writes=[r_Rrun[1]])
                c.op("act", lambda: A.copy(Rbb[1 - rb_cur][:], Rrun[:, 1, :]), reads=[r_Rrun[1]], writes=[r_Rbb[1 - rb_cur]])

        def r5():
            c.op("dve", lambda: V.bn_stats(s[:, 0:6], PO[:, 0:64]), reads=[r_PO], writes=[rs])
            c.op("dve", lambda: V.bn_stats(s[:, 6:12], PO[:, 64:128]), reads=[r_PO], writes=[rs])
            c.op("dve", lambda: V.bn_aggr(s[:, 12:14], s[:, 0:6]), reads=[rs], writes=[rs])
            c.op("dve", lambda: V.bn_aggr(s[:, 14:16], s[:, 6:12]), reads=[rs], writes=[rs])
            c.op("act", lambda: A.activation(s[:, 16:18], s[:, 13:16:2], AF.Ln, bias=epsT[:, 0:1], scale=1.0), reads=[rs, r_eps], writes=[rs])
            c.op("act", lambda: A.activation(s[:, 16:18], s[:, 16:18], AF.Exp, scale=-0.5), reads=[rs], writes=[rs])
            c.op("dve", lambda: V.tensor_scalar(sg[b][:], sg[b][:], 1.0, None, ALU.add), reads=[r_sg[b]], writes=[r_sg[b]])
            c.op("dve", lambda: V.reciprocal(sg[b][:], sg[b][:]), reads=[r_sg[b]], writes=[r_sg[b]])
            c.op("dve", lambda: V.tensor_tensor(sg[b][:], sg[b][:], PV[:, 0:128], ALU.mult), reads=[r_sg[b], r_PV], writes=[r_sg[b]])

        def r6():
            c.op("dve", lambda: V.tensor_scalar(xn[b][:, 0:64], PO[:, 0:64], s[:, 12:13], s[:, 16:17], ALU.subtract, ALU.mult),
                 reads=[r_PO, rs], writes=[r_xn[b]])
            c.op("dve", lambda: V.tensor_scalar(xn[b][:, 64:128], PO[:, 64:128], s[:, 14:15], s[:, 17:18], ALU.subtract, ALU.mult),
                 reads=[r_PO, rs], writes=[r_xn[b]])
            c.op("pool", lambda: nc.gpsimd.tensor_tensor(xn[b][:], xn[b][:], sg[b][:], ALU.mult), reads=[r_xn[b], r_sg[b]], writes=[r_xn[b]])
            c.op("pool", lambda: nc.gpsimd.tensor_tensor(yt[b][:, 0:64], xn[b][:, 0:64], xn[b][:, 64:128], ALU.add),
                 reads=[r_xn[b]], writes=[r_yt[b]])
        return [r1, r2, r3, r4, r5, r6]

    def dil_steps(n):
        b = n % NB
        sl = slice(n * 128, (n + 1) * 128)
        s2 = st[b]; rs = r_st[b]
        kts = [kt for kt in range(n - 8, n + 9) if 0 <= kt < NCH]
        groups = [kts[i:i + 4] for i in range(0, len(kts), 4)]
        G = len(groups)

        def qk(gi):
            grp = groups[gi]; pb = gi % 3
            for j, kt in enumerate(grp):
                c.op("pe", lambda: nc.tensor.matmul(PD[pb][:, j * 128:(j + 1) * 128], qkT[64:128, 1, kt * 128:(kt + 1) * 128],
                                                    qkT[64:128, 0, sl], start=True, stop=True),
                     reads=[r_qk[kt], r_qk[n]], writes=[r_PD[pb]], acc=(j > 0))
            w = len(grp) * 128
            o0 = (grp[0] - n + 8) * 128
            c.op("act", lambda: A.activation(pd[pb][:, 0:w], PD[pb][:, 0:w], AF.Exp), reads=[r_PD[pb]], writes=[r_pd[pb]])
            c.op("pool", lambda: nc.gpsimd.tensor_tensor(pd[pb][:, 0:w], pd[pb][:, 0:w], mmb[:, o0:o0 + w], ALU.mult),
                 reads=[r_pd[pb], r_mm], writes=[r_pd[pb]])

        def pv(gi):
            grp = groups[gi]; pb = gi % 3
            for j, kt in enumerate(grp):
                first = (gi == 0 and j == 0)
                last = (gi == G - 1 and j == len(grp) - 1)
                c.op("pe", lambda: nc.tensor.matmul(PA[:, 0:65], pd[pb][:, j * 128:(j + 1) * 128], dvall[:, kt, 0:65],
                                                    start=first, stop=last),
                     reads=[r_pd[pb], r_dv[kt]], writes=[r_PA], acc=(not first))

        def fin():
            c.op("dve", lambda: V.reciprocal(s2[:, 20:21], PA[:, 64:65]), reads=[r_PA, rs], writes=[rs])
            c.op("dve", lambda: V.tensor_scalar(yt[b][:, 64:128], PA[:, 0:64], s2[:, 20:21], None, ALU.mult),
                 reads=[r_PA, rs], writes=[r_yt[b]])
        steps = []
        from functools import partial
        steps.append(partial(qk, 0))
        if G > 1:
            steps.append(partial(qk, 1))
        for k in range(G):
            if k + 2 < G:
                steps.append(partial(qk, k + 2))
            steps.append(partial(pv, k))
        steps.append(fin)
        return steps

    for n in range(NCH - 1, -1, -1):
        b = n % NB
        if n % 4 == 3 and n // 4 - 1 >= 0:
            load_slab(n // 4 - 1)
        rs_ = ret_steps(n)
        ds_ = dil_steps(n)
        i = j = 0
        while i < len(ds_) or j < len(rs_):
            if i < len(ds_):
                ds_[i](); i += 1
            if j < len(rs_):
                rs_[j](); j += 1
        c.dma("sp", y[n * 128:(n + 1) * 128, :], yt[b][:], reads=[r_yt[b]], writes=[r_y[b]])
    c.finish("sp", r_y)
    print("ME: ins", c.n_ins, "waits", c.n_wait, "sbuf left", nc.sbuf_bytes_remaining)


NEG = -30000.0


def mo_patterns(NCH):
    pats = []
    for o in range(5):
        pats.append((4, 4 + o - 2))
    for n in (0, 1, NCH - 2, NCH - 1):
        kts = range(0, 4) if n < 2 else range(NCH - 4, NCH)
        for kt in kts:
            pats.append((n, kt))
    return pats


def mo_keys(n, NCH):
    if 2 <= n <= NCH - 3:
        return [(n + o - 2, o) for o in range(5)]
    idx = {0: 0, 1: 1, NCH - 2: 2, NCH - 1: 3}[n]
    kts = range(0, 4) if n < 2 else range(NCH - 4, NCH)
    return [(kt, 5 + idx * 4 + j) for j, kt in enumerate(kts)]


def mo_host_bias(rpb2, T):
    NCH = T // 128
    rows = T // 64
    pats = mo_patterns(NCH)
    m = np.arange(128)[:, None]
    c = np.arange(128)[None, :]
    out = np.full((128, 2, len(pats), 128), NEG, np.float32)
    for pi, (n, kt) in enumerate(pats):
        rm = 2 * kt + m // 64; wm = m % 64
        rc = 2 * n + c // 64; wc = c % 64
        r0 = np.clip(rc - 4, 0, rows - 8)
        c0 = np.clip(wc - 8, 0, 64 - 16)
        valid = (rm >= r0) & (rm < r0 + 8) & (wm >= c0) & (wm < c0 + 16)
        ro = np.clip(rm - rc + 7, 0, 14)
        co = np.clip(wm - wc + 15, 0, 30)
        for h in range(2):
            g = rpb2[h][ro, co]
            out[:, h, pi, :] = np.where(valid, g, np.float32(NEG))
    return out.reshape(128, 2 * len(pats) * 128)


def mo_weight_cols(core):
    h0, h1 = 2 * core, 2 * core + 1
    rng = lambda blk, h: list(range(blk * 1024 + h * 64, blk * 1024 + (h + 1) * 64))
    return np.array(rng(0, h0) + rng(0, h1) + rng(1, h0) + rng(1, h1) + rng(2, h0) + rng(2, h1))


def build_MO(T=16384):
    nc = bass.Bass("TRN2", target_bir_lowering=False)
    c = Ctx(nc)
    xT = nc.dram_tensor("xT", [1024, T], F32, kind="ExternalInput").ap()
    wsel = nc.dram_tensor("wsel", [1024, 384], F32, kind="ExternalInput").ap()
    bias = nc.dram_tensor("bias", [128, 2 * 21 * 128], F32, kind="ExternalInput").ap()
    idn = nc.dram_tensor("idn", [128, 128], F32, kind="ExternalInput").ap()
    y = nc.dram_tensor("y", [T, 128], F32, kind="ExternalOutput").ap()
    emit_MO(nc, c, xT, wsel, bias, idn, y, T)
    return nc


def emit_MO(nc, c, xT, wsel, bias, idn, y, T, pfx="O"):
    NCH = T // 128
    sb = lambda n, s, d: c.sb(pfx + n, s, d)
    V = nc.vector
    A = nc.scalar
    identb = sb("identb", [128, 128], BF16); r_identb = Res("identb")
    c.dma("pool", identb[:], idn, writes=[r_identb])
    wb = sb("wb", [128, 8, 384], BF16); r_wb = Res("wb")
    c.dma("pool", wb[:], wsel.rearrange("(c p) e -> p c e", p=128), writes=[r_wb])
    biasb = sb("biasb", [128, 2, 21, 128], BF16); r_bias = Res("bias")
    c.dma("pool", biasb[:].rearrange("p a b c -> p (a b c)"), bias, writes=[r_bias])

    qkT = sb("qkT", [128, 2, T], BF16); r_qk = [Res("qk%d" % n) for n in range(NCH)]
    vall = sb("vall", [128, NCH, 2, 66], BF16); r_v = [Res("v%d" % n) for n in range(NCH)]
    ones_dst = Res("ones")
    c.op("pool", lambda: nc.gpsimd.memset(vall[:, :, :, 64:66], 1.0), writes=[ones_dst])
    NB = 2
    xTt = [sb("xTt%d" % i, [128, 8, 128], BF16) for i in range(NB)]; r_xTt = [Res("xTt%d" % i) for i in range(NB)]
    pd = [sb("pd%d" % i, [128, 640], BF16) for i in range(3)]; r_pd = [Res("pd%d" % i) for i in range(3)]
    yt = [sb("yt%d" % i, [128, 128], F32) for i in range(NB)]; r_yt = [Res("yt%d" % i) for i in range(NB)]
    st = [sb("st%d" % i, [128, 8], F32) for i in range(NB)]; r_st = [Res("st%d" % i) for i in range(NB)]
    r_y = [Res("y0"), Res("y1")]
    PA = c.ps(pfx + "PA", [128, 512], F32); r_PA = Res("PA")
    PV = c.ps(pfx + "PV", [128, 512], F32); r_PV = Res("PV")
    PO = c.ps(pfx + "PO", [128, 512], F32); r_PO = Res("PO")
    PD = [c.ps(pfx + "PD%d" % i, [128, 1024], F32) for i in range(2)]; r_PD = [Res("PD%d" % i) for i in range(2)]

    def load_x(n):
        b = n % NB
        c.dma("pool", xTt[b][:], xT[:, n * 128:(n + 1) * 128].rearrange("(c p) t -> p c t", p=128), writes=[r_xTt[b]])

    load_x(0)
    for n in range(NCH):
        b = n % NB
        if n + 1 < NCH:
            load_x(n + 1)
        for g in range(2):
            for k in range(8):
                c.op("pe", lambda: nc.tensor.matmul(PA[:, g * 128:(g + 1) * 128], wb[:, k, g * 128:(g + 1) * 128], xTt[b][:, k, :],
                                                    start=(k == 0), stop=(k == 7)),
                     reads=[r_wb, r_xTt[b]], writes=[r_PA], acc=(g + k > 0))
        for k in range(8):
            c.op("pe", lambda: nc.tensor.matmul(PV[:, 0:128], xTt[b][:, k, :], wb[:, k, 256:384], start=(k == 0), stop=(k == 7)),
                 reads=[r_wb, r_xTt[b]], writes=[r_PV], acc=(k > 0))
        c.op("act", lambda: A.activation(qkT[:, 0, n * 128:(n + 1) * 128], PA[:, 0:128], AF.Copy, scale=0.125),
             reads=[r_PA], writes=[r_qk[n]])
        c.op("dve", lambda: V.tensor_copy(qkT[:, 1, n * 128:(n + 1) * 128], PA[:, 128:256]), reads=[r_PA], writes=[r_qk[n]])
        c.op("dve", lambda: V.tensor_copy(vall[:, n, :, 0:64], PV[:, 0:128].rearrange("p (h d) -> p h d", h=2)),
             reads=[r_PV, ones_dst], writes=[r_v[n]])

    POs = [PO, PA]; r_POs = [r_PO, r_PA]
    units = [(n, hh) for n in range(NCH) for hh in range(2)]

    def qk(ui):
        n, hh = units[ui]
        sl = slice(n * 128, (n + 1) * 128)
        keys = mo_keys(n, NCH)
        hp = slice(hh * 64, (hh + 1) * 64)
        pb = ui % 2; sbi = ui % 3
        for j, (kt, pat) in enumerate(keys):
            c.op("pe", lambda: nc.tensor.matmul(PD[pb][:, j * 128:(j + 1) * 128], qkT[hp, 1, kt * 128:(kt + 1) * 128],
                                                qkT[hp, 0, sl], start=True, stop=False),
                 reads=[r_qk[kt], r_qk[n]], writes=[r_PD[pb]], acc=(j > 0))
            c.op("pe", lambda: nc.tensor.matmul(PD[pb][:, j * 128:(j + 1) * 128], identb[:], biasb[:, hh, pat, :],
                                                start=False, stop=True),
                 reads=[r_identb, r_bias], writes=[r_PD[pb]], acc=True)
        nk = len(keys)
        w0 = min(nk, 4) * 128
        c.op("act", lambda: A.activation(pd[sbi][:, 0:w0], PD[pb][:, 0:w0], AF.Exp), reads=[r_PD[pb]], writes=[r_pd[sbi]])
        if nk > 4:
            c.op("act", lambda: A.activation(pd[sbi][:, 512:640], PD[pb][:, 512:640], AF.Exp), reads=[r_PD[pb]], writes=[r_pd[sbi]], acc=True)

    def pv(ui):
        n, hh = units[ui]
        keys = mo_keys(n, NCH)
        nk = len(keys)
        sbi = ui % 3
        po = POs[n % 2]; r_po = r_POs[n % 2]
        for j, (kt, pat) in enumerate(keys):
            c.op("pe", lambda: nc.tensor.matmul(po[:, hh * 128:hh * 128 + 65], pd[sbi][:, j * 128:(j + 1) * 128], vall[:, kt, hh, 0:65],
                                                start=(j == 0), stop=(j == nk - 1)),
                 reads=[r_pd[sbi], r_v[kt]], writes=[r_po], acc=(hh + j > 0))

    def fin(n):
        b = n % NB
        s = st[b]; rs = r_st[b]
        po = POs[n % 2]; r_po = r_POs[n % 2]
        for hh in range(2):
            c.op("dve", lambda: V.reciprocal(s[:, hh:hh + 1], po[:, hh * 128 + 64:hh * 128 + 65]), reads=[r_po, rs], writes=[rs])
            c.op("dve", lambda: V.tensor_scalar(yt[b][:, hh * 64:(hh + 1) * 64], po[:, hh * 128:hh * 128 + 64], s[:, hh:hh + 1], None, ALU.mult),
                 reads=[r_po, rs], writes=[r_yt[b]])
        c.dma("sp", y[n * 128:(n + 1) * 128, :], yt[b][:], reads=[r_yt[b]], writes=[r_y[b]])

    qk(0)
    for ui in range(len(units)):
        if ui + 1 < len(units):
            qk(ui + 1)
        pv(ui)
        if units[ui][1] == 1:
            fin(units[ui][0])
    c.finish("sp", r_y)
    print("MO: ins", c.n_ins, "waits", c.n_wait, "sbuf left", nc.sbuf_bytes_remaining)


N_CORES = 8
T_SEQ = 16384
_PROGS = {}


def _prog(name):
    if name not in _PROGS:
        _PROGS[name] = {"ME": lambda: build_ME(T_SEQ), "MO": lambda: build_MO(T_SEQ), "F": lambda: build_F2(32, 2)}[name]()
    return _PROGS[name]


def _launch(nc, in_maps):
    res = run_bass_kernel_spmd(nc, in_maps, core_ids=list(range(N_CORES)))
    return res.results


def kernel(x, ab_w_in, ab_w_out, ret_decay, c_w_in, c_w_out, c_rpb, ln_g, ln_b,
           router_w, router_b, exp_w_up, exp_b_up, exp_w_down, exp_b_down):
    f32 = np.float32
    xs = np.ascontiguousarray(np.asarray(x, f32)[0])
    T = xs.shape[0]
    idn = np.eye(128, dtype=f32)
    ii = np.arange(128)
    fcst = np.ascontiguousarray(np.concatenate([np.tile(np.arange(256, dtype=f32)[None, :], (128, 1)),
                                                (ii[:, None] < ii[None, :]).astype(f32), np.ones((128, 128), f32)], 1))
    me_c = me_host_consts(T)
    per = T // N_CORES
    for layer in range(4):
        j = layer // 2
        xT = np.ascontiguousarray(xs.T)
        ycat = np.empty((T, 1024), f32)
        if layer % 2 == 0:
            w_in = np.asarray(ab_w_in[j], f32)
            dec = np.asarray(ret_decay[j], f32)
            in_maps = [dict(xT=xT, wsel=np.ascontiguousarray(w_in[:, me_weight_cols(h)]),
                            dec=np.ascontiguousarray(dec[:, h][None, :]), **me_c) for h in range(N_CORES)]
            outs = _launch(_prog("ME"), in_maps)
            for h in range(N_CORES):
                yh = outs[h]["y"]
                ycat[:, h * 64:(h + 1) * 64] = yh[:, 0:64]
                ycat[:, 512 + h * 64:512 + (h + 1) * 64] = yh[:, 64:128]
            w_out = np.asarray(ab_w_out[j], f32)
        else:
            w_in = np.asarray(c_w_in[j], f32)
            rpb = np.asarray(c_rpb[j], f32)
            in_maps = [dict(xT=xT, wsel=np.ascontiguousarray(w_in[:, mo_weight_cols(cc)]),
                            bias=mo_host_bias(rpb[2 * cc:2 * cc + 2], T), idn=idn) for cc in range(N_CORES)]
            outs = _launch(_prog("MO"), in_maps)
            for cc in range(N_CORES):
                ycat[:, cc * 128:(cc + 1) * 128] = outs[cc]["y"]
            w_out = np.asarray(c_w_out[j], f32)
        lnp = np.ascontiguousarray(np.stack([ln_g[layer, 0], ln_b[layer, 0], ln_g[layer, 1], ln_b[layer, 1]], 0).astype(f32))
        common = dict(w_out=np.ascontiguousarray(w_out), lnp=lnp,
                      rw=np.ascontiguousarray(np.asarray(router_w[layer], f32)),
                      rb=np.ascontiguousarray(np.asarray(router_b[layer], f32)[None, :]),
                      wup=np.ascontiguousarray(np.asarray(exp_w_up[layer], f32)),
                      bup=np.ascontiguousarray(np.asarray(exp_b_up[layer], f32).reshape(256, 256)),
                      wdn=np.ascontiguousarray(np.asarray(exp_w_down[layer], f32)),
                      bdn=np.ascontiguousarray(np.asarray(exp_b_down[layer], f32)), idn=idn, fcst=fcst)
        in_maps = []
        for cc in range(N_CORES):
            sl = slice(cc * per, (cc + 1) * per)
            in_maps.append(dict(common, x=np.ascontiguousarray(xs[sl]), yT=np.ascontiguousarray(ycat[sl].T)))
        outs = _launch(_prog("F"), in_maps)
        xs = np.concatenate([outs[cc]["xo"] for cc in range(N_CORES)], 0)
    return xs[None].astype(f32)
```
